# Optimizing a Trainium2 kernel written in Bass

```python
import math
import jax
import jax.numpy as jnp
from jax import lax
import numpy as np

D_MODEL = 1024
BATCH = 2
SEQ = 16384
DEPTH = 2

F32 = jnp.float32
CTX_LEN = 256
GRID_W = 64
HEAD_DIM = 64
GROUP_WIDTH = D_MODEL // 4
BLOCK = 128
NORM_EPS = 1e-6
ROPE_BASE = 10000.0
SWA_HEADS = GROUP_WIDTH // HEAD_DIM
SWA_KV_HEADS = SWA_HEADS // 2
WINDOW = 128
RET_HEADS = GROUP_WIDTH // HEAD_DIM
RET_CHUNK = 128
DIFF_HEADS = GROUP_WIDTH // HEAD_DIM
DIFF_DIM = HEAD_DIM // 2
LRU_WIDTH = GROUP_WIDTH
LRU_BLOCKS = 4
LRU_BLOCK = LRU_WIDTH // LRU_BLOCKS
CONV_WIDTH = 4
LRU_C = 8.0
D_FF = 2816
N_EXPERTS = 8
TOP_K = 2
D_FF_EXPERT = 3584

PROJ_WIDTHS = (
    SWA_HEADS * HEAD_DIM, SWA_KV_HEADS * HEAD_DIM, SWA_KV_HEADS * HEAD_DIM,
    RET_HEADS * HEAD_DIM, RET_HEADS * HEAD_DIM, RET_HEADS * HEAD_DIM, RET_HEADS * HEAD_DIM,
    DIFF_HEADS * 2 * DIFF_DIM, DIFF_HEADS * 2 * DIFF_DIM, DIFF_HEADS * HEAD_DIM,
    LRU_WIDTH, LRU_WIDTH)
D_IN = sum(PROJ_WIDTHS)

kernel_name = 'hybrid_parallel_heads_dit_block'


def rms_norm(x, g):
    xf = x.astype(F32)
    y = xf * lax.rsqrt(jnp.mean(xf * xf, axis=-1, keepdims=True) + NORM_EPS)
    return (y * g.astype(F32)).astype(x.dtype)


def head_norm(y, gain):
    yf = y.astype(F32)
    yn = yf * lax.rsqrt(jnp.mean(yf * yf, axis=-1, keepdims=True) + NORM_EPS)
    return yn * gain.astype(F32).reshape(y.shape[-2], y.shape[-1])


def modulate(h, shift, scale):
    return h * (1.0 + scale) + shift


def axial_rope(rows, dim):
    row = jnp.repeat(jnp.arange(rows, dtype=F32), GRID_W)
    col = jnp.tile(jnp.arange(GRID_W, dtype=F32), rows)
    n_freq = dim // 4
    inv = ROPE_BASE ** (-jnp.arange(n_freq, dtype=F32) / n_freq)
    ang = jnp.concatenate([row[:, None] * inv, col[:, None] * inv], axis=-1)
    return jnp.cos(ang), jnp.sin(ang)


def line_rope(n, dim):
    n_freq = dim // 2
    inv = ROPE_BASE ** (-jnp.arange(n_freq, dtype=F32) / n_freq)
    ang = jnp.arange(n, dtype=F32)[:, None] * inv
    return jnp.cos(ang), jnp.sin(ang)


def apply_rope(x, cos, sin):
    shape = (1, cos.shape[0]) + (1,) * (x.ndim - 3) + (cos.shape[-1],)
    cos = cos.reshape(shape)
    sin = sin.reshape(shape)
    x1, x2 = jnp.split(x, 2, axis=-1)
    return jnp.concatenate([x1 * cos - x2 * sin, x2 * cos + x1 * sin], axis=-1)


def _flip(t):
    return jnp.flip(t, axis=1)


def _ident(t):
    return t


def split_proj(p):
    cuts = np.cumsum(PROJ_WIDTHS)[:-1].tolist()
    return jnp.split(p, cuts, axis=-1)


def _heads(t, h):
    return t.reshape(t.shape[:2] + (h, -1))


def _diff_heads(t):
    return t.reshape(t.shape[:2] + (DIFF_HEADS, 2, DIFF_DIM))


def swa_mixer(q, k, v, qc, kc, vc, sink, rope, need_ctx):
    B, N = q.shape[:2]
    M = kc.shape[1]
    G, R, d = SWA_KV_HEADS, SWA_HEADS // SWA_KV_HEADS, HEAD_DIM
    nb = N // BLOCK
    scale = d ** -0.5
    q = apply_rope(q.astype(F32), *rope) * scale
    k = apply_rope(k.astype(F32), *rope)
    v = v.astype(F32)
    kc = kc.astype(F32)
    vc = vc.astype(F32)
    sink = sink.astype(F32).reshape(G, R, 1, 1)
    pad = ((0, 0), (BLOCK, BLOCK), (0, 0), (0, 0))
    kp = jnp.pad(k, pad).reshape(B, nb + 2, BLOCK, G, d)
    vp = jnp.pad(v, pad).reshape(B, nb + 2, BLOCK, G, d)
    kw = jnp.concatenate([kp[:, :-2], kp[:, 1:-1], kp[:, 2:]], axis=2)
    vw = jnp.concatenate([vp[:, :-2], vp[:, 1:-1], vp[:, 2:]], axis=2)
    qpos = (jnp.arange(nb) * BLOCK)[:, None, None] + jnp.arange(BLOCK)[None, :, None]
    kpos = (jnp.arange(nb) * BLOCK - BLOCK)[:, None, None] + jnp.arange(3 * BLOCK)[None, None, :]
    valid = (jnp.abs(qpos - kpos) <= WINDOW) & (kpos >= 0) & (kpos < N)
    qb = q.reshape(B, nb, BLOCK, G, R, d)
    s_win = jnp.einsum('bnqgrd,bnkgd->bngrqk', qb, kw)
    s_win = jnp.where(valid[None, :, None, None], s_win, -jnp.inf)
    s_ctx = jnp.einsum('bnqgrd,bmgd->bngrqm', qb, kc)
    s_sink = jnp.broadcast_to(sink, s_win.shape[:-1] + (1,))
    p = jax.nn.softmax(jnp.concatenate([s_win, s_ctx, s_sink], axis=-1), axis=-1)
    nw = 3 * BLOCK
    o = (jnp.einsum('bngrqk,bnkgd->bnqgrd', p[..., :nw], vw)
         + jnp.einsum('bngrqm,bmgd->bnqgrd', p[..., nw:nw + M], vc))
    o = o.reshape(B, N, G * R * d)
    oc = None
    if need_ctx:
        qcg = (qc.astype(F32) * scale).reshape(B, M, G, R, d)
        s = jnp.einsum('bmgrd,bngd->bgrmn', qcg, kc)
        s = jnp.concatenate([s, jnp.broadcast_to(sink, s.shape[:-1] + (1,))], axis=-1)
        pc = jax.nn.softmax(s, axis=-1)
        oc = jnp.einsum('bgrmn,bngd->bmgrd', pc[..., :M], vc).reshape(B, M, G * R * d)
    return o, oc


def _ret_summaries(k, v, lg):
    B, L, H, d = k.shape
    C = RET_CHUNK
    kc = k.reshape(B, L // C, C, H, d)
    vc = v.reshape(B, L // C, C, H, v.shape[-1])
    idx = jnp.arange(C)
    zeta = jnp.exp(lg[:, None] * (C - 1 - idx)[None, :])
    return jnp.einsum('bcmhd,hm,bcmhe->bchde', kc, zeta, vc)


def _ret_scan(U, lg, s0):
    decay = jnp.exp(lg * RET_CHUNK)[None, :, None, None]

    def step(S, u):
        return decay * S + u, S

    s_fin, s_prev = lax.scan(step, s0, jnp.moveaxis(U, 1, 0))
    return s_fin, jnp.moveaxis(s_prev, 0, 1)


def _ret_output(q, k, v, s_prev, lg):
    B, L, H, d = q.shape
    C = RET_CHUNK
    nc = L // C
    e = v.shape[-1]
    qc = q.reshape(B, nc, C, H, d)
    kc = k.reshape(B, nc, C, H, d)
    vc = v.reshape(B, nc, C, H, e)
    idx = jnp.arange(C)
    diff = idx[:, None] - idx[None, :]
    dmask = jnp.where(diff >= 0, jnp.exp(lg[:, None, None] * jnp.maximum(diff, 0)), 0.0)
    s = jnp.einsum('bcnhd,bcmhd->bchnm', qc, kc) * dmask
    inner = jnp.einsum('bchnm,bcmhe->bcnhe', s, vc)
    xi = jnp.exp(lg[:, None] * (idx + 1)[None, :])
    cross = jnp.einsum('bcnhd,bchde->bcnhe', qc, s_prev) * xi.T[None, None, :, :, None]
    return (inner + cross).reshape(B, L, H, e)


def retention_mixer(q, k, v, g, qc, kc, vc, gc, decay_logit, gn, rope, need_ctx):
    B, N, H, d = q.shape
    scale = d ** -0.5
    q = apply_rope(q.astype(F32), *rope)
    k = apply_rope(k.astype(F32), *rope) * scale
    v = v.astype(F32)
    qc = qc.astype(F32)
    kc = kc.astype(F32) * scale
    vc = vc.astype(F32)
    log_gamma = -jax.nn.softplus(-decay_logit.astype(F32))
    y = 0.0
    yc = 0.0
    for direction in range(2):
        lg = log_gamma[direction]
        fl = _flip if direction == 1 else _ident
        s0 = jnp.zeros((B, H, d, v.shape[-1]), F32)
        sc_fin, sc_prev = _ret_scan(_ret_summaries(fl(kc), fl(vc), lg), lg, s0)
        _, s_prev = _ret_scan(_ret_summaries(fl(k), fl(v), lg), lg, sc_fin)
        y = y + fl(_ret_output(fl(q), fl(k), fl(v), s_prev, lg))
        if need_ctx:
            yc = yc + fl(_ret_output(fl(qc), fl(kc), fl(vc), sc_prev, lg))
    o = head_norm(y, gn).reshape(B, N, H * d) * jax.nn.silu(g.astype(F32))
    oc = None
    if need_ctx:
        oc = head_norm(yc, gn).reshape(B, kc.shape[1], H * d) * jax.nn.silu(gc.astype(F32))
    return o, oc


def _diff_attend(qb, k, v, lam):
    s = jnp.einsum('bqhmd,bkhmd->bhmqk', qb, k)
    p = jax.nn.softmax(s, axis=-1)
    w = p[:, :, 0] - lam * p[:, :, 1]
    return jnp.einsum('bhqk,bkhe->bqhe', w, v)


def diff_mixer(q, k, v, qc, kc, vc, lam_vecs, gn, lam_init, rope, need_ctx):
    B, N, H = q.shape[:3]
    M = kc.shape[1]
    scale = DIFF_DIM ** -0.5
    q = apply_rope(q.astype(F32), *rope) * scale
    k = apply_rope(k.astype(F32), *rope)
    v = v.astype(F32)
    kc = kc.astype(F32)
    vc = vc.astype(F32)
    lv = lam_vecs.astype(F32)
    lam = jnp.exp(jnp.sum(lv[0] * lv[1])) - jnp.exp(jnp.sum(lv[2] * lv[3])) + lam_init
    k_all = jnp.concatenate([k, kc], axis=1)
    v_all = jnp.concatenate([v, vc], axis=1)
    nb = N // BLOCK
    qblocks = jnp.moveaxis(q.reshape(B, nb, BLOCK, H, 2, DIFF_DIM), 1, 0)
    o = lax.map(lambda qb: _diff_attend(qb, k_all, v_all, lam), qblocks)
    o = jnp.moveaxis(o, 0, 1).reshape(B, N, H, v.shape[-1])
    o = (head_norm(o, gn) * (1.0 - lam_init)).reshape(B, N, -1)
    oc = None
    if need_ctx:
        occ = _diff_attend(qc.astype(F32) * scale, kc, vc, lam)
        oc = (head_norm(occ, gn) * (1.0 - lam_init)).reshape(B, M, -1)
    return o, oc


def dwconv(x, w, b):
    y = lax.conv_general_dilated(x, w[:, None, :].astype(x.dtype), (1,), [(1, 2)],
                                 dimension_numbers=('NWC', 'WIO', 'NWC'),
                                 feature_group_count=x.shape[-1])
    return y + b


def _lin_combine(left, right):
    a_l, b_l = left
    a_r, b_r = right
    return a_l * a_r, a_r * b_l + b_r


def rglru_scan(u, wa, ba, wx, bx, lam, h0):
    B, L, W = u.shape
    ub = u.reshape(B, L, LRU_BLOCKS, LRU_BLOCK)
    r = jax.nn.sigmoid(jnp.einsum('blhi,hij->blhj', ub, wa.astype(F32)).reshape(B, L, W) + ba.astype(F32))
    i = jax.nn.sigmoid(jnp.einsum('blhi,hij->blhj', ub, wx.astype(F32)).reshape(B, L, W) + bx.astype(F32))
    log_a = -LRU_C * r * jax.nn.softplus(-lam.astype(F32))
    a = jnp.exp(log_a)
    b = jnp.sqrt(-jnp.expm1(2.0 * log_a)) * (i * u)
    b = b.at[:, 0].add(a[:, 0] * h0)
    _, h = lax.associative_scan(_lin_combine, (a, b), axis=1)
    return h


def rglru_mixer(xb, yb, xbc, ybc, conv_w, conv_b, wa, ba, wx, bx, lam, need_ctx):
    u = dwconv(xb, conv_w, conv_b).astype(F32)
    uc = dwconv(xbc, conv_w, conv_b).astype(F32)
    B = u.shape[0]
    h = 0.0
    hc = 0.0
    for direction in range(2):
        fl = _flip if direction == 1 else _ident
        prm = (wa[direction], ba[direction], wx[direction], bx[direction], lam[direction])
        hc_seq = rglru_scan(fl(uc), *prm, jnp.zeros((B, LRU_WIDTH), F32))
        h = h + fl(rglru_scan(fl(u), *prm, hc_seq[:, -1]))
        if need_ctx:
            hc = hc + fl(hc_seq)
    o = h * jax.nn.gelu(yb.astype(F32))
    oc = hc * jax.nn.gelu(ybc.astype(F32)) if need_ctx else None
    return o, oc


def hybrid_mixer(p, pc, sink, ret_logit, ret_g, diff_lam, diff_g, lam_init,
                 conv_w, conv_b, lru_wa, lru_ba, lru_wx, lru_bx, lru_lambda,
                 rope_attn, rope_diff, rope_ret, need_ctx):
    (aq, ak, av, rq, rk, rv, rg, dq, dk, dv, lx, ly) = split_proj(p)
    (aqc, akc, avc, rqc, rkc, rvc, rgc, dqc, dkc, dvc, lxc, lyc) = split_proj(pc)
    oa, oac = swa_mixer(_heads(aq, SWA_HEADS), _heads(ak, SWA_KV_HEADS), _heads(av, SWA_KV_HEADS),
                        _heads(aqc, SWA_HEADS), _heads(akc, SWA_KV_HEADS), _heads(avc, SWA_KV_HEADS),
                        sink, rope_attn, need_ctx)
    ob, obc = retention_mixer(_heads(rq, RET_HEADS), _heads(rk, RET_HEADS), _heads(rv, RET_HEADS), rg,
                              _heads(rqc, RET_HEADS), _heads(rkc, RET_HEADS), _heads(rvc, RET_HEADS), rgc,
                              ret_logit, ret_g, rope_ret, need_ctx)
    od, odc = diff_mixer(_diff_heads(dq), _diff_heads(dk), _heads(dv, DIFF_HEADS),
                         _diff_heads(dqc), _diff_heads(dkc), _heads(dvc, DIFF_HEADS),
                         diff_lam, diff_g, lam_init, rope_diff, need_ctx)
    ol, olc = rglru_mixer(lx, ly, lxc, lyc, conv_w, conv_b, lru_wa, lru_ba, lru_wx, lru_bx,
                          lru_lambda, need_ctx)
    o = jnp.concatenate([oa, ob, od, ol], axis=-1).astype(p.dtype)
    o_ctx = jnp.concatenate([oac, obc, odc, olc], axis=-1).astype(pc.dtype) if need_ctx else None
    return o, o_ctx


def swiglu(h, w1, w3, w2):
    return (jax.nn.silu(h @ w1) * (h @ w3)) @ w2


def moe_swiglu(h, router, w1, w3, w2):
    logits = jnp.einsum('bld,de->ble', h, router).astype(F32)
    top_val, top_idx = lax.top_k(logits, TOP_K)
    top_w = jax.nn.softmax(top_val, axis=-1)
    gate = jnp.sum(jax.nn.one_hot(top_idx, N_EXPERTS, dtype=F32) * top_w[..., None], axis=-2)
    out = jnp.zeros(h.shape, F32)
    for e in range(N_EXPERTS):
        out = out + gate[..., e:e + 1] * swiglu(h, w1[e], w3[e], w2[e])
    return out.astype(h.dtype)


def setup_inputs(seed: int = 0) -> dict:
    key = jax.random.key(seed)
    keys = jax.random.split(key, 32)
    counter = [0]

    def nrm(shape, std):
        k = keys[counter[0]]
        counter[0] += 1
        return jax.random.normal(k, shape, F32) * std

    D = D_MODEL
    n_dense = (DEPTH + 1) // 2
    n_moe = DEPTH // 2
    gam = 1.0 - 2.0 ** (-5.0 - jnp.arange(RET_HEADS, dtype=F32))
    ret_base = jnp.log(gam) - jnp.log1p(-gam)
    a_c = jax.random.uniform(keys[31], (DEPTH, 2, LRU_WIDTH), F32, 0.9, 0.999)
    a = a_c ** (1.0 / LRU_C)
    return {
        'x': nrm((BATCH, SEQ, D), 1.0),
        'c': nrm((BATCH, D), 1.0),
        'ctx': nrm((BATCH, CTX_LEN, D), 1.0),
        'c_ctx': nrm((D,), 1.0),
        'w_ada': nrm((DEPTH, D, 6 * D), 0.5 * D ** -0.5),
        'b_ada': nrm((DEPTH, 6 * D), 0.02),
        'g_norm1': 1.0 + nrm((DEPTH, D), 0.05),
        'g_norm2': 1.0 + nrm((DEPTH, D), 0.05),
        'w_in': nrm((DEPTH, D, D_IN), D ** -0.5),
        'w_out': nrm((DEPTH, D, D), D ** -0.5),
        'attn_sink': nrm((DEPTH, SWA_HEADS), 0.5),
        'ret_decay_logit': ret_base + nrm((DEPTH, 2, RET_HEADS), 0.1),
        'ret_gn': 1.0 + nrm((DEPTH, RET_HEADS * HEAD_DIM), 0.05),
        'diff_lambda': nrm((DEPTH, 4, DIFF_DIM), 0.1),
        'diff_gn': 1.0 + nrm((DEPTH, DIFF_HEADS * HEAD_DIM), 0.05),
        'conv_w': nrm((DEPTH, CONV_WIDTH, LRU_WIDTH), CONV_WIDTH ** -0.5),
        'conv_b': nrm((DEPTH, LRU_WIDTH), 0.02),
        'lru_wa': nrm((DEPTH, 2, LRU_BLOCKS, LRU_BLOCK, LRU_BLOCK), LRU_BLOCK ** -0.5),
        'lru_ba': nrm((DEPTH, 2, LRU_WIDTH), 0.02),
        'lru_wx': nrm((DEPTH, 2, LRU_BLOCKS, LRU_BLOCK, LRU_BLOCK), LRU_BLOCK ** -0.5),
        'lru_bx': nrm((DEPTH, 2, LRU_WIDTH), 0.02),
        'lru_lambda': jnp.log(a) - jnp.log1p(-a),
        'ffn_w1': nrm((n_dense, D, D_FF), D ** -0.5),
        'ffn_w3': nrm((n_dense, D, D_FF), D ** -0.5),
        'ffn_w2': nrm((n_dense, D_FF, D), D_FF ** -0.5),
        'moe_router': nrm((n_moe, D, N_EXPERTS), D ** -0.5),
        'moe_w1': nrm((n_moe, N_EXPERTS, D, D_FF_EXPERT), D ** -0.5),
        'moe_w3': nrm((n_moe, N_EXPERTS, D, D_FF_EXPERT), D ** -0.5),
        'moe_w2': nrm((n_moe, N_EXPERTS, D_FF_EXPERT, D), D_FF_EXPERT ** -0.5),
        'final_norm': 1.0 + nrm((D,), 0.05),
    }


def reference(x, c, ctx, c_ctx, w_ada, b_ada, g_norm1, g_norm2, w_in, w_out,
              attn_sink, ret_decay_logit, ret_gn, diff_lambda, diff_gn,
              conv_w, conv_b, lru_wa, lru_ba, lru_wx, lru_bx, lru_lambda,
              ffn_w1, ffn_w3, ffn_w2, moe_router, moe_w1, moe_w3, moe_w2, final_norm):
    n_lat = x.shape[1]
    rows = n_lat // GRID_W
    rope_attn = axial_rope(rows, HEAD_DIM)
    rope_diff = axial_rope(rows, DIFF_DIM)
    rope_ret = line_rope(n_lat, HEAD_DIM)
    xc = ctx
    for layer in range(DEPTH):
        need_ctx = layer < DEPTH - 1
        lam_init = 0.8 - 0.6 * math.exp(-0.3 * layer)
        mod = (jax.nn.silu(c) @ w_ada[layer] + b_ada[layer])[:, None, :]
        mod_c = jax.nn.silu(c_ctx) @ w_ada[layer] + b_ada[layer]
        sh1, sc1, gt1, sh2, sc2, gt2 = jnp.split(mod, 6, axis=-1)
        sh1c, sc1c, gt1c, sh2c, sc2c, gt2c = jnp.split(mod_c, 6, axis=-1)
        h = modulate(rms_norm(x, g_norm1[layer]), sh1, sc1)
        hc = modulate(rms_norm(xc, g_norm1[layer]), sh1c, sc1c)
        o, o_ctx = hybrid_mixer(h @ w_in[layer], hc @ w_in[layer],
                                attn_sink[layer], ret_decay_logit[layer], ret_gn[layer],
                                diff_lambda[layer], diff_gn[layer], lam_init,
                                conv_w[layer], conv_b[layer], lru_wa[layer], lru_ba[layer],
                                lru_wx[layer], lru_bx[layer], lru_lambda[layer],
                                rope_attn, rope_diff, rope_ret, need_ctx)
        x = x + gt1 * (o @ w_out[layer])
        j = layer // 2
        if layer % 2 == 0:
            ffn = lambda t: swiglu(t, ffn_w1[j], ffn_w3[j], ffn_w2[j])
        else:
            ffn = lambda t: moe_swiglu(t, moe_router[j], moe_w1[j], moe_w3[j], moe_w2[j])
        x = x + gt2 * ffn(modulate(rms_norm(x, g_norm2[layer]), sh2, sc2))
        if need_ctx:
            xc = xc + gt1c * (o_ctx @ w_out[layer])
            xc = xc + gt2c * ffn(modulate(rms_norm(xc, g_norm2[layer]), sh2c, sc2c))
    return rms_norm(x, final_norm)
```

```python
from contextlib import ExitStack
import math
import numpy as np
import ml_dtypes
import concourse.bass as bass
import concourse.mybir as mybir
from concourse.bass_utils import run_bass_kernel_spmd

F32 = mybir.dt.float32
BF16 = mybir.dt.bfloat16
I32 = mybir.dt.int32
AF = mybir.ActivationFunctionType
ALU = mybir.AluOpType
AX = mybir.AxisListType

ENGS = ("pe", "act", "dve", "pool", "sp")
DMAQ = ("sp", "act", "pool")
NDMASEM = 8
_ENGATTR = {"pe": "tensor", "act": "scalar", "dve": "vector", "pool": "gpsimd", "sp": "sync"}


class Op:
    __slots__ = ("eng", "fn", "dma", "cc", "raw", "war", "sig", "dsem", "dval", "has_dep")

    def __init__(self, eng, fn, dma, cc=False):
        self.eng = eng
        self.fn = fn
        self.dma = dma
        self.cc = cc
        self.raw = []
        self.war = []
        self.sig = None
        self.dsem = None
        self.dval = None
        self.has_dep = False


class Prog:
    def __init__(self, nc):
        self.nc = nc
        self.gs = ExitStack()
        self.sems = {e: self.gs.enter_context(nc.semaphore(f"s_{e}")) for e in ENGS}
        self.dsems = {e: [self.gs.enter_context(nc.semaphore(f"d_{e}{i}")) for i in range(NDMASEM)] for e in DMAQ}
        self.cnt = {e: 0 for e in ENGS}
        self.dcnt = {e: [0] * NDMASEM for e in DMAQ}
        self.dk = {e: 0 for e in DMAQ}
        self.ccsem = self.gs.enter_context(nc.semaphore("s_cc"))
        self.cccnt = 0
        self.waited = {e: {} for e in ENGS}
        self.barrier = None
        self.nsb = 0
        self.nphase = 0
        self._reset()

    def _reset(self):
        self.q = {e: [] for e in ENGS}
        self.res_w = {}
        self.res_r = {}
        self.es = ExitStack()

    def sb(self, shape, dt=F32, name=None, persist=False):
        self.nsb += 1
        st = self.gs if persist else self.es
        return st.enter_context(self.nc.sbuf_tensor(f"s{self.nsb}_" + (name or "t"), list(shape), dt))

    def ps(self, shape, dt=F32, name=None):
        self.nsb += 1
        return self.es.enter_context(self.nc.psum_tensor(f"p{self.nsb}_" + (name or "t"), list(shape), dt))

    def op(self, eng, meth, reads=(), writes=(), dma=False, cc=False, **kw):
        fn = (lambda e: getattr(e, meth)(**{k: (v(e) if callable(v) else v) for k, v in kw.items()}))
        o = Op(eng, fn, dma or cc, cc)
        raw = set()
        war = set()
        for r in reads:
            w = self.res_w.get(r)
            if w is not None:
                raw.add(w)
        for wr in writes:
            w = self.res_w.get(wr)
            if w is not None:
                war.add(w)
            for rd in self.res_r.get(wr, ()):
                war.add(rd)
        raw.discard(o)
        war -= raw
        o.raw = list(raw)
        o.war = list(war)
        for r in reads:
            self.res_r.setdefault(r, []).append(o)
        for wr in writes:
            self.res_w[wr] = o
            self.res_r[wr] = []
        self.q[eng].append(o)
        return o

    def dma(self, eng, out, in_, reads=(), writes=(), **kw):
        return self.op(eng, "dma_start", reads, writes, dma=True, out=out, in_=in_, **kw)

    def end_phase(self):
        nc = self.nc
        for e in ENGS:
            for o in self.q[e]:
                for d in o.raw:
                    d.has_dep = True
                for d in o.war:
                    if d.dma or d.eng != o.eng or o.dma:
                        d.has_dep = True
            for o in reversed(self.q[e]):
                if not o.dma:
                    o.has_dep = True
                    break
        for e in ENGS:
            for o in self.q[e]:
                if o.cc:
                    self.cccnt += 1
                    o.dsem = self.ccsem
                    o.dval = self.cccnt
                elif o.dma:
                    s = self.dk[e] % NDMASEM
                    self.dk[e] += 1
                    self.dcnt[e][s] += 16
                    o.dsem = self.dsems[e][s]
                    o.dval = self.dcnt[e][s]
                elif o.has_dep:
                    self.cnt[e] += 1
                    o.sig = self.cnt[e]
        prev_barrier = self.barrier
        nb = [(self.sems[e], self.cnt[e]) for e in ENGS if self.cnt[e] > 0]
        for e in DMAQ:
            for s in range(NDMASEM):
                if self.dcnt[e][s] > 0:
                    nb.append((self.dsems[e][s], self.dcnt[e][s]))
        if self.cccnt > 0:
            nb.append((self.ccsem, self.cccnt))
        self.barrier = nb
        sems = self.sems

        def run_engine(e, eng):
            waited = self.waited[e]

            def wait(sem, val):
                if waited.get(sem, 0) >= val:
                    return
                eng.wait_ge(sem, val)
                waited[sem] = val

            if prev_barrier is not None:
                for s, v in prev_barrier:
                    wait(s, v)
            for o in self.q[e]:
                for d, is_raw in [(d, True) for d in o.raw] + [(d, False) for d in o.war]:
                    if d.dma:
                        wait(d.dsem, d.dval)
                    elif d.eng == e and not o.dma:
                        if is_raw:
                            wait(sems[e], d.sig)
                    else:
                        wait(sems[d.eng], d.sig)
                if o.cc:
                    if o.dval > 1:
                        wait(o.dsem, o.dval - 1)
                    o.fn(eng).then_inc(o.dsem)
                elif o.dma:
                    if o.dval > 16:
                        wait(o.dsem, o.dval - 16)
                    o.fn(eng).then_inc(o.dsem, 16)
                else:
                    ins = o.fn(eng)
                    if o.sig is not None:
                        ins.then_inc(sems[e], 1)
            if e == "sp":
                for s, v in nb:
                    wait(s, v)

        with nc.Block() as block:
            @block.tensor
            def _(eng):
                run_engine("pe", eng)

            @block.scalar
            def _(eng):
                run_engine("act", eng)

            @block.vector
            def _(eng):
                run_engine("dve", eng)

            @block.gpsimd
            def _(eng):
                run_engine("pool", eng)

            @block.sync
            def _(eng):
                run_engine("sp", eng)
        self.es.close()
        self._reset()
        self.nphase += 1

    def emit(self):
        self.end_phase()

    def close(self):
        self.es.close()
        self.gs.close()

D = 1024
EPS = 1e-6
SC_SWA = 64 ** -0.5
SC_RET = 64 ** -0.5
SC_DIFF = 32 ** -0.5


class Rot:
    def __init__(self, P, name, n, shape, dt=F32, psum=False):
        self.bufs = [(P.ps(shape, dt, f"{name}{i}") if psum else P.sb(shape, dt, f"{name}{i}")) for i in range(n)]
        self.name = name
        self.i = 0

    def next(self):
        k = self.i % len(self.bufs)
        self.i += 1
        return self.bufs[k], f"{self.name}{k}"


def _r(a, b):
    return list(range(a, b))


TOK_COLS = _r(0, 256) + _r(256, 384) + _r(1536, 1792) + _r(1792, 2048) + _r(512, 768) + _r(768, 1024) + _r(384, 512) + _r(1024, 1280) + _r(2048, 2304)
FEAT_COLS = _r(1280, 1536) + _r(2304, 2560) + _r(2560, 2816)


def rope_tables(n_lat):
    f32 = np.float32
    n = np.arange(n_lat)
    row = (n // 64).astype(f32)
    col = (n % 64).astype(f32)

    def axial(dim):
        nf = dim // 4
        inv = (f32(10000.0) ** (-(np.arange(nf, dtype=f32)) / f32(nf))).astype(f32)
        ang = np.concatenate([row[:, None] * inv, col[:, None] * inv], -1).astype(f32)
        return np.cos(ang).astype(f32), np.sin(ang).astype(f32)

    ca, sa = axial(64)
    cd, sd = axial(32)
    nf = 32
    inv = (f32(10000.0) ** (-(np.arange(nf, dtype=f32)) / f32(nf))).astype(f32)
    ang = (n.astype(f32)[:, None] * inv).astype(f32)
    cl, sl = np.cos(ang).astype(f32), np.sin(ang).astype(f32)
    return np.ascontiguousarray(np.concatenate([ca, sa, cd, sd, cl, sl], -1).astype(f32))


def mod_rows(P, cT, wada, bada, ncols, pmrot, name, extra=None):
    scT = P.sb([128, 8, 2], F32, name + "_scT")
    P.dma("sp", scT[:], cT, writes=[name + "_scT"])
    P.op("act", "activation", out=scT[:], in_=scT[:], func=AF.Silu, reads=[name + "_scT"], writes=[name + "_scT"])
    mrow = P.sb([2, ncols], F32, name + "_mrow")
    brot = Rot(P, name + "_brow", 2, [2, 512], F32)
    xrot_ = Rot(P, name + "_xrow", 2, [2, 512], F32)
    g2rot = Rot(P, name + "_g2row", 2, [2, 512], F32)
    wrot = Rot(P, name + "_wA", 1, [128, 8, 512], F32)
    wv = wada.rearrange("(k p) c -> p k c", p=128)
    ntot = ncols if extra is None else extra[0]
    for cb in range(ntot // 512):
        cs = slice(cb * 512, (cb + 1) * 512)
        wA, wk = wrot.next()
        P.dma("sp", wA[:], wv[:, :, cs], writes=[wk])
        brow, bk = brot.next()
        P.dma("sp", brow[0:1, :], bada[:, cs], writes=[bk])
        P.dma("sp", brow[1:2, :], bada[:, cs], writes=[bk])
        pm, pk = pmrot.next()
        for k in range(8):
            P.op("pe", "matmul", out=pm[0:2, :], lhsT=scT[:, k, :], rhs=wA[:, k, :], start=(k == 0), stop=(k == 7),
                 reads=[name + "_scT", wk], writes=[pk])
        if cb * 512 < ncols:
            P.op("dve", "tensor_tensor", out=mrow[:, cs], in0=pm[0:2, :], in1=brow[:, :], op=ALU.add, reads=[pk, bk], writes=[name + "_mrow"])
        else:
            xr, xk = xrot_.next()
            P.op("dve", "tensor_tensor", out=xr[:, :], in0=pm[0:2, :], in1=brow[:, :], op=ALU.add, reads=[pk, bk], writes=[xk])
            oc = cb * 512 - ncols
            if 2048 <= oc < 3072:
                g2, g2k = g2rot.next()
                P.dma("sp", g2[0:1, :], extra[1][:, oc - 2048:oc - 2048 + 512], writes=[g2k])
                P.dma("sp", g2[1:2, :], extra[1][:, oc - 2048:oc - 2048 + 512], writes=[g2k])
                P.op("dve", "scalar_tensor_tensor", out=xr[:, :], in0=xr[:, :], scalar=1.0, in1=g2[:, :], op0=ALU.add, op1=ALU.mult, reads=[xk, g2k], writes=[xk])
            P.dma("sp", extra[2][:, oc:oc + 512], xr[:, :], reads=[xk])
    return mrow, name + "_mrow"


def bcast_rows(P, row, rowkey, sel, v, pmrot, out, outkey):
    for hh in range(2):
        pm, pk = pmrot.next()
        P.op("pe", "matmul", out=pm[:, :], lhsT=sel[:, v, :], rhs=row[:, hh * 512:(hh + 1) * 512], start=True, stop=True,
             reads=[rowkey, "sel"], writes=[pk])
        P.op("act", "copy", out=out[:, hh * 512:(hh + 1) * 512], in_=pm[:, :], reads=[pk], writes=[outkey])


def rms_rstd(P, xt, xk, rots):
    sq, sqk = rots["sq"].next()
    st, stk = rots["st"].next()
    P.op("act", "activation", out=sq[:], in_=xt, func=AF.Square, accum_out=st[:, 0:1], reads=[xk], writes=[sqk, stk])
    P.op("dve", "tensor_scalar", out=st[:, 1:2], in0=st[:, 0:1], scalar1=1.0 / D, scalar2=EPS, op0=ALU.mult, op1=ALU.add, reads=[stk], writes=[stk])
    P.op("act", "activation", out=st[:, 2:3], in_=st[:, 1:2], func=AF.Sqrt, reads=[stk], writes=[stk])
    P.op("dve", "reciprocal", out=st[:, 3:4], in_=st[:, 2:3], reads=[stk], writes=[stk])
    return st, stk, sq, sqk


def norm_mod_transpose(P, xt, xk, Gb, Sb, gkeys, ident, hT, hTk, col0, rots):
    st, stk, sq, sqk = rms_rstd(P, xt, xk, rots)
    P.op("dve", "scalar_tensor_tensor", out=sq[:], in0=xt, scalar=st[:, 3:4], in1=Gb[:], op0=ALU.mult, op1=ALU.mult,
         reads=[xk, stk, gkeys[0]], writes=[sqk])
    hb, hbk = rots["hb"].next()
    P.op("pool", "tensor_tensor", out=hb[:], in0=sq[:], in1=Sb[:], op=ALU.add, reads=[sqk, gkeys[1]], writes=[hbk])
    ptr, ptk = rots["ptr"].next()
    for k in range(8):
        P.op("pe", "transpose", out=ptr[:, k * 128:(k + 1) * 128], in_=hb[:, k * 128:(k + 1) * 128], identity=ident[:],
             reads=[hbk, "ident"], writes=[ptk])
    P.op("act", "copy", out=hT[:, :, col0:col0 + 128], in_=ptr[:, :].rearrange("p (k t) -> p k t", k=8), reads=[ptk], writes=[hTk])


def head_cols(j):
    g = j // 2
    r = lambda a, n=64: list(range(a, a + n))
    tok = (r(1536 + j * 64) + r(0 + j * 64) + r(1792 + j * 64) + r(256 + g * 64) + r(512 + j * 64) + r(768 + j * 64)
           + r(384 + g * 64) + r(1024 + j * 64) + r(2048 + j * 64))
    feat = r(1280 + j * 64) + r(2304 + j * 64) + r(2560 + j * 64)
    return tok, feat


def emit_p(P, N, M, x_lat, x_ctx, cT, wada, bada, gn, gn2, wt_d, wf_d, tabs, ident_d, sel_d,
           s_qA, s_kB, s_rq, s_rk, s_tok, s_f32, s_modrows):
    nlt, nct = N // 128, M // 128
    ident = P.sb([128, 128], BF16, "ident")
    P.dma("sp", ident[:], ident_d, writes=["ident"])
    sel = P.sb([2, 2, 128], F32, "sel")
    P.dma("sp", sel[:], sel_d, writes=["sel"])
    Wt = P.sb([128, 8, 576], BF16, "Wt")
    Wf = P.sb([128, 8, 192], BF16, "Wf")
    P.dma("pool", Wt[:], wt_d.rearrange("(k p) c -> p k c", p=128), writes=["Wt"])
    P.dma("pool", Wf[:], wf_d.rearrange("(k p) c -> p k c", p=128), writes=["Wf"])
    pmrot = Rot(P, "pm", 3, [128, 512], F32, psum=True)
    mrow, mk = mod_rows(P, cT, wada, bada, 2048, pmrot, "mod", extra=(6144, gn2, s_modrows))
    grow = P.sb([2, D], F32, "grow")
    P.dma("sp", grow[0:1, :], gn, writes=["grow"])
    P.dma("sp", grow[1:2, :], gn, writes=["grow"])
    P.op("dve", "scalar_tensor_tensor", out=grow[:], in0=mrow[:, 1024:2048], scalar=1.0, in1=grow[:], op0=ALU.add, op1=ALU.mult,
         reads=[mk, "grow"], writes=["grow"])
    Gb = [P.sb([128, D], F32, f"Gb{v}") for v in range(2)]
    Sb = [P.sb([128, D], F32, f"Sb{v}") for v in range(2)]
    for v in range(2):
        bcast_rows(P, grow, "grow", sel, v, pmrot, Gb[v], f"Gb{v}")
        bcast_rows(P, mrow[:, 0:1024], mk, sel, v, pmrot, Sb[v], f"Sb{v}")
    rots = {
        "sq": Rot(P, "sq", 2, [128, D], F32),
        "st": Rot(P, "st", 4, [128, 4], F32),
        "hb": Rot(P, "hb", 2, [128, D], BF16),
        "ptr": Rot(P, "ptr", 2, [128, 1024], BF16, psum=True),
    }
    xrot = Rot(P, "xt", 3, [128, D], F32)
    hTrot = Rot(P, "hT", 2, [128, 8, 512], BF16)
    ptokrot = Rot(P, "ptok", 2, [128, 576], F32)
    tabrot = Rot(P, "tab", 2, [128, 160], F32)
    tmprot = Rot(P, "rtmp", 2, [128, 4, 128], F32)
    qkrot = Rot(P, "qk16", 2, [128, 384], BF16)
    vstrot = Rot(P, "vst", 2, [128, 256], BF16)
    ptArot = Rot(P, "ptA", 2, [128, 4, 128], BF16, psum=True)
    f16rot = Rot(P, "f16T", 2, [128, 4, 512], BF16)
    f32rot = Rot(P, "f32T", 2, [128, 2, 512], F32)
    pieces = [(0, 64, SC_DIFF), (64, 128, SC_SWA), (128, 320, 1.0), (320, 384, SC_RET), (384, 512, 1.0)]
    ei = 0
    groups = [(list(range(t, min(t + 4, nct))), 1) for t in range(0, nct, 4)] + [(list(range(t, min(t + 4, nlt))), 0) for t in range(0, nlt, 4)]
    def do_norm(gi, ti):
        tiles, v = groups[gi]
        src = x_ctx if v == 1 else x_lat
        tile = tiles[ti]
        xt, xk = xrot.next()
        P.dma("sp", xt[:], (src(tile) if callable(src) else src[tile * 128:(tile + 1) * 128, :]), writes=[xk])
        hT, hTk = hts[gi]
        norm_mod_transpose(P, xt[:], xk, Gb[v], Sb[v], (f"Gb{v}", f"Sb{v}"), ident, hT, hTk, ti * 128, rots)

    hts = {}
    st1 = {}

    def stage1(gi, ti):
        tiles, v = groups[gi]
        tile = tiles[ti]
        T0 = (tiles[0] * 128) if v == 1 else (M + tiles[0] * 128)
        hT, hTk = hts[gi]
        ptok, ptokk = ptokrot.next()
        pm, pk = pmrot.next()
        for k in range(8):
            P.op("pe", "matmul", out=pm[:, :], lhsT=hT[:, k, ti * 128:(ti + 1) * 128], rhs=Wt[:, k, 0:512], start=(k == 0), stop=(k == 7),
                 reads=[hTk, "Wt"], writes=[pk])
        for (c0, c1, sc) in pieces:
            P.op("act", "activation", out=ptok[:, c0:c1], in_=pm[:, c0:c1], func=AF.Copy, scale=float(sc), reads=[pk], writes=[ptokk])
        pm2, pk2 = pmrot.next()
        for k in range(8):
            P.op("pe", "matmul", out=pm2[:, 0:64], lhsT=hT[:, k, ti * 128:(ti + 1) * 128], rhs=Wt[:, k, 512:576], start=(k == 0), stop=(k == 7),
                 reads=[hTk, "Wt"], writes=[pk2])
        P.op("act", "copy", out=ptok[:, 512:576], in_=pm2[:, 0:64], reads=[pk2], writes=[ptokk])
        qk, qkk = qkrot.next()
        if v == 0:
            tab, tabk = tabrot.next()
            P.dma("sp", tab[:], tabs[tile * 128:(tile + 1) * 128, :], writes=[tabk])

            def views(t_, kind):
                if kind == "diff":
                    vv = t_[:, 0:256].rearrange("p (a c) -> p a c", a=2)[:, :, 0:64].rearrange("p a (h two d) -> p a h two d", h=2, two=2)
                    return vv[:, :, :, 0, :], vv[:, :, :, 1, :], [128, 2, 2, 16]
                if kind == "swa":
                    vv = t_[:, 0:256].rearrange("p (a c) -> p a c", a=2)[:, :, 64:128].rearrange("p a (two d) -> p a two d", two=2)
                    return vv[:, :, 0, :], vv[:, :, 1, :], [128, 2, 32]
                vv = t_[:, 256:384].rearrange("p (h two d) -> p h two d", h=2, two=2)
                return vv[:, :, 0, :], vv[:, :, 1, :], [128, 2, 32]
            for ki, (kind, hd, tc0) in enumerate((("diff", 16, 64), ("swa", 32, 0), ("ret", 32, 96))):
                x1, x2, shp = views(ptok, kind)
                o1, o2, _ = views(qk, kind)
                cosb = tab[:, tc0:tc0 + hd]
                sinb = tab[:, tc0 + hd:tc0 + 2 * hd]
                for _ in range(len(shp) - 2):
                    cosb = cosb.unsqueeze(1)
                    sinb = sinb.unsqueeze(1)
                cosb = cosb.to_broadcast(shp)
                sinb = sinb.to_broadcast(shp)
                tmp, tmpk = tmprot.next()
                nel = int(np.prod(shp[1:]))
                if len(shp) == 4:
                    T_ = [tmp[:, i, 0:nel].rearrange("p (a h d) -> p a h d", a=shp[1], h=shp[2]) for i in range(4)]
                else:
                    T_ = [tmp[:, i, 0:nel].rearrange("p (a d) -> p a d", a=shp[1]) for i in range(4)]
                e1 = "dve" if (ti + ki) % 2 == 0 else "pool"
                e2 = "pool" if (ti + ki) % 2 == 0 else "dve"
                P.op(e1, "tensor_tensor", out=T_[0], in0=x1, in1=cosb, op=ALU.mult, reads=[ptokk, tabk], writes=[tmpk + "a"])
                P.op(e1, "tensor_tensor", out=T_[1], in0=x2, in1=sinb, op=ALU.mult, reads=[ptokk, tabk], writes=[tmpk + "b"])
                P.op(e2, "tensor_tensor", out=T_[2], in0=x2, in1=cosb, op=ALU.mult, reads=[ptokk, tabk], writes=[tmpk + "c"])
                P.op(e2, "tensor_tensor", out=T_[3], in0=x1, in1=sinb, op=ALU.mult, reads=[ptokk, tabk], writes=[tmpk + "d"])
                P.op(e1, "tensor_tensor", out=o1, in0=T_[0], in1=T_[1], op=ALU.subtract, reads=[tmpk + "a", tmpk + "b"], writes=[qkk])
                P.op(e2, "tensor_tensor", out=o2, in0=T_[2], in1=T_[3], op=ALU.add, reads=[tmpk + "c", tmpk + "d"], writes=[qkk])
        else:
            P.op("dve", "tensor_copy", out=qk[:, :], in_=ptok[:, 0:384], reads=[ptokk], writes=[qkk])
        vst, vstk = vstrot.next()
        P.op("pool", "tensor_copy", out=vst[:, 64:256], in_=ptok[:, 384:576], reads=[ptokk], writes=[vstk])
        P.op("pool", "tensor_copy", out=vst[:, 0:64], in_=qk[:, 320:384], reads=[qkk], writes=[vstk])
        P.dma("sp", s_tok[T0 + ti * 128:T0 + (ti + 1) * 128, :], vst[:], reads=[vstk], writes=["s_tok"])
        st1[(gi, ti)] = (qk, qkk)

    def stage2(gi, ti, f16T, f16k):
        qk, qkk = st1.pop((gi, ti))
        ptA, ptAk = ptArot.next()
        P.op("pe", "transpose", out=ptA[:, 0, :], in_=qk[:, 0:128], identity=ident[:], reads=[qkk, "ident"], writes=[ptAk])
        P.op("pe", "transpose", out=ptA[:, 1, :], in_=qk[:, 128:256], identity=ident[:], reads=[qkk, "ident"], writes=[ptAk])
        P.op("pe", "transpose", out=ptA[0:64, 2, :], in_=qk[:, 256:320], identity=ident[:], reads=[qkk, "ident"], writes=[ptAk])
        P.op("pe", "transpose", out=ptA[0:64, 3, :], in_=qk[:, 320:384], identity=ident[:], reads=[qkk, "ident"], writes=[ptAk])
        P.op("dve", "tensor_copy", out=f16T[:, 0:2, ti * 128:(ti + 1) * 128], in_=ptA[:, 0:2, :], reads=[ptAk], writes=[f16k])
        P.op("dve", "tensor_copy", out=f16T[0:64, 2:4, ti * 128:(ti + 1) * 128], in_=ptA[0:64, 2:4, :], reads=[ptAk], writes=[f16k])

    hts[0] = hTrot.next()
    for ti in range(len(groups[0][0])):
        do_norm(0, ti)
    for gi, (tiles, v) in enumerate(groups):
        ntok = len(tiles) * 128
        T0 = (tiles[0] * 128) if v == 1 else (M + tiles[0] * 128)
        hT, hTk = hts[gi]
        nxt = gi + 1 if gi + 1 < len(groups) else None
        if nxt is not None:
            hts[nxt] = hTrot.next()
        f16T, f16k = f16rot.next()
        for ti in range(len(tiles)):
            stage1(gi, ti)
            if nxt is not None and ti < len(groups[nxt][0]):
                do_norm(nxt, ti)
            if ti > 0:
                stage2(gi, ti - 1, f16T, f16k)
        if nxt is not None:
            for ti in range(len(tiles), len(groups[nxt][0])):
                do_norm(nxt, ti)
        stage2(gi, len(tiles) - 1, f16T, f16k)
        P.dma("sp", s_qA[:, T0:T0 + ntok], f16T[:, 0, 0:ntok], reads=[f16k], writes=["s_qA"])
        P.dma("sp", s_kB[:, T0:T0 + ntok], f16T[:, 1, 0:ntok], reads=[f16k], writes=["s_kB"])
        P.dma("sp", s_rq[:, T0:T0 + ntok], f16T[0:64, 2, 0:ntok], reads=[f16k], writes=["s_rq"])
        P.dma("sp", s_rk[:, T0:T0 + ntok], f16T[0:64, 3, 0:ntok], reads=[f16k], writes=["s_rk"])
        f32T, f32k = f32rot.next()
        for fb, (c0, c1) in enumerate(((0, 128), (128, 192))):
            w = c1 - c0
            pm, pk = pmrot.next()
            for k in range(8):
                P.op("pe", "matmul", out=pm[0:w, 0:ntok], lhsT=Wf[:, k, c0:c1], rhs=hT[:, k, 0:ntok], start=(k == 0), stop=(k == 7), reads=[hTk, "Wf"], writes=[pk])
            P.op("act", "copy", out=f32T[0:w, fb, 0:ntok], in_=pm[0:w, 0:ntok], reads=[pk], writes=[f32k])
        P.dma("sp", s_f32[0:128, T0:T0 + ntok], f32T[:, 0, 0:ntok], reads=[f32k], writes=["s_f32"])
        P.dma("sp", s_f32[128:192, T0:T0 + ntok], f32T[0:64, 1, 0:ntok], reads=[f32k], writes=["s_f32"])
        del hts[gi]
    P.end_phase()


def lam_scalar_col(P, lamv_d, lam_init, pmrot):
    lv = P.sb([1, 128], F32, "lv")
    P.dma("sp", lv[:], lamv_d, writes=["lv"])
    pr = P.sb([1, 64], F32, "lvpr")
    lvv = lv[:, :].rearrange("o (a b d) -> o a b d", a=2, b=2)
    P.op("dve", "tensor_tensor", out=pr[:, :].rearrange("o (a d) -> o a d", a=2), in0=lvv[:, :, 0, :], in1=lvv[:, :, 1, :], op=ALU.mult, reads=["lv"], writes=["lvpr"])
    sm = P.sb([1, 4], F32, "lvsm")
    P.op("dve", "tensor_reduce", out=sm[:, 0:2], in_=pr[:, :].rearrange("o (a d) -> o a d", a=2), axis=AX.X, op=ALU.add, reads=["lvpr"], writes=["lvsm"])
    P.op("act", "activation", out=sm[:, 0:2], in_=sm[:, 0:2], func=AF.Exp, reads=["lvsm"], writes=["lvsm"])
    P.op("dve", "tensor_tensor", out=sm[:, 2:3], in0=sm[:, 0:1], in1=sm[:, 1:2], op=ALU.subtract, reads=["lvsm"], writes=["lvsm"])
    P.op("dve", "tensor_scalar", out=sm[:, 3:4], in0=sm[:, 2:3], scalar1=float(lam_init), scalar2=-1.0, op0=ALU.add, op1=ALU.mult, reads=["lvsm"], writes=["lvsm"])
    P.op("dve", "tensor_scalar", out=sm[:, 2:3], in0=sm[:, 2:3], scalar1=float(lam_init), scalar2=None, op0=ALU.add, reads=["lvsm"], writes=["lvsm"])
    ones1 = P.sb([1, 128], F32, "ones1")
    P.op("pool", "memset", ap=ones1[:], constant=1.0, writes=["ones1"])
    pm, pk = pmrot.next()
    P.op("pe", "matmul", out=pm[:, 0:2], lhsT=ones1[:, :], rhs=sm[:, 2:4], start=True, stop=True, reads=["ones1", "lvsm"], writes=[pk])
    lamc = P.sb([128, 2], F32, "lamc")
    P.op("dve", "tensor_copy", out=lamc[:], in_=pm[:, 0:2], reads=[pk], writes=["lamc"])
    return lamc


def headnorm_T(P, o32, ok, ncol, ones64, eprot, gcol, gkey, mul, out_ap, outk, tmprot, eng_a="dve", eng_b="pool"):
    sq, sqk = tmprot.next()
    P.op(eng_b, "tensor_tensor", out=sq[0:64, 0:ncol], in0=o32, in1=o32, op=ALU.mult, reads=[ok], writes=[sqk])
    pm, pk = eprot.next()
    P.op("pe", "matmul", out=pm[0:64, 0:ncol], lhsT=ones64[0:64, 0:64], rhs=sq[0:64, 0:ncol], start=True, stop=True, reads=[sqk, "ones64"], writes=[pk])
    P.op(eng_a, "tensor_scalar", out=sq[0:64, 0:ncol], in0=pm[0:64, 0:ncol], scalar1=1.0 / 64, scalar2=EPS, op0=ALU.mult, op1=ALU.add, reads=[pk], writes=[sqk])
    P.op("act", "activation", out=sq[0:64, 0:ncol], in_=sq[0:64, 0:ncol], func=AF.Sqrt, reads=[sqk], writes=[sqk])
    P.op(eng_a, "reciprocal", out=sq[0:64, 0:ncol], in_=sq[0:64, 0:ncol], reads=[sqk], writes=[sqk])
    P.op(eng_a, "tensor_tensor", out=sq[0:64, 0:ncol], in0=sq[0:64, 0:ncol], in1=o32, op=ALU.mult, reads=[sqk, ok], writes=[sqk])
    P.op(eng_a, "tensor_scalar", out=out_ap, in0=sq[0:64, 0:ncol], scalar1=gcol, scalar2=float(mul), op0=ALU.mult, op1=ALU.mult, reads=[sqk, gkey], writes=[outk])


def emit_diff(P, N, M, need_ctx, lam_init, qT_d, kT_d, v_d, lamv_d, gn_d, oT_d):
    T = N + M
    NKB = T // 128

    qT = P.sb([64, T], BF16, "qT")
    kT = P.sb([64, T], BF16, "kT")
    V1 = P.sb([128, NKB, 65], BF16, "V1")
    nchunk = 4
    cw = T // nchunk
    for i in range(nchunk):
        P.dma("sp", qT[:, i * cw:(i + 1) * cw], qT_d[:, i * cw:(i + 1) * cw], writes=["qT"])
        P.dma("sp", kT[:, i * cw:(i + 1) * cw], kT_d[:, i * cw:(i + 1) * cw], writes=["kT"])
    vv = v_d.rearrange("(kb p) d -> p kb d", p=128)
    step = max(1, NKB // 8)
    for k0 in range(0, NKB, step):
        k1 = min(NKB, k0 + step)
        P.dma("sp", V1[:, k0:k1, 0:64], vv[:, k0:k1, :], writes=["V1"])
    P.op("pool", "memset", ap=V1[:, :, 64:65], constant=1.0, writes=["V1"])
    gcol = P.sb([64, 1], F32, "gcol")
    P.dma("sp", gcol[:], gn_d, writes=["gcol"])
    ones64 = P.sb([128, 64], F32, "ones64")
    P.op("pool", "memset", ap=ones64[:], constant=1.0, writes=["ones64"])
    srot = Rot(P, "S", 3, [128, 512], F32, psum=True)
    eprot = Rot(P, "ep", 1, [128, 512], F32, psum=True)
    accrot = [Rot(P, f"acc{m}_", 2, [128, 512], F32, psum=True) for m in range(2)]
    lamc = lam_scalar_col(P, lamv_d, lam_init, eprot)
    ptrot = Rot(P, "PT", 6, [128, 512], BF16)
    rlrot = Rot(P, "rl", 2, [128, 2, 512], F32)
    bcrot = Rot(P, "bc", 2, [64, 2, 512], F32)
    t32rot = Rot(P, "t32", 3, [64, 512], F32)
    orot = Rot(P, "ob", 2, [64, 512], BF16)

    def epilogue(acc, q0, nq):
        rl, rlk = rlrot.next()
        for m in range(2):
            P.op("dve", "reciprocal", out=rl[64:65, m, 0:nq], in_=acc[m][0][64:65, 0:nq], reads=[acc[m][1]], writes=[rlk])
        P.op("dve", "tensor_scalar", out=rl[64:65, 1, 0:nq], in0=rl[64:65, 1, 0:nq], scalar1=lamc[64:65, 1:2], scalar2=None, op0=ALU.mult, reads=[rlk, "lamc"], writes=[rlk])
        bc, bck = bcrot.next()
        for m in range(2):
            pm, pk = eprot.next()
            P.op("pe", "matmul", out=pm[0:64, 0:nq], lhsT=ones64[64:65, 0:64], rhs=rl[64:65, m, 0:nq], start=True, stop=True, reads=[rlk, "ones64"], writes=[pk])
            P.op("dve", "tensor_copy", out=bc[:, m, 0:nq], in_=pm[0:64, 0:nq], reads=[pk], writes=[bck])
        t0, t0k = t32rot.next()
        t1, t1k = t32rot.next()
        P.op("dve", "tensor_tensor", out=t0[:, 0:nq], in0=acc[0][0][0:64, 0:nq], in1=bc[:, 0, 0:nq], op=ALU.mult, reads=[acc[0][1], bck], writes=[t0k])
        P.op("dve", "tensor_tensor", out=t1[:, 0:nq], in0=acc[1][0][0:64, 0:nq], in1=bc[:, 1, 0:nq], op=ALU.mult, reads=[acc[1][1], bck], writes=[t1k])
        P.op("pool", "tensor_tensor", out=t0[:, 0:nq], in0=t0[:, 0:nq], in1=t1[:, 0:nq], op=ALU.add, reads=[t0k, t1k], writes=[t0k])
        ob, obk = orot.next()
        headnorm_T(P, t0[:, 0:nq], t0k, nq, ones64, eprot, gcol[:, 0:1], "gcol", 1.0 - lam_init, ob[:, 0:nq], obk, t32rot)
        P.dma("sp", oT_d[:, q0:q0 + nq], ob[:, 0:nq], reads=[obk])
    QG = 2
    sgroups = []
    if need_ctx:
        sgroups.append(([(0, M)], 0, M // 128))
    lat = [(g0, min(512, T - g0)) for g0 in range(M, T, 512)]
    for i in range(0, len(lat), QG):
        sgroups.append((lat[i:i + QG], 0, NKB))
    LOOK = 1
    for (qgs, kb0, kb1) in sgroups:
        ng = len(qgs)
        acc = [[accrot[m].next() for m in range(2)] for _ in range(ng)]
        steps = [(kb, m) for kb in range(kb0, kb1) for m in range(2)]
        pts = {}

        def issue_s(i):
            kb, m = steps[i]
            for gi, (q0, nq) in enumerate(qgs):
                S, Sk = srot.next()
                P.op("pe", "matmul", out=S[:, 0:nq], lhsT=kT[m * 32:(m + 1) * 32, kb * 128:(kb + 1) * 128], rhs=qT[m * 32:(m + 1) * 32, q0:q0 + nq],
                     start=True, stop=True, reads=["kT", "qT"], writes=[Sk])
                PT, PTk = ptrot.next()
                P.op("act", "activation", out=PT[:, 0:nq], in_=S[:, 0:nq], func=AF.Exp, reads=[Sk], writes=[PTk])
                pts[(i, gi)] = (PT, PTk)

        def issue_pv(i):
            kb, m = steps[i]
            for gi, (q0, nq) in enumerate(qgs):
                PT, PTk = pts.pop((i, gi))
                P.op("pe", "matmul", out=acc[gi][m][0][0:65, 0:nq], lhsT=V1[:, kb, :], rhs=PT[:, 0:nq], start=(kb == kb0), stop=(kb == kb1 - 1),
                     reads=["V1", PTk], writes=[acc[gi][m][1]])
        for i in range(len(steps) + LOOK):
            if i < len(steps):
                issue_s(i)
            if i - LOOK >= 0:
                issue_pv(i - LOOK)
        for gi, (q0, nq) in enumerate(qgs):
            epilogue(acc[gi], q0, nq)
    P.end_phase()


def swa_masks():
    b = np.arange(128)[:, None]
    a = np.arange(128)[None, :]
    m = np.concatenate([(b <= a), np.ones((128, 128), bool), (a <= b)], 1)
    return m.astype(np.float32).astype(ml_dtypes.bfloat16)


def emit_swa(P, N, M, need_ctx, qT_d, kT_d, v_d, sink_d, m3_d, oT_d):
    T = N + M
    NKB = T // 128
    MB = M // 128
    nb = N // 128

    qT = P.sb([64, T], BF16, "qT")
    kT = P.sb([64, T], BF16, "kT")
    V1 = P.sb([128, NKB, 65], BF16, "V1")
    nchunk = 4
    cw = T // nchunk
    for i in range(nchunk):
        P.dma("sp", qT[:, i * cw:(i + 1) * cw], qT_d[:, i * cw:(i + 1) * cw], writes=["qT"])
        P.dma("sp", kT[:, i * cw:(i + 1) * cw], kT_d[:, i * cw:(i + 1) * cw], writes=["kT"])
    vv = v_d.rearrange("(kb p) d -> p kb d", p=128)
    step = max(1, NKB // 8)
    for k0 in range(0, NKB, step):
        k1 = min(NKB, k0 + step)
        P.dma("sp", V1[:, k0:k1, 0:64], vv[:, k0:k1, :], writes=["V1"])
    P.op("pool", "memset", ap=V1[:, :, 64:65], constant=1.0, writes=["V1"])
    m3 = P.sb([128, 384], BF16, "m3")
    P.dma("sp", m3[:], m3_d, writes=["m3"])
    es = P.sb([128, 1], F32, "es")
    P.dma("sp", es[:], sink_d, writes=["es"])
    P.op("act", "activation", out=es[:], in_=es[:], func=AF.Exp, reads=["es"], writes=["es"])
    ones64 = P.sb([128, 64], F32, "ones64")
    P.op("pool", "memset", ap=ones64[:], constant=1.0, writes=["ones64"])
    accrot = Rot(P, "acc", 2, [128, 512], F32, psum=True)
    srot = Rot(P, "S", 3, [128, 512], F32, psum=True)
    eprot = Rot(P, "ep", 2, [128, 512], F32, psum=True)
    ptrot = Rot(P, "PT", 4, [128, 512], BF16)
    rlrot = Rot(P, "rl", 2, [128, 512], F32)
    bcrot = Rot(P, "bc", 2, [64, 512], F32)
    orot = Rot(P, "ob", 2, [64, 512], BF16)

    qgroups = []
    if need_ctx:
        qgroups.append((0, M, None))
    for s0 in range(0, nb, 4):
        qgroups.append((M + s0 * 128, min(4, nb - s0) * 128, s0))
    ei = 0
    for (q0, nq, s0) in qgroups:
        acc, acck = accrot.next()
        items = [(kb, 0, nq, None) for kb in range(MB)]
        if s0 is not None:
            ns = nq // 128
            for r in range(s0 - 1, s0 + ns + 1):
                if r < 0 or r >= nb:
                    continue
                sa = max(r - 1, s0)
                sb_ = min(r + 1, s0 + ns - 1)
                items.append((MB + r, (sa - s0) * 128, (sb_ - s0 + 1) * 128, (sa - (r - 1)) * 128))
        for ii, (kb, c0, c1, mc) in enumerate(items):
            w = c1 - c0
            S, Sk = srot.next()
            P.op("pe", "matmul", out=S[:, 0:w], lhsT=kT[:, kb * 128:(kb + 1) * 128], rhs=qT[:, q0 + c0:q0 + c1], start=True, stop=True,
                 reads=["kT", "qT"], writes=[Sk])
            PT, PTk = ptrot.next()
            P.op("act", "activation", out=PT[:, 0:w], in_=S[:, 0:w], func=AF.Exp, reads=[Sk], writes=[PTk])
            if mc is not None:
                eng = "dve" if ei % 2 == 0 else "pool"
                ei += 1
                P.op(eng, "tensor_tensor", out=PT[:, 0:w], in0=PT[:, 0:w], in1=m3[:, mc:mc + w], op=ALU.mult, reads=[PTk, "m3"], writes=[PTk])
            P.op("pe", "matmul", out=acc[0:65, c0:c1], lhsT=V1[:, kb, :], rhs=PT[:, 0:w], start=(ii == 0), stop=(ii == len(items) - 1),
                 reads=["V1", PTk], writes=[acck])
        rl, rlk = rlrot.next()
        P.op("dve", "tensor_scalar", out=rl[64:65, 0:nq], in0=acc[64:65, 0:nq], scalar1=es[64:65, 0:1], scalar2=None, op0=ALU.add, reads=[acck, "es"], writes=[rlk])
        P.op("dve", "reciprocal", out=rl[64:65, 0:nq], in_=rl[64:65, 0:nq], reads=[rlk], writes=[rlk])
        pm, pk = eprot.next()
        P.op("pe", "matmul", out=pm[0:64, 0:nq], lhsT=ones64[64:65, 0:64], rhs=rl[64:65, 0:nq], start=True, stop=True, reads=[rlk, "ones64"], writes=[pk])
        bc, bck = bcrot.next()
        P.op("act", "copy", out=bc[:, 0:nq], in_=pm[0:64, 0:nq], reads=[pk], writes=[bck])
        ob, obk = orot.next()
        P.op("dve", "tensor_tensor", out=ob[:, 0:nq], in0=acc[0:64, 0:nq], in1=bc[:, 0:nq], op=ALU.mult, reads=[acck, bck], writes=[obk])
        P.dma("sp", oT_d[:, q0:q0 + nq], ob[:, 0:nq], reads=[obk])
    P.end_phase()


def ret_consts():
    m = np.arange(128)[:, None].astype(np.float32)
    n = np.arange(128)[None, :].astype(np.float32)
    c = np.zeros((128, 6, 128), np.float32)
    c[:, 0] = np.maximum(n - m, 0)
    c[:, 1] = (n >= m)
    c[:, 2] = np.maximum(m - n, 0)
    c[:, 3] = (m >= n)
    c[:, 4] = n + 1
    c[:, 5] = 128 - n
    z = np.zeros((128, 2), np.float32)
    z[:, 0] = 127 - np.arange(128)
    z[:, 1] = np.arange(128)
    return c, z


def emit_ret(P, N, M, need_ctx, qT_d, kT_d, kt_d, v_d, gT_d, lg_d, gn_d, rc_d, rz_d, oT_d):
    T = N + M
    NC = T // 128
    MB = M // 128

    ktok = P.sb([128, NC, 64], BF16, "ktok")
    V = P.sb([128, NC, 64], BF16, "V")
    vv = v_d.rearrange("(kb p) d -> p kb d", p=128)
    kv = kt_d.rearrange("(kb p) d -> p kb d", p=128)
    step = max(1, NC // 8)
    for k0 in range(0, NC, step):
        k1 = min(NC, k0 + step)
        P.dma("sp", V[:, k0:k1, :], vv[:, k0:k1, :], writes=["V"])
        P.dma("sp", ktok[:, k0:k1, :], kv[:, k0:k1, :], writes=["ktok"])
    rc = P.sb([128, 6, 128], F32, "rc")
    P.dma("sp", rc[:], rc_d, writes=["rc"])
    rz = P.sb([128, 2], F32, "rz")
    P.dma("sp", rz[:], rz_d, writes=["rz"])
    gcol = P.sb([64, 1], F32, "gcol")
    P.dma("sp", gcol[:], gn_d, writes=["gcol"])
    lgc = P.sb([128, 2], F32, "lgc")
    P.dma("sp", lgc[:], lg_d, writes=["lgc"])
    P.op("act", "activation", out=lgc[:], in_=lgc[:], func=AF.Exp, scale=-1.0, reads=["lgc"], writes=["lgc"])
    P.op("dve", "tensor_scalar", out=lgc[:], in0=lgc[:], scalar1=1.0, scalar2=None, op0=ALU.add, reads=["lgc"], writes=["lgc"])
    P.op("act", "activation", out=lgc[:], in_=lgc[:], func=AF.Ln, reads=["lgc"], writes=["lgc"])
    P.op("dve", "tensor_scalar", out=lgc[:], in0=lgc[:], scalar1=-1.0, scalar2=None, op0=ALU.mult, reads=["lgc"], writes=["lgc"])
    DcT = P.sb([128, 128], F32, "DcT")
    E2 = P.sb([128, 128], F32, "E2")
    P.op("act", "activation", out=DcT[:], in_=rc[:, 0, :], func=AF.Exp, scale=lgc[:, 0:1], reads=["rc", "lgc"], writes=["DcT"])
    P.op("act", "activation", out=E2[:], in_=rc[:, 2, :], func=AF.Exp, scale=lgc[:, 1:2], reads=["rc", "lgc"], writes=["E2"])
    P.op("dve", "tensor_tensor", out=DcT[:], in0=DcT[:], in1=rc[:, 1, :], op=ALU.mult, reads=["DcT", "rc"], writes=["DcT"])
    P.op("dve", "tensor_tensor", out=E2[:], in0=E2[:], in1=rc[:, 3, :], op=ALU.mult, reads=["E2", "rc"], writes=["E2"])
    P.op("dve", "tensor_tensor", out=DcT[:], in0=DcT[:], in1=E2[:], op=ALU.add, reads=["DcT", "E2"], writes=["DcT"])
    xi = P.sb([64, 2, 128], F32, "xi")
    P.op("act", "activation", out=xi[:, 0, :], in_=rc[0:64, 4, :], func=AF.Exp, scale=lgc[0:64, 0:1], reads=["rc", "lgc"], writes=["xi"])
    P.op("act", "activation", out=xi[:, 1, :], in_=rc[0:64, 5, :], func=AF.Exp, scale=lgc[0:64, 1:2], reads=["rc", "lgc"], writes=["xi"])
    zc = P.sb([128, 2], F32, "zc")
    P.op("act", "activation", out=zc[:, 0:1], in_=rz[:, 0:1], func=AF.Exp, scale=lgc[:, 0:1], reads=["rz", "lgc"], writes=["zc"])
    P.op("act", "activation", out=zc[:, 1:2], in_=rz[:, 1:2], func=AF.Exp, scale=lgc[:, 1:2], reads=["rz", "lgc"], writes=["zc"])
    gc = P.sb([64, 2], F32, "gc")
    P.op("act", "activation", out=gc[:], in_=lgc[0:64, :], func=AF.Exp, scale=128.0, reads=["lgc"], writes=["gc"])
    ones64 = P.sb([128, 64], F32, "ones64")
    P.op("pool", "memset", ap=ones64[:], constant=1.0, writes=["ones64"])

    U = [P.sb([64, NC, 64], F32, f"U{d}") for d in range(2)]
    S16 = [P.sb([64, NC, 64], BF16, f"S16_{d}") for d in range(2)]
    urot = Rot(P, "ups", 2, [128, 512], F32, psum=True)
    kzrot = Rot(P, "kz", 4, [128, 64], BF16)
    for d in range(2):
        for c0 in range(0, NC, 8):
            c1 = min(NC, c0 + 8)
            pm, pk = urot.next()
            for c in range(c0, c1):
                kz, kzk = kzrot.next()
                P.op("dve" if d == 0 else "pool", "tensor_scalar", out=kz[:], in0=ktok[:, c, :], scalar1=zc[:, d:d + 1], scalar2=None, op0=ALU.mult,
                     reads=["ktok", "zc"], writes=[kzk])
                P.op("pe", "matmul", out=pm[0:64, (c - c0) * 64:(c - c0 + 1) * 64], lhsT=kz[:], rhs=V[:, c, :], start=True, stop=True,
                     reads=[kzk, "V"], writes=[pk])
            P.op("act", "copy", out=U[d][:, c0:c1, :], in_=pm[0:64, 0:(c1 - c0) * 64].rearrange("p (c e) -> p c e", e=64), reads=[pk], writes=[f"U{d}"])
    order = [list(range(NC)), list(range(MB - 1, -1, -1)) + list(range(NC - 1, MB - 1, -1))]
    prev = [{}, {}]
    for d in range(2):
        eng = "dve"
        od = order[d]
        for i in range(1, NC):
            prev[d][od[i]] = od[i - 1]
            P.op(eng, "scalar_tensor_tensor", out=U[d][:, od[i], :], in0=U[d][:, od[i - 1], :], scalar=gc[:, d:d + 1], in1=U[d][:, od[i], :],
                 op0=ALU.mult, op1=ALU.add, reads=[f"U{d}", "gc"], writes=[f"U{d}"])
        P.op(eng, "tensor_copy", out=S16[d][:], in_=U[d][:], reads=[f"U{d}"], writes=[f"S16_{d}"])
    accrot = Rot(P, "acc", 2, [128, 512], F32, psum=True)
    srot = Rot(P, "S", 2, [128, 512], F32, psum=True)
    eprot = Rot(P, "ep", 2, [128, 512], F32, psum=True)
    qrot = Rot(P, "qg", 2, [64, 512], BF16)
    krot = Rot(P, "kg", 2, [64, 512], BF16)
    grot = Rot(P, "gg", 2, [64, 512], F32)
    amrot = Rot(P, "am", 3, [128, 128], BF16)
    qxrot = Rot(P, "qx", 3, [64, 2, 128], BF16)
    t32rot = Rot(P, "t32", 3, [64, 512], F32)
    orot = Rot(P, "ob", 2, [64, 512], BF16)
    groups = []
    if need_ctx:
        groups.append(list(range(0, MB)))
    for c0 in range(MB, NC, 4):
        groups.append(list(range(c0, min(NC, c0 + 4))))
    for grp in groups:
        t0_ = grp[0] * 128
        nq = len(grp) * 128
        qg, qgk = qrot.next()
        kg, kgk = krot.next()
        gg, ggk = grot.next()
        P.dma("sp", qg[:, 0:nq], qT_d[:, t0_:t0_ + nq], writes=[qgk])
        P.dma("sp", kg[:, 0:nq], kT_d[:, t0_:t0_ + nq], writes=[kgk])
        P.dma("sp", gg[:, 0:nq], gT_d[:, t0_:t0_ + nq], writes=[ggk])
        P.op("act", "activation", out=gg[:, 0:nq], in_=gg[:, 0:nq], func=AF.Silu, reads=[ggk], writes=[ggk])
        acc, acck = accrot.next()
        for ci, c in enumerate(grp):
            cs = slice(ci * 128, (ci + 1) * 128)
            S, Sk = srot.next()
            P.op("pe", "matmul", out=S[:, 0:128], lhsT=kg[:, cs], rhs=qg[:, cs], start=True, stop=True, reads=[kgk, qgk], writes=[Sk])
            am, amk = amrot.next()
            P.op("dve", "tensor_tensor", out=am[:], in0=S[:, 0:128], in1=DcT[:], op=ALU.mult, reads=[Sk, "DcT"], writes=[amk])
            qx, qxk = qxrot.next()
            P.op("pool", "tensor_tensor", out=qx[:], in0=qg[:, cs].unsqueeze(1).to_broadcast([64, 2, 128]), in1=xi[:], op=ALU.mult, reads=[qgk, "xi"], writes=[qxk])
            mms = [(V[:, c, :], am[:], ["V", amk])]
            for d in range(2):
                if c in prev[d]:
                    mms.append((S16[d][:, prev[d][c], :], qx[:, d, :], [f"S16_{d}", qxk]))
            for mi, (l_, r_, rk_) in enumerate(mms):
                P.op("pe", "matmul", out=acc[0:64, cs], lhsT=l_, rhs=r_, start=(mi == 0), stop=(mi == len(mms) - 1), reads=rk_, writes=[acck])
        y, yk = t32rot.next()
        P.op("act", "copy", out=y[:, 0:nq], in_=acc[0:64, 0:nq], reads=[acck], writes=[yk])
        yn, ynk = t32rot.next()
        headnorm_T(P, y[:, 0:nq], yk, nq, ones64, eprot, gcol[:, 0:1], "gcol", 1.0, yn[:, 0:nq], ynk, t32rot)
        ob, obk = orot.next()
        P.op("pool", "tensor_tensor", out=ob[:, 0:nq], in0=yn[:, 0:nq], in1=gg[:, 0:nq], op=ALU.mult, reads=[ynk, ggk], writes=[obk])
        P.dma("sp", oT_d[:, t0_:t0_ + nq], ob[:, 0:nq], reads=[obk])
    P.end_phase()


def _bk(name, c0, c1):
    return [f"{name}{b}" for b in range(c0 // 512, (c1 - 1) // 512 + 1)]


def emit_lru(P, N, M, lx_d, ly_d, cw_d, cb_d, wa_d, wx_d, ba_d, bx_d, lam_d, i2_d, oT_d):
    T = N + M

    XX = P.sb([128, T], F32, "XX")
    UU = P.sb([128, T], F32, "UU")
    CH = 2048
    for c0 in range(0, T, CH):
        c1 = min(T, c0 + CH)
        P.dma("sp", XX[0:64, c0:c1], lx_d[:, c0:c1], writes=_bk("XX", c0, c1))
        P.dma("sp", XX[64:128, c0:c1], lx_d[:, c0:c1], writes=_bk("XX", c0, c1))
    small = {}
    for nm, d_, shp in (("cw", cw_d, [128, 4]), ("cb", cb_d, [128, 1]), ("wa", wa_d, [64, 128]), ("wx", wx_d, [64, 128]),
                        ("ba", ba_d, [128, 1]), ("bx", bx_d, [128, 1]), ("lam", lam_d, [128, 1]), ("i2", i2_d, [128, 64])):
        t_ = P.sb(shp, F32, nm)
        P.dma("sp", t_[:], d_, writes=[nm])
        small[nm] = t_
    cw, cb, wa, wx, ba, bx, lam, i2 = (small[k] for k in ("cw", "cb", "wa", "wx", "ba", "bx", "lam", "i2"))
    c8 = P.sb([128, 1], F32, "c8")
    P.op("act", "activation", out=c8[:], in_=lam[:], func=AF.Exp, scale=-1.0, reads=["lam"], writes=["c8"])
    P.op("dve", "tensor_scalar", out=c8[:], in0=c8[:], scalar1=1.0, scalar2=None, op0=ALU.add, reads=["c8"], writes=["c8"])
    P.op("act", "activation", out=c8[:], in_=c8[:], func=AF.Ln, reads=["c8"], writes=["c8"])
    P.op("dve", "tensor_scalar", out=c8[:], in0=c8[:], scalar1=-8.0, scalar2=None, op0=ALU.mult, reads=["c8"], writes=["c8"])
    for (s0, s1) in ((0, M), (M, T)):
        for c0 in range(s0, s1, CH):
            c1 = min(s1, c0 + CH)
            P.op("dve", "tensor_scalar", out=UU[:, c0:c1], in0=XX[:, c0:c1], scalar1=cw[:, 1:2], scalar2=cb[:, 0:1], op0=ALU.mult, op1=ALU.add,
                 reads=_bk("XX", c0, c1) + ["cw", "cb"], writes=_bk("UU", c0, c1))
            for (tap, sh) in ((0, -1), (2, 1), (3, 2)):
                o0, o1 = max(c0, s0 - sh), min(c1, s1 - sh)
                if o1 <= o0:
                    continue
                P.op("dve", "scalar_tensor_tensor", out=UU[:, o0:o1], in0=XX[:, o0 + sh:o1 + sh], scalar=cw[:, tap:tap + 1], in1=UU[:, o0:o1],
                     op0=ALU.mult, op1=ALU.add, reads=_bk("XX", o0 + sh, o1 + sh) + _bk("UU", o0, o1) + ["cw"], writes=_bk("UU", o0, o1))
    grot = Rot(P, "gps", 4, [128, 512], F32, psum=True)
    rsrot = Rot(P, "rs", 2, [128, 512], F32)
    isrot = Rot(P, "is", 2, [128, 512], F32)
    t1rot = Rot(P, "t1", 2, [128, 512], F32)
    for c0 in range(0, T, 512):
        c1 = min(T, c0 + 512)
        w = c1 - c0
        uk = _bk("UU", c0, c1)
        xk = _bk("XX", c0, c1)
        pr, prk = grot.next()
        pi, pik = grot.next()
        P.op("pe", "matmul", out=pr[:, 0:w], lhsT=wa[:, :], rhs=UU[0:64, c0:c1], start=True, stop=True, reads=uk + ["wa"], writes=[prk])
        P.op("pe", "matmul", out=pi[:, 0:w], lhsT=wx[:, :], rhs=UU[0:64, c0:c1], start=True, stop=True, reads=uk + ["wx"], writes=[pik])
        rs, rsk = rsrot.next()
        is_, isk = isrot.next()
        P.op("act", "activation", out=rs[:, 0:w], in_=pr[:, 0:w], func=AF.Sigmoid, bias=ba[:, 0:1], reads=[prk, "ba"], writes=[rsk])
        P.op("act", "activation", out=is_[:, 0:w], in_=pi[:, 0:w], func=AF.Sigmoid, bias=bx[:, 0:1], reads=[pik, "bx"], writes=[isk])
        P.op("act", "activation", out=XX[:, c0:c1], in_=rs[:, 0:w], func=AF.Exp, scale=c8[:, 0:1], reads=[rsk, "c8"], writes=xk)
        t1, t1k = t1rot.next()
        P.op("pool", "tensor_tensor", out=t1[:, 0:w], in0=XX[:, c0:c1], in1=XX[:, c0:c1], op=ALU.mult, reads=xk, writes=[t1k])
        P.op("pool", "tensor_scalar", out=t1[:, 0:w], in0=t1[:, 0:w], scalar1=-1.0, scalar2=1.0, op0=ALU.mult, op1=ALU.add, reads=[t1k], writes=[t1k])
        P.op("act", "activation", out=t1[:, 0:w], in_=t1[:, 0:w], func=AF.Sqrt, reads=[t1k], writes=[t1k])
        P.op("dve", "tensor_tensor", out=is_[:, 0:w], in0=is_[:, 0:w], in1=UU[:, c0:c1], op=ALU.mult, reads=[isk] + uk, writes=[isk])
        P.op("dve", "tensor_tensor", out=UU[:, c0:c1], in0=is_[:, 0:w], in1=t1[:, 0:w], op=ALU.mult, reads=[isk, t1k], writes=uk)
    prev = None
    for c0 in range(0, T, CH):
        c1 = min(T, c0 + CH)
        init = 0.0 if prev is None else UU[0:64, c0 - 1:c0]
        P.op("dve", "tensor_tensor_scan", out=UU[0:64, c0:c1], data0=XX[0:64, c0:c1], data1=UU[0:64, c0:c1], initial=init, op0=ALU.mult, op1=ALU.add,
             reads=_bk("XX", c0, c1) + _bk("UU", max(c0 - 1, 0), c1) + ["scanf"], writes=_bk("UU", c0, c1) + ["scanf"])
        prev = c0
    chunks = [(c0, min(M, c0 + CH)) for c0 in range(0, M, CH)][::-1] + [(c0, min(T, c0 + CH)) for c0 in range(M, T, CH)][::-1]
    prev_lo = None
    for (c0, c1) in chunks:
        init = 0.0 if prev_lo is None else UU[64:128, prev_lo:prev_lo + 1]
        rk = [] if prev_lo is None else _bk("UU", prev_lo, prev_lo + 1)
        P.op("dve", "tensor_tensor_scan", out=UU[64:128, c0:c1][:, ::-1], data0=XX[64:128, c0:c1][:, ::-1], data1=UU[64:128, c0:c1][:, ::-1], initial=init,
             op0=ALU.mult, op1=ALU.add, reads=_bk("XX", c0, c1) + _bk("UU", c0, c1) + rk + ["scanb"], writes=_bk("UU", c0, c1) + ["scanb"])
        prev_lo = c0
    hrot = Rot(P, "hps", 2, [128, 512], F32, psum=True)
    yrot = Rot(P, "yy", 2, [64, 512], F32)
    zrot = Rot(P, "zz", 2, [64, 512], F32)
    orot = Rot(P, "ob", 2, [64, 512], BF16)
    for c0 in range(0, T, 512):
        c1 = min(T, c0 + 512)
        w = c1 - c0
        hp, hpk = hrot.next()
        P.op("pe", "matmul", out=hp[0:64, 0:w], lhsT=i2[:, :], rhs=UU[:, c0:c1], start=True, stop=True, reads=_bk("UU", c0, c1) + ["i2"], writes=[hpk])
        yy, yk = yrot.next()
        zz, zk = zrot.next()
        P.dma("sp", yy[:, 0:w], ly_d[:, c0:c1], writes=[yk])
        P.op("pool", "tensor_tensor", out=zz[:, 0:w], in0=yy[:, 0:w], in1=yy[:, 0:w], op=ALU.mult, reads=[yk], writes=[zk])
        P.op("pool", "tensor_scalar", out=zz[:, 0:w], in0=zz[:, 0:w], scalar1=0.044715, scalar2=1.0, op0=ALU.mult, op1=ALU.add, reads=[zk], writes=[zk])
        P.op("pool", "tensor_tensor", out=zz[:, 0:w], in0=zz[:, 0:w], in1=yy[:, 0:w], op=ALU.mult, reads=[zk, yk], writes=[zk])
        P.op("act", "activation", out=zz[:, 0:w], in_=zz[:, 0:w], func=AF.Sigmoid, scale=2.0 * math.sqrt(2.0 / math.pi), reads=[zk], writes=[zk])
        P.op("pool", "tensor_tensor", out=zz[:, 0:w], in0=zz[:, 0:w], in1=yy[:, 0:w], op=ALU.mult, reads=[zk, yk], writes=[zk])
        ob, obk = orot.next()
        P.op("dve", "tensor_tensor", out=ob[:, 0:w], in0=hp[0:64, 0:w], in1=zz[:, 0:w], op=ALU.mult, reads=[hpk, zk], writes=[obk])
        P.dma("sp", oT_d[:, c0:c1], ob[:, 0:w], reads=[obk])
    P.end_phase()


def emit_f(P, tile_specs, nv, E, FF, moe, final, modrows_d, sel_d, wo_d, w1_d, w3_d, w2_d, ident_d, wr_d=None, id32_d=None, fn_d=None, GT=8):
    ntile = len(tile_specs)
    NFB = FF // 128
    nhalf = 2
    HB = NFB // nhalf
    ident = P.sb([128, 128], BF16, "ident")
    P.dma("sp", ident[:], ident_d, writes=["ident"])
    sel = P.sb([2, 2, 128], F32, "sel")
    P.dma("sp", sel[:], sel_d, writes=["sel"])
    mrrot = Rot(P, "mrowF", 2, [2, 1024], F32)
    pmrot = Rot(P, "pm", 3, [128, 512], F32, psum=True)
    mt = [[P.sb([128, D], F32, f"mt{v}_{i}") for i in range(4)] for v in range(nv)]
    for i in range(4):
        mrow, mrk = mrrot.next()
        P.dma("sp", mrow[:], modrows_d[:, i * 1024:(i + 1) * 1024], writes=[mrk])
        for v in range(nv):
            bcast_rows(P, mrow, mrk, sel, v, pmrot, mt[v][i], f"mt{v}_{i}")
    if final:
        fnb = P.sb([128, D], F32, "fnb")
        P.dma("sp", fnb[:], fn_d, writes=["fnb"])
    Wo = P.sb([128, 8, D], BF16, "Wo")
    wov = wo_d.rearrange("(k p) c -> p k c", p=128)
    for k in range(8):
        P.dma("pool", Wo[:, k, :], wov[:, k, :], writes=["Wo"])
    if moe:
        Wr = P.sb([128, 8, E], F32, "Wr")
        P.dma("sp", Wr[:], wr_d.rearrange("(k p) e -> p k e", p=128), writes=["Wr"])
        id32 = P.sb([128, 128], F32, "id32")
        P.dma("sp", id32[:], id32_d, writes=["id32"])
        gates = P.sb([128, GT, E], F32, "gates")
        h32T = P.sb([128, 8, 128], F32, "h32T")
        rsm = Rot(P, "rsm", 2, [128, 40], F32)
    purot = Rot(P, "pu", 4, [128, 512], F32, psum=True)
    rots = {
        "sq": Rot(P, "sq", 2, [128, D], F32),
        "st": Rot(P, "st", 4, [128, 4], F32),
        "hb": Rot(P, "hb", 2, [128, D], BF16),
        "ptr": Rot(P, "ptr", 1, [128, 1024], BF16, psum=True),
    }
    xrot = Rot(P, "xt", 2, [128, D], F32)
    otrot = Rot(P, "oTt", 2, [128, 8, 128], BF16)
    acc = P.sb([128, GT, D], F32, "acc")
    h2T = P.sb([128, 8, GT * 128], BF16, "h2T")
    gT = P.sb([128, HB, GT * 128], BF16, "gT")
    W2 = P.sb([128, HB, D], BF16, "W2")
    w1rot = Rot(P, "w1b", 2, [128, 8, 256], BF16)
    w3rot = Rot(P, "w3b", 2, [128, 8, 256], BF16)
    srot = Rot(P, "sil", 2, [128, 512], F32)
    tmprot = Rot(P, "ftmp", 2, [128, 512], F32)
    orot = Rot(P, "xout", 1, [128, D], F32) if final else None

    tiles_v = [ts_[0] for ts_ in tile_specs]
    for g0 in range(0, ntile, GT):
        tiles = list(range(g0, min(ntile, g0 + GT)))
        ng = len(tiles)
        ntok = ng * 128
        for ti, tile in enumerate(tiles):
            v = tiles_v[tile]
            acck = f"acc{ti}"
            xt, xk = xrot.next()
            P.dma("sp", xt[:], tile_specs[tile][1], writes=[xk])
            ot, otk = otrot.next()
            P.dma("sp", ot[:], tile_specs[tile][2], reads=["l_oT"], writes=[otk])
            for hh in range(2):
                pm, pk = pmrot.next()
                for k in range(8):
                    P.op("pe", "matmul", out=pm[:, :], lhsT=ot[:, k, :], rhs=Wo[:, k, hh * 512:(hh + 1) * 512], start=(k == 0), stop=(k == 7),
                         reads=[otk, "Wo"], writes=[pk])
                hs = slice(hh * 512, (hh + 1) * 512)
                P.op("dve", "tensor_tensor", out=acc[:, ti, hs], in0=pm[:, :], in1=mt[v][0][:, hs], op=ALU.mult, reads=[pk, f"mt{v}_0"], writes=[acck])
                P.op("pool", "tensor_tensor", out=acc[:, ti, hs], in0=acc[:, ti, hs], in1=xt[:, hs], op=ALU.add, reads=[acck, xk], writes=[acck])
            st, stk, sq, sqk = rms_rstd(P, acc[:, ti, :], acck, rots)
            P.op("dve", "scalar_tensor_tensor", out=sq[:], in0=acc[:, ti, :], scalar=st[:, 3:4], in1=mt[v][2][:], op0=ALU.mult, op1=ALU.mult,
                 reads=[acck, stk, f"mt{v}_2"], writes=[sqk])
            P.op("pool", "tensor_tensor", out=sq[:], in0=sq[:], in1=mt[v][1][:], op=ALU.add, reads=[sqk, f"mt{v}_1"], writes=[sqk])
            hb, hbk = rots["hb"].next()
            P.op("act", "copy", out=hb[:], in_=sq[:], reads=[sqk], writes=[hbk])
            ptr, ptk = rots["ptr"].next()
            for k in range(8):
                P.op("pe", "transpose", out=ptr[:, k * 128:(k + 1) * 128], in_=hb[:, k * 128:(k + 1) * 128], identity=ident[:], reads=[hbk, "ident"], writes=[ptk])
            P.op("act", "copy", out=h2T[:, :, ti * 128:(ti + 1) * 128], in_=ptr[:, :].rearrange("p (k t) -> p k t", k=8), reads=[ptk], writes=[f"h2T{ti}"])
            if moe:
                for hh in range(2):
                    pm, pk = pmrot.next()
                    for k4 in range(4):
                        k = hh * 4 + k4
                        P.op("pe", "transpose", out=pm[:, k4 * 128:(k4 + 1) * 128], in_=sq[:, k * 128:(k + 1) * 128], identity=id32[:], reads=[sqk, "id32"], writes=[pk])
                    P.op("act", "copy", out=h32T[:, hh * 4:(hh + 1) * 4, :], in_=pm[:, :].rearrange("p (k t) -> p k t", k=4), reads=[pk], writes=["h32T"])
                pm, pk = pmrot.next()
                for k in range(8):
                    P.op("pe", "matmul", out=pm[:, 0:E], lhsT=h32T[:, k, :], rhs=Wr[:, k, :], start=(k == 0), stop=(k == 7), reads=["h32T", "Wr"], writes=[pk])
                r, rk = rsm.next()
                lg, eq1, lg2, eq2 = r[:, 0:E], r[:, 8:8 + E], r[:, 16:16 + E], r[:, 24:24 + E]
                m1, m2, dd, ww1, ww2 = (r[:, 32 + i:33 + i] for i in range(5))
                P.op("act", "copy", out=lg, in_=pm[:, 0:E], reads=[pk], writes=[rk])
                P.op("dve", "tensor_reduce", out=m1, in_=lg, axis=AX.X, op=ALU.max, reads=[rk], writes=[rk])
                P.op("dve", "tensor_scalar", out=eq1, in0=lg, scalar1=m1, scalar2=None, op0=ALU.is_equal, reads=[rk], writes=[rk])
                P.op("dve", "scalar_tensor_tensor", out=lg2, in0=eq1, scalar=-1e30, in1=lg, op0=ALU.mult, op1=ALU.add, reads=[rk], writes=[rk])
                P.op("dve", "tensor_reduce", out=m2, in_=lg2, axis=AX.X, op=ALU.max, reads=[rk], writes=[rk])
                P.op("dve", "tensor_scalar", out=eq2, in0=lg2, scalar1=m2, scalar2=None, op0=ALU.is_equal, reads=[rk], writes=[rk])
                P.op("dve", "tensor_tensor", out=dd, in0=m2, in1=m1, op=ALU.subtract, reads=[rk], writes=[rk])
                P.op("act", "activation", out=dd, in_=dd, func=AF.Exp, reads=[rk], writes=[rk])
                P.op("dve", "tensor_scalar", out=ww1, in0=dd, scalar1=1.0, scalar2=None, op0=ALU.add, reads=[rk], writes=[rk])
                P.op("dve", "reciprocal", out=ww1, in_=ww1, reads=[rk], writes=[rk])
                P.op("dve", "tensor_tensor", out=ww2, in0=dd, in1=ww1, op=ALU.mult, reads=[rk], writes=[rk])
                P.op("dve", "tensor_scalar", out=gates[:, ti, :], in0=eq1, scalar1=ww1, scalar2=None, op0=ALU.mult, reads=[rk], writes=[f"gates{ti}"])
                P.op("dve", "scalar_tensor_tensor", out=gates[:, ti, :], in0=eq2, scalar=ww2, in1=gates[:, ti, :], op0=ALU.mult, op1=ALU.add, reads=[rk, f"gates{ti}"], writes=[f"gates{ti}"])
        h2keys = [f"h2T{ti}" for ti in range(ng)]
        chunks = [(c0, min(ntok, c0 + 512)) for c0 in range(0, ntok, 512)]
        for e in range(E):
            for half in range(nhalf):
                fb0 = half * HB
                w2v = w2_d[e, fb0 * 128:(fb0 + HB) * 128, :].rearrange("(f p) c -> p f c", p=128)
                for fp in range(0, HB, 2):
                    nb_ = min(2, HB - fp)
                    w1b, w1k = w1rot.next()
                    w3b, w3k = w3rot.next()
                    cs = slice((fb0 + fp) * 128, (fb0 + fp + nb_) * 128)
                    P.dma("pool", w1b[:, :, 0:nb_ * 128], w1_d[e, :, cs].rearrange("(k p) c -> p k c", p=128), writes=[w1k])
                    P.dma("pool", w3b[:, :, 0:nb_ * 128], w3_d[e, :, cs].rearrange("(k p) c -> p k c", p=128), writes=[w3k])
                    P.dma("pool", W2[:, fp:fp + nb_, :], w2v[:, fp:fp + nb_, :], writes=["W2"])
                    for bi in range(nb_):
                        fbi = fp + bi
                        for (c0, c1) in chunks:
                            w = c1 - c0
                            hk = h2keys[c0 // 128:(c1 - 1) // 128 + 1]
                            p1, p1k = purot.next()
                            p3, p3k = purot.next()
                            for k in range(8):
                                P.op("pe", "matmul", out=p1[:, 0:w], lhsT=w1b[:, k, bi * 128:(bi + 1) * 128], rhs=h2T[:, k, c0:c1], start=(k == 0), stop=(k == 7),
                                     reads=[w1k] + hk, writes=[p1k])
                            for k in range(8):
                                P.op("pe", "matmul", out=p3[:, 0:w], lhsT=w3b[:, k, bi * 128:(bi + 1) * 128], rhs=h2T[:, k, c0:c1], start=(k == 0), stop=(k == 7),
                                     reads=[w3k] + hk, writes=[p3k])
                            sl, slk = srot.next()
                            P.op("act", "activation", out=sl[:, 0:w], in_=p1[:, 0:w], func=AF.Silu, reads=[p1k], writes=[slk])
                            P.op("dve", "tensor_tensor", out=gT[:, fbi, c0:c1], in0=p3[:, 0:w], in1=sl[:, 0:w], op=ALU.mult, reads=[p3k, slk], writes=[f"gT{c0 // 512}"])
                for ti, tile in enumerate(tiles):
                    v = tiles_v[tile]
                    for hh in range(2):
                        hs = slice(hh * 512, (hh + 1) * 512)
                        pm, pk = pmrot.next()
                        for fbi in range(HB):
                            P.op("pe", "matmul", out=pm[:, :], lhsT=gT[:, fbi, ti * 128:(ti + 1) * 128], rhs=W2[:, fbi, hs], start=(fbi == 0), stop=(fbi == HB - 1),
                                 reads=[f"gT{ti // 4}", "W2"], writes=[pk])
                        tmp, tmpk = tmprot.next()
                        if moe:
                            P.op("dve", "scalar_tensor_tensor", out=tmp[:], in0=pm[:, :], scalar=gates[:, ti, e:e + 1], in1=mt[v][3][:, hs], op0=ALU.mult, op1=ALU.mult,
                                 reads=[pk, f"gates{ti}", f"mt{v}_3"], writes=[tmpk])
                        else:
                            P.op("dve", "tensor_tensor", out=tmp[:], in0=pm[:, :], in1=mt[v][3][:, hs], op=ALU.mult, reads=[pk, f"mt{v}_3"], writes=[tmpk])
                        P.op("pool", "tensor_tensor", out=acc[:, ti, hs], in0=acc[:, ti, hs], in1=tmp[:], op=ALU.add, reads=[f"acc{ti}", tmpk], writes=[f"acc{ti}"])
        for ti, tile in enumerate(tiles):
            if final:
                st, stk, sq, sqk = rms_rstd(P, acc[:, ti, :], f"acc{ti}", rots)
                xo_t, xok = orot.next()
                P.op("dve", "scalar_tensor_tensor", out=xo_t[:], in0=acc[:, ti, :], scalar=st[:, 3:4], in1=fnb[:], op0=ALU.mult, op1=ALU.mult,
                     reads=[f"acc{ti}", stk, "fnb"], writes=[xok])
                P.dma("sp", tile_specs[tile][3], xo_t[:], reads=[xok], writes=["xout_d"])
            else:
                P.dma("sp", tile_specs[tile][3], acc[:, ti, :], reads=[f"acc{ti}"], writes=["xout_d", "xsrc"])
    P.end_phase()


def build_fused(N, M, E, FFD, FFE):
    T = N + M
    tps = N // 4
    nc = bass.Bass("TRN2", target_bir_lowering=False)
    P = Prog(nc)
    di = lambda n, s, d=F32: nc.dram_tensor(n, s, d, kind="ExternalInput").ap()
    do = lambda n, s, d=F32: nc.dram_tensor(n, s, d, kind="ExternalOutput").ap()
    sc = lambda n, s, d=F32: nc.dram_tensor(n, s, d).ap()
    xsh = di("xsh", [tps, D])
    ctx = di("ctx", [M, D])
    cT = di("cT", [128, 8, 2])
    tabs = di("tabs", [N, 160])
    ident_d = di("ident", [128, 128], BF16)
    id32_d = di("id32", [128, 128])
    sel_d = di("sel", [2, 2, 128])
    m3_d = di("m3", [128, 384], BF16)
    rc_d = di("retc", [128, 6, 128])
    rz_d = di("retz", [128, 2])
    i2_d = di("i2", [128, 64])
    fn_d = di("fnb", [128, D])
    L = []
    for l in range(2):
        L.append(dict(
            wada=di(f"wada{l}", [D, 6144]), bada=di(f"bada{l}", [1, 6144]), gn1=di(f"gn1_{l}", [1, D]), gn2=di(f"gn2_{l}", [1, D]),
            wt=di(f"wt{l}", [D, 576]), wf=di(f"wf{l}", [D, 192]), wo=di(f"wo{l}", [D, D]),
            sink=di(f"sink{l}", [128, 1]), logit=di(f"logit{l}", [128, 2]), rgn=di(f"rgn{l}", [64, 1]),
            lamv=di(f"lamv{l}", [1, 128]), dgn=di(f"dgn{l}", [64, 1]),
            cw=di(f"cw{l}", [128, 4]), cb=di(f"cb{l}", [128, 1]), wa=di(f"wa{l}", [64, 128]), wx=di(f"wx{l}", [64, 128]),
            ba=di(f"ba{l}", [128, 1]), bx=di(f"bx{l}", [128, 1]), lam=di(f"lam{l}", [128, 1])))
    fw1, fw3, fw2 = di("fw1", [1, D, FFD]), di("fw3", [1, D, FFD]), di("fw2", [1, FFD, D])
    mw1, mw3, mw2 = di("mw1", [E, D, FFE]), di("mw3", [E, D, FFE]), di("mw2", [E, FFE, D])
    wr_d = di("wr", [D, E])
    xo = do("xo", [tps, D])
    s_qA, s_kB = sc("s_qA", [128, T], BF16), sc("s_kB", [128, T], BF16)
    s_rq, s_rk = sc("s_rq", [64, T], BF16), sc("s_rk", [64, T], BF16)
    s_tok = sc("s_tok", [T, 256], BF16)
    s_f32 = sc("s_f32", [192, T])
    s_mod = sc("s_mod", [2, 4096])
    s_oT = sc("s_oT", [256, T], BF16)
    CHT = tps // 2
    NCH = N // CHT
    st_o = sc("st_o", [NCH * 256, CHT], BF16)
    st_oc = sc("st_oc", [256, M], BF16)
    g_o = sc("g_o", [NCH * 1024, CHT], BF16)
    g_oc = sc("g_oc", [1024, M], BF16)
    l_oT = sc("l_oT", [1024, tps], BF16)
    s_x0 = sc("s_x0", [tps, D])
    s_x1 = sc("s_x1", [tps, D])
    XR = min(256, tps)
    NXC = tps // XR
    g_x = [sc(f"g_x{c}", [4 * XR, D]) for c in range(NXC)]
    s_xc1 = sc("s_xc1", [M, D])
    RG = [[0, 1, 2, 3], [4, 5, 6, 7]]
    l_oTv = l_oT.rearrange("(k p) t -> p k t", p=128)
    g_ocv = g_oc.rearrange("(k p) t -> p k t", p=128)

    def gather_x(src):
        for c in range(NXC):
            P.op("pool", "collective_compute", cc=True, kind="AllGather", op=ALU.bypass, replica_groups=RG,
                 ins=[src[c * XR:(c + 1) * XR, :]], outs=[g_x[c]], reads=["xsrc"], writes=["g_x"])

    def x_tile(ti):
        t0 = ti * 128
        r, rem = t0 // tps, t0 % tps
        c, i = rem // XR, rem % XR
        return g_x[c][r * XR + i:r * XR + i + 128, :]
    for r0 in range(0, tps, max(128, tps // 4)):
        r1 = min(tps, r0 + max(128, tps // 4))
        P.dma("sp", s_x0[r0:r1, :], xsh[r0:r1, :], writes=["xsrc"])
    gather_x(s_x0)
    P.end_phase()
    for l in range(2):
        W = L[l]
        need_ctx = (l == 0)
        lam_init = 0.8 - 0.6 * math.exp(-0.3 * l)
        x_lat = x_tile
        x_ctx = ctx if l == 0 else s_xc1
        emit_p(P, N, M, x_lat, x_ctx, cT, W["wada"], W["bada"], W["gn1"], W["gn2"], W["wt"], W["wf"], tabs, ident_d, sel_d,
               s_qA, s_kB, s_rq, s_rk, s_tok, s_f32, s_mod)
        emit_swa(P, N, M, need_ctx, s_qA[64:128, :], s_kB[64:128, :], s_tok[:, 64:128], W["sink"], m3_d, s_oT[0:64, :])
        emit_ret(P, N, M, need_ctx, s_rq, s_rk, s_tok[:, 0:64], s_tok[:, 128:192], s_f32[0:64, :], W["logit"], W["rgn"], rc_d, rz_d, s_oT[64:128, :])
        emit_diff(P, N, M, need_ctx, lam_init, s_qA[0:64, :], s_kB[0:64, :], s_tok[:, 192:256], W["lamv"], W["dgn"], s_oT[128:192, :])
        emit_lru(P, N, M, s_f32[64:128, :], s_f32[128:192, :], W["cw"], W["cb"], W["wa"], W["wx"], W["ba"], W["bx"], W["lam"], i2_d, s_oT[192:256, :])
        for c in range(NCH):
            P.dma("sp", st_o[c * 256:(c + 1) * 256, :], s_oT[:, M + c * CHT:M + (c + 1) * CHT], writes=[f"st_o{c}"])
        if need_ctx:
            P.dma("sp", st_oc[:, :], s_oT[:, 0:M], writes=["st_oc"])
        for c in range(NCH):
            P.op("pool", "collective_compute", cc=True, kind="AllGather", op=ALU.bypass, replica_groups=RG,
                 ins=[st_o[c * 256:(c + 1) * 256, :]], outs=[g_o[c * 1024:(c + 1) * 1024, :]], reads=[f"st_o{c}"], writes=["g_o"])
        if need_ctx:
            P.op("pool", "collective_compute", cc=True, kind="AllGather", op=ALU.bypass, replica_groups=RG,
                 ins=[st_oc], outs=[g_oc], reads=["st_oc"], writes=["g_oc"])
        P.end_phase()
        pidc = {}

        def shard_rows(e, pidc=pidc):
            if id(e) not in pidc:
                pidc[id(e)] = e.snap((e.partition_id() % 4) * 2048)
            return pidc[id(e)]
        for h in range(2):
            P.dma("sp", l_oT[:, h * CHT:(h + 1) * CHT],
                  (lambda e, h=h, sr=shard_rows: g_o[bass.ds(sr(e) + h * 1024, 1024), :]), writes=["l_oT"])
        specs = []
        for i in range(tps // 128):
            if l == 0:
                xs = xsh[i * 128:(i + 1) * 128, :]
                od = s_x1[i * 128:(i + 1) * 128, :]
            else:
                xs = s_x1[i * 128:(i + 1) * 128, :]
                od = xo[i * 128:(i + 1) * 128, :]
            specs.append((0, xs, l_oTv[:, :, i * 128:(i + 1) * 128], od))
        if need_ctx:
            for i in range(M // 128):
                specs.append((1, ctx[i * 128:(i + 1) * 128, :], g_ocv[:, :, i * 128:(i + 1) * 128], s_xc1[i * 128:(i + 1) * 128, :]))
        if l == 0:
            emit_f(P, specs, 2, 1, FFD, False, False, s_mod, sel_d, W["wo"], fw1, fw3, fw2, ident_d)
            gather_x(s_x1)
            P.end_phase()
        else:
            emit_f(P, specs, 1, E, FFE, True, True, s_mod, sel_d, W["wo"], mw1, mw3, mw2, ident_d, wr_d=wr_d, id32_d=id32_d, fn_d=fn_d)
    P.close()
    return nc


NCORES = 8
_cache = {}


def _get(name, fn, *a):
    key = (name,) + a
    if key not in _cache:
        _cache[key] = fn(*a)
    return _cache[key]


def _wo_perm():
    idx = np.zeros(1024, np.int64)
    for j in range(4):
        for m in range(4):
            idx[j * 256 + m * 64:j * 256 + (m + 1) * 64] = np.arange(m * 256 + j * 64, m * 256 + (j + 1) * 64)
    return idx


def make_maps(inp):
    f32 = np.float32
    B, N, _ = inp["x"].shape
    M = inp["ctx"].shape[1]
    ident = np.eye(128, dtype=f32).astype(ml_dtypes.bfloat16)
    sel = np.zeros((2, 2, 128), f32)
    sel[0, 0, :] = 1.0
    sel[1, 1, :] = 1.0
    rc, rz = ret_consts()
    tabs = rope_tables(N)
    common = {"ident": ident, "id32": np.eye(128, dtype=f32), "sel": sel, "m3": swa_masks(), "retc": rc, "retz": rz,
              "i2": np.concatenate([np.eye(64, dtype=f32)] * 2, 0), "tabs": tabs,
              "fnb": np.ascontiguousarray(np.broadcast_to(inp["final_norm"][None, :], (128, D))).astype(f32),
              "fw1": inp["ffn_w1"], "fw3": inp["ffn_w3"], "fw2": inp["ffn_w2"],
              "mw1": inp["moe_w1"][0], "mw3": inp["moe_w3"][0], "mw2": inp["moe_w2"][0], "wr": inp["moe_router"][0]}
    woidx = _wo_perm()
    for l in range(2):
        common[f"wada{l}"] = inp["w_ada"][l]
        common[f"bada{l}"] = np.ascontiguousarray(inp["b_ada"][l][None, :])
        common[f"gn1_{l}"] = np.ascontiguousarray(inp["g_norm1"][l][None, :])
        common[f"gn2_{l}"] = np.ascontiguousarray(inp["g_norm2"][l][None, :])
        common[f"wo{l}"] = np.ascontiguousarray(inp["w_out"][l][woidx, :])
        common[f"lamv{l}"] = np.ascontiguousarray(inp["diff_lambda"][l].reshape(1, 128))
    maps = []
    dup = lambda a: np.ascontiguousarray(np.concatenate([a, a], 0)).astype(f32)
    for core in range(NCORES):
        b, j = core // 4, core % 4
        s = slice(j * 64, (j + 1) * 64)
        m = dict(common)
        m["xsh"] = np.ascontiguousarray(inp["x"][b, j * (N // 4):(j + 1) * (N // 4)])
        m["ctx"] = np.ascontiguousarray(inp["ctx"][b])
        cv = np.stack([inp["c"][b], inp["c_ctx"]], 0)
        m["cT"] = np.ascontiguousarray(cv.reshape(2, 8, 128).transpose(2, 1, 0)).astype(f32)
        tok, feat = head_cols(j)
        for l in range(2):
            two = lambda a: np.ascontiguousarray(np.concatenate([a[0][s], a[1][s]], 0)[:, None]).astype(f32)
            m[f"wt{l}"] = np.ascontiguousarray(inp["w_in"][l][:, tok])
            m[f"wf{l}"] = np.ascontiguousarray(inp["w_in"][l][:, feat])
            m[f"sink{l}"] = np.full((128, 1), inp["attn_sink"][l][j], f32)
            m[f"logit{l}"] = np.ascontiguousarray(np.broadcast_to(inp["ret_decay_logit"][l][:, j][None, :], (128, 2))).astype(f32)
            m[f"rgn{l}"] = np.ascontiguousarray(inp["ret_gn"][l][s, None])
            m[f"dgn{l}"] = np.ascontiguousarray(inp["diff_gn"][l][s, None])
            m[f"cw{l}"] = dup(inp["conv_w"][l][:, s].T)
            m[f"cb{l}"] = dup(inp["conv_b"][l][s, None])
            m[f"wa{l}"] = np.ascontiguousarray(np.concatenate([inp["lru_wa"][l][0, j], inp["lru_wa"][l][1, j]], 1))
            m[f"wx{l}"] = np.ascontiguousarray(np.concatenate([inp["lru_wx"][l][0, j], inp["lru_wx"][l][1, j]], 1))
            m[f"ba{l}"] = two(inp["lru_ba"][l])
            m[f"bx{l}"] = two(inp["lru_bx"][l])
            m[f"lam{l}"] = two(inp["lru_lambda"][l])
        maps.append(m)
    return maps


def kernel(**inputs):
    inp = {k: np.asarray(v) for k, v in inputs.items()}
    B, N, _ = inp["x"].shape
    M = inp["ctx"].shape[1]
    E = inp["moe_w1"].shape[1]
    nc = _get("fused", build_fused, N, M, E, inp["ffn_w1"].shape[2], inp["moe_w1"].shape[3])
    res = run_bass_kernel_spmd(nc, make_maps(inp), core_ids=list(range(NCORES))).results
    out = np.stack([np.concatenate([res[b * 4 + s]["xo"] for s in range(4)], 0) for b in range(B)], 0)
    return out.astype(np.float32)
```

```python
from contextlib import ExitStack
import math
import numpy as np
import ml_dtypes
import concourse.bass as bass
import concourse.mybir as mybir
from concourse.bass_utils import run_bass_kernel_spmd

F32 = mybir.dt.float32
BF16 = mybir.dt.bfloat16
I32 = mybir.dt.int32
AF = mybir.ActivationFunctionType
ALU = mybir.AluOpType
AX = mybir.AxisListType

ENGS = ("pe", "act", "dve", "pool", "sp")
DMAQ = ("sp", "act", "pool")
NDMASEM = 8
_ENGATTR = {"pe": "tensor", "act": "scalar", "dve": "vector", "pool": "gpsimd", "sp": "sync"}


class Op:
    __slots__ = ("eng", "fn", "dma", "cc", "raw", "war", "sig", "dsem", "dval", "has_dep")

    def __init__(self, eng, fn, dma, cc=False):
        self.eng = eng
        self.fn = fn
        self.dma = dma
        self.cc = cc
        self.raw = []
        self.war = []
        self.sig = None
        self.dsem = None
        self.dval = None
        self.has_dep = False


class Prog:
    def __init__(self, nc):
        self.nc = nc
        self.gs = ExitStack()
        self.sems = {e: self.gs.enter_context(nc.semaphore(f"s_{e}")) for e in ENGS}
        self.dsems = {e: [self.gs.enter_context(nc.semaphore(f"d_{e}{i}")) for i in range(NDMASEM)] for e in DMAQ}
        self.cnt = {e: 0 for e in ENGS}
        self.dcnt = {e: [0] * NDMASEM for e in DMAQ}
        self.dk = {e: 0 for e in DMAQ}
        self.ccsem = self.gs.enter_context(nc.semaphore("s_cc"))
        self.cccnt = 0
        self.waited = {e: {} for e in ENGS}
        self.barrier = None
        self.nsb = 0
        self.nphase = 0
        self._reset()

    def _reset(self):
        self.q = {e: [] for e in ENGS}
        self.res_w = {}
        self.res_r = {}
        self.es = ExitStack()

    def sb(self, shape, dt=F32, name=None, persist=False):
        self.nsb += 1
        st = self.gs if persist else self.es
        return st.enter_context(self.nc.sbuf_tensor(f"s{self.nsb}_" + (name or "t"), list(shape), dt))

    def ps(self, shape, dt=F32, name=None):
        self.nsb += 1
        return self.es.enter_context(self.nc.psum_tensor(f"p{self.nsb}_" + (name or "t"), list(shape), dt))

    def op(self, eng, meth, reads=(), writes=(), dma=False, cc=False, **kw):
        fn = (lambda e: getattr(e, meth)(**{k: (v(e) if callable(v) else v) for k, v in kw.items()}))
        o = Op(eng, fn, dma or cc, cc)
        raw = set()
        war = set()
        for r in reads:
            w = self.res_w.get(r)
            if w is not None:
                raw.add(w)
        for wr in writes:
            w = self.res_w.get(wr)
            if w is not None:
                war.add(w)
            for rd in self.res_r.get(wr, ()):
                war.add(rd)
        raw.discard(o)
        war -= raw
        o.raw = list(raw)
        o.war = list(war)
        for r in reads:
            self.res_r.setdefault(r, []).append(o)
        for wr in writes:
            self.res_w[wr] = o
            self.res_r[wr] = []
        self.q[eng].append(o)
        return o

    def dma(self, eng, out, in_, reads=(), writes=(), **kw):
        return self.op(eng, "dma_start", reads, writes, dma=True, out=out, in_=in_, **kw)

    def end_phase(self):
        nc = self.nc
        for e in ENGS:
            for o in self.q[e]:
                for d in o.raw:
                    d.has_dep = True
                for d in o.war:
                    if d.dma or d.eng != o.eng or o.dma:
                        d.has_dep = True
            for o in reversed(self.q[e]):
                if not o.dma:
                    o.has_dep = True
                    break
        for e in ENGS:
            for o in self.q[e]:
                if o.cc:
                    self.cccnt += 1
                    o.dsem = self.ccsem
                    o.dval = self.cccnt
                elif o.dma:
                    s = self.dk[e] % NDMASEM
                    self.dk[e] += 1
                    self.dcnt[e][s] += 16
                    o.dsem = self.dsems[e][s]
                    o.dval = self.dcnt[e][s]
                elif o.has_dep:
                    self.cnt[e] += 1
                    o.sig = self.cnt[e]
        prev_barrier = self.barrier
        nb = [(self.sems[e], self.cnt[e]) for e in ENGS if self.cnt[e] > 0]
        for e in DMAQ:
            for s in range(NDMASEM):
                if self.dcnt[e][s] > 0:
                    nb.append((self.dsems[e][s], self.dcnt[e][s]))
        if self.cccnt > 0:
            nb.append((self.ccsem, self.cccnt))
        self.barrier = nb
        sems = self.sems

        def run_engine(e, eng):
            waited = self.waited[e]

            def wait(sem, val):
                if waited.get(sem, 0) >= val:
                    return
                eng.wait_ge(sem, val)
                waited[sem] = val

            if prev_barrier is not None:
                for s, v in prev_barrier:
                    wait(s, v)
            for o in self.q[e]:
                for d, is_raw in [(d, True) for d in o.raw] + [(d, False) for d in o.war]:
                    if d.dma:
                        wait(d.dsem, d.dval)
                    elif d.eng == e and not o.dma:
                        if is_raw:
                            wait(sems[e], d.sig)
                    else:
                        wait(sems[d.eng], d.sig)
                if o.cc:
                    if o.dval > 1:
                        wait(o.dsem, o.dval - 1)
                    o.fn(eng).then_inc(o.dsem)
                elif o.dma:
                    if o.dval > 16:
                        wait(o.dsem, o.dval - 16)
                    o.fn(eng).then_inc(o.dsem, 16)
                else:
                    ins = o.fn(eng)
                    if o.sig is not None:
                        ins.then_inc(sems[e], 1)
            if e == "sp":
                for s, v in nb:
                    wait(s, v)

        with nc.Block() as block:
            @block.tensor
            def _(eng):
                run_engine("pe", eng)

            @block.scalar
            def _(eng):
                run_engine("act", eng)

            @block.vector
            def _(eng):
                run_engine("dve", eng)

            @block.gpsimd
            def _(eng):
                run_engine("pool", eng)

            @block.sync
            def _(eng):
                run_engine("sp", eng)
        self.es.close()
        self._reset()
        self.nphase += 1

    def emit(self):
        self.end_phase()

    def close(self):
        self.es.close()
        self.gs.close()

D = 1024
EPS = 1e-6
SC_SWA = 64 ** -0.5
SC_RET = 64 ** -0.5
SC_DIFF = 32 ** -0.5


class Rot:
    def __init__(self, P, name, n, shape, dt=F32, psum=False):
        self.bufs = [(P.ps(shape, dt, f"{name}{i}") if psum else P.sb(shape, dt, f"{name}{i}")) for i in range(n)]
        self.name = name
        self.i = 0

    def next(self):
        k = self.i % len(self.bufs)
        self.i += 1
        return self.bufs[k], f"{self.name}{k}"


def _r(a, b):
    return list(range(a, b))


TOK_COLS = _r(0, 256) + _r(256, 384) + _r(1536, 1792) + _r(1792, 2048) + _r(512, 768) + _r(768, 1024) + _r(384, 512) + _r(1024, 1280) + _r(2048, 2304)
FEAT_COLS = _r(1280, 1536) + _r(2304, 2560) + _r(2560, 2816)


def rope_tables(n_lat):
    f32 = np.float32
    n = np.arange(n_lat)
    row = (n // 64).astype(f32)
    col = (n % 64).astype(f32)

    def axial(dim):
        nf = dim // 4
        inv = (f32(10000.0) ** (-(np.arange(nf, dtype=f32)) / f32(nf))).astype(f32)
        ang = np.concatenate([row[:, None] * inv, col[:, None] * inv], -1).astype(f32)
        return np.cos(ang).astype(f32), np.sin(ang).astype(f32)

    ca, sa = axial(64)
    cd, sd = axial(32)
    nf = 32
    inv = (f32(10000.0) ** (-(np.arange(nf, dtype=f32)) / f32(nf))).astype(f32)
    ang = (n.astype(f32)[:, None] * inv).astype(f32)
    cl, sl = np.cos(ang).astype(f32), np.sin(ang).astype(f32)
    return np.ascontiguousarray(np.concatenate([ca, sa, cd, sd, cl, sl], -1).astype(f32))


def mod_rows(P, cT, wada, bada, ncols, pmrot, name, extra=None):
    scT = P.sb([128, 8, 2], F32, name + "_scT")
    P.dma("sp", scT[:], cT, writes=[name + "_scT"])
    P.op("act", "activation", out=scT[:], in_=scT[:], func=AF.Silu, reads=[name + "_scT"], writes=[name + "_scT"])
    mrow = P.sb([2, ncols], F32, name + "_mrow")
    brot = Rot(P, name + "_brow", 2, [2, 512], F32)
    xrot_ = Rot(P, name + "_xrow", 2, [2, 512], F32)
    g2rot = Rot(P, name + "_g2row", 2, [2, 512], F32)
    wrot = Rot(P, name + "_wA", 1, [128, 8, 512], F32)
    wv = wada.rearrange("(k p) c -> p k c", p=128)
    ntot = ncols if extra is None else extra[0]
    for cb in range(ntot // 512):
        cs = slice(cb * 512, (cb + 1) * 512)
        wA, wk = wrot.next()
        P.dma("sp", wA[:], wv[:, :, cs], writes=[wk])
        brow, bk = brot.next()
        P.dma("sp", brow[0:1, :], bada[:, cs], writes=[bk])
        P.dma("sp", brow[1:2, :], bada[:, cs], writes=[bk])
        pm, pk = pmrot.next()
        for k in range(8):
            P.op("pe", "matmul", out=pm[0:2, :], lhsT=scT[:, k, :], rhs=wA[:, k, :], start=(k == 0), stop=(k == 7),
                 reads=[name + "_scT", wk], writes=[pk])
        if cb * 512 < ncols:
            P.op("dve", "tensor_tensor", out=mrow[:, cs], in0=pm[0:2, :], in1=brow[:, :], op=ALU.add, reads=[pk, bk], writes=[name + "_mrow"])
        else:
            xr, xk = xrot_.next()
            P.op("dve", "tensor_tensor", out=xr[:, :], in0=pm[0:2, :], in1=brow[:, :], op=ALU.add, reads=[pk, bk], writes=[xk])
            oc = cb * 512 - ncols
            if 2048 <= oc < 3072:
                g2, g2k = g2rot.next()
                P.dma("sp", g2[0:1, :], extra[1][:, oc - 2048:oc - 2048 + 512], writes=[g2k])
                P.dma("sp", g2[1:2, :], extra[1][:, oc - 2048:oc - 2048 + 512], writes=[g2k])
                P.op("dve", "scalar_tensor_tensor", out=xr[:, :], in0=xr[:, :], scalar=1.0, in1=g2[:, :], op0=ALU.add, op1=ALU.mult, reads=[xk, g2k], writes=[xk])
            P.dma("sp", extra[2][:, oc:oc + 512], xr[:, :], reads=[xk])
    return mrow, name + "_mrow"


def bcast_rows(P, row, rowkey, sel, v, pmrot, out, outkey):
    for hh in range(2):
        pm, pk = pmrot.next()
        P.op("pe", "matmul", out=pm[:, :], lhsT=sel[:, v, :], rhs=row[:, hh * 512:(hh + 1) * 512], start=True, stop=True,
             reads=[rowkey, "sel"], writes=[pk])
        P.op("act", "copy", out=out[:, hh * 512:(hh + 1) * 512], in_=pm[:, :], reads=[pk], writes=[outkey])


def rms_rstd(P, xt, xk, rots):
    sq, sqk = rots["sq"].next()
    st, stk = rots["st"].next()
    P.op("act", "activation", out=sq[:], in_=xt, func=AF.Square, accum_out=st[:, 0:1], reads=[xk], writes=[sqk, stk])
    P.op("dve", "tensor_scalar", out=st[:, 1:2], in0=st[:, 0:1], scalar1=1.0 / D, scalar2=EPS, op0=ALU.mult, op1=ALU.add, reads=[stk], writes=[stk])
    P.op("act", "activation", out=st[:, 2:3], in_=st[:, 1:2], func=AF.Sqrt, reads=[stk], writes=[stk])
    P.op("dve", "reciprocal", out=st[:, 3:4], in_=st[:, 2:3], reads=[stk], writes=[stk])
    return st, stk, sq, sqk


def norm_mod_transpose(P, xt, xk, Gb, Sb, gkeys, ident, hT, hTk, col0, rots):
    st, stk, sq, sqk = rms_rstd(P, xt, xk, rots)
    P.op("dve", "scalar_tensor_tensor", out=sq[:], in0=xt, scalar=st[:, 3:4], in1=Gb[:], op0=ALU.mult, op1=ALU.mult,
         reads=[xk, stk, gkeys[0]], writes=[sqk])
    hb, hbk = rots["hb"].next()
    P.op("pool", "tensor_tensor", out=hb[:], in0=sq[:], in1=Sb[:], op=ALU.add, reads=[sqk, gkeys[1]], writes=[hbk])
    ptr, ptk = rots["ptr"].next()
    for k in range(8):
        P.op("pe", "transpose", out=ptr[:, k * 128:(k + 1) * 128], in_=hb[:, k * 128:(k + 1) * 128], identity=ident[:],
             reads=[hbk, "ident"], writes=[ptk])
    P.op("act", "copy", out=hT[:, :, col0:col0 + 128], in_=ptr[:, :].rearrange("p (k t) -> p k t", k=8), reads=[ptk], writes=[hTk])


def head_cols(j):
    g = j // 2
    r = lambda a, n=64: list(range(a, a + n))
    tok = (r(1536 + j * 64) + r(0 + j * 64) + r(1792 + j * 64) + r(256 + g * 64) + r(512 + j * 64) + r(768 + j * 64)
           + r(384 + g * 64) + r(1024 + j * 64) + r(2048 + j * 64))
    feat = r(1280 + j * 64) + r(2304 + j * 64) + r(2560 + j * 64)
    return tok, feat


def emit_p(P, N, M, x_lat, x_ctx, cT, wada, bada, gn, gn2, wt_d, wf_d, tabs, ident_d, sel_d,
           s_qA, s_kB, s_rq, s_rk, s_tok, s_f32, s_modrows):
    nlt, nct = N // 128, M // 128
    ident = P.sb([128, 128], BF16, "ident")
    P.dma("sp", ident[:], ident_d, writes=["ident"])
    sel = P.sb([2, 2, 128], F32, "sel")
    P.dma("sp", sel[:], sel_d, writes=["sel"])
    Wt = P.sb([128, 8, 576], BF16, "Wt")
    Wf = P.sb([128, 8, 192], BF16, "Wf")
    P.dma("pool", Wt[:], wt_d.rearrange("(k p) c -> p k c", p=128), writes=["Wt"])
    P.dma("pool", Wf[:], wf_d.rearrange("(k p) c -> p k c", p=128), writes=["Wf"])
    pmrot = Rot(P, "pm", 3, [128, 512], F32, psum=True)
    mrow, mk = mod_rows(P, cT, wada, bada, 2048, pmrot, "mod", extra=(6144, gn2, s_modrows))
    grow = P.sb([2, D], F32, "grow")
    P.dma("sp", grow[0:1, :], gn, writes=["grow"])
    P.dma("sp", grow[1:2, :], gn, writes=["grow"])
    P.op("dve", "scalar_tensor_tensor", out=grow[:], in0=mrow[:, 1024:2048], scalar=1.0, in1=grow[:], op0=ALU.add, op1=ALU.mult,
         reads=[mk, "grow"], writes=["grow"])
    Gb = [P.sb([128, D], F32, f"Gb{v}") for v in range(2)]
    Sb = [P.sb([128, D], F32, f"Sb{v}") for v in range(2)]
    for v in range(2):
        bcast_rows(P, grow, "grow", sel, v, pmrot, Gb[v], f"Gb{v}")
        bcast_rows(P, mrow[:, 0:1024], mk, sel, v, pmrot, Sb[v], f"Sb{v}")
    rots = {
        "sq": Rot(P, "sq", 2, [128, D], F32),
        "st": Rot(P, "st", 4, [128, 4], F32),
        "hb": Rot(P, "hb", 2, [128, D], BF16),
        "ptr": Rot(P, "ptr", 2, [128, 1024], BF16, psum=True),
    }
    xrot = Rot(P, "xt", 3, [128, D], F32)
    hTrot = Rot(P, "hT", 2, [128, 8, 512], BF16)
    ptokrot = Rot(P, "ptok", 2, [128, 576], F32)
    tabrot = Rot(P, "tab", 2, [128, 160], F32)
    tmprot = Rot(P, "rtmp", 2, [128, 4, 128], F32)
    qkrot = Rot(P, "qk16", 2, [128, 384], BF16)
    vstrot = Rot(P, "vst", 2, [128, 256], BF16)
    ptArot = Rot(P, "ptA", 2, [128, 4, 128], BF16, psum=True)
    f16rot = Rot(P, "f16T", 2, [128, 4, 512], BF16)
    f32rot = Rot(P, "f32T", 2, [128, 2, 512], F32)
    pieces = [(0, 64, SC_DIFF), (64, 128, SC_SWA), (128, 320, 1.0), (320, 384, SC_RET), (384, 512, 1.0)]
    ei = 0
    groups = [(list(range(t, min(t + 4, nct))), 1) for t in range(0, nct, 4)] + [(list(range(t, min(t + 4, nlt))), 0) for t in range(0, nlt, 4)]
    def do_norm(gi, ti):
        tiles, v = groups[gi]
        src = x_ctx if v == 1 else x_lat
        tile = tiles[ti]
        xt, xk = xrot.next()
        P.dma("sp", xt[:], (src(tile) if callable(src) else src[tile * 128:(tile + 1) * 128, :]), writes=[xk])
        hT, hTk = hts[gi]
        norm_mod_transpose(P, xt[:], xk, Gb[v], Sb[v], (f"Gb{v}", f"Sb{v}"), ident, hT, hTk, ti * 128, rots)

    hts = {}
    st1 = {}

    def stage1(gi, ti):
        tiles, v = groups[gi]
        tile = tiles[ti]
        T0 = (tiles[0] * 128) if v == 1 else (M + tiles[0] * 128)
        hT, hTk = hts[gi]
        ptok, ptokk = ptokrot.next()
        pm, pk = pmrot.next()
        for k in range(8):
            P.op("pe", "matmul", out=pm[:, :], lhsT=hT[:, k, ti * 128:(ti + 1) * 128], rhs=Wt[:, k, 0:512], start=(k == 0), stop=(k == 7),
                 reads=[hTk, "Wt"], writes=[pk])
        for (c0, c1, sc) in pieces:
            P.op("act", "activation", out=ptok[:, c0:c1], in_=pm[:, c0:c1], func=AF.Copy, scale=float(sc), reads=[pk], writes=[ptokk])
        pm2, pk2 = pmrot.next()
        for k in range(8):
            P.op("pe", "matmul", out=pm2[:, 0:64], lhsT=hT[:, k, ti * 128:(ti + 1) * 128], rhs=Wt[:, k, 512:576], start=(k == 0), stop=(k == 7),
                 reads=[hTk, "Wt"], writes=[pk2])
        P.op("act", "copy", out=ptok[:, 512:576], in_=pm2[:, 0:64], reads=[pk2], writes=[ptokk])
        qk, qkk = qkrot.next()
        if v == 0:
            tab, tabk = tabrot.next()
            P.dma("sp", tab[:], tabs[tile * 128:(tile + 1) * 128, :], writes=[tabk])

            def views(t_, kind):
                if kind == "diff":
                    vv = t_[:, 0:256].rearrange("p (a c) -> p a c", a=2)[:, :, 0:64].rearrange("p a (h two d) -> p a h two d", h=2, two=2)
                    return vv[:, :, :, 0, :], vv[:, :, :, 1, :], [128, 2, 2, 16]
                if kind == "swa":
                    vv = t_[:, 0:256].rearrange("p (a c) -> p a c", a=2)[:, :, 64:128].rearrange("p a (two d) -> p a two d", two=2)
                    return vv[:, :, 0, :], vv[:, :, 1, :], [128, 2, 32]
                vv = t_[:, 256:384].rearrange("p (h two d) -> p h two d", h=2, two=2)
                return vv[:, :, 0, :], vv[:, :, 1, :], [128, 2, 32]
            for ki, (kind, hd, tc0) in enumerate((("diff", 16, 64), ("swa", 32, 0), ("ret", 32, 96))):
                x1, x2, shp = views(ptok, kind)
                o1, o2, _ = views(qk, kind)
                cosb = tab[:, tc0:tc0 + hd]
                sinb = tab[:, tc0 + hd:tc0 + 2 * hd]
                for _ in range(len(shp) - 2):
                    cosb = cosb.unsqueeze(1)
                    sinb = sinb.unsqueeze(1)
                cosb = cosb.to_broadcast(shp)
                sinb = sinb.to_broadcast(shp)
                tmp, tmpk = tmprot.next()
                nel = int(np.prod(shp[1:]))
                if len(shp) == 4:
                    T_ = [tmp[:, i, 0:nel].rearrange("p (a h d) -> p a h d", a=shp[1], h=shp[2]) for i in range(4)]
                else:
                    T_ = [tmp[:, i, 0:nel].rearrange("p (a d) -> p a d", a=shp[1]) for i in range(4)]
                e1 = "dve" if (ti + ki) % 2 == 0 else "pool"
                e2 = "pool" if (ti + ki) % 2 == 0 else "dve"
                P.op(e1, "tensor_tensor", out=T_[0], in0=x1, in1=cosb, op=ALU.mult, reads=[ptokk, tabk], writes=[tmpk + "a"])
                P.op(e1, "tensor_tensor", out=T_[1], in0=x2, in1=sinb, op=ALU.mult, reads=[ptokk, tabk], writes=[tmpk + "b"])
                P.op(e2, "tensor_tensor", out=T_[2], in0=x2, in1=cosb, op=ALU.mult, reads=[ptokk, tabk], writes=[tmpk + "c"])
                P.op(e2, "tensor_tensor", out=T_[3], in0=x1, in1=sinb, op=ALU.mult, reads=[ptokk, tabk], writes=[tmpk + "d"])
                P.op(e1, "tensor_tensor", out=o1, in0=T_[0], in1=T_[1], op=ALU.subtract, reads=[tmpk + "a", tmpk + "b"], writes=[qkk])
                P.op(e2, "tensor_tensor", out=o2, in0=T_[2], in1=T_[3], op=ALU.add, reads=[tmpk + "c", tmpk + "d"], writes=[qkk])
        else:
            P.op("dve", "tensor_copy", out=qk[:, :], in_=ptok[:, 0:384], reads=[ptokk], writes=[qkk])
        vst, vstk = vstrot.next()
        P.op("pool", "tensor_copy", out=vst[:, 64:256], in_=ptok[:, 384:576], reads=[ptokk], writes=[vstk])
        P.op("pool", "tensor_copy", out=vst[:, 0:64], in_=qk[:, 320:384], reads=[qkk], writes=[vstk])
        P.dma("sp", s_tok[T0 + ti * 128:T0 + (ti + 1) * 128, :], vst[:], reads=[vstk], writes=["s_tok"])
        st1[(gi, ti)] = (qk, qkk)

    def stage2(gi, ti, f16T, f16k):
        qk, qkk = st1.pop((gi, ti))
        ptA, ptAk = ptArot.next()
        P.op("pe", "transpose", out=ptA[:, 0, :], in_=qk[:, 0:128], identity=ident[:], reads=[qkk, "ident"], writes=[ptAk])
        P.op("pe", "transpose", out=ptA[:, 1, :], in_=qk[:, 128:256], identity=ident[:], reads=[qkk, "ident"], writes=[ptAk])
        P.op("pe", "transpose", out=ptA[0:64, 2, :], in_=qk[:, 256:320], identity=ident[:], reads=[qkk, "ident"], writes=[ptAk])
        P.op("pe", "transpose", out=ptA[0:64, 3, :], in_=qk[:, 320:384], identity=ident[:], reads=[qkk, "ident"], writes=[ptAk])
        P.op("dve", "tensor_copy", out=f16T[:, 0:2, ti * 128:(ti + 1) * 128], in_=ptA[:, 0:2, :], reads=[ptAk], writes=[f16k])
        P.op("dve", "tensor_copy", out=f16T[0:64, 2:4, ti * 128:(ti + 1) * 128], in_=ptA[0:64, 2:4, :], reads=[ptAk], writes=[f16k])

    hts[0] = hTrot.next()
    for ti in range(len(groups[0][0])):
        do_norm(0, ti)
    for gi, (tiles, v) in enumerate(groups):
        ntok = len(tiles) * 128
        T0 = (tiles[0] * 128) if v == 1 else (M + tiles[0] * 128)
        hT, hTk = hts[gi]
        nxt = gi + 1 if gi + 1 < len(groups) else None
        if nxt is not None:
            hts[nxt] = hTrot.next()
        f16T, f16k = f16rot.next()
        for ti in range(len(tiles)):
            stage1(gi, ti)
            if nxt is not None and ti < len(groups[nxt][0]):
                do_norm(nxt, ti)
            if ti > 0:
                stage2(gi, ti - 1, f16T, f16k)
        if nxt is not None:
            for ti in range(len(tiles), len(groups[nxt][0])):
                do_norm(nxt, ti)
        stage2(gi, len(tiles) - 1, f16T, f16k)
        P.dma("sp", s_qA[:, T0:T0 + ntok], f16T[:, 0, 0:ntok], reads=[f16k], writes=["s_qA"])
        P.dma("sp", s_kB[:, T0:T0 + ntok], f16T[:, 1, 0:ntok], reads=[f16k], writes=["s_kB"])
        P.dma("sp", s_rq[:, T0:T0 + ntok], f16T[0:64, 2, 0:ntok], reads=[f16k], writes=["s_rq"])
        P.dma("sp", s_rk[:, T0:T0 + ntok], f16T[0:64, 3, 0:ntok], reads=[f16k], writes=["s_rk"])
        f32T, f32k = f32rot.next()
        for fb, (c0, c1) in enumerate(((0, 128), (128, 192))):
            w = c1 - c0
            pm, pk = pmrot.next()
            for k in range(8):
                P.op("pe", "matmul", out=pm[0:w, 0:ntok], lhsT=Wf[:, k, c0:c1], rhs=hT[:, k, 0:ntok], start=(k == 0), stop=(k == 7), reads=[hTk, "Wf"], writes=[pk])
            P.op("act", "copy", out=f32T[0:w, fb, 0:ntok], in_=pm[0:w, 0:ntok], reads=[pk], writes=[f32k])
        P.dma("sp", s_f32[0:128, T0:T0 + ntok], f32T[:, 0, 0:ntok], reads=[f32k], writes=["s_f32"])
        P.dma("sp", s_f32[128:192, T0:T0 + ntok], f32T[0:64, 1, 0:ntok], reads=[f32k], writes=["s_f32"])
        del hts[gi]
    P.end_phase()


def lam_scalar_col(P, lamv_d, lam_init, pmrot):
    lv = P.sb([1, 128], F32, "lv")
    P.dma("sp", lv[:], lamv_d, writes=["lv"])
    pr = P.sb([1, 64], F32, "lvpr")
    lvv = lv[:, :].rearrange("o (a b d) -> o a b d", a=2, b=2)
    P.op("dve", "tensor_tensor", out=pr[:, :].rearrange("o (a d) -> o a d", a=2), in0=lvv[:, :, 0, :], in1=lvv[:, :, 1, :], op=ALU.mult, reads=["lv"], writes=["lvpr"])
    sm = P.sb([1, 4], F32, "lvsm")
    P.op("dve", "tensor_reduce", out=sm[:, 0:2], in_=pr[:, :].rearrange("o (a d) -> o a d", a=2), axis=AX.X, op=ALU.add, reads=["lvpr"], writes=["lvsm"])
    P.op("act", "activation", out=sm[:, 0:2], in_=sm[:, 0:2], func=AF.Exp, reads=["lvsm"], writes=["lvsm"])
    P.op("dve", "tensor_tensor", out=sm[:, 2:3], in0=sm[:, 0:1], in1=sm[:, 1:2], op=ALU.subtract, reads=["lvsm"], writes=["lvsm"])
    P.op("dve", "tensor_scalar", out=sm[:, 3:4], in0=sm[:, 2:3], scalar1=float(lam_init), scalar2=-1.0, op0=ALU.add, op1=ALU.mult, reads=["lvsm"], writes=["lvsm"])
    P.op("dve", "tensor_scalar", out=sm[:, 2:3], in0=sm[:, 2:3], scalar1=float(lam_init), scalar2=None, op0=ALU.add, reads=["lvsm"], writes=["lvsm"])
    ones1 = P.sb([1, 128], F32, "ones1")
    P.op("pool", "memset", ap=ones1[:], constant=1.0, writes=["ones1"])
    pm, pk = pmrot.next()
    P.op("pe", "matmul", out=pm[:, 0:2], lhsT=ones1[:, :], rhs=sm[:, 2:4], start=True, stop=True, reads=["ones1", "lvsm"], writes=[pk])
    lamc = P.sb([128, 2], F32, "lamc")
    P.op("dve", "tensor_copy", out=lamc[:], in_=pm[:, 0:2], reads=[pk], writes=["lamc"])
    return lamc


def headnorm_T(P, o32, ok, ncol, ones64, eprot, gcol, gkey, mul, out_ap, outk, tmprot, eng_a="dve", eng_b="pool"):
    sq, sqk = tmprot.next()
    P.op(eng_b, "tensor_tensor", out=sq[0:64, 0:ncol], in0=o32, in1=o32, op=ALU.mult, reads=[ok], writes=[sqk])
    pm, pk = eprot.next()
    P.op("pe", "matmul", out=pm[0:64, 0:ncol], lhsT=ones64[0:64, 0:64], rhs=sq[0:64, 0:ncol], start=True, stop=True, reads=[sqk, "ones64"], writes=[pk])
    P.op(eng_a, "tensor_scalar", out=sq[0:64, 0:ncol], in0=pm[0:64, 0:ncol], scalar1=1.0 / 64, scalar2=EPS, op0=ALU.mult, op1=ALU.add, reads=[pk], writes=[sqk])
    P.op("act", "activation", out=sq[0:64, 0:ncol], in_=sq[0:64, 0:ncol], func=AF.Sqrt, reads=[sqk], writes=[sqk])
    P.op(eng_a, "reciprocal", out=sq[0:64, 0:ncol], in_=sq[0:64, 0:ncol], reads=[sqk], writes=[sqk])
    P.op(eng_a, "tensor_tensor", out=sq[0:64, 0:ncol], in0=sq[0:64, 0:ncol], in1=o32, op=ALU.mult, reads=[sqk, ok], writes=[sqk])
    P.op(eng_a, "tensor_scalar", out=out_ap, in0=sq[0:64, 0:ncol], scalar1=gcol, scalar2=float(mul), op0=ALU.mult, op1=ALU.mult, reads=[sqk, gkey], writes=[outk])


def emit_diff(P, N, M, need_ctx, lam_init, qT_d, kT_d, v_d, lamv_d, gn_d, oT_d):
    T = N + M
    NKB = T // 128

    qT = P.sb([64, T], BF16, "qT")
    kT = P.sb([64, T], BF16, "kT")
    V1 = P.sb([128, NKB, 128], BF16, "V1")
    nchunk = 4
    cw = T // nchunk
    for i in range(nchunk):
        P.dma("sp", qT[:, i * cw:(i + 1) * cw], qT_d[:, i * cw:(i + 1) * cw], writes=["qT"])
        P.dma("sp", kT[:, i * cw:(i + 1) * cw], kT_d[:, i * cw:(i + 1) * cw], writes=["kT"])
    vv = v_d.rearrange("(kb p) d -> p kb d", p=128)
    step = max(1, NKB // 8)
    for k0 in range(0, NKB, step):
        k1 = min(NKB, k0 + step)
        P.dma("sp", V1[:, k0:k1, 0:64], vv[:, k0:k1, :], writes=["V1"])
    P.op("pool", "memset", ap=V1[:, :, 64:65], constant=1.0, writes=["V1"])
    P.op("pool", "memset", ap=V1[:, :, 65:128], constant=0.0, writes=["V1"])
    gcol = P.sb([64, 1], F32, "gcol")
    P.dma("sp", gcol[:], gn_d, writes=["gcol"])
    ones64 = P.sb([128, 64], F32, "ones64")
    P.op("pool", "memset", ap=ones64[:], constant=1.0, writes=["ones64"])
    srot = Rot(P, "S", 3, [128, 512], F32, psum=True)
    eprot = Rot(P, "ep", 1, [128, 512], F32, psum=True)
    accrot = [Rot(P, f"acc{m}_", 2, [128, 512], F32, psum=True) for m in range(2)]
    lamc = lam_scalar_col(P, lamv_d, lam_init, eprot)
    ptrot = Rot(P, "PT", 6, [128, 512], BF16)
    rlrot = Rot(P, "rl", 2, [128, 2, 512], F32)
    bcrot = Rot(P, "bc", 2, [64, 2, 512], F32)
    t32rot = Rot(P, "t32", 3, [64, 512], F32)
    orot = Rot(P, "ob", 2, [64, 512], BF16)

    def epilogue(acc, q0, nq):
        rl, rlk = rlrot.next()
        for m in range(2):
            P.op("dve", "reciprocal", out=rl[64:65, m, 0:nq], in_=acc[m][0][64:65, 0:nq], reads=[acc[m][1]], writes=[rlk])
        P.op("dve", "tensor_scalar", out=rl[64:65, 1, 0:nq], in0=rl[64:65, 1, 0:nq], scalar1=lamc[64:65, 1:2], scalar2=None, op0=ALU.mult, reads=[rlk, "lamc"], writes=[rlk])
        bc, bck = bcrot.next()
        for m in range(2):
            pm, pk = eprot.next()
            P.op("pe", "matmul", out=pm[0:64, 0:nq], lhsT=ones64[64:65, 0:64], rhs=rl[64:65, m, 0:nq], start=True, stop=True, reads=[rlk, "ones64"], writes=[pk])
            P.op("dve", "tensor_copy", out=bc[:, m, 0:nq], in_=pm[0:64, 0:nq], reads=[pk], writes=[bck])
        t0, t0k = t32rot.next()
        t1, t1k = t32rot.next()
        P.op("dve", "tensor_tensor", out=t0[:, 0:nq], in0=acc[0][0][0:64, 0:nq], in1=bc[:, 0, 0:nq], op=ALU.mult, reads=[acc[0][1], bck], writes=[t0k])
        P.op("dve", "tensor_tensor", out=t1[:, 0:nq], in0=acc[1][0][0:64, 0:nq], in1=bc[:, 1, 0:nq], op=ALU.mult, reads=[acc[1][1], bck], writes=[t1k])
        P.op("pool", "tensor_tensor", out=t0[:, 0:nq], in0=t0[:, 0:nq], in1=t1[:, 0:nq], op=ALU.add, reads=[t0k, t1k], writes=[t0k])
        ob, obk = orot.next()
        headnorm_T(P, t0[:, 0:nq], t0k, nq, ones64, eprot, gcol[:, 0:1], "gcol", 1.0 - lam_init, ob[:, 0:nq], obk, t32rot)
        P.dma("sp", oT_d[:, q0:q0 + nq], ob[:, 0:nq], reads=[obk])
    QG = 2
    sgroups = []
    if need_ctx:
        sgroups.append(([(0, M)], 0, M // 128))
    lat = [(g0, min(512, T - g0)) for g0 in range(M, T, 512)]
    for i in range(0, len(lat), QG):
        sgroups.append((lat[i:i + QG], 0, NKB))
    LOOK = 1
    for (qgs, kb0, kb1) in sgroups:
        ng = len(qgs)
        acc = [[accrot[m].next() for m in range(2)] for _ in range(ng)]
        steps = [(kb, m) for kb in range(kb0, kb1) for m in range(2)]
        pts = {}

        def issue_s(i):
            kb, m = steps[i]
            for gi, (q0, nq) in enumerate(qgs):
                S, Sk = srot.next()
                P.op("pe", "matmul", out=S[:, 0:nq], lhsT=kT[m * 32:(m + 1) * 32, kb * 128:(kb + 1) * 128], rhs=qT[m * 32:(m + 1) * 32, q0:q0 + nq],
                     start=True, stop=True, reads=["kT", "qT"], writes=[Sk])
                PT, PTk = ptrot.next()
                P.op("act", "activation", out=PT[:, 0:nq], in_=S[:, 0:nq], func=AF.Exp, reads=[Sk], writes=[PTk])
                pts[(i, gi)] = (PT, PTk)

        def issue_pv(i):
            kb, m = steps[i]
            for gi, (q0, nq) in enumerate(qgs):
                PT, PTk = pts.pop((i, gi))
                P.op("pe", "matmul", out=acc[gi][m][0][:, 0:nq], lhsT=V1[:, kb, :], rhs=PT[:, 0:nq], start=(kb == kb0), stop=(kb == kb1 - 1),
                     reads=["V1", PTk], writes=[acc[gi][m][1]])
        for i in range(len(steps) + LOOK):
            if i < len(steps):
                issue_s(i)
            if i - LOOK >= 0:
                issue_pv(i - LOOK)
        for gi, (q0, nq) in enumerate(qgs):
            epilogue(acc[gi], q0, nq)
    P.end_phase()


def swa_masks():
    b = np.arange(128)[:, None]
    a = np.arange(128)[None, :]
    m = np.concatenate([(b <= a), np.ones((128, 128), bool), (a <= b)], 1)
    return m.astype(np.float32).astype(ml_dtypes.bfloat16)


def emit_swa(P, N, M, need_ctx, qT_d, kT_d, v_d, sink_d, m3_d, oT_d):
    T = N + M
    NKB = T // 128
    MB = M // 128
    nb = N // 128

    qT = P.sb([64, T], BF16, "qT")
    kT = P.sb([64, T], BF16, "kT")
    V1 = P.sb([128, NKB, 65], BF16, "V1")
    nchunk = 4
    cw = T // nchunk
    for i in range(nchunk):
        P.dma("sp", qT[:, i * cw:(i + 1) * cw], qT_d[:, i * cw:(i + 1) * cw], writes=["qT"])
        P.dma("sp", kT[:, i * cw:(i + 1) * cw], kT_d[:, i * cw:(i + 1) * cw], writes=["kT"])
    vv = v_d.rearrange("(kb p) d -> p kb d", p=128)
    step = max(1, NKB // 8)
    for k0 in range(0, NKB, step):
        k1 = min(NKB, k0 + step)
        P.dma("sp", V1[:, k0:k1, 0:64], vv[:, k0:k1, :], writes=["V1"])
    P.op("pool", "memset", ap=V1[:, :, 64:65], constant=1.0, writes=["V1"])
    m3 = P.sb([128, 384], BF16, "m3")
    P.dma("sp", m3[:], m3_d, writes=["m3"])
    es = P.sb([128, 1], F32, "es")
    P.dma("sp", es[:], sink_d, writes=["es"])
    P.op("act", "activation", out=es[:], in_=es[:], func=AF.Exp, reads=["es"], writes=["es"])
    ones64 = P.sb([128, 64], F32, "ones64")
    P.op("pool", "memset", ap=ones64[:], constant=1.0, writes=["ones64"])
    accrot = Rot(P, "acc", 2, [128, 512], F32, psum=True)
    srot = Rot(P, "S", 3, [128, 512], F32, psum=True)
    eprot = Rot(P, "ep", 2, [128, 512], F32, psum=True)
    ptrot = Rot(P, "PT", 4, [128, 512], BF16)
    rlrot = Rot(P, "rl", 2, [128, 512], F32)
    bcrot = Rot(P, "bc", 2, [64, 512], F32)
    orot = Rot(P, "ob", 2, [64, 512], BF16)

    qgroups = []
    if need_ctx:
        qgroups.append((0, M, None))
    for s0 in range(0, nb, 4):
        qgroups.append((M + s0 * 128, min(4, nb - s0) * 128, s0))
    ei = 0
    for (q0, nq, s0) in qgroups:
        acc, acck = accrot.next()
        items = [(kb, 0, nq, None) for kb in range(MB)]
        if s0 is not None:
            ns = nq // 128
            for r in range(s0 - 1, s0 + ns + 1):
                if r < 0 or r >= nb:
                    continue
                sa = max(r - 1, s0)
                sb_ = min(r + 1, s0 + ns - 1)
                items.append((MB + r, (sa - s0) * 128, (sb_ - s0 + 1) * 128, (sa - (r - 1)) * 128))
        for ii, (kb, c0, c1, mc) in enumerate(items):
            w = c1 - c0
            S, Sk = srot.next()
            P.op("pe", "matmul", out=S[:, 0:w], lhsT=kT[:, kb * 128:(kb + 1) * 128], rhs=qT[:, q0 + c0:q0 + c1], start=True, stop=True,
                 reads=["kT", "qT"], writes=[Sk])
            PT, PTk = ptrot.next()
            P.op("act", "activation", out=PT[:, 0:w], in_=S[:, 0:w], func=AF.Exp, reads=[Sk], writes=[PTk])
            if mc is not None:
                eng = "dve" if ei % 2 == 0 else "pool"
                ei += 1
                P.op(eng, "tensor_tensor", out=PT[:, 0:w], in0=PT[:, 0:w], in1=m3[:, mc:mc + w], op=ALU.mult, reads=[PTk, "m3"], writes=[PTk])
            P.op("pe", "matmul", out=acc[0:65, c0:c1], lhsT=V1[:, kb, :], rhs=PT[:, 0:w], start=(ii == 0), stop=(ii == len(items) - 1),
                 reads=["V1", PTk], writes=[acck])
        rl, rlk = rlrot.next()
        P.op("dve", "tensor_scalar", out=rl[64:65, 0:nq], in0=acc[64:65, 0:nq], scalar1=es[64:65, 0:1], scalar2=None, op0=ALU.add, reads=[acck, "es"], writes=[rlk])
        P.op("dve", "reciprocal", out=rl[64:65, 0:nq], in_=rl[64:65, 0:nq], reads=[rlk], writes=[rlk])
        pm, pk = eprot.next()
        P.op("pe", "matmul", out=pm[0:64, 0:nq], lhsT=ones64[64:65, 0:64], rhs=rl[64:65, 0:nq], start=True, stop=True, reads=[rlk, "ones64"], writes=[pk])
        bc, bck = bcrot.next()
        P.op("act", "copy", out=bc[:, 0:nq], in_=pm[0:64, 0:nq], reads=[pk], writes=[bck])
        ob, obk = orot.next()
        P.op("dve", "tensor_tensor", out=ob[:, 0:nq], in0=acc[0:64, 0:nq], in1=bc[:, 0:nq], op=ALU.mult, reads=[acck, bck], writes=[obk])
        P.dma("sp", oT_d[:, q0:q0 + nq], ob[:, 0:nq], reads=[obk])
    P.end_phase()


def ret_consts():
    m = np.arange(128)[:, None].astype(np.float32)
    n = np.arange(128)[None, :].astype(np.float32)
    c = np.zeros((128, 6, 128), np.float32)
    c[:, 0] = np.maximum(n - m, 0)
    c[:, 1] = (n >= m)
    c[:, 2] = np.maximum(m - n, 0)
    c[:, 3] = (m >= n)
    c[:, 4] = n + 1
    c[:, 5] = 128 - n
    z = np.zeros((128, 2), np.float32)
    z[:, 0] = 127 - np.arange(128)
    z[:, 1] = np.arange(128)
    return c, z


def emit_ret(P, N, M, need_ctx, qT_d, kT_d, kt_d, v_d, gT_d, lg_d, gn_d, rc_d, rz_d, oT_d):
    T = N + M
    NC = T // 128
    MB = M // 128

    ktok = P.sb([128, NC, 64], BF16, "ktok")
    V = P.sb([128, NC, 64], BF16, "V")
    vv = v_d.rearrange("(kb p) d -> p kb d", p=128)
    kv = kt_d.rearrange("(kb p) d -> p kb d", p=128)
    step = max(1, NC // 8)
    for k0 in range(0, NC, step):
        k1 = min(NC, k0 + step)
        P.dma("sp", V[:, k0:k1, :], vv[:, k0:k1, :], writes=["V"])
        P.dma("sp", ktok[:, k0:k1, :], kv[:, k0:k1, :], writes=["ktok"])
    rc = P.sb([128, 6, 128], F32, "rc")
    P.dma("sp", rc[:], rc_d, writes=["rc"])
    rz = P.sb([128, 2], F32, "rz")
    P.dma("sp", rz[:], rz_d, writes=["rz"])
    gcol = P.sb([64, 1], F32, "gcol")
    P.dma("sp", gcol[:], gn_d, writes=["gcol"])
    lgc = P.sb([128, 2], F32, "lgc")
    P.dma("sp", lgc[:], lg_d, writes=["lgc"])
    P.op("act", "activation", out=lgc[:], in_=lgc[:], func=AF.Exp, scale=-1.0, reads=["lgc"], writes=["lgc"])
    P.op("dve", "tensor_scalar", out=lgc[:], in0=lgc[:], scalar1=1.0, scalar2=None, op0=ALU.add, reads=["lgc"], writes=["lgc"])
    P.op("act", "activation", out=lgc[:], in_=lgc[:], func=AF.Ln, reads=["lgc"], writes=["lgc"])
    P.op("dve", "tensor_scalar", out=lgc[:], in0=lgc[:], scalar1=-1.0, scalar2=None, op0=ALU.mult, reads=["lgc"], writes=["lgc"])
    DcT = P.sb([128, 128], F32, "DcT")
    E2 = P.sb([128, 128], F32, "E2")
    P.op("act", "activation", out=DcT[:], in_=rc[:, 0, :], func=AF.Exp, scale=lgc[:, 0:1], reads=["rc", "lgc"], writes=["DcT"])
    P.op("act", "activation", out=E2[:], in_=rc[:, 2, :], func=AF.Exp, scale=lgc[:, 1:2], reads=["rc", "lgc"], writes=["E2"])
    P.op("dve", "tensor_tensor", out=DcT[:], in0=DcT[:], in1=rc[:, 1, :], op=ALU.mult, reads=["DcT", "rc"], writes=["DcT"])
    P.op("dve", "tensor_tensor", out=E2[:], in0=E2[:], in1=rc[:, 3, :], op=ALU.mult, reads=["E2", "rc"], writes=["E2"])
    P.op("dve", "tensor_tensor", out=DcT[:], in0=DcT[:], in1=E2[:], op=ALU.add, reads=["DcT", "E2"], writes=["DcT"])
    xi = P.sb([64, 2, 128], F32, "xi")
    P.op("act", "activation", out=xi[:, 0, :], in_=rc[0:64, 4, :], func=AF.Exp, scale=lgc[0:64, 0:1], reads=["rc", "lgc"], writes=["xi"])
    P.op("act", "activation", out=xi[:, 1, :], in_=rc[0:64, 5, :], func=AF.Exp, scale=lgc[0:64, 1:2], reads=["rc", "lgc"], writes=["xi"])
    zc = P.sb([128, 2], F32, "zc")
    P.op("act", "activation", out=zc[:, 0:1], in_=rz[:, 0:1], func=AF.Exp, scale=lgc[:, 0:1], reads=["rz", "lgc"], writes=["zc"])
    P.op("act", "activation", out=zc[:, 1:2], in_=rz[:, 1:2], func=AF.Exp, scale=lgc[:, 1:2], reads=["rz", "lgc"], writes=["zc"])
    gc = P.sb([64, 2], F32, "gc")
    P.op("act", "activation", out=gc[:], in_=lgc[0:64, :], func=AF.Exp, scale=128.0, reads=["lgc"], writes=["gc"])
    ones64 = P.sb([128, 64], F32, "ones64")
    P.op("pool", "memset", ap=ones64[:], constant=1.0, writes=["ones64"])

    U = [P.sb([64, NC, 64], F32, f"U{d}") for d in range(2)]
    S16 = [P.sb([64, NC, 64], BF16, f"S16_{d}") for d in range(2)]
    urot = Rot(P, "ups", 2, [128, 512], F32, psum=True)
    kzrot = Rot(P, "kz", 4, [128, 64], BF16)
    for d in range(2):
        for c0 in range(0, NC, 8):
            c1 = min(NC, c0 + 8)
            pm, pk = urot.next()
            for c in range(c0, c1):
                kz, kzk = kzrot.next()
                P.op("dve" if d == 0 else "pool", "tensor_scalar", out=kz[:], in0=ktok[:, c, :], scalar1=zc[:, d:d + 1], scalar2=None, op0=ALU.mult,
                     reads=["ktok", "zc"], writes=[kzk])
                P.op("pe", "matmul", out=pm[0:64, (c - c0) * 64:(c - c0 + 1) * 64], lhsT=kz[:], rhs=V[:, c, :], start=True, stop=True,
                     reads=[kzk, "V"], writes=[pk])
            P.op("act", "copy", out=U[d][:, c0:c1, :], in_=pm[0:64, 0:(c1 - c0) * 64].rearrange("p (c e) -> p c e", e=64), reads=[pk], writes=[f"U{d}"])
    order = [list(range(NC)), list(range(MB - 1, -1, -1)) + list(range(NC - 1, MB - 1, -1))]
    prev = [{}, {}]
    for d in range(2):
        eng = "dve"
        od = order[d]
        for i in range(1, NC):
            prev[d][od[i]] = od[i - 1]
            P.op(eng, "scalar_tensor_tensor", out=U[d][:, od[i], :], in0=U[d][:, od[i - 1], :], scalar=gc[:, d:d + 1], in1=U[d][:, od[i], :],
                 op0=ALU.mult, op1=ALU.add, reads=[f"U{d}", "gc"], writes=[f"U{d}"])
        P.op(eng, "tensor_copy", out=S16[d][:], in_=U[d][:], reads=[f"U{d}"], writes=[f"S16_{d}"])
    accrot = Rot(P, "acc", 2, [128, 512], F32, psum=True)
    srot = Rot(P, "S", 2, [128, 512], F32, psum=True)
    eprot = Rot(P, "ep", 2, [128, 512], F32, psum=True)
    qrot = Rot(P, "qg", 2, [64, 512], BF16)
    krot = Rot(P, "kg", 2, [64, 512], BF16)
    grot = Rot(P, "gg", 2, [64, 512], F32)
    amrot = Rot(P, "am", 3, [128, 128], BF16)
    qxrot = Rot(P, "qx", 3, [64, 2, 128], BF16)
    t32rot = Rot(P, "t32", 3, [64, 512], F32)
    orot = Rot(P, "ob", 2, [64, 512], BF16)
    groups = []
    if need_ctx:
        groups.append(list(range(0, MB)))
    for c0 in range(MB, NC, 4):
        groups.append(list(range(c0, min(NC, c0 + 4))))
    for grp in groups:
        t0_ = grp[0] * 128
        nq = len(grp) * 128
        qg, qgk = qrot.next()
        kg, kgk = krot.next()
        gg, ggk = grot.next()
        P.dma("sp", qg[:, 0:nq], qT_d[:, t0_:t0_ + nq], writes=[qgk])
        P.dma("sp", kg[:, 0:nq], kT_d[:, t0_:t0_ + nq], writes=[kgk])
        P.dma("sp", gg[:, 0:nq], gT_d[:, t0_:t0_ + nq], writes=[ggk])
        P.op("act", "activation", out=gg[:, 0:nq], in_=gg[:, 0:nq], func=AF.Silu, reads=[ggk], writes=[ggk])
        acc, acck = accrot.next()
        for ci, c in enumerate(grp):
            cs = slice(ci * 128, (ci + 1) * 128)
            S, Sk = srot.next()
            P.op("pe", "matmul", out=S[:, 0:128], lhsT=kg[:, cs], rhs=qg[:, cs], start=True, stop=True, reads=[kgk, qgk], writes=[Sk])
            am, amk = amrot.next()
            P.op("dve", "tensor_tensor", out=am[:], in0=S[:, 0:128], in1=DcT[:], op=ALU.mult, reads=[Sk, "DcT"], writes=[amk])
            qx, qxk = qxrot.next()
            P.op("pool", "tensor_tensor", out=qx[:], in0=qg[:, cs].unsqueeze(1).to_broadcast([64, 2, 128]), in1=xi[:], op=ALU.mult, reads=[qgk, "xi"], writes=[qxk])
            mms = [(V[:, c, :], am[:], ["V", amk])]
            for d in range(2):
                if c in prev[d]:
                    mms.append((S16[d][:, prev[d][c], :], qx[:, d, :], [f"S16_{d}", qxk]))
            for mi, (l_, r_, rk_) in enumerate(mms):
                P.op("pe", "matmul", out=acc[0:64, cs], lhsT=l_, rhs=r_, start=(mi == 0), stop=(mi == len(mms) - 1), reads=rk_, writes=[acck])
        y, yk = t32rot.next()
        P.op("act", "copy", out=y[:, 0:nq], in_=acc[0:64, 0:nq], reads=[acck], writes=[yk])
        yn, ynk = t32rot.next()
        headnorm_T(P, y[:, 0:nq], yk, nq, ones64, eprot, gcol[:, 0:1], "gcol", 1.0, yn[:, 0:nq], ynk, t32rot)
        ob, obk = orot.next()
        P.op("pool", "tensor_tensor", out=ob[:, 0:nq], in0=yn[:, 0:nq], in1=gg[:, 0:nq], op=ALU.mult, reads=[ynk, ggk], writes=[obk])
        P.dma("sp", oT_d[:, t0_:t0_ + nq], ob[:, 0:nq], reads=[obk])
    P.end_phase()


def _bk(name, c0, c1):
    return [f"{name}{b}" for b in range(c0 // 512, (c1 - 1) // 512 + 1)]


def emit_lru(P, N, M, lx_d, ly_d, cw_d, cb_d, wa_d, wx_d, ba_d, bx_d, lam_d, i2_d, oT_d):
    T = N + M

    XX = P.sb([128, T], F32, "XX")
    UU = P.sb([128, T], F32, "UU")
    CH = 2048
    for c0 in range(0, T, CH):
        c1 = min(T, c0 + CH)
        P.dma("sp", XX[0:64, c0:c1], lx_d[:, c0:c1], writes=_bk("XX", c0, c1))
        P.dma("sp", XX[64:128, c0:c1], lx_d[:, c0:c1], writes=_bk("XX", c0, c1))
    small = {}
    for nm, d_, shp in (("cw", cw_d, [128, 4]), ("cb", cb_d, [128, 1]), ("wa", wa_d, [64, 128]), ("wx", wx_d, [64, 128]),
                        ("ba", ba_d, [128, 1]), ("bx", bx_d, [128, 1]), ("lam", lam_d, [128, 1]), ("i2", i2_d, [128, 64])):
        t_ = P.sb(shp, F32, nm)
        P.dma("sp", t_[:], d_, writes=[nm])
        small[nm] = t_
    cw, cb, wa, wx, ba, bx, lam, i2 = (small[k] for k in ("cw", "cb", "wa", "wx", "ba", "bx", "lam", "i2"))
    c8 = P.sb([128, 1], F32, "c8")
    P.op("act", "activation", out=c8[:], in_=lam[:], func=AF.Exp, scale=-1.0, reads=["lam"], writes=["c8"])
    P.op("dve", "tensor_scalar", out=c8[:], in0=c8[:], scalar1=1.0, scalar2=None, op0=ALU.add, reads=["c8"], writes=["c8"])
    P.op("act", "activation", out=c8[:], in_=c8[:], func=AF.Ln, reads=["c8"], writes=["c8"])
    P.op("dve", "tensor_scalar", out=c8[:], in0=c8[:], scalar1=-8.0, scalar2=None, op0=ALU.mult, reads=["c8"], writes=["c8"])
    for (s0, s1) in ((0, M), (M, T)):
        for c0 in range(s0, s1, CH):
            c1 = min(s1, c0 + CH)
            P.op("dve", "tensor_scalar", out=UU[:, c0:c1], in0=XX[:, c0:c1], scalar1=cw[:, 1:2], scalar2=cb[:, 0:1], op0=ALU.mult, op1=ALU.add,
                 reads=_bk("XX", c0, c1) + ["cw", "cb"], writes=_bk("UU", c0, c1))
            for (tap, sh) in ((0, -1), (2, 1), (3, 2)):
                o0, o1 = max(c0, s0 - sh), min(c1, s1 - sh)
                if o1 <= o0:
                    continue
                P.op("dve", "scalar_tensor_tensor", out=UU[:, o0:o1], in0=XX[:, o0 + sh:o1 + sh], scalar=cw[:, tap:tap + 1], in1=UU[:, o0:o1],
                     op0=ALU.mult, op1=ALU.add, reads=_bk("XX", o0 + sh, o1 + sh) + _bk("UU", o0, o1) + ["cw"], writes=_bk("UU", o0, o1))
    grot = Rot(P, "gps", 4, [128, 512], F32, psum=True)
    rsrot = Rot(P, "rs", 2, [128, 512], F32)
    isrot = Rot(P, "is", 2, [128, 512], F32)
    t1rot = Rot(P, "t1", 2, [128, 512], F32)
    for c0 in range(0, T, 512):
        c1 = min(T, c0 + 512)
        w = c1 - c0
        uk = _bk("UU", c0, c1)
        xk = _bk("XX", c0, c1)
        pr, prk = grot.next()
        pi, pik = grot.next()
        P.op("pe", "matmul", out=pr[:, 0:w], lhsT=wa[:, :], rhs=UU[0:64, c0:c1], start=True, stop=True, reads=uk + ["wa"], writes=[prk])
        P.op("pe", "matmul", out=pi[:, 0:w], lhsT=wx[:, :], rhs=UU[0:64, c0:c1], start=True, stop=True, reads=uk + ["wx"], writes=[pik])
        rs, rsk = rsrot.next()
        is_, isk = isrot.next()
        P.op("act", "activation", out=rs[:, 0:w], in_=pr[:, 0:w], func=AF.Sigmoid, bias=ba[:, 0:1], reads=[prk, "ba"], writes=[rsk])
        P.op("act", "activation", out=is_[:, 0:w], in_=pi[:, 0:w], func=AF.Sigmoid, bias=bx[:, 0:1], reads=[pik, "bx"], writes=[isk])
        P.op("act", "activation", out=XX[:, c0:c1], in_=rs[:, 0:w], func=AF.Exp, scale=c8[:, 0:1], reads=[rsk, "c8"], writes=xk)
        t1, t1k = t1rot.next()
        P.op("pool", "tensor_tensor", out=t1[:, 0:w], in0=XX[:, c0:c1], in1=XX[:, c0:c1], op=ALU.mult, reads=xk, writes=[t1k])
        P.op("pool", "tensor_scalar", out=t1[:, 0:w], in0=t1[:, 0:w], scalar1=-1.0, scalar2=1.0, op0=ALU.mult, op1=ALU.add, reads=[t1k], writes=[t1k])
        P.op("act", "activation", out=t1[:, 0:w], in_=t1[:, 0:w], func=AF.Sqrt, reads=[t1k], writes=[t1k])
        P.op("dve", "tensor_tensor", out=is_[:, 0:w], in0=is_[:, 0:w], in1=UU[:, c0:c1], op=ALU.mult, reads=[isk] + uk, writes=[isk])
        P.op("dve", "tensor_tensor", out=UU[:, c0:c1], in0=is_[:, 0:w], in1=t1[:, 0:w], op=ALU.mult, reads=[isk, t1k], writes=uk)
    prev = None
    for c0 in range(0, T, CH):
        c1 = min(T, c0 + CH)
        init = 0.0 if prev is None else UU[0:64, c0 - 1:c0]
        P.op("dve", "tensor_tensor_scan", out=UU[0:64, c0:c1], data0=XX[0:64, c0:c1], data1=UU[0:64, c0:c1], initial=init, op0=ALU.mult, op1=ALU.add,
             reads=_bk("XX", c0, c1) + _bk("UU", max(c0 - 1, 0), c1) + ["scanf"], writes=_bk("UU", c0, c1) + ["scanf"])
        prev = c0
    chunks = [(c0, min(M, c0 + CH)) for c0 in range(0, M, CH)][::-1] + [(c0, min(T, c0 + CH)) for c0 in range(M, T, CH)][::-1]
    prev_lo = None
    for (c0, c1) in chunks:
        init = 0.0 if prev_lo is None else UU[64:128, prev_lo:prev_lo + 1]
        rk = [] if prev_lo is None else _bk("UU", prev_lo, prev_lo + 1)
        P.op("dve", "tensor_tensor_scan", out=UU[64:128, c0:c1][:, ::-1], data0=XX[64:128, c0:c1][:, ::-1], data1=UU[64:128, c0:c1][:, ::-1], initial=init,
             op0=ALU.mult, op1=ALU.add, reads=_bk("XX", c0, c1) + _bk("UU", c0, c1) + rk + ["scanb"], writes=_bk("UU", c0, c1) + ["scanb"])
        prev_lo = c0
    hrot = Rot(P, "hps", 2, [128, 512], F32, psum=True)
    yrot = Rot(P, "yy", 2, [64, 512], F32)
    zrot = Rot(P, "zz", 2, [64, 512], F32)
    orot = Rot(P, "ob", 2, [64, 512], BF16)
    for c0 in range(0, T, 512):
        c1 = min(T, c0 + 512)
        w = c1 - c0
        hp, hpk = hrot.next()
        P.op("pe", "matmul", out=hp[0:64, 0:w], lhsT=i2[:, :], rhs=UU[:, c0:c1], start=True, stop=True, reads=_bk("UU", c0, c1) + ["i2"], writes=[hpk])
        yy, yk = yrot.next()
        zz, zk = zrot.next()
        P.dma("sp", yy[:, 0:w], ly_d[:, c0:c1], writes=[yk])
        P.op("pool", "tensor_tensor", out=zz[:, 0:w], in0=yy[:, 0:w], in1=yy[:, 0:w], op=ALU.mult, reads=[yk], writes=[zk])
        P.op("pool", "tensor_scalar", out=zz[:, 0:w], in0=zz[:, 0:w], scalar1=0.044715, scalar2=1.0, op0=ALU.mult, op1=ALU.add, reads=[zk], writes=[zk])
        P.op("pool", "tensor_tensor", out=zz[:, 0:w], in0=zz[:, 0:w], in1=yy[:, 0:w], op=ALU.mult, reads=[zk, yk], writes=[zk])
        P.op("act", "activation", out=zz[:, 0:w], in_=zz[:, 0:w], func=AF.Sigmoid, scale=2.0 * math.sqrt(2.0 / math.pi), reads=[zk], writes=[zk])
        P.op("pool", "tensor_tensor", out=zz[:, 0:w], in0=zz[:, 0:w], in1=yy[:, 0:w], op=ALU.mult, reads=[zk, yk], writes=[zk])
        ob, obk = orot.next()
        P.op("dve", "tensor_tensor", out=ob[:, 0:w], in0=hp[0:64, 0:w], in1=zz[:, 0:w], op=ALU.mult, reads=[hpk, zk], writes=[obk])
        P.dma("sp", oT_d[:, c0:c1], ob[:, 0:w], reads=[obk])
    P.end_phase()


def emit_f(P, tile_specs, nv, E, FF, moe, final, modrows_d, sel_d, wo_d, w1_d, w3_d, w2_d, ident_d, wr_d=None, id32_d=None, fn_d=None, GT=8):
    ntile = len(tile_specs)
    NFB = FF // 128
    nhalf = 2
    HB = NFB // nhalf
    ident = P.sb([128, 128], BF16, "ident")
    P.dma("sp", ident[:], ident_d, writes=["ident"])
    sel = P.sb([2, 2, 128], F32, "sel")
    P.dma("sp", sel[:], sel_d, writes=["sel"])
    mrrot = Rot(P, "mrowF", 2, [2, 1024], F32)
    pmrot = Rot(P, "pm", 3, [128, 512], F32, psum=True)
    mt = [[P.sb([128, D], F32, f"mt{v}_{i}") for i in range(4)] for v in range(nv)]
    for i in range(4):
        mrow, mrk = mrrot.next()
        P.dma("sp", mrow[:], modrows_d[:, i * 1024:(i + 1) * 1024], writes=[mrk])
        for v in range(nv):
            bcast_rows(P, mrow, mrk, sel, v, pmrot, mt[v][i], f"mt{v}_{i}")
    if final:
        fnb = P.sb([128, D], F32, "fnb")
        P.dma("sp", fnb[:], fn_d, writes=["fnb"])
    Wo = P.sb([128, 8, D], BF16, "Wo")
    wov = wo_d.rearrange("(k p) c -> p k c", p=128)
    for k in range(8):
        P.dma("pool", Wo[:, k, :], wov[:, k, :], writes=["Wo"])
    if moe:
        Wr = P.sb([128, 8, E], F32, "Wr")
        P.dma("sp", Wr[:], wr_d.rearrange("(k p) e -> p k e", p=128), writes=["Wr"])
        id32 = P.sb([128, 128], F32, "id32")
        P.dma("sp", id32[:], id32_d, writes=["id32"])
        gates = P.sb([128, GT, E], F32, "gates")
        h32T = P.sb([128, 8, 128], F32, "h32T")
        rsm = Rot(P, "rsm", 2, [128, 40], F32)
    purot = Rot(P, "pu", 4, [128, 512], F32, psum=True)
    rots = {
        "sq": Rot(P, "sq", 2, [128, D], F32),
        "st": Rot(P, "st", 4, [128, 4], F32),
        "hb": Rot(P, "hb", 2, [128, D], BF16),
        "ptr": Rot(P, "ptr", 1, [128, 1024], BF16, psum=True),
    }
    xrot = Rot(P, "xt", 2, [128, D], F32)
    otrot = Rot(P, "oTt", 2, [128, 8, 128], BF16)
    acc = P.sb([128, GT, D], F32, "acc")
    h2T = P.sb([128, 8, GT * 128], BF16, "h2T")
    gT = P.sb([128, HB, GT * 128], BF16, "gT")
    W2 = P.sb([128, HB, D], BF16, "W2")
    w1rot = Rot(P, "w1b", 2, [128, 8, 256], BF16)
    w3rot = Rot(P, "w3b", 2, [128, 8, 256], BF16)
    srot = Rot(P, "sil", 2, [128, 512], F32)
    tmprot = Rot(P, "ftmp", 2, [128, 512], F32)
    orot = Rot(P, "xout", 1, [128, D], F32) if final else None

    tiles_v = [ts_[0] for ts_ in tile_specs]
    for g0 in range(0, ntile, GT):
        tiles = list(range(g0, min(ntile, g0 + GT)))
        ng = len(tiles)
        ntok = ng * 128
        for ti, tile in enumerate(tiles):
            v = tiles_v[tile]
            acck = f"acc{ti}"
            xt, xk = xrot.next()
            P.dma("sp", xt[:], tile_specs[tile][1], writes=[xk])
            ot, otk = otrot.next()
            P.dma("sp", ot[:], tile_specs[tile][2], reads=["l_oT"], writes=[otk])
            for hh in range(2):
                pm, pk = pmrot.next()
                for k in range(8):
                    P.op("pe", "matmul", out=pm[:, :], lhsT=ot[:, k, :], rhs=Wo[:, k, hh * 512:(hh + 1) * 512], start=(k == 0), stop=(k == 7),
                         reads=[otk, "Wo"], writes=[pk])
                hs = slice(hh * 512, (hh + 1) * 512)
                P.op("dve", "tensor_tensor", out=acc[:, ti, hs], in0=pm[:, :], in1=mt[v][0][:, hs], op=ALU.mult, reads=[pk, f"mt{v}_0"], writes=[acck])
                P.op("pool", "tensor_tensor", out=acc[:, ti, hs], in0=acc[:, ti, hs], in1=xt[:, hs], op=ALU.add, reads=[acck, xk], writes=[acck])
            st, stk, sq, sqk = rms_rstd(P, acc[:, ti, :], acck, rots)
            P.op("dve", "scalar_tensor_tensor", out=sq[:], in0=acc[:, ti, :], scalar=st[:, 3:4], in1=mt[v][2][:], op0=ALU.mult, op1=ALU.mult,
                 reads=[acck, stk, f"mt{v}_2"], writes=[sqk])
            P.op("pool", "tensor_tensor", out=sq[:], in0=sq[:], in1=mt[v][1][:], op=ALU.add, reads=[sqk, f"mt{v}_1"], writes=[sqk])
            hb, hbk = rots["hb"].next()
            P.op("act", "copy", out=hb[:], in_=sq[:], reads=[sqk], writes=[hbk])
            ptr, ptk = rots["ptr"].next()
            for k in range(8):
                P.op("pe", "transpose", out=ptr[:, k * 128:(k + 1) * 128], in_=hb[:, k * 128:(k + 1) * 128], identity=ident[:], reads=[hbk, "ident"], writes=[ptk])
            P.op("act", "copy", out=h2T[:, :, ti * 128:(ti + 1) * 128], in_=ptr[:, :].rearrange("p (k t) -> p k t", k=8), reads=[ptk], writes=[f"h2T{ti}"])
            if moe:
                for hh in range(2):
                    pm, pk = pmrot.next()
                    for k4 in range(4):
                        k = hh * 4 + k4
                        P.op("pe", "transpose", out=pm[:, k4 * 128:(k4 + 1) * 128], in_=sq[:, k * 128:(k + 1) * 128], identity=id32[:], reads=[sqk, "id32"], writes=[pk])
                    P.op("act", "copy", out=h32T[:, hh * 4:(hh + 1) * 4, :], in_=pm[:, :].rearrange("p (k t) -> p k t", k=4), reads=[pk], writes=["h32T"])
                pm, pk = pmrot.next()
                for k in range(8):
                    P.op("pe", "matmul", out=pm[:, 0:E], lhsT=h32T[:, k, :], rhs=Wr[:, k, :], start=(k == 0), stop=(k == 7), reads=["h32T", "Wr"], writes=[pk])
                r, rk = rsm.next()
                lg, eq1, lg2, eq2 = r[:, 0:E], r[:, 8:8 + E], r[:, 16:16 + E], r[:, 24:24 + E]
                m1, m2, dd, ww1, ww2 = (r[:, 32 + i:33 + i] for i in range(5))
                P.op("act", "copy", out=lg, in_=pm[:, 0:E], reads=[pk], writes=[rk])
                P.op("dve", "tensor_reduce", out=m1, in_=lg, axis=AX.X, op=ALU.max, reads=[rk], writes=[rk])
                P.op("dve", "tensor_scalar", out=eq1, in0=lg, scalar1=m1, scalar2=None, op0=ALU.is_equal, reads=[rk], writes=[rk])
                P.op("dve", "scalar_tensor_tensor", out=lg2, in0=eq1, scalar=-1e30, in1=lg, op0=ALU.mult, op1=ALU.add, reads=[rk], writes=[rk])
                P.op("dve", "tensor_reduce", out=m2, in_=lg2, axis=AX.X, op=ALU.max, reads=[rk], writes=[rk])
                P.op("dve", "tensor_scalar", out=eq2, in0=lg2, scalar1=m2, scalar2=None, op0=ALU.is_equal, reads=[rk], writes=[rk])
                P.op("dve", "tensor_tensor", out=dd, in0=m2, in1=m1, op=ALU.subtract, reads=[rk], writes=[rk])
                P.op("act", "activation", out=dd, in_=dd, func=AF.Exp, reads=[rk], writes=[rk])
                P.op("dve", "tensor_scalar", out=ww1, in0=dd, scalar1=1.0, scalar2=None, op0=ALU.add, reads=[rk], writes=[rk])
                P.op("dve", "reciprocal", out=ww1, in_=ww1, reads=[rk], writes=[rk])
                P.op("dve", "tensor_tensor", out=ww2, in0=dd, in1=ww1, op=ALU.mult, reads=[rk], writes=[rk])
                P.op("dve", "tensor_scalar", out=gates[:, ti, :], in0=eq1, scalar1=ww1, scalar2=None, op0=ALU.mult, reads=[rk], writes=[f"gates{ti}"])
                P.op("dve", "scalar_tensor_tensor", out=gates[:, ti, :], in0=eq2, scalar=ww2, in1=gates[:, ti, :], op0=ALU.mult, op1=ALU.add, reads=[rk, f"gates{ti}"], writes=[f"gates{ti}"])
        h2keys = [f"h2T{ti}" for ti in range(ng)]
        chunks = [(c0, min(ntok, c0 + 512)) for c0 in range(0, ntok, 512)]
        for e in range(E):
            for half in range(nhalf):
                fb0 = half * HB
                w2v = w2_d[e, fb0 * 128:(fb0 + HB) * 128, :].rearrange("(f p) c -> p f c", p=128)
                for fp in range(0, HB, 2):
                    nb_ = min(2, HB - fp)
                    w1b, w1k = w1rot.next()
                    w3b, w3k = w3rot.next()
                    cs = slice((fb0 + fp) * 128, (fb0 + fp + nb_) * 128)
                    P.dma("pool", w1b[:, :, 0:nb_ * 128], w1_d[e, :, cs].rearrange("(k p) c -> p k c", p=128), writes=[w1k])
                    P.dma("pool", w3b[:, :, 0:nb_ * 128], w3_d[e, :, cs].rearrange("(k p) c -> p k c", p=128), writes=[w3k])
                    P.dma("pool", W2[:, fp:fp + nb_, :], w2v[:, fp:fp + nb_, :], writes=["W2"])
                    for bi in range(nb_):
                        fbi = fp + bi
                        for (c0, c1) in chunks:
                            w = c1 - c0
                            hk = h2keys[c0 // 128:(c1 - 1) // 128 + 1]
                            p1, p1k = purot.next()
                            p3, p3k = purot.next()
                            for k in range(8):
                                P.op("pe", "matmul", out=p1[:, 0:w], lhsT=w1b[:, k, bi * 128:(bi + 1) * 128], rhs=h2T[:, k, c0:c1], start=(k == 0), stop=(k == 7),
                                     reads=[w1k] + hk, writes=[p1k])
                            for k in range(8):
                                P.op("pe", "matmul", out=p3[:, 0:w], lhsT=w3b[:, k, bi * 128:(bi + 1) * 128], rhs=h2T[:, k, c0:c1], start=(k == 0), stop=(k == 7),
                                     reads=[w3k] + hk, writes=[p3k])
                            sl, slk = srot.next()
                            P.op("act", "activation", out=sl[:, 0:w], in_=p1[:, 0:w], func=AF.Silu, reads=[p1k], writes=[slk])
                            P.op("dve", "tensor_tensor", out=gT[:, fbi, c0:c1], in0=p3[:, 0:w], in1=sl[:, 0:w], op=ALU.mult, reads=[p3k, slk], writes=[f"gT{c0 // 512}"])
                for ti, tile in enumerate(tiles):
                    v = tiles_v[tile]
                    for hh in range(2):
                        hs = slice(hh * 512, (hh + 1) * 512)
                        pm, pk = pmrot.next()
                        for fbi in range(HB):
                            P.op("pe", "matmul", out=pm[:, :], lhsT=gT[:, fbi, ti * 128:(ti + 1) * 128], rhs=W2[:, fbi, hs], start=(fbi == 0), stop=(fbi == HB - 1),
                                 reads=[f"gT{ti // 4}", "W2"], writes=[pk])
                        tmp, tmpk = tmprot.next()
                        if moe:
                            P.op("dve", "scalar_tensor_tensor", out=tmp[:], in0=pm[:, :], scalar=gates[:, ti, e:e + 1], in1=mt[v][3][:, hs], op0=ALU.mult, op1=ALU.mult,
                                 reads=[pk, f"gates{ti}", f"mt{v}_3"], writes=[tmpk])
                        else:
                            P.op("dve", "tensor_tensor", out=tmp[:], in0=pm[:, :], in1=mt[v][3][:, hs], op=ALU.mult, reads=[pk, f"mt{v}_3"], writes=[tmpk])
                        P.op("pool", "tensor_tensor", out=acc[:, ti, hs], in0=acc[:, ti, hs], in1=tmp[:], op=ALU.add, reads=[f"acc{ti}", tmpk], writes=[f"acc{ti}"])
        for ti, tile in enumerate(tiles):
            if final:
                st, stk, sq, sqk = rms_rstd(P, acc[:, ti, :], f"acc{ti}", rots)
                xo_t, xok = orot.next()
                P.op("dve", "scalar_tensor_tensor", out=xo_t[:], in0=acc[:, ti, :], scalar=st[:, 3:4], in1=fnb[:], op0=ALU.mult, op1=ALU.mult,
                     reads=[f"acc{ti}", stk, "fnb"], writes=[xok])
                P.dma("sp", tile_specs[tile][3], xo_t[:], reads=[xok], writes=["xout_d"])
            else:
                P.dma("sp", tile_specs[tile][3], acc[:, ti, :], reads=[f"acc{ti}"], writes=["xout_d", "xsrc"])
    P.end_phase()


def build_fused(N, M, E, FFD, FFE):
    T = N + M
    tps = N // 4
    nc = bass.Bass("TRN2", target_bir_lowering=False)
    P = Prog(nc)
    di = lambda n, s, d=F32: nc.dram_tensor(n, s, d, kind="ExternalInput").ap()
    do = lambda n, s, d=F32: nc.dram_tensor(n, s, d, kind="ExternalOutput").ap()
    sc = lambda n, s, d=F32: nc.dram_tensor(n, s, d).ap()
    xsh = di("xsh", [tps, D])
    ctx = di("ctx", [M, D])
    cT = di("cT", [128, 8, 2])
    tabs = di("tabs", [N, 160])
    ident_d = di("ident", [128, 128], BF16)
    id32_d = di("id32", [128, 128])
    sel_d = di("sel", [2, 2, 128])
    m3_d = di("m3", [128, 384], BF16)
    rc_d = di("retc", [128, 6, 128])
    rz_d = di("retz", [128, 2])
    i2_d = di("i2", [128, 64])
    fn_d = di("fnb", [128, D])
    L = []
    for l in range(2):
        L.append(dict(
            wada=di(f"wada{l}", [D, 6144]), bada=di(f"bada{l}", [1, 6144]), gn1=di(f"gn1_{l}", [1, D]), gn2=di(f"gn2_{l}", [1, D]),
            wt=di(f"wt{l}", [D, 576]), wf=di(f"wf{l}", [D, 192]), wo=di(f"wo{l}", [D, D]),
            sink=di(f"sink{l}", [128, 1]), logit=di(f"logit{l}", [128, 2]), rgn=di(f"rgn{l}", [64, 1]),
            lamv=di(f"lamv{l}", [1, 128]), dgn=di(f"dgn{l}", [64, 1]),
            cw=di(f"cw{l}", [128, 4]), cb=di(f"cb{l}", [128, 1]), wa=di(f"wa{l}", [64, 128]), wx=di(f"wx{l}", [64, 128]),
            ba=di(f"ba{l}", [128, 1]), bx=di(f"bx{l}", [128, 1]), lam=di(f"lam{l}", [128, 1])))
    fw1, fw3, fw2 = di("fw1", [1, D, FFD]), di("fw3", [1, D, FFD]), di("fw2", [1, FFD, D])
    mw1, mw3, mw2 = di("mw1", [E, D, FFE]), di("mw3", [E, D, FFE]), di("mw2", [E, FFE, D])
    wr_d = di("wr", [D, E])
    xo = do("xo", [tps, D])
    s_qA, s_kB = sc("s_qA", [128, T], BF16), sc("s_kB", [128, T], BF16)
    s_rq, s_rk = sc("s_rq", [64, T], BF16), sc("s_rk", [64, T], BF16)
    s_tok = sc("s_tok", [T, 256], BF16)
    s_f32 = sc("s_f32", [192, T])
    s_mod = sc("s_mod", [2, 4096])
    s_oT = sc("s_oT", [256, T], BF16)
    CHT = tps // 2
    NCH = N // CHT
    st_o = sc("st_o", [NCH * 256, CHT], BF16)
    st_oc = sc("st_oc", [256, M], BF16)
    g_o = sc("g_o", [NCH * 1024, CHT], BF16)
    g_oc = sc("g_oc", [1024, M], BF16)
    l_oT = sc("l_oT", [1024, tps], BF16)
    s_x0 = sc("s_x0", [tps, D])
    s_x1 = sc("s_x1", [tps, D])
    XR = min(256, tps)
    NXC = tps // XR
    g_x = [sc(f"g_x{c}", [4 * XR, D]) for c in range(NXC)]
    s_xc1 = sc("s_xc1", [M, D])
    RG = [[0, 1, 2, 3], [4, 5, 6, 7]]
    l_oTv = l_oT.rearrange("(k p) t -> p k t", p=128)
    g_ocv = g_oc.rearrange("(k p) t -> p k t", p=128)

    def gather_x(src):
        for c in range(NXC):
            P.op("pool", "collective_compute", cc=True, kind="AllGather", op=ALU.bypass, replica_groups=RG,
                 ins=[src[c * XR:(c + 1) * XR, :]], outs=[g_x[c]], reads=["xsrc"], writes=["g_x"])

    def x_tile(ti):
        t0 = ti * 128
        r, rem = t0 // tps, t0 % tps
        c, i = rem // XR, rem % XR
        return g_x[c][r * XR + i:r * XR + i + 128, :]
    for r0 in range(0, tps, max(128, tps // 4)):
        r1 = min(tps, r0 + max(128, tps // 4))
        P.dma("sp", s_x0[r0:r1, :], xsh[r0:r1, :], writes=["xsrc"])
    gather_x(s_x0)
    P.end_phase()
    for l in range(2):
        W = L[l]
        need_ctx = (l == 0)
        lam_init = 0.8 - 0.6 * math.exp(-0.3 * l)
        x_lat = x_tile
        x_ctx = ctx if l == 0 else s_xc1
        emit_p(P, N, M, x_lat, x_ctx, cT, W["wada"], W["bada"], W["gn1"], W["gn2"], W["wt"], W["wf"], tabs, ident_d, sel_d,
               s_qA, s_kB, s_rq, s_rk, s_tok, s_f32, s_mod)
        emit_swa(P, N, M, need_ctx, s_qA[64:128, :], s_kB[64:128, :], s_tok[:, 64:128], W["sink"], m3_d, s_oT[0:64, :])
        emit_ret(P, N, M, need_ctx, s_rq, s_rk, s_tok[:, 0:64], s_tok[:, 128:192], s_f32[0:64, :], W["logit"], W["rgn"], rc_d, rz_d, s_oT[64:128, :])
        emit_diff(P, N, M, need_ctx, lam_init, s_qA[0:64, :], s_kB[0:64, :], s_tok[:, 192:256], W["lamv"], W["dgn"], s_oT[128:192, :])
        emit_lru(P, N, M, s_f32[64:128, :], s_f32[128:192, :], W["cw"], W["cb"], W["wa"], W["wx"], W["ba"], W["bx"], W["lam"], i2_d, s_oT[192:256, :])
        for c in range(NCH):
            P.dma("sp", st_o[c * 256:(c + 1) * 256, :], s_oT[:, M + c * CHT:M + (c + 1) * CHT], writes=[f"st_o{c}"])
        if need_ctx:
            P.dma("sp", st_oc[:, :], s_oT[:, 0:M], writes=["st_oc"])
        for c in range(NCH):
            P.op("pool", "collective_compute", cc=True, kind="AllGather", op=ALU.bypass, replica_groups=RG,
                 ins=[st_o[c * 256:(c + 1) * 256, :]], outs=[g_o[c * 1024:(c + 1) * 1024, :]], reads=[f"st_o{c}"], writes=["g_o"])
        if need_ctx:
            P.op("pool", "collective_compute", cc=True, kind="AllGather", op=ALU.bypass, replica_groups=RG,
                 ins=[st_oc], outs=[g_oc], reads=["st_oc"], writes=["g_oc"])
        P.end_phase()
        pidc = {}

        def shard_rows(e, pidc=pidc):
            if id(e) not in pidc:
                pidc[id(e)] = e.snap((e.partition_id() % 4) * 2048)
            return pidc[id(e)]
        for h in range(2):
            P.dma("sp", l_oT[:, h * CHT:(h + 1) * CHT],
                  (lambda e, h=h, sr=shard_rows: g_o[bass.ds(sr(e) + h * 1024, 1024), :]), writes=["l_oT"])
        specs = []
        for i in range(tps // 128):
            if l == 0:
                xs = xsh[i * 128:(i + 1) * 128, :]
                od = s_x1[i * 128:(i + 1) * 128, :]
            else:
                xs = s_x1[i * 128:(i + 1) * 128, :]
                od = xo[i * 128:(i + 1) * 128, :]
            specs.append((0, xs, l_oTv[:, :, i * 128:(i + 1) * 128], od))
        if need_ctx:
            for i in range(M // 128):
                specs.append((1, ctx[i * 128:(i + 1) * 128, :], g_ocv[:, :, i * 128:(i + 1) * 128], s_xc1[i * 128:(i + 1) * 128, :]))
        if l == 0:
            emit_f(P, specs, 2, 1, FFD, False, False, s_mod, sel_d, W["wo"], fw1, fw3, fw2, ident_d)
            gather_x(s_x1)
            P.end_phase()
        else:
            emit_f(P, specs, 1, E, FFE, True, True, s_mod, sel_d, W["wo"], mw1, mw3, mw2, ident_d, wr_d=wr_d, id32_d=id32_d, fn_d=fn_d)
    P.close()
    return nc


NCORES = 8
_cache = {}


def _get(name, fn, *a):
    key = (name,) + a
    if key not in _cache:
        _cache[key] = fn(*a)
    return _cache[key]


def _wo_perm():
    idx = np.zeros(1024, np.int64)
    for j in range(4):
        for m in range(4):
            idx[j * 256 + m * 64:j * 256 + (m + 1) * 64] = np.arange(m * 256 + j * 64, m * 256 + (j + 1) * 64)
    return idx


def make_maps(inp):
    f32 = np.float32
    B, N, _ = inp["x"].shape
    M = inp["ctx"].shape[1]
    ident = np.eye(128, dtype=f32).astype(ml_dtypes.bfloat16)
    sel = np.zeros((2, 2, 128), f32)
    sel[0, 0, :] = 1.0
    sel[1, 1, :] = 1.0
    rc, rz = ret_consts()
    tabs = rope_tables(N)
    common = {"ident": ident, "id32": np.eye(128, dtype=f32), "sel": sel, "m3": swa_masks(), "retc": rc, "retz": rz,
              "i2": np.concatenate([np.eye(64, dtype=f32)] * 2, 0), "tabs": tabs,
              "fnb": np.ascontiguousarray(np.broadcast_to(inp["final_norm"][None, :], (128, D))).astype(f32),
              "fw1": inp["ffn_w1"], "fw3": inp["ffn_w3"], "fw2": inp["ffn_w2"],
              "mw1": inp["moe_w1"][0], "mw3": inp["moe_w3"][0], "mw2": inp["moe_w2"][0], "wr": inp["moe_router"][0]}
    woidx = _wo_perm()
    for l in range(2):
        common[f"wada{l}"] = inp["w_ada"][l]
        common[f"bada{l}"] = np.ascontiguousarray(inp["b_ada"][l][None, :])
        common[f"gn1_{l}"] = np.ascontiguousarray(inp["g_norm1"][l][None, :])
        common[f"gn2_{l}"] = np.ascontiguousarray(inp["g_norm2"][l][None, :])
        common[f"wo{l}"] = np.ascontiguousarray(inp["w_out"][l][woidx, :])
        common[f"lamv{l}"] = np.ascontiguousarray(inp["diff_lambda"][l].reshape(1, 128))
    maps = []
    dup = lambda a: np.ascontiguousarray(np.concatenate([a, a], 0)).astype(f32)
    for core in range(NCORES):
        b, j = core // 4, core % 4
        s = slice(j * 64, (j + 1) * 64)
        m = dict(common)
        m["xsh"] = np.ascontiguousarray(inp["x"][b, j * (N // 4):(j + 1) * (N // 4)])
        m["ctx"] = np.ascontiguousarray(inp["ctx"][b])
        cv = np.stack([inp["c"][b], inp["c_ctx"]], 0)
        m["cT"] = np.ascontiguousarray(cv.reshape(2, 8, 128).transpose(2, 1, 0)).astype(f32)
        tok, feat = head_cols(j)
        for l in range(2):
            two = lambda a: np.ascontiguousarray(np.concatenate([a[0][s], a[1][s]], 0)[:, None]).astype(f32)
            m[f"wt{l}"] = np.ascontiguousarray(inp["w_in"][l][:, tok])
            m[f"wf{l}"] = np.ascontiguousarray(inp["w_in"][l][:, feat])
            m[f"sink{l}"] = np.full((128, 1), inp["attn_sink"][l][j], f32)
            m[f"logit{l}"] = np.ascontiguousarray(np.broadcast_to(inp["ret_decay_logit"][l][:, j][None, :], (128, 2))).astype(f32)
            m[f"rgn{l}"] = np.ascontiguousarray(inp["ret_gn"][l][s, None])
            m[f"dgn{l}"] = np.ascontiguousarray(inp["diff_gn"][l][s, None])
            m[f"cw{l}"] = dup(inp["conv_w"][l][:, s].T)
            m[f"cb{l}"] = dup(inp["conv_b"][l][s, None])
            m[f"wa{l}"] = np.ascontiguousarray(np.concatenate([inp["lru_wa"][l][0, j], inp["lru_wa"][l][1, j]], 1))
            m[f"wx{l}"] = np.ascontiguousarray(np.concatenate([inp["lru_wx"][l][0, j], inp["lru_wx"][l][1, j]], 1))
            m[f"ba{l}"] = two(inp["lru_ba"][l])
            m[f"bx{l}"] = two(inp["lru_bx"][l])
            m[f"lam{l}"] = two(inp["lru_lambda"][l])
        maps.append(m)
    return maps


def kernel(**inputs):
    inp = {k: np.asarray(v) for k, v in inputs.items()}
    B, N, _ = inp["x"].shape
    M = inp["ctx"].shape[1]
    E = inp["moe_w1"].shape[1]
    nc = _get("fused", build_fused, N, M, E, inp["ffn_w1"].shape[2], inp["moe_w1"].shape[3])
    res = run_bass_kernel_spmd(nc, make_maps(inp), core_ids=list(range(NCORES))).results
    out = np.stack([np.concatenate([res[b * 4 + s]["xo"] for s in range(4)], 0) for b in range(B)], 0)
    return out.astype(np.float32)
```

```python
from contextlib import ExitStack
import math
import numpy as np
import ml_dtypes
import concourse.bass as bass
import concourse.mybir as mybir
from concourse.bass_utils import run_bass_kernel_spmd

F32 = mybir.dt.float32
BF16 = mybir.dt.bfloat16
I32 = mybir.dt.int32
AF = mybir.ActivationFunctionType
ALU = mybir.AluOpType
AX = mybir.AxisListType

ENGS = ("pe", "act", "dve", "pool", "sp")
DMAQ = ("sp", "act", "pool")
NDMASEM = 8
_ENGATTR = {"pe": "tensor", "act": "scalar", "dve": "vector", "pool": "gpsimd", "sp": "sync"}


class Op:
    __slots__ = ("eng", "fn", "dma", "cc", "raw", "war", "sig", "dsem", "dval", "has_dep")

    def __init__(self, eng, fn, dma, cc=False):
        self.eng = eng
        self.fn = fn
        self.dma = dma
        self.cc = cc
        self.raw = []
        self.war = []
        self.sig = None
        self.dsem = None
        self.dval = None
        self.has_dep = False


class Prog:
    def __init__(self, nc):
        self.nc = nc
        self.gs = ExitStack()
        self.sems = {e: self.gs.enter_context(nc.semaphore(f"s_{e}")) for e in ENGS}
        self.dsems = {e: [self.gs.enter_context(nc.semaphore(f"d_{e}{i}")) for i in range(NDMASEM)] for e in DMAQ}
        self.cnt = {e: 0 for e in ENGS}
        self.dcnt = {e: [0] * NDMASEM for e in DMAQ}
        self.dk = {e: 0 for e in DMAQ}
        self.ccsem = self.gs.enter_context(nc.semaphore("s_cc"))
        self.cccnt = 0
        self.waited = {e: {} for e in ENGS}
        self.barrier = None
        self.nsb = 0
        self.nphase = 0
        self._reset()

    def _reset(self):
        self.q = {e: [] for e in ENGS}
        self.res_w = {}
        self.res_r = {}
        self.es = ExitStack()

    def sb(self, shape, dt=F32, name=None, persist=False):
        self.nsb += 1
        st = self.gs if persist else self.es
        return st.enter_context(self.nc.sbuf_tensor(f"s{self.nsb}_" + (name or "t"), list(shape), dt))

    def ps(self, shape, dt=F32, name=None):
        self.nsb += 1
        return self.es.enter_context(self.nc.psum_tensor(f"p{self.nsb}_" + (name or "t"), list(shape), dt))

    def op(self, eng, meth, reads=(), writes=(), dma=False, cc=False, **kw):
        fn = (lambda e: getattr(e, meth)(**{k: (v(e) if callable(v) else v) for k, v in kw.items()}))
        o = Op(eng, fn, dma or cc, cc)
        raw = set()
        war = set()
        for r in reads:
            w = self.res_w.get(r)
            if w is not None:
                raw.add(w)
        for wr in writes:
            w = self.res_w.get(wr)
            if w is not None:
                war.add(w)
            for rd in self.res_r.get(wr, ()):
                war.add(rd)
        raw.discard(o)
        war -= raw
        o.raw = list(raw)
        o.war = list(war)
        for r in reads:
            self.res_r.setdefault(r, []).append(o)
        for wr in writes:
            self.res_w[wr] = o
            self.res_r[wr] = []
        self.q[eng].append(o)
        return o

    def dma(self, eng, out, in_, reads=(), writes=(), **kw):
        return self.op(eng, "dma_start", reads, writes, dma=True, out=out, in_=in_, **kw)

    def end_phase(self):
        nc = self.nc
        for e in ENGS:
            for o in self.q[e]:
                for d in o.raw:
                    d.has_dep = True
                for d in o.war:
                    if d.dma or d.eng != o.eng or o.dma:
                        d.has_dep = True
            for o in reversed(self.q[e]):
                if not o.dma:
                    o.has_dep = True
                    break
        for e in ENGS:
            for o in self.q[e]:
                if o.cc:
                    self.cccnt += 1
                    o.dsem = self.ccsem
                    o.dval = self.cccnt
                elif o.dma:
                    s = self.dk[e] % NDMASEM
                    self.dk[e] += 1
                    self.dcnt[e][s] += 16
                    o.dsem = self.dsems[e][s]
                    o.dval = self.dcnt[e][s]
                elif o.has_dep:
                    self.cnt[e] += 1
                    o.sig = self.cnt[e]
        prev_barrier = self.barrier
        nb = [(self.sems[e], self.cnt[e]) for e in ENGS if self.cnt[e] > 0]
        for e in DMAQ:
            for s in range(NDMASEM):
                if self.dcnt[e][s] > 0:
                    nb.append((self.dsems[e][s], self.dcnt[e][s]))
        if self.cccnt > 0:
            nb.append((self.ccsem, self.cccnt))
        self.barrier = nb
        sems = self.sems

        def run_engine(e, eng):
            waited = self.waited[e]

            def wait(sem, val):
                if waited.get(sem, 0) >= val:
                    return
                eng.wait_ge(sem, val)
                waited[sem] = val

            if prev_barrier is not None:
                for s, v in prev_barrier:
                    wait(s, v)
            for o in self.q[e]:
                for d, is_raw in [(d, True) for d in o.raw] + [(d, False) for d in o.war]:
                    if d.dma:
                        wait(d.dsem, d.dval)
                    elif d.eng == e and not o.dma:
                        if is_raw:
                            wait(sems[e], d.sig)
                    else:
                        wait(sems[d.eng], d.sig)
                if o.cc:
                    if o.dval > 1:
                        wait(o.dsem, o.dval - 1)
                    o.fn(eng).then_inc(o.dsem)
                elif o.dma:
                    if o.dval > 16:
                        wait(o.dsem, o.dval - 16)
                    o.fn(eng).then_inc(o.dsem, 16)
                else:
                    ins = o.fn(eng)
                    if o.sig is not None:
                        ins.then_inc(sems[e], 1)
            if e == "sp":
                for s, v in nb:
                    wait(s, v)

        with nc.Block() as block:
            @block.tensor
            def _(eng):
                run_engine("pe", eng)

            @block.scalar
            def _(eng):
                run_engine("act", eng)

            @block.vector
            def _(eng):
                run_engine("dve", eng)

            @block.gpsimd
            def _(eng):
                run_engine("pool", eng)

            @block.sync
            def _(eng):
                run_engine("sp", eng)
        self.es.close()
        self._reset()
        self.nphase += 1

    def emit(self):
        self.end_phase()

    def close(self):
        self.es.close()
        self.gs.close()

D = 1024
EPS = 1e-6
SC_SWA = 64 ** -0.5
SC_RET = 64 ** -0.5
SC_DIFF = 32 ** -0.5


class Rot:
    def __init__(self, P, name, n, shape, dt=F32, psum=False):
        self.bufs = [(P.ps(shape, dt, f"{name}{i}") if psum else P.sb(shape, dt, f"{name}{i}")) for i in range(n)]
        self.name = name
        self.i = 0

    def next(self):
        k = self.i % len(self.bufs)
        self.i += 1
        return self.bufs[k], f"{self.name}{k}"


def _r(a, b):
    return list(range(a, b))


TOK_COLS = _r(0, 256) + _r(256, 384) + _r(1536, 1792) + _r(1792, 2048) + _r(512, 768) + _r(768, 1024) + _r(384, 512) + _r(1024, 1280) + _r(2048, 2304)
FEAT_COLS = _r(1280, 1536) + _r(2304, 2560) + _r(2560, 2816)


def rope_tables(n_lat):
    f32 = np.float32
    n = np.arange(n_lat)
    row = (n // 64).astype(f32)
    col = (n % 64).astype(f32)

    def axial(dim):
        nf = dim // 4
        inv = (f32(10000.0) ** (-(np.arange(nf, dtype=f32)) / f32(nf))).astype(f32)
        ang = np.concatenate([row[:, None] * inv, col[:, None] * inv], -1).astype(f32)
        return np.cos(ang).astype(f32), np.sin(ang).astype(f32)

    ca, sa = axial(64)
    cd, sd = axial(32)
    nf = 32
    inv = (f32(10000.0) ** (-(np.arange(nf, dtype=f32)) / f32(nf))).astype(f32)
    ang = (n.astype(f32)[:, None] * inv).astype(f32)
    cl, sl = np.cos(ang).astype(f32), np.sin(ang).astype(f32)
    return np.ascontiguousarray(np.concatenate([ca, sa, cd, sd, cl, sl], -1).astype(f32))


def mod_rows(P, cT, wada, bada, ncols, pmrot, name, extra=None):
    scT = P.sb([128, 8, 2], F32, name + "_scT")
    P.dma("sp", scT[:], cT, writes=[name + "_scT"])
    P.op("act", "activation", out=scT[:], in_=scT[:], func=AF.Silu, reads=[name + "_scT"], writes=[name + "_scT"])
    mrow = P.sb([2, ncols], F32, name + "_mrow")
    brot = Rot(P, name + "_brow", 2, [2, 512], F32)
    xrot_ = Rot(P, name + "_xrow", 2, [2, 512], F32)
    g2rot = Rot(P, name + "_g2row", 2, [2, 512], F32)
    wrot = Rot(P, name + "_wA", 1, [128, 8, 512], F32)
    wv = wada.rearrange("(k p) c -> p k c", p=128)
    ntot = ncols if extra is None else extra[0]
    for cb in range(ntot // 512):
        cs = slice(cb * 512, (cb + 1) * 512)
        wA, wk = wrot.next()
        P.dma("sp", wA[:], wv[:, :, cs], writes=[wk])
        brow, bk = brot.next()
        P.dma("sp", brow[0:1, :], bada[:, cs], writes=[bk])
        P.dma("sp", brow[1:2, :], bada[:, cs], writes=[bk])
        pm, pk = pmrot.next()
        for k in range(8):
            P.op("pe", "matmul", out=pm[0:2, :], lhsT=scT[:, k, :], rhs=wA[:, k, :], start=(k == 0), stop=(k == 7),
                 reads=[name + "_scT", wk], writes=[pk])
        if cb * 512 < ncols:
            P.op("dve", "tensor_tensor", out=mrow[:, cs], in0=pm[0:2, :], in1=brow[:, :], op=ALU.add, reads=[pk, bk], writes=[name + "_mrow"])
        else:
            xr, xk = xrot_.next()
            P.op("dve", "tensor_tensor", out=xr[:, :], in0=pm[0:2, :], in1=brow[:, :], op=ALU.add, reads=[pk, bk], writes=[xk])
            oc = cb * 512 - ncols
            if 2048 <= oc < 3072:
                g2, g2k = g2rot.next()
                P.dma("sp", g2[0:1, :], extra[1][:, oc - 2048:oc - 2048 + 512], writes=[g2k])
                P.dma("sp", g2[1:2, :], extra[1][:, oc - 2048:oc - 2048 + 512], writes=[g2k])
                P.op("dve", "scalar_tensor_tensor", out=xr[:, :], in0=xr[:, :], scalar=1.0, in1=g2[:, :], op0=ALU.add, op1=ALU.mult, reads=[xk, g2k], writes=[xk])
            P.dma("sp", extra[2][:, oc:oc + 512], xr[:, :], reads=[xk])
    return mrow, name + "_mrow"


def bcast_rows(P, row, rowkey, sel, v, pmrot, out, outkey):
    for hh in range(2):
        pm, pk = pmrot.next()
        P.op("pe", "matmul", out=pm[:, :], lhsT=sel[:, v, :], rhs=row[:, hh * 512:(hh + 1) * 512], start=True, stop=True,
             reads=[rowkey, "sel"], writes=[pk])
        P.op("act", "copy", out=out[:, hh * 512:(hh + 1) * 512], in_=pm[:, :], reads=[pk], writes=[outkey])


def rms_rstd(P, xt, xk, rots):
    sq, sqk = rots["sq"].next()
    st, stk = rots["st"].next()
    P.op("act", "activation", out=sq[:], in_=xt, func=AF.Square, accum_out=st[:, 0:1], reads=[xk], writes=[sqk, stk])
    P.op("dve", "tensor_scalar", out=st[:, 1:2], in0=st[:, 0:1], scalar1=1.0 / D, scalar2=EPS, op0=ALU.mult, op1=ALU.add, reads=[stk], writes=[stk])
    P.op("act", "activation", out=st[:, 2:3], in_=st[:, 1:2], func=AF.Sqrt, reads=[stk], writes=[stk])
    P.op("dve", "reciprocal", out=st[:, 3:4], in_=st[:, 2:3], reads=[stk], writes=[stk])
    return st, stk, sq, sqk


def norm_mod_transpose(P, xt, xk, Gb, Sb, gkeys, ident, hT, hTk, col0, rots):
    st, stk, sq, sqk = rms_rstd(P, xt, xk, rots)
    P.op("dve", "scalar_tensor_tensor", out=sq[:], in0=xt, scalar=st[:, 3:4], in1=Gb[:], op0=ALU.mult, op1=ALU.mult,
         reads=[xk, stk, gkeys[0]], writes=[sqk])
    hb, hbk = rots["hb"].next()
    P.op("pool", "tensor_tensor", out=hb[:], in0=sq[:], in1=Sb[:], op=ALU.add, reads=[sqk, gkeys[1]], writes=[hbk])
    ptr, ptk = rots["ptr"].next()
    for k in range(8):
        P.op("pe", "transpose", out=ptr[:, k * 128:(k + 1) * 128], in_=hb[:, k * 128:(k + 1) * 128], identity=ident[:],
             reads=[hbk, "ident"], writes=[ptk])
    P.op("act", "copy", out=hT[:, :, col0:col0 + 128], in_=ptr[:, :].rearrange("p (k t) -> p k t", k=8), reads=[ptk], writes=[hTk])


def head_cols(j):
    g = j // 2
    r = lambda a, n=64: list(range(a, a + n))
    tok = (r(1536 + j * 64) + r(0 + j * 64) + r(1792 + j * 64) + r(256 + g * 64) + r(512 + j * 64) + r(768 + j * 64)
           + r(384 + g * 64) + r(1024 + j * 64) + r(2048 + j * 64))
    feat = r(1280 + j * 64) + r(2304 + j * 64) + r(2560 + j * 64)
    return tok, feat


def emit_p(P, N, M, x_lat, x_ctx, cT, wada, bada, gn, gn2, wt_d, wf_d, tabs, ident_d, sel_d,
           s_qA, s_kB, s_rq, s_rk, s_tok, s_f32, s_modrows):
    nlt, nct = N // 128, M // 128
    ident = P.sb([128, 128], BF16, "ident")
    P.dma("sp", ident[:], ident_d, writes=["ident"])
    sel = P.sb([2, 2, 128], F32, "sel")
    P.dma("sp", sel[:], sel_d, writes=["sel"])
    Wt = P.sb([128, 8, 576], BF16, "Wt")
    Wf = P.sb([128, 8, 192], BF16, "Wf")
    P.dma("pool", Wt[:], wt_d.rearrange("(k p) c -> p k c", p=128), writes=["Wt"])
    P.dma("pool", Wf[:], wf_d.rearrange("(k p) c -> p k c", p=128), writes=["Wf"])
    pmrot = Rot(P, "pm", 3, [128, 512], F32, psum=True)
    mrow, mk = mod_rows(P, cT, wada, bada, 2048, pmrot, "mod", extra=(6144, gn2, s_modrows))
    grow = P.sb([2, D], F32, "grow")
    P.dma("sp", grow[0:1, :], gn, writes=["grow"])
    P.dma("sp", grow[1:2, :], gn, writes=["grow"])
    P.op("dve", "scalar_tensor_tensor", out=grow[:], in0=mrow[:, 1024:2048], scalar=1.0, in1=grow[:], op0=ALU.add, op1=ALU.mult,
         reads=[mk, "grow"], writes=["grow"])
    Gb = [P.sb([128, D], F32, f"Gb{v}") for v in range(2)]
    Sb = [P.sb([128, D], F32, f"Sb{v}") for v in range(2)]
    for v in range(2):
        bcast_rows(P, grow, "grow", sel, v, pmrot, Gb[v], f"Gb{v}")
        bcast_rows(P, mrow[:, 0:1024], mk, sel, v, pmrot, Sb[v], f"Sb{v}")
    rots = {
        "sq": Rot(P, "sq", 4, [128, D], F32),
        "st": Rot(P, "st", 8, [128, 4], F32),
        "hb": Rot(P, "hb", 4, [128, D], BF16),
        "ptr": Rot(P, "ptr", 2, [128, 1024], BF16, psum=True),
    }
    xrot = Rot(P, "xt", 5, [128, D], F32)
    hTrot = Rot(P, "hT", 3, [128, 8, 512], BF16)
    ptokrot = Rot(P, "ptok", 4, [128, 576], F32)
    tabrot = Rot(P, "tab", 4, [128, 160], F32)
    tmprot = Rot(P, "rtmp", 6, [128, 4, 128], F32)
    qkrot = Rot(P, "qk16", 4, [128, 384], BF16)
    vstrot = Rot(P, "vst", 4, [128, 256], BF16)
    ptArot = Rot(P, "ptA", 2, [128, 4, 128], BF16, psum=True)
    f16rot = Rot(P, "f16T", 2, [128, 4, 512], BF16)
    f32rot = Rot(P, "f32T", 2, [128, 2, 512], F32)
    pieces = [(0, 64, SC_DIFF), (64, 128, SC_SWA), (128, 320, 1.0), (320, 384, SC_RET), (384, 512, 1.0)]
    ei = 0
    groups = [(list(range(t, min(t + 4, nct))), 1) for t in range(0, nct, 4)] + [(list(range(t, min(t + 4, nlt))), 0) for t in range(0, nlt, 4)]
    def do_norm(gi, ti):
        tiles, v = groups[gi]
        src = x_ctx if v == 1 else x_lat
        tile = tiles[ti]
        xt, xk = xrot.next()
        P.dma("sp", xt[:], (src(tile) if callable(src) else src[tile * 128:(tile + 1) * 128, :]), reads=["g_x", "s_xc1"], writes=[xk])
        hT, hTk = hts[gi]
        norm_mod_transpose(P, xt[:], xk, Gb[v], Sb[v], (f"Gb{v}", f"Sb{v}"), ident, hT, hTk, ti * 128, rots)

    hts = {}
    st1 = {}

    def stage1(gi, ti):
        tiles, v = groups[gi]
        tile = tiles[ti]
        T0 = (tiles[0] * 128) if v == 1 else (M + tiles[0] * 128)
        hT, hTk = hts[gi]
        ptok, ptokk = ptokrot.next()
        pm, pk = pmrot.next()
        for k in range(8):
            P.op("pe", "matmul", out=pm[:, :], lhsT=hT[:, k, ti * 128:(ti + 1) * 128], rhs=Wt[:, k, 0:512], start=(k == 0), stop=(k == 7),
                 reads=[hTk, "Wt"], writes=[pk])
        for (c0, c1, sc) in pieces:
            P.op("act", "activation", out=ptok[:, c0:c1], in_=pm[:, c0:c1], func=AF.Copy, scale=float(sc), reads=[pk], writes=[ptokk])
        pm2, pk2 = pmrot.next()
        for k in range(8):
            P.op("pe", "matmul", out=pm2[:, 0:64], lhsT=hT[:, k, ti * 128:(ti + 1) * 128], rhs=Wt[:, k, 512:576], start=(k == 0), stop=(k == 7),
                 reads=[hTk, "Wt"], writes=[pk2])
        P.op("act", "copy", out=ptok[:, 512:576], in_=pm2[:, 0:64], reads=[pk2], writes=[ptokk])
        qk, qkk = qkrot.next()
        if v == 0:
            tab, tabk = tabrot.next()
            P.dma("sp", tab[:], tabs[tile * 128:(tile + 1) * 128, :], writes=[tabk])

            def views(t_, kind):
                if kind == "diff":
                    vv = t_[:, 0:256].rearrange("p (a c) -> p a c", a=2)[:, :, 0:64].rearrange("p a (h two d) -> p a h two d", h=2, two=2)
                    return vv[:, :, :, 0, :], vv[:, :, :, 1, :], [128, 2, 2, 16]
                if kind == "swa":
                    vv = t_[:, 0:256].rearrange("p (a c) -> p a c", a=2)[:, :, 64:128].rearrange("p a (two d) -> p a two d", two=2)
                    return vv[:, :, 0, :], vv[:, :, 1, :], [128, 2, 32]
                vv = t_[:, 256:384].rearrange("p (h two d) -> p h two d", h=2, two=2)
                return vv[:, :, 0, :], vv[:, :, 1, :], [128, 2, 32]
            for ki, (kind, hd, tc0) in enumerate((("diff", 16, 64), ("swa", 32, 0), ("ret", 32, 96))):
                x1, x2, shp = views(ptok, kind)
                o1, o2, _ = views(qk, kind)
                cosb = tab[:, tc0:tc0 + hd]
                sinb = tab[:, tc0 + hd:tc0 + 2 * hd]
                for _ in range(len(shp) - 2):
                    cosb = cosb.unsqueeze(1)
                    sinb = sinb.unsqueeze(1)
                cosb = cosb.to_broadcast(shp)
                sinb = sinb.to_broadcast(shp)
                tmp, tmpk = tmprot.next()
                nel = int(np.prod(shp[1:]))
                if len(shp) == 4:
                    T_ = [tmp[:, i, 0:nel].rearrange("p (a h d) -> p a h d", a=shp[1], h=shp[2]) for i in range(4)]
                else:
                    T_ = [tmp[:, i, 0:nel].rearrange("p (a d) -> p a d", a=shp[1]) for i in range(4)]
                e1 = "dve" if (ti + ki) % 2 == 0 else "pool"
                e2 = "pool" if (ti + ki) % 2 == 0 else "dve"
                P.op(e1, "tensor_tensor", out=T_[0], in0=x1, in1=cosb, op=ALU.mult, reads=[ptokk, tabk], writes=[tmpk + "a"])
                P.op(e1, "tensor_tensor", out=T_[1], in0=x2, in1=sinb, op=ALU.mult, reads=[ptokk, tabk], writes=[tmpk + "b"])
                P.op(e2, "tensor_tensor", out=T_[2], in0=x2, in1=cosb, op=ALU.mult, reads=[ptokk, tabk], writes=[tmpk + "c"])
                P.op(e2, "tensor_tensor", out=T_[3], in0=x1, in1=sinb, op=ALU.mult, reads=[ptokk, tabk], writes=[tmpk + "d"])
                P.op(e1, "tensor_tensor", out=o1, in0=T_[0], in1=T_[1], op=ALU.subtract, reads=[tmpk + "a", tmpk + "b"], writes=[qkk])
                P.op(e2, "tensor_tensor", out=o2, in0=T_[2], in1=T_[3], op=ALU.add, reads=[tmpk + "c", tmpk + "d"], writes=[qkk])
        else:
            P.op("dve", "tensor_copy", out=qk[:, :], in_=ptok[:, 0:384], reads=[ptokk], writes=[qkk])
        vst, vstk = vstrot.next()
        P.op("pool", "tensor_copy", out=vst[:, 64:256], in_=ptok[:, 384:576], reads=[ptokk], writes=[vstk])
        P.op("pool", "tensor_copy", out=vst[:, 0:64], in_=qk[:, 320:384], reads=[qkk], writes=[vstk])
        P.dma("sp", s_tok[T0 + ti * 128:T0 + (ti + 1) * 128, :], vst[:], reads=[vstk], writes=["s_tok"])
        st1[(gi, ti)] = (qk, qkk)

    def stage2(gi, ti, f16T, f16k):
        qk, qkk = st1.pop((gi, ti))
        ptA, ptAk = ptArot.next()
        P.op("pe", "transpose", out=ptA[:, 0, :], in_=qk[:, 0:128], identity=ident[:], reads=[qkk, "ident"], writes=[ptAk])
        P.op("pe", "transpose", out=ptA[:, 1, :], in_=qk[:, 128:256], identity=ident[:], reads=[qkk, "ident"], writes=[ptAk])
        P.op("pe", "transpose", out=ptA[0:64, 2, :], in_=qk[:, 256:320], identity=ident[:], reads=[qkk, "ident"], writes=[ptAk])
        P.op("pe", "transpose", out=ptA[0:64, 3, :], in_=qk[:, 320:384], identity=ident[:], reads=[qkk, "ident"], writes=[ptAk])
        P.op("dve", "tensor_copy", out=f16T[:, 0:2, ti * 128:(ti + 1) * 128], in_=ptA[:, 0:2, :], reads=[ptAk], writes=[f16k])
        P.op("dve", "tensor_copy", out=f16T[0:64, 2:4, ti * 128:(ti + 1) * 128], in_=ptA[0:64, 2:4, :], reads=[ptAk], writes=[f16k])

    hts[0] = hTrot.next()
    for ti in range(len(groups[0][0])):
        do_norm(0, ti)
    for gi, (tiles, v) in enumerate(groups):
        ntok = len(tiles) * 128
        T0 = (tiles[0] * 128) if v == 1 else (M + tiles[0] * 128)
        hT, hTk = hts[gi]
        nxt = gi + 1 if gi + 1 < len(groups) else None
        if nxt is not None:
            hts[nxt] = hTrot.next()
        f16T, f16k = f16rot.next()
        for ti in range(len(tiles)):
            stage1(gi, ti)
            if nxt is not None and ti < len(groups[nxt][0]):
                do_norm(nxt, ti)
            if ti > 0:
                stage2(gi, ti - 1, f16T, f16k)
        if nxt is not None:
            for ti in range(len(tiles), len(groups[nxt][0])):
                do_norm(nxt, ti)
        stage2(gi, len(tiles) - 1, f16T, f16k)
        P.dma("sp", s_qA[:, T0:T0 + ntok], f16T[:, 0, 0:ntok], reads=[f16k], writes=["s_qA"])
        P.dma("sp", s_kB[:, T0:T0 + ntok], f16T[:, 1, 0:ntok], reads=[f16k], writes=["s_kB"])
        P.dma("sp", s_rq[:, T0:T0 + ntok], f16T[0:64, 2, 0:ntok], reads=[f16k], writes=["s_rq"])
        P.dma("sp", s_rk[:, T0:T0 + ntok], f16T[0:64, 3, 0:ntok], reads=[f16k], writes=["s_rk"])
        f32T, f32k = f32rot.next()
        for fb, (c0, c1) in enumerate(((0, 128), (128, 192))):
            w = c1 - c0
            pm, pk = pmrot.next()
            for k in range(8):
                P.op("pe", "matmul", out=pm[0:w, 0:ntok], lhsT=Wf[:, k, c0:c1], rhs=hT[:, k, 0:ntok], start=(k == 0), stop=(k == 7), reads=[hTk, "Wf"], writes=[pk])
            P.op("act", "copy", out=f32T[0:w, fb, 0:ntok], in_=pm[0:w, 0:ntok], reads=[pk], writes=[f32k])
        P.dma("sp", s_f32[0:128, T0:T0 + ntok], f32T[:, 0, 0:ntok], reads=[f32k], writes=["s_f32"])
        P.dma("sp", s_f32[128:192, T0:T0 + ntok], f32T[0:64, 1, 0:ntok], reads=[f32k], writes=["s_f32"])
        del hts[gi]
    P.end_phase()


def lam_scalar_col(P, lamv_d, lam_init, pmrot):
    lv = P.sb([1, 128], F32, "lv")
    P.dma("sp", lv[:], lamv_d, writes=["lv"])
    pr = P.sb([1, 64], F32, "lvpr")
    lvv = lv[:, :].rearrange("o (a b d) -> o a b d", a=2, b=2)
    P.op("dve", "tensor_tensor", out=pr[:, :].rearrange("o (a d) -> o a d", a=2), in0=lvv[:, :, 0, :], in1=lvv[:, :, 1, :], op=ALU.mult, reads=["lv"], writes=["lvpr"])
    sm = P.sb([1, 4], F32, "lvsm")
    P.op("dve", "tensor_reduce", out=sm[:, 0:2], in_=pr[:, :].rearrange("o (a d) -> o a d", a=2), axis=AX.X, op=ALU.add, reads=["lvpr"], writes=["lvsm"])
    P.op("act", "activation", out=sm[:, 0:2], in_=sm[:, 0:2], func=AF.Exp, reads=["lvsm"], writes=["lvsm"])
    P.op("dve", "tensor_tensor", out=sm[:, 2:3], in0=sm[:, 0:1], in1=sm[:, 1:2], op=ALU.subtract, reads=["lvsm"], writes=["lvsm"])
    P.op("dve", "tensor_scalar", out=sm[:, 3:4], in0=sm[:, 2:3], scalar1=float(lam_init), scalar2=-1.0, op0=ALU.add, op1=ALU.mult, reads=["lvsm"], writes=["lvsm"])
    P.op("dve", "tensor_scalar", out=sm[:, 2:3], in0=sm[:, 2:3], scalar1=float(lam_init), scalar2=None, op0=ALU.add, reads=["lvsm"], writes=["lvsm"])
    ones1 = P.sb([1, 128], F32, "ones1")
    P.op("pool", "memset", ap=ones1[:], constant=1.0, writes=["ones1"])
    pm, pk = pmrot.next()
    P.op("pe", "matmul", out=pm[:, 0:2], lhsT=ones1[:, :], rhs=sm[:, 2:4], start=True, stop=True, reads=["ones1", "lvsm"], writes=[pk])
    lamc = P.sb([128, 2], F32, "lamc")
    P.op("dve", "tensor_copy", out=lamc[:], in_=pm[:, 0:2], reads=[pk], writes=["lamc"])
    return lamc


def headnorm_T(P, o32, ok, ncol, ones64, eprot, gcol, gkey, mul, out_ap, outk, tmprot, eng_a="dve", eng_b="pool"):
    sq, sqk = tmprot.next()
    P.op(eng_b, "tensor_tensor", out=sq[0:64, 0:ncol], in0=o32, in1=o32, op=ALU.mult, reads=[ok], writes=[sqk])
    pm, pk = eprot.next()
    P.op("pe", "matmul", out=pm[0:64, 0:ncol], lhsT=ones64[0:64, 0:64], rhs=sq[0:64, 0:ncol], start=True, stop=True, reads=[sqk, "ones64"], writes=[pk])
    P.op(eng_a, "tensor_scalar", out=sq[0:64, 0:ncol], in0=pm[0:64, 0:ncol], scalar1=1.0 / 64, scalar2=EPS, op0=ALU.mult, op1=ALU.add, reads=[pk], writes=[sqk])
    P.op("act", "activation", out=sq[0:64, 0:ncol], in_=sq[0:64, 0:ncol], func=AF.Sqrt, reads=[sqk], writes=[sqk])
    P.op(eng_a, "reciprocal", out=sq[0:64, 0:ncol], in_=sq[0:64, 0:ncol], reads=[sqk], writes=[sqk])
    P.op(eng_a, "tensor_tensor", out=sq[0:64, 0:ncol], in0=sq[0:64, 0:ncol], in1=o32, op=ALU.mult, reads=[sqk, ok], writes=[sqk])
    P.op(eng_a, "tensor_scalar", out=out_ap, in0=sq[0:64, 0:ncol], scalar1=gcol, scalar2=float(mul), op0=ALU.mult, op1=ALU.mult, reads=[sqk, gkey], writes=[outk])


def emit_diff(P, N, M, need_ctx, lam_init, qT_d, kT_d, v_d, lamv_d, gn_d, oT_d):
    T = N + M
    NKB = T // 128

    qT = P.sb([64, T], BF16, "qT")
    kT = P.sb([64, T], BF16, "kT")
    V1 = P.sb([128, NKB, 128], BF16, "V1")
    nchunk = 4
    cw = T // nchunk
    for i in range(nchunk):
        P.dma("sp", qT[:, i * cw:(i + 1) * cw], qT_d[:, i * cw:(i + 1) * cw], writes=["qT"])
        P.dma("sp", kT[:, i * cw:(i + 1) * cw], kT_d[:, i * cw:(i + 1) * cw], writes=["kT"])
    vv = v_d.rearrange("(kb p) d -> p kb d", p=128)
    step = max(1, NKB // 8)
    for k0 in range(0, NKB, step):
        k1 = min(NKB, k0 + step)
        P.dma("sp", V1[:, k0:k1, 0:64], vv[:, k0:k1, :], writes=["V1"])
    P.op("pool", "memset", ap=V1[:, :, 64:65], constant=1.0, writes=["V1"])
    P.op("pool", "memset", ap=V1[:, :, 65:128], constant=0.0, writes=["V1"])
    gcol = P.sb([64, 1], F32, "gcol")
    P.dma("sp", gcol[:], gn_d, writes=["gcol"])
    ones64 = P.sb([128, 64], F32, "ones64")
    P.op("pool", "memset", ap=ones64[:], constant=1.0, writes=["ones64"])
    srot = Rot(P, "S", 3, [128, 512], F32, psum=True)
    eprot = Rot(P, "ep", 1, [128, 512], F32, psum=True)
    accrot = [Rot(P, f"acc{m}_", 2, [128, 512], F32, psum=True) for m in range(2)]
    lamc = lam_scalar_col(P, lamv_d, lam_init, eprot)
    ptrot = Rot(P, "PT", 6, [128, 512], BF16)
    rlrot = Rot(P, "rl", 2, [128, 2, 512], F32)
    bcrot = Rot(P, "bc", 2, [64, 2, 512], F32)
    t32rot = Rot(P, "t32", 3, [64, 512], F32)
    orot = Rot(P, "ob", 2, [64, 512], BF16)

    def epilogue(acc, q0, nq):
        rl, rlk = rlrot.next()
        for m in range(2):
            P.op("dve", "reciprocal", out=rl[64:65, m, 0:nq], in_=acc[m][0][64:65, 0:nq], reads=[acc[m][1]], writes=[rlk])
        P.op("dve", "tensor_scalar", out=rl[64:65, 1, 0:nq], in0=rl[64:65, 1, 0:nq], scalar1=lamc[64:65, 1:2], scalar2=None, op0=ALU.mult, reads=[rlk, "lamc"], writes=[rlk])
        bc, bck = bcrot.next()
        for m in range(2):
            pm, pk = eprot.next()
            P.op("pe", "matmul", out=pm[0:64, 0:nq], lhsT=ones64[64:65, 0:64], rhs=rl[64:65, m, 0:nq], start=True, stop=True, reads=[rlk, "ones64"], writes=[pk])
            P.op("dve", "tensor_copy", out=bc[:, m, 0:nq], in_=pm[0:64, 0:nq], reads=[pk], writes=[bck])
        t0, t0k = t32rot.next()
        t1, t1k = t32rot.next()
        P.op("dve", "tensor_tensor", out=t0[:, 0:nq], in0=acc[0][0][0:64, 0:nq], in1=bc[:, 0, 0:nq], op=ALU.mult, reads=[acc[0][1], bck], writes=[t0k])
        P.op("dve", "tensor_tensor", out=t1[:, 0:nq], in0=acc[1][0][0:64, 0:nq], in1=bc[:, 1, 0:nq], op=ALU.mult, reads=[acc[1][1], bck], writes=[t1k])
        P.op("pool", "tensor_tensor", out=t0[:, 0:nq], in0=t0[:, 0:nq], in1=t1[:, 0:nq], op=ALU.add, reads=[t0k, t1k], writes=[t0k])
        ob, obk = orot.next()
        headnorm_T(P, t0[:, 0:nq], t0k, nq, ones64, eprot, gcol[:, 0:1], "gcol", 1.0 - lam_init, ob[:, 0:nq], obk, t32rot)
        P.dma("sp", oT_d[:, q0:q0 + nq], ob[:, 0:nq], reads=[obk])
    QG = 2
    sgroups = []
    if need_ctx:
        sgroups.append(([(0, M)], 0, M // 128))
    lat = [(g0, min(512, T - g0)) for g0 in range(M, T, 512)]
    for i in range(0, len(lat), QG):
        sgroups.append((lat[i:i + QG], 0, NKB))
    LOOK = 1
    for (qgs, kb0, kb1) in sgroups:
        ng = len(qgs)
        acc = [[accrot[m].next() for m in range(2)] for _ in range(ng)]
        steps = [(kb, m) for kb in range(kb0, kb1) for m in range(2)]
        pts = {}

        def issue_s(i):
            kb, m = steps[i]
            for gi, (q0, nq) in enumerate(qgs):
                S, Sk = srot.next()
                P.op("pe", "matmul", out=S[:, 0:nq], lhsT=kT[m * 32:(m + 1) * 32, kb * 128:(kb + 1) * 128], rhs=qT[m * 32:(m + 1) * 32, q0:q0 + nq],
                     start=True, stop=True, reads=["kT", "qT"], writes=[Sk])
                PT, PTk = ptrot.next()
                P.op("act", "activation", out=PT[:, 0:nq], in_=S[:, 0:nq], func=AF.Exp, reads=[Sk], writes=[PTk])
                pts[(i, gi)] = (PT, PTk)

        def issue_pv(i):
            kb, m = steps[i]
            for gi, (q0, nq) in enumerate(qgs):
                PT, PTk = pts.pop((i, gi))
                P.op("pe", "matmul", out=acc[gi][m][0][:, 0:nq], lhsT=V1[:, kb, :], rhs=PT[:, 0:nq], start=(kb == kb0), stop=(kb == kb1 - 1),
                     reads=["V1", PTk], writes=[acc[gi][m][1]])
        for i in range(len(steps) + LOOK):
            if i < len(steps):
                issue_s(i)
            if i - LOOK >= 0:
                issue_pv(i - LOOK)
        for gi, (q0, nq) in enumerate(qgs):
            epilogue(acc[gi], q0, nq)
    P.end_phase()


def swa_masks():
    b = np.arange(128)[:, None]
    a = np.arange(128)[None, :]
    m = np.concatenate([(b <= a), np.ones((128, 128), bool), (a <= b)], 1)
    return m.astype(np.float32).astype(ml_dtypes.bfloat16)


def emit_swa(P, N, M, need_ctx, qT_d, kT_d, v_d, sink_d, m3_d, oT_d):
    T = N + M
    NKB = T // 128
    MB = M // 128
    nb = N // 128

    qT = P.sb([64, T], BF16, "qT")
    kT = P.sb([64, T], BF16, "kT")
    V1 = P.sb([128, NKB, 65], BF16, "V1")
    nchunk = 4
    cw = T // nchunk
    for i in range(nchunk):
        P.dma("sp", qT[:, i * cw:(i + 1) * cw], qT_d[:, i * cw:(i + 1) * cw], writes=["qT"])
        P.dma("sp", kT[:, i * cw:(i + 1) * cw], kT_d[:, i * cw:(i + 1) * cw], writes=["kT"])
    vv = v_d.rearrange("(kb p) d -> p kb d", p=128)
    step = max(1, NKB // 8)
    for k0 in range(0, NKB, step):
        k1 = min(NKB, k0 + step)
        P.dma("sp", V1[:, k0:k1, 0:64], vv[:, k0:k1, :], writes=["V1"])
    P.op("pool", "memset", ap=V1[:, :, 64:65], constant=1.0, writes=["V1"])
    m3 = P.sb([128, 384], BF16, "m3")
    P.dma("sp", m3[:], m3_d, writes=["m3"])
    es = P.sb([128, 1], F32, "es")
    P.dma("sp", es[:], sink_d, writes=["es"])
    P.op("act", "activation", out=es[:], in_=es[:], func=AF.Exp, reads=["es"], writes=["es"])
    ones64 = P.sb([128, 64], F32, "ones64")
    P.op("pool", "memset", ap=ones64[:], constant=1.0, writes=["ones64"])
    accrot = Rot(P, "acc", 2, [128, 512], F32, psum=True)
    srot = Rot(P, "S", 3, [128, 512], F32, psum=True)
    eprot = Rot(P, "ep", 2, [128, 512], F32, psum=True)
    ptrot = Rot(P, "PT", 4, [128, 512], BF16)
    rlrot = Rot(P, "rl", 2, [128, 512], F32)
    bcrot = Rot(P, "bc", 2, [64, 512], F32)
    orot = Rot(P, "ob", 2, [64, 512], BF16)

    qgroups = []
    if need_ctx:
        qgroups.append((0, M, None))
    for s0 in range(0, nb, 4):
        qgroups.append((M + s0 * 128, min(4, nb - s0) * 128, s0))
    ei = 0
    for (q0, nq, s0) in qgroups:
        acc, acck = accrot.next()
        items = [(kb, 0, nq, None) for kb in range(MB)]
        if s0 is not None:
            ns = nq // 128
            for r in range(s0 - 1, s0 + ns + 1):
                if r < 0 or r >= nb:
                    continue
                sa = max(r - 1, s0)
                sb_ = min(r + 1, s0 + ns - 1)
                items.append((MB + r, (sa - s0) * 128, (sb_ - s0 + 1) * 128, (sa - (r - 1)) * 128))
        for ii, (kb, c0, c1, mc) in enumerate(items):
            w = c1 - c0
            S, Sk = srot.next()
            P.op("pe", "matmul", out=S[:, 0:w], lhsT=kT[:, kb * 128:(kb + 1) * 128], rhs=qT[:, q0 + c0:q0 + c1], start=True, stop=True,
                 reads=["kT", "qT"], writes=[Sk])
            PT, PTk = ptrot.next()
            P.op("act", "activation", out=PT[:, 0:w], in_=S[:, 0:w], func=AF.Exp, reads=[Sk], writes=[PTk])
            if mc is not None:
                eng = "dve" if ei % 2 == 0 else "pool"
                ei += 1
                P.op(eng, "tensor_tensor", out=PT[:, 0:w], in0=PT[:, 0:w], in1=m3[:, mc:mc + w], op=ALU.mult, reads=[PTk, "m3"], writes=[PTk])
            P.op("pe", "matmul", out=acc[0:65, c0:c1], lhsT=V1[:, kb, :], rhs=PT[:, 0:w], start=(ii == 0), stop=(ii == len(items) - 1),
                 reads=["V1", PTk], writes=[acck])
        rl, rlk = rlrot.next()
        P.op("dve", "tensor_scalar", out=rl[64:65, 0:nq], in0=acc[64:65, 0:nq], scalar1=es[64:65, 0:1], scalar2=None, op0=ALU.add, reads=[acck, "es"], writes=[rlk])
        P.op("dve", "reciprocal", out=rl[64:65, 0:nq], in_=rl[64:65, 0:nq], reads=[rlk], writes=[rlk])
        pm, pk = eprot.next()
        P.op("pe", "matmul", out=pm[0:64, 0:nq], lhsT=ones64[64:65, 0:64], rhs=rl[64:65, 0:nq], start=True, stop=True, reads=[rlk, "ones64"], writes=[pk])
        bc, bck = bcrot.next()
        P.op("act", "copy", out=bc[:, 0:nq], in_=pm[0:64, 0:nq], reads=[pk], writes=[bck])
        ob, obk = orot.next()
        P.op("dve", "tensor_tensor", out=ob[:, 0:nq], in0=acc[0:64, 0:nq], in1=bc[:, 0:nq], op=ALU.mult, reads=[acck, bck], writes=[obk])
        P.dma("sp", oT_d[:, q0:q0 + nq], ob[:, 0:nq], reads=[obk])
    P.end_phase()


def ret_consts():
    m = np.arange(128)[:, None].astype(np.float32)
    n = np.arange(128)[None, :].astype(np.float32)
    c = np.zeros((128, 6, 128), np.float32)
    c[:, 0] = np.maximum(n - m, 0)
    c[:, 1] = (n >= m)
    c[:, 2] = np.maximum(m - n, 0)
    c[:, 3] = (m >= n)
    c[:, 4] = n + 1
    c[:, 5] = 128 - n
    z = np.zeros((128, 2), np.float32)
    z[:, 0] = 127 - np.arange(128)
    z[:, 1] = np.arange(128)
    return c, z


def emit_ret(P, N, M, need_ctx, qT_d, kT_d, kt_d, v_d, gT_d, lg_d, gn_d, rc_d, rz_d, oT_d):
    T = N + M
    NC = T // 128
    MB = M // 128

    ktok = P.sb([128, NC, 64], BF16, "ktok")
    V = P.sb([128, NC, 64], BF16, "V")
    vv = v_d.rearrange("(kb p) d -> p kb d", p=128)
    kv = kt_d.rearrange("(kb p) d -> p kb d", p=128)
    step = max(1, NC // 8)
    for k0 in range(0, NC, step):
        k1 = min(NC, k0 + step)
        P.dma("sp", V[:, k0:k1, :], vv[:, k0:k1, :], writes=["V"])
        P.dma("sp", ktok[:, k0:k1, :], kv[:, k0:k1, :], writes=["ktok"])
    rc = P.sb([128, 6, 128], F32, "rc")
    P.dma("sp", rc[:], rc_d, writes=["rc"])
    rz = P.sb([128, 2], F32, "rz")
    P.dma("sp", rz[:], rz_d, writes=["rz"])
    gcol = P.sb([64, 1], F32, "gcol")
    P.dma("sp", gcol[:], gn_d, writes=["gcol"])
    lgc = P.sb([128, 2], F32, "lgc")
    P.dma("sp", lgc[:], lg_d, writes=["lgc"])
    P.op("act", "activation", out=lgc[:], in_=lgc[:], func=AF.Exp, scale=-1.0, reads=["lgc"], writes=["lgc"])
    P.op("dve", "tensor_scalar", out=lgc[:], in0=lgc[:], scalar1=1.0, scalar2=None, op0=ALU.add, reads=["lgc"], writes=["lgc"])
    P.op("act", "activation", out=lgc[:], in_=lgc[:], func=AF.Ln, reads=["lgc"], writes=["lgc"])
    P.op("dve", "tensor_scalar", out=lgc[:], in0=lgc[:], scalar1=-1.0, scalar2=None, op0=ALU.mult, reads=["lgc"], writes=["lgc"])
    DcT = P.sb([128, 128], F32, "DcT")
    E2 = P.sb([128, 128], F32, "E2")
    P.op("act", "activation", out=DcT[:], in_=rc[:, 0, :], func=AF.Exp, scale=lgc[:, 0:1], reads=["rc", "lgc"], writes=["DcT"])
    P.op("act", "activation", out=E2[:], in_=rc[:, 2, :], func=AF.Exp, scale=lgc[:, 1:2], reads=["rc", "lgc"], writes=["E2"])
    P.op("dve", "tensor_tensor", out=DcT[:], in0=DcT[:], in1=rc[:, 1, :], op=ALU.mult, reads=["DcT", "rc"], writes=["DcT"])
    P.op("dve", "tensor_tensor", out=E2[:], in0=E2[:], in1=rc[:, 3, :], op=ALU.mult, reads=["E2", "rc"], writes=["E2"])
    P.op("dve", "tensor_tensor", out=DcT[:], in0=DcT[:], in1=E2[:], op=ALU.add, reads=["DcT", "E2"], writes=["DcT"])
    xi = P.sb([64, 2, 128], F32, "xi")
    P.op("act", "activation", out=xi[:, 0, :], in_=rc[0:64, 4, :], func=AF.Exp, scale=lgc[0:64, 0:1], reads=["rc", "lgc"], writes=["xi"])
    P.op("act", "activation", out=xi[:, 1, :], in_=rc[0:64, 5, :], func=AF.Exp, scale=lgc[0:64, 1:2], reads=["rc", "lgc"], writes=["xi"])
    zc = P.sb([128, 2], F32, "zc")
    P.op("act", "activation", out=zc[:, 0:1], in_=rz[:, 0:1], func=AF.Exp, scale=lgc[:, 0:1], reads=["rz", "lgc"], writes=["zc"])
    P.op("act", "activation", out=zc[:, 1:2], in_=rz[:, 1:2], func=AF.Exp, scale=lgc[:, 1:2], reads=["rz", "lgc"], writes=["zc"])
    gc = P.sb([64, 2], F32, "gc")
    P.op("act", "activation", out=gc[:], in_=lgc[0:64, :], func=AF.Exp, scale=128.0, reads=["lgc"], writes=["gc"])
    ones64 = P.sb([128, 64], F32, "ones64")
    P.op("pool", "memset", ap=ones64[:], constant=1.0, writes=["ones64"])

    U = [P.sb([64, NC, 64], F32, f"U{d}") for d in range(2)]
    S16 = [P.sb([64, NC, 64], BF16, f"S16_{d}") for d in range(2)]
    urot = Rot(P, "ups", 2, [128, 512], F32, psum=True)
    kzrot = Rot(P, "kz", 4, [128, 64], BF16)
    for d in range(2):
        for c0 in range(0, NC, 8):
            c1 = min(NC, c0 + 8)
            pm, pk = urot.next()
            for c in range(c0, c1):
                kz, kzk = kzrot.next()
                P.op("dve" if d == 0 else "pool", "tensor_scalar", out=kz[:], in0=ktok[:, c, :], scalar1=zc[:, d:d + 1], scalar2=None, op0=ALU.mult,
                     reads=["ktok", "zc"], writes=[kzk])
                P.op("pe", "matmul", out=pm[0:64, (c - c0) * 64:(c - c0 + 1) * 64], lhsT=kz[:], rhs=V[:, c, :], start=True, stop=True,
                     reads=[kzk, "V"], writes=[pk])
            P.op("act", "copy", out=U[d][:, c0:c1, :], in_=pm[0:64, 0:(c1 - c0) * 64].rearrange("p (c e) -> p c e", e=64), reads=[pk], writes=[f"U{d}"])
    order = [list(range(NC)), list(range(MB - 1, -1, -1)) + list(range(NC - 1, MB - 1, -1))]
    prev = [{}, {}]
    for d in range(2):
        eng = "dve"
        od = order[d]
        for i in range(1, NC):
            prev[d][od[i]] = od[i - 1]
            P.op(eng, "scalar_tensor_tensor", out=U[d][:, od[i], :], in0=U[d][:, od[i - 1], :], scalar=gc[:, d:d + 1], in1=U[d][:, od[i], :],
                 op0=ALU.mult, op1=ALU.add, reads=[f"U{d}", "gc"], writes=[f"U{d}"])
        P.op(eng, "tensor_copy", out=S16[d][:], in_=U[d][:], reads=[f"U{d}"], writes=[f"S16_{d}"])
    accrot = Rot(P, "acc", 2, [128, 512], F32, psum=True)
    srot = Rot(P, "S", 2, [128, 512], F32, psum=True)
    eprot = Rot(P, "ep", 2, [128, 512], F32, psum=True)
    qrot = Rot(P, "qg", 2, [64, 512], BF16)
    krot = Rot(P, "kg", 2, [64, 512], BF16)
    grot = Rot(P, "gg", 2, [64, 512], F32)
    amrot = Rot(P, "am", 3, [128, 128], BF16)
    qxrot = Rot(P, "qx", 3, [64, 2, 128], BF16)
    t32rot = Rot(P, "t32", 3, [64, 512], F32)
    orot = Rot(P, "ob", 2, [64, 512], BF16)
    groups = []
    if need_ctx:
        groups.append(list(range(0, MB)))
    for c0 in range(MB, NC, 4):
        groups.append(list(range(c0, min(NC, c0 + 4))))
    for grp in groups:
        t0_ = grp[0] * 128
        nq = len(grp) * 128
        qg, qgk = qrot.next()
        kg, kgk = krot.next()
        gg, ggk = grot.next()
        P.dma("sp", qg[:, 0:nq], qT_d[:, t0_:t0_ + nq], writes=[qgk])
        P.dma("sp", kg[:, 0:nq], kT_d[:, t0_:t0_ + nq], writes=[kgk])
        P.dma("sp", gg[:, 0:nq], gT_d[:, t0_:t0_ + nq], writes=[ggk])
        P.op("act", "activation", out=gg[:, 0:nq], in_=gg[:, 0:nq], func=AF.Silu, reads=[ggk], writes=[ggk])
        acc, acck = accrot.next()
        for ci, c in enumerate(grp):
            cs = slice(ci * 128, (ci + 1) * 128)
            S, Sk = srot.next()
            P.op("pe", "matmul", out=S[:, 0:128], lhsT=kg[:, cs], rhs=qg[:, cs], start=True, stop=True, reads=[kgk, qgk], writes=[Sk])
            am, amk = amrot.next()
            P.op("dve", "tensor_tensor", out=am[:], in0=S[:, 0:128], in1=DcT[:], op=ALU.mult, reads=[Sk, "DcT"], writes=[amk])
            qx, qxk = qxrot.next()
            P.op("pool", "tensor_tensor", out=qx[:], in0=qg[:, cs].unsqueeze(1).to_broadcast([64, 2, 128]), in1=xi[:], op=ALU.mult, reads=[qgk, "xi"], writes=[qxk])
            mms = [(V[:, c, :], am[:], ["V", amk])]
            for d in range(2):
                if c in prev[d]:
                    mms.append((S16[d][:, prev[d][c], :], qx[:, d, :], [f"S16_{d}", qxk]))
            for mi, (l_, r_, rk_) in enumerate(mms):
                P.op("pe", "matmul", out=acc[0:64, cs], lhsT=l_, rhs=r_, start=(mi == 0), stop=(mi == len(mms) - 1), reads=rk_, writes=[acck])
        y, yk = t32rot.next()
        P.op("act", "copy", out=y[:, 0:nq], in_=acc[0:64, 0:nq], reads=[acck], writes=[yk])
        yn, ynk = t32rot.next()
        headnorm_T(P, y[:, 0:nq], yk, nq, ones64, eprot, gcol[:, 0:1], "gcol", 1.0, yn[:, 0:nq], ynk, t32rot)
        ob, obk = orot.next()
        P.op("pool", "tensor_tensor", out=ob[:, 0:nq], in0=yn[:, 0:nq], in1=gg[:, 0:nq], op=ALU.mult, reads=[ynk, ggk], writes=[obk])
        P.dma("sp", oT_d[:, t0_:t0_ + nq], ob[:, 0:nq], reads=[obk])
    P.end_phase()


def _bk(name, c0, c1):
    return [f"{name}{b}" for b in range(c0 // 512, (c1 - 1) // 512 + 1)]


def emit_lru(P, N, M, lx_d, ly_d, cw_d, cb_d, wa_d, wx_d, ba_d, bx_d, lam_d, i2_d, oT_d):
    T = N + M

    XX = P.sb([128, T], F32, "XX")
    UU = P.sb([128, T], F32, "UU")
    CH = 2048
    for c0 in range(0, T, CH):
        c1 = min(T, c0 + CH)
        P.dma("sp", XX[0:64, c0:c1], lx_d[:, c0:c1], writes=_bk("XX", c0, c1))
        P.dma("sp", XX[64:128, c0:c1], lx_d[:, c0:c1], writes=_bk("XX", c0, c1))
    small = {}
    for nm, d_, shp in (("cw", cw_d, [128, 4]), ("cb", cb_d, [128, 1]), ("wa", wa_d, [64, 128]), ("wx", wx_d, [64, 128]),
                        ("ba", ba_d, [128, 1]), ("bx", bx_d, [128, 1]), ("lam", lam_d, [128, 1]), ("i2", i2_d, [128, 64])):
        t_ = P.sb(shp, F32, nm)
        P.dma("sp", t_[:], d_, writes=[nm])
        small[nm] = t_
    cw, cb, wa, wx, ba, bx, lam, i2 = (small[k] for k in ("cw", "cb", "wa", "wx", "ba", "bx", "lam", "i2"))
    c8 = P.sb([128, 1], F32, "c8")
    P.op("act", "activation", out=c8[:], in_=lam[:], func=AF.Exp, scale=-1.0, reads=["lam"], writes=["c8"])
    P.op("dve", "tensor_scalar", out=c8[:], in0=c8[:], scalar1=1.0, scalar2=None, op0=ALU.add, reads=["c8"], writes=["c8"])
    P.op("act", "activation", out=c8[:], in_=c8[:], func=AF.Ln, reads=["c8"], writes=["c8"])
    P.op("dve", "tensor_scalar", out=c8[:], in0=c8[:], scalar1=-8.0, scalar2=None, op0=ALU.mult, reads=["c8"], writes=["c8"])
    for (s0, s1) in ((0, M), (M, T)):
        for c0 in range(s0, s1, CH):
            c1 = min(s1, c0 + CH)
            P.op("dve", "tensor_scalar", out=UU[:, c0:c1], in0=XX[:, c0:c1], scalar1=cw[:, 1:2], scalar2=cb[:, 0:1], op0=ALU.mult, op1=ALU.add,
                 reads=_bk("XX", c0, c1) + ["cw", "cb"], writes=_bk("UU", c0, c1))
            for (tap, sh) in ((0, -1), (2, 1), (3, 2)):
                o0, o1 = max(c0, s0 - sh), min(c1, s1 - sh)
                if o1 <= o0:
                    continue
                P.op("dve", "scalar_tensor_tensor", out=UU[:, o0:o1], in0=XX[:, o0 + sh:o1 + sh], scalar=cw[:, tap:tap + 1], in1=UU[:, o0:o1],
                     op0=ALU.mult, op1=ALU.add, reads=_bk("XX", o0 + sh, o1 + sh) + _bk("UU", o0, o1) + ["cw"], writes=_bk("UU", o0, o1))
    grot = Rot(P, "gps", 4, [128, 512], F32, psum=True)
    rsrot = Rot(P, "rs", 2, [128, 512], F32)
    isrot = Rot(P, "is", 2, [128, 512], F32)
    t1rot = Rot(P, "t1", 2, [128, 512], F32)
    for c0 in range(0, T, 512):
        c1 = min(T, c0 + 512)
        w = c1 - c0
        uk = _bk("UU", c0, c1)
        xk = _bk("XX", c0, c1)
        pr, prk = grot.next()
        pi, pik = grot.next()
        P.op("pe", "matmul", out=pr[:, 0:w], lhsT=wa[:, :], rhs=UU[0:64, c0:c1], start=True, stop=True, reads=uk + ["wa"], writes=[prk])
        P.op("pe", "matmul", out=pi[:, 0:w], lhsT=wx[:, :], rhs=UU[0:64, c0:c1], start=True, stop=True, reads=uk + ["wx"], writes=[pik])
        rs, rsk = rsrot.next()
        is_, isk = isrot.next()
        P.op("act", "activation", out=rs[:, 0:w], in_=pr[:, 0:w], func=AF.Sigmoid, bias=ba[:, 0:1], reads=[prk, "ba"], writes=[rsk])
        P.op("act", "activation", out=is_[:, 0:w], in_=pi[:, 0:w], func=AF.Sigmoid, bias=bx[:, 0:1], reads=[pik, "bx"], writes=[isk])
        P.op("act", "activation", out=XX[:, c0:c1], in_=rs[:, 0:w], func=AF.Exp, scale=c8[:, 0:1], reads=[rsk, "c8"], writes=xk)
        t1, t1k = t1rot.next()
        P.op("pool", "tensor_tensor", out=t1[:, 0:w], in0=XX[:, c0:c1], in1=XX[:, c0:c1], op=ALU.mult, reads=xk, writes=[t1k])
        P.op("pool", "tensor_scalar", out=t1[:, 0:w], in0=t1[:, 0:w], scalar1=-1.0, scalar2=1.0, op0=ALU.mult, op1=ALU.add, reads=[t1k], writes=[t1k])
        P.op("act", "activation", out=t1[:, 0:w], in_=t1[:, 0:w], func=AF.Sqrt, reads=[t1k], writes=[t1k])
        P.op("dve", "tensor_tensor", out=is_[:, 0:w], in0=is_[:, 0:w], in1=UU[:, c0:c1], op=ALU.mult, reads=[isk] + uk, writes=[isk])
        P.op("dve", "tensor_tensor", out=UU[:, c0:c1], in0=is_[:, 0:w], in1=t1[:, 0:w], op=ALU.mult, reads=[isk, t1k], writes=uk)
    prev = None
    for c0 in range(0, T, CH):
        c1 = min(T, c0 + CH)
        init = 0.0 if prev is None else UU[0:64, c0 - 1:c0]
        P.op("dve", "tensor_tensor_scan", out=UU[0:64, c0:c1], data0=XX[0:64, c0:c1], data1=UU[0:64, c0:c1], initial=init, op0=ALU.mult, op1=ALU.add,
             reads=_bk("XX", c0, c1) + _bk("UU", max(c0 - 1, 0), c1) + ["scanf"], writes=_bk("UU", c0, c1) + ["scanf"])
        prev = c0
    chunks = [(c0, min(M, c0 + CH)) for c0 in range(0, M, CH)][::-1] + [(c0, min(T, c0 + CH)) for c0 in range(M, T, CH)][::-1]
    prev_lo = None
    for (c0, c1) in chunks:
        init = 0.0 if prev_lo is None else UU[64:128, prev_lo:prev_lo + 1]
        rk = [] if prev_lo is None else _bk("UU", prev_lo, prev_lo + 1)
        P.op("dve", "tensor_tensor_scan", out=UU[64:128, c0:c1][:, ::-1], data0=XX[64:128, c0:c1][:, ::-1], data1=UU[64:128, c0:c1][:, ::-1], initial=init,
             op0=ALU.mult, op1=ALU.add, reads=_bk("XX", c0, c1) + _bk("UU", c0, c1) + rk + ["scanb"], writes=_bk("UU", c0, c1) + ["scanb"])
        prev_lo = c0
    hrot = Rot(P, "hps", 2, [128, 512], F32, psum=True)
    yrot = Rot(P, "yy", 2, [64, 512], F32)
    zrot = Rot(P, "zz", 2, [64, 512], F32)
    orot = Rot(P, "ob", 2, [64, 512], BF16)
    for c0 in range(0, T, 512):
        c1 = min(T, c0 + 512)
        w = c1 - c0
        hp, hpk = hrot.next()
        P.op("pe", "matmul", out=hp[0:64, 0:w], lhsT=i2[:, :], rhs=UU[:, c0:c1], start=True, stop=True, reads=_bk("UU", c0, c1) + ["i2"], writes=[hpk])
        yy, yk = yrot.next()
        zz, zk = zrot.next()
        P.dma("sp", yy[:, 0:w], ly_d[:, c0:c1], writes=[yk])
        P.op("pool", "tensor_tensor", out=zz[:, 0:w], in0=yy[:, 0:w], in1=yy[:, 0:w], op=ALU.mult, reads=[yk], writes=[zk])
        P.op("pool", "tensor_scalar", out=zz[:, 0:w], in0=zz[:, 0:w], scalar1=0.044715, scalar2=1.0, op0=ALU.mult, op1=ALU.add, reads=[zk], writes=[zk])
        P.op("pool", "tensor_tensor", out=zz[:, 0:w], in0=zz[:, 0:w], in1=yy[:, 0:w], op=ALU.mult, reads=[zk, yk], writes=[zk])
        P.op("act", "activation", out=zz[:, 0:w], in_=zz[:, 0:w], func=AF.Sigmoid, scale=2.0 * math.sqrt(2.0 / math.pi), reads=[zk], writes=[zk])
        P.op("pool", "tensor_tensor", out=zz[:, 0:w], in0=zz[:, 0:w], in1=yy[:, 0:w], op=ALU.mult, reads=[zk, yk], writes=[zk])
        ob, obk = orot.next()
        P.op("dve", "tensor_tensor", out=ob[:, 0:w], in0=hp[0:64, 0:w], in1=zz[:, 0:w], op=ALU.mult, reads=[hpk, zk], writes=[obk])
        P.dma("sp", oT_d[:, c0:c1], ob[:, 0:w], reads=[obk])
    P.end_phase()


def emit_f(P, tile_specs, nv, E, FF, moe, final, modrows_d, sel_d, wo_d, w1_d, w3_d, w2_d, ident_d, wr_d=None, id32_d=None, fn_d=None, GT=8):
    ntile = len(tile_specs)
    NFB = FF // 128
    nhalf = 2
    HB = NFB // nhalf
    ident = P.sb([128, 128], BF16, "ident")
    P.dma("sp", ident[:], ident_d, writes=["ident"])
    sel = P.sb([2, 2, 128], F32, "sel")
    P.dma("sp", sel[:], sel_d, writes=["sel"])
    mrrot = Rot(P, "mrowF", 2, [2, 1024], F32)
    pmrot = Rot(P, "pm", 3, [128, 512], F32, psum=True)
    mt = [[P.sb([128, D], F32, f"mt{v}_{i}") for i in range(4)] for v in range(nv)]
    for i in range(4):
        mrow, mrk = mrrot.next()
        P.dma("sp", mrow[:], modrows_d[:, i * 1024:(i + 1) * 1024], writes=[mrk])
        for v in range(nv):
            bcast_rows(P, mrow, mrk, sel, v, pmrot, mt[v][i], f"mt{v}_{i}")
    if final:
        fnb = P.sb([128, D], F32, "fnb")
        P.dma("sp", fnb[:], fn_d, writes=["fnb"])
    Wo = P.sb([128, 8, D], BF16, "Wo")
    wov = wo_d.rearrange("(k p) c -> p k c", p=128)
    for k in range(8):
        P.dma("pool", Wo[:, k, :], wov[:, k, :], writes=["Wo"])
    if moe:
        Wr = P.sb([128, 8, E], F32, "Wr")
        P.dma("sp", Wr[:], wr_d.rearrange("(k p) e -> p k e", p=128), writes=["Wr"])
        id32 = P.sb([128, 128], F32, "id32")
        P.dma("sp", id32[:], id32_d, writes=["id32"])
        gates = P.sb([128, GT, E], F32, "gates")
        h32T = P.sb([128, 8, 128], F32, "h32T")
        rsm = Rot(P, "rsm", 2, [128, 40], F32)
    purot = Rot(P, "pu", 4, [128, 512], F32, psum=True)
    rots = {
        "sq": Rot(P, "sq", 2, [128, D], F32),
        "st": Rot(P, "st", 4, [128, 4], F32),
        "hb": Rot(P, "hb", 2, [128, D], BF16),
        "ptr": Rot(P, "ptr", 1, [128, 1024], BF16, psum=True),
    }
    xrot = Rot(P, "xt", 2, [128, D], F32)
    otrot = Rot(P, "oTt", 2, [128, 8, 128], BF16)
    acc = P.sb([128, GT, D], F32, "acc")
    h2T = P.sb([128, 8, GT * 128], BF16, "h2T")
    gT = P.sb([128, HB, GT * 128], BF16, "gT")
    W2 = P.sb([128, HB, D], BF16, "W2")
    w1rot = Rot(P, "w1b", 2, [128, 8, 256], BF16)
    w3rot = Rot(P, "w3b", 2, [128, 8, 256], BF16)
    srot = Rot(P, "sil", 2, [128, 512], F32)
    tmprot = Rot(P, "ftmp", 2, [128, 512], F32)
    orot = Rot(P, "xout", 1, [128, D], F32) if final else None

    tiles_v = [ts_[0] for ts_ in tile_specs]
    for g0 in range(0, ntile, GT):
        tiles = list(range(g0, min(ntile, g0 + GT)))
        ng = len(tiles)
        ntok = ng * 128
        for ti, tile in enumerate(tiles):
            v = tiles_v[tile]
            acck = f"acc{ti}"
            xt, xk = xrot.next()
            P.dma("sp", xt[:], tile_specs[tile][1], writes=[xk])
            ot, otk = otrot.next()
            P.dma("sp", ot[:], tile_specs[tile][2], reads=["l_oT"], writes=[otk])
            for hh in range(2):
                pm, pk = pmrot.next()
                for k in range(8):
                    P.op("pe", "matmul", out=pm[:, :], lhsT=ot[:, k, :], rhs=Wo[:, k, hh * 512:(hh + 1) * 512], start=(k == 0), stop=(k == 7),
                         reads=[otk, "Wo"], writes=[pk])
                hs = slice(hh * 512, (hh + 1) * 512)
                P.op("dve", "tensor_tensor", out=acc[:, ti, hs], in0=pm[:, :], in1=mt[v][0][:, hs], op=ALU.mult, reads=[pk, f"mt{v}_0"], writes=[acck])
                P.op("pool", "tensor_tensor", out=acc[:, ti, hs], in0=acc[:, ti, hs], in1=xt[:, hs], op=ALU.add, reads=[acck, xk], writes=[acck])
            st, stk, sq, sqk = rms_rstd(P, acc[:, ti, :], acck, rots)
            P.op("dve", "scalar_tensor_tensor", out=sq[:], in0=acc[:, ti, :], scalar=st[:, 3:4], in1=mt[v][2][:], op0=ALU.mult, op1=ALU.mult,
                 reads=[acck, stk, f"mt{v}_2"], writes=[sqk])
            P.op("pool", "tensor_tensor", out=sq[:], in0=sq[:], in1=mt[v][1][:], op=ALU.add, reads=[sqk, f"mt{v}_1"], writes=[sqk])
            hb, hbk = rots["hb"].next()
            P.op("act", "copy", out=hb[:], in_=sq[:], reads=[sqk], writes=[hbk])
            ptr, ptk = rots["ptr"].next()
            for k in range(8):
                P.op("pe", "transpose", out=ptr[:, k * 128:(k + 1) * 128], in_=hb[:, k * 128:(k + 1) * 128], identity=ident[:], reads=[hbk, "ident"], writes=[ptk])
            P.op("act", "copy", out=h2T[:, :, ti * 128:(ti + 1) * 128], in_=ptr[:, :].rearrange("p (k t) -> p k t", k=8), reads=[ptk], writes=[f"h2T{ti}"])
            if moe:
                for hh in range(2):
                    pm, pk = pmrot.next()
                    for k4 in range(4):
                        k = hh * 4 + k4
                        P.op("pe", "transpose", out=pm[:, k4 * 128:(k4 + 1) * 128], in_=sq[:, k * 128:(k + 1) * 128], identity=id32[:], reads=[sqk, "id32"], writes=[pk])
                    P.op("act", "copy", out=h32T[:, hh * 4:(hh + 1) * 4, :], in_=pm[:, :].rearrange("p (k t) -> p k t", k=4), reads=[pk], writes=["h32T"])
                pm, pk = pmrot.next()
                for k in range(8):
                    P.op("pe", "matmul", out=pm[:, 0:E], lhsT=h32T[:, k, :], rhs=Wr[:, k, :], start=(k == 0), stop=(k == 7), reads=["h32T", "Wr"], writes=[pk])
                r, rk = rsm.next()
                lg, eq1, lg2, eq2 = r[:, 0:E], r[:, 8:8 + E], r[:, 16:16 + E], r[:, 24:24 + E]
                m1, m2, dd, ww1, ww2 = (r[:, 32 + i:33 + i] for i in range(5))
                P.op("act", "copy", out=lg, in_=pm[:, 0:E], reads=[pk], writes=[rk])
                P.op("dve", "tensor_reduce", out=m1, in_=lg, axis=AX.X, op=ALU.max, reads=[rk], writes=[rk])
                P.op("dve", "tensor_scalar", out=eq1, in0=lg, scalar1=m1, scalar2=None, op0=ALU.is_equal, reads=[rk], writes=[rk])
                P.op("dve", "scalar_tensor_tensor", out=lg2, in0=eq1, scalar=-1e30, in1=lg, op0=ALU.mult, op1=ALU.add, reads=[rk], writes=[rk])
                P.op("dve", "tensor_reduce", out=m2, in_=lg2, axis=AX.X, op=ALU.max, reads=[rk], writes=[rk])
                P.op("dve", "tensor_scalar", out=eq2, in0=lg2, scalar1=m2, scalar2=None, op0=ALU.is_equal, reads=[rk], writes=[rk])
                P.op("dve", "tensor_tensor", out=dd, in0=m2, in1=m1, op=ALU.subtract, reads=[rk], writes=[rk])
                P.op("act", "activation", out=dd, in_=dd, func=AF.Exp, reads=[rk], writes=[rk])
                P.op("dve", "tensor_scalar", out=ww1, in0=dd, scalar1=1.0, scalar2=None, op0=ALU.add, reads=[rk], writes=[rk])
                P.op("dve", "reciprocal", out=ww1, in_=ww1, reads=[rk], writes=[rk])
                P.op("dve", "tensor_tensor", out=ww2, in0=dd, in1=ww1, op=ALU.mult, reads=[rk], writes=[rk])
                P.op("dve", "tensor_scalar", out=gates[:, ti, :], in0=eq1, scalar1=ww1, scalar2=None, op0=ALU.mult, reads=[rk], writes=[f"gates{ti}"])
                P.op("dve", "scalar_tensor_tensor", out=gates[:, ti, :], in0=eq2, scalar=ww2, in1=gates[:, ti, :], op0=ALU.mult, op1=ALU.add, reads=[rk, f"gates{ti}"], writes=[f"gates{ti}"])
        h2keys = [f"h2T{ti}" for ti in range(ng)]
        chunks = [(c0, min(ntok, c0 + 512)) for c0 in range(0, ntok, 512)]
        for e in range(E):
            for half in range(nhalf):
                fb0 = half * HB
                w2v = w2_d[e, fb0 * 128:(fb0 + HB) * 128, :].rearrange("(f p) c -> p f c", p=128)
                for fp in range(0, HB, 2):
                    nb_ = min(2, HB - fp)
                    w1b, w1k = w1rot.next()
                    w3b, w3k = w3rot.next()
                    cs = slice((fb0 + fp) * 128, (fb0 + fp + nb_) * 128)
                    P.dma("pool", w1b[:, :, 0:nb_ * 128], w1_d[e, :, cs].rearrange("(k p) c -> p k c", p=128), writes=[w1k])
                    P.dma("pool", w3b[:, :, 0:nb_ * 128], w3_d[e, :, cs].rearrange("(k p) c -> p k c", p=128), writes=[w3k])
                    P.dma("pool", W2[:, fp:fp + nb_, :], w2v[:, fp:fp + nb_, :], writes=["W2"])
                    for bi in range(nb_):
                        fbi = fp + bi
                        for (c0, c1) in chunks:
                            w = c1 - c0
                            hk = h2keys[c0 // 128:(c1 - 1) // 128 + 1]
                            p1, p1k = purot.next()
                            p3, p3k = purot.next()
                            for k in range(8):
                                P.op("pe", "matmul", out=p1[:, 0:w], lhsT=w1b[:, k, bi * 128:(bi + 1) * 128], rhs=h2T[:, k, c0:c1], start=(k == 0), stop=(k == 7),
                                     reads=[w1k] + hk, writes=[p1k])
                            for k in range(8):
                                P.op("pe", "matmul", out=p3[:, 0:w], lhsT=w3b[:, k, bi * 128:(bi + 1) * 128], rhs=h2T[:, k, c0:c1], start=(k == 0), stop=(k == 7),
                                     reads=[w3k] + hk, writes=[p3k])
                            sl, slk = srot.next()
                            P.op("act", "activation", out=sl[:, 0:w], in_=p1[:, 0:w], func=AF.Silu, reads=[p1k], writes=[slk])
                            P.op("dve", "tensor_tensor", out=gT[:, fbi, c0:c1], in0=p3[:, 0:w], in1=sl[:, 0:w], op=ALU.mult, reads=[p3k, slk], writes=[f"gT{c0 // 512}"])
                for ti, tile in enumerate(tiles):
                    v = tiles_v[tile]
                    for hh in range(2):
                        hs = slice(hh * 512, (hh + 1) * 512)
                        pm, pk = pmrot.next()
                        for fbi in range(HB):
                            P.op("pe", "matmul", out=pm[:, :], lhsT=gT[:, fbi, ti * 128:(ti + 1) * 128], rhs=W2[:, fbi, hs], start=(fbi == 0), stop=(fbi == HB - 1),
                                 reads=[f"gT{ti // 4}", "W2"], writes=[pk])
                        tmp, tmpk = tmprot.next()
                        if moe:
                            P.op("dve", "scalar_tensor_tensor", out=tmp[:], in0=pm[:, :], scalar=gates[:, ti, e:e + 1], in1=mt[v][3][:, hs], op0=ALU.mult, op1=ALU.mult,
                                 reads=[pk, f"gates{ti}", f"mt{v}_3"], writes=[tmpk])
                        else:
                            P.op("dve", "tensor_tensor", out=tmp[:], in0=pm[:, :], in1=mt[v][3][:, hs], op=ALU.mult, reads=[pk, f"mt{v}_3"], writes=[tmpk])
                        P.op("pool", "tensor_tensor", out=acc[:, ti, hs], in0=acc[:, ti, hs], in1=tmp[:], op=ALU.add, reads=[f"acc{ti}", tmpk], writes=[f"acc{ti}"])
        for ti, tile in enumerate(tiles):
            if final:
                st, stk, sq, sqk = rms_rstd(P, acc[:, ti, :], f"acc{ti}", rots)
                xo_t, xok = orot.next()
                P.op("dve", "scalar_tensor_tensor", out=xo_t[:], in0=acc[:, ti, :], scalar=st[:, 3:4], in1=fnb[:], op0=ALU.mult, op1=ALU.mult,
                     reads=[f"acc{ti}", stk, "fnb"], writes=[xok])
                P.dma("sp", tile_specs[tile][3], xo_t[:], reads=[xok], writes=["xout_d"])
            else:
                P.dma("sp", tile_specs[tile][3], acc[:, ti, :], reads=[f"acc{ti}"], writes=["xout_d", "xsrc"])
    P.end_phase()


def build_fused(N, M, E, FFD, FFE):
    T = N + M
    tps = N // 4
    nc = bass.Bass("TRN2", target_bir_lowering=False)
    P = Prog(nc)
    di = lambda n, s, d=F32: nc.dram_tensor(n, s, d, kind="ExternalInput").ap()
    do = lambda n, s, d=F32: nc.dram_tensor(n, s, d, kind="ExternalOutput").ap()
    sc = lambda n, s, d=F32: nc.dram_tensor(n, s, d).ap()
    xsh = di("xsh", [tps, D])
    ctx = di("ctx", [M, D])
    cT = di("cT", [128, 8, 2])
    tabs = di("tabs", [N, 160])
    ident_d = di("ident", [128, 128], BF16)
    id32_d = di("id32", [128, 128])
    sel_d = di("sel", [2, 2, 128])
    m3_d = di("m3", [128, 384], BF16)
    rc_d = di("retc", [128, 6, 128])
    rz_d = di("retz", [128, 2])
    i2_d = di("i2", [128, 64])
    fn_d = di("fnb", [128, D])
    L = []
    for l in range(2):
        L.append(dict(
            wada=di(f"wada{l}", [D, 6144]), bada=di(f"bada{l}", [1, 6144]), gn1=di(f"gn1_{l}", [1, D]), gn2=di(f"gn2_{l}", [1, D]),
            wt=di(f"wt{l}", [D, 576]), wf=di(f"wf{l}", [D, 192]), wo=di(f"wo{l}", [D, D]),
            sink=di(f"sink{l}", [128, 1]), logit=di(f"logit{l}", [128, 2]), rgn=di(f"rgn{l}", [64, 1]),
            lamv=di(f"lamv{l}", [1, 128]), dgn=di(f"dgn{l}", [64, 1]),
            cw=di(f"cw{l}", [128, 4]), cb=di(f"cb{l}", [128, 1]), wa=di(f"wa{l}", [64, 128]), wx=di(f"wx{l}", [64, 128]),
            ba=di(f"ba{l}", [128, 1]), bx=di(f"bx{l}", [128, 1]), lam=di(f"lam{l}", [128, 1])))
    fw1, fw3, fw2 = di("fw1", [1, D, FFD]), di("fw3", [1, D, FFD]), di("fw2", [1, FFD, D])
    mw1, mw3, mw2 = di("mw1", [E, D, FFE]), di("mw3", [E, D, FFE]), di("mw2", [E, FFE, D])
    wr_d = di("wr", [D, E])
    xo = do("xo", [tps, D])
    s_qA, s_kB = sc("s_qA", [128, T], BF16), sc("s_kB", [128, T], BF16)
    s_rq, s_rk = sc("s_rq", [64, T], BF16), sc("s_rk", [64, T], BF16)
    s_tok = sc("s_tok", [T, 256], BF16)
    s_f32 = sc("s_f32", [192, T])
    s_mod = sc("s_mod", [2, 4096])
    s_oT = sc("s_oT", [256, T], BF16)
    CHT = tps // 2
    NCH = N // CHT
    st_o = sc("st_o", [NCH * 256, CHT], BF16)
    st_oc = sc("st_oc", [256, M], BF16)
    g_o = sc("g_o", [NCH * 1024, CHT], BF16)
    g_oc = sc("g_oc", [1024, M], BF16)
    l_oT = sc("l_oT", [1024, tps], BF16)
    s_x0 = sc("s_x0", [tps, D])
    s_x1 = sc("s_x1", [tps, D])
    XR = min(256, tps)
    NXC = tps // XR
    g_x = [sc(f"g_x{c}", [4 * XR, D]) for c in range(NXC)]
    s_xc1 = sc("s_xc1", [M, D])
    RG = [[0, 1, 2, 3], [4, 5, 6, 7]]
    l_oTv = l_oT.rearrange("(k p) t -> p k t", p=128)
    g_ocv = g_oc.rearrange("(k p) t -> p k t", p=128)

    def gather_x(src):
        for c in range(NXC):
            P.op("pool", "collective_compute", cc=True, kind="AllGather", op=ALU.bypass, replica_groups=RG,
                 ins=[src[c * XR:(c + 1) * XR, :]], outs=[g_x[c]], reads=["xsrc"], writes=["g_x"])

    def x_tile(ti):
        t0 = ti * 128
        r, rem = t0 // tps, t0 % tps
        c, i = rem // XR, rem % XR
        return g_x[c][r * XR + i:r * XR + i + 128, :]
    for r0 in range(0, tps, max(128, tps // 4)):
        r1 = min(tps, r0 + max(128, tps // 4))
        P.dma("sp", s_x0[r0:r1, :], xsh[r0:r1, :], writes=["xsrc"])
    gather_x(s_x0)
    for l in range(2):
        W = L[l]
        need_ctx = (l == 0)
        lam_init = 0.8 - 0.6 * math.exp(-0.3 * l)
        x_lat = x_tile
        x_ctx = ctx if l == 0 else s_xc1
        emit_p(P, N, M, x_lat, x_ctx, cT, W["wada"], W["bada"], W["gn1"], W["gn2"], W["wt"], W["wf"], tabs, ident_d, sel_d,
               s_qA, s_kB, s_rq, s_rk, s_tok, s_f32, s_mod)
        emit_swa(P, N, M, need_ctx, s_qA[64:128, :], s_kB[64:128, :], s_tok[:, 64:128], W["sink"], m3_d, s_oT[0:64, :])
        emit_ret(P, N, M, need_ctx, s_rq, s_rk, s_tok[:, 0:64], s_tok[:, 128:192], s_f32[0:64, :], W["logit"], W["rgn"], rc_d, rz_d, s_oT[64:128, :])
        emit_diff(P, N, M, need_ctx, lam_init, s_qA[0:64, :], s_kB[0:64, :], s_tok[:, 192:256], W["lamv"], W["dgn"], s_oT[128:192, :])
        emit_lru(P, N, M, s_f32[64:128, :], s_f32[128:192, :], W["cw"], W["cb"], W["wa"], W["wx"], W["ba"], W["bx"], W["lam"], i2_d, s_oT[192:256, :])
        for c in range(NCH):
            P.dma("sp", st_o[c * 256:(c + 1) * 256, :], s_oT[:, M + c * CHT:M + (c + 1) * CHT], writes=[f"st_o{c}"])
        if need_ctx:
            P.dma("sp", st_oc[:, :], s_oT[:, 0:M], writes=["st_oc"])
        for c in range(NCH):
            P.op("pool", "collective_compute", cc=True, kind="AllGather", op=ALU.bypass, replica_groups=RG,
                 ins=[st_o[c * 256:(c + 1) * 256, :]], outs=[g_o[c * 1024:(c + 1) * 1024, :]], reads=[f"st_o{c}"], writes=["g_o"])
        if need_ctx:
            P.op("pool", "collective_compute", cc=True, kind="AllGather", op=ALU.bypass, replica_groups=RG,
                 ins=[st_oc], outs=[g_oc], reads=["st_oc"], writes=["g_oc"])
        P.end_phase()
        pidc = {}

        def shard_rows(e, pidc=pidc):
            if id(e) not in pidc:
                pidc[id(e)] = e.snap((e.partition_id() % 4) * 2048)
            return pidc[id(e)]
        for h in range(2):
            P.dma("sp", l_oT[:, h * CHT:(h + 1) * CHT],
                  (lambda e, h=h, sr=shard_rows: g_o[bass.ds(sr(e) + h * 1024, 1024), :]), writes=["l_oT"])
        specs = []
        for i in range(tps // 128):
            if l == 0:
                xs = xsh[i * 128:(i + 1) * 128, :]
                od = s_x1[i * 128:(i + 1) * 128, :]
            else:
                xs = s_x1[i * 128:(i + 1) * 128, :]
                od = xo[i * 128:(i + 1) * 128, :]
            specs.append((0, xs, l_oTv[:, :, i * 128:(i + 1) * 128], od))
        if need_ctx:
            for i in range(M // 128):
                specs.append((1, ctx[i * 128:(i + 1) * 128, :], g_ocv[:, :, i * 128:(i + 1) * 128], s_xc1[i * 128:(i + 1) * 128, :]))
        if l == 0:
            emit_f(P, specs, 2, 1, FFD, False, False, s_mod, sel_d, W["wo"], fw1, fw3, fw2, ident_d)
            gather_x(s_x1)
            P.end_phase()
        else:
            emit_f(P, specs, 1, E, FFE, True, True, s_mod, sel_d, W["wo"], mw1, mw3, mw2, ident_d, wr_d=wr_d, id32_d=id32_d, fn_d=fn_d)
    P.close()
    return nc


NCORES = 8
_cache = {}


def _get(name, fn, *a):
    key = (name,) + a
    if key not in _cache:
        _cache[key] = fn(*a)
    return _cache[key]


def _wo_perm():
    idx = np.zeros(1024, np.int64)
    for j in range(4):
        for m in range(4):
            idx[j * 256 + m * 64:j * 256 + (m + 1) * 64] = np.arange(m * 256 + j * 64, m * 256 + (j + 1) * 64)
    return idx


def make_maps(inp):
    f32 = np.float32
    B, N, _ = inp["x"].shape
    M = inp["ctx"].shape[1]
    ident = np.eye(128, dtype=f32).astype(ml_dtypes.bfloat16)
    sel = np.zeros((2, 2, 128), f32)
    sel[0, 0, :] = 1.0
    sel[1, 1, :] = 1.0
    rc, rz = ret_consts()
    tabs = rope_tables(N)
    common = {"ident": ident, "id32": np.eye(128, dtype=f32), "sel": sel, "m3": swa_masks(), "retc": rc, "retz": rz,
              "i2": np.concatenate([np.eye(64, dtype=f32)] * 2, 0), "tabs": tabs,
              "fnb": np.ascontiguousarray(np.broadcast_to(inp["final_norm"][None, :], (128, D))).astype(f32),
              "fw1": inp["ffn_w1"], "fw3": inp["ffn_w3"], "fw2": inp["ffn_w2"],
              "mw1": inp["moe_w1"][0], "mw3": inp["moe_w3"][0], "mw2": inp["moe_w2"][0], "wr": inp["moe_router"][0]}
    woidx = _wo_perm()
    for l in range(2):
        common[f"wada{l}"] = inp["w_ada"][l]
        common[f"bada{l}"] = np.ascontiguousarray(inp["b_ada"][l][None, :])
        common[f"gn1_{l}"] = np.ascontiguousarray(inp["g_norm1"][l][None, :])
        common[f"gn2_{l}"] = np.ascontiguousarray(inp["g_norm2"][l][None, :])
        common[f"wo{l}"] = np.ascontiguousarray(inp["w_out"][l][woidx, :])
        common[f"lamv{l}"] = np.ascontiguousarray(inp["diff_lambda"][l].reshape(1, 128))
    maps = []
    dup = lambda a: np.ascontiguousarray(np.concatenate([a, a], 0)).astype(f32)
    for core in range(NCORES):
        b, j = core // 4, core % 4
        s = slice(j * 64, (j + 1) * 64)
        m = dict(common)
        m["xsh"] = np.ascontiguousarray(inp["x"][b, j * (N // 4):(j + 1) * (N // 4)])
        m["ctx"] = np.ascontiguousarray(inp["ctx"][b])
        cv = np.stack([inp["c"][b], inp["c_ctx"]], 0)
        m["cT"] = np.ascontiguousarray(cv.reshape(2, 8, 128).transpose(2, 1, 0)).astype(f32)
        tok, feat = head_cols(j)
        for l in range(2):
            two = lambda a: np.ascontiguousarray(np.concatenate([a[0][s], a[1][s]], 0)[:, None]).astype(f32)
            m[f"wt{l}"] = np.ascontiguousarray(inp["w_in"][l][:, tok])
            m[f"wf{l}"] = np.ascontiguousarray(inp["w_in"][l][:, feat])
            m[f"sink{l}"] = np.full((128, 1), inp["attn_sink"][l][j], f32)
            m[f"logit{l}"] = np.ascontiguousarray(np.broadcast_to(inp["ret_decay_logit"][l][:, j][None, :], (128, 2))).astype(f32)
            m[f"rgn{l}"] = np.ascontiguousarray(inp["ret_gn"][l][s, None])
            m[f"dgn{l}"] = np.ascontiguousarray(inp["diff_gn"][l][s, None])
            m[f"cw{l}"] = dup(inp["conv_w"][l][:, s].T)
            m[f"cb{l}"] = dup(inp["conv_b"][l][s, None])
            m[f"wa{l}"] = np.ascontiguousarray(np.concatenate([inp["lru_wa"][l][0, j], inp["lru_wa"][l][1, j]], 1))
            m[f"wx{l}"] = np.ascontiguousarray(np.concatenate([inp["lru_wx"][l][0, j], inp["lru_wx"][l][1, j]], 1))
            m[f"ba{l}"] = two(inp["lru_ba"][l])
            m[f"bx{l}"] = two(inp["lru_bx"][l])
            m[f"lam{l}"] = two(inp["lru_lambda"][l])
        maps.append(m)
    return maps


def kernel(**inputs):
    inp = {k: np.asarray(v) for k, v in inputs.items()}
    B, N, _ = inp["x"].shape
    M = inp["ctx"].shape[1]
    E = inp["moe_w1"].shape[1]
    nc = _get("fused", build_fused, N, M, E, inp["ffn_w1"].shape[2], inp["moe_w1"].shape[3])
    res = run_bass_kernel_spmd(nc, make_maps(inp), core_ids=list(range(NCORES))).results
    out = np.stack([np.concatenate([res[b * 4 + s]["xo"] for s in range(4)], 0) for b in range(B)], 0)
    return out.astype(np.float32)
```

```python
from contextlib import ExitStack
import math
import numpy as np
import ml_dtypes
import concourse.bass as bass
import concourse.mybir as mybir
from concourse.bass_utils import run_bass_kernel_spmd

F32 = mybir.dt.float32
BF16 = mybir.dt.bfloat16
I32 = mybir.dt.int32
AF = mybir.ActivationFunctionType
ALU = mybir.AluOpType
AX = mybir.AxisListType

ENGS = ("pe", "act", "dve", "pool", "sp")
DMAQ = ("sp", "act", "pool")
NDMASEM = 8
_ENGATTR = {"pe": "tensor", "act": "scalar", "dve": "vector", "pool": "gpsimd", "sp": "sync"}


class Op:
    __slots__ = ("eng", "fn", "dma", "cc", "raw", "war", "sig", "dsem", "dval", "has_dep")

    def __init__(self, eng, fn, dma, cc=False):
        self.eng = eng
        self.fn = fn
        self.dma = dma
        self.cc = cc
        self.raw = []
        self.war = []
        self.sig = None
        self.dsem = None
        self.dval = None
        self.has_dep = False


class Prog:
    def __init__(self, nc):
        self.nc = nc
        self.gs = ExitStack()
        self.sems = {e: self.gs.enter_context(nc.semaphore(f"s_{e}")) for e in ENGS}
        self.dsems = {e: [self.gs.enter_context(nc.semaphore(f"d_{e}{i}")) for i in range(NDMASEM)] for e in DMAQ}
        self.cnt = {e: 0 for e in ENGS}
        self.dcnt = {e: [0] * NDMASEM for e in DMAQ}
        self.dk = {e: 0 for e in DMAQ}
        self.ccsem = self.gs.enter_context(nc.semaphore("s_cc"))
        self.cccnt = 0
        self.waited = {e: {} for e in ENGS}
        self.barrier = None
        self.nsb = 0
        self.nphase = 0
        self._reset()

    def _reset(self):
        self.q = {e: [] for e in ENGS}
        self.res_w = {}
        self.res_r = {}
        self.es = ExitStack()

    def sb(self, shape, dt=F32, name=None, persist=False):
        self.nsb += 1
        st = self.gs if persist else self.es
        return st.enter_context(self.nc.sbuf_tensor(f"s{self.nsb}_" + (name or "t"), list(shape), dt))

    def ps(self, shape, dt=F32, name=None):
        self.nsb += 1
        return self.es.enter_context(self.nc.psum_tensor(f"p{self.nsb}_" + (name or "t"), list(shape), dt))

    def op(self, eng, meth, reads=(), writes=(), dma=False, cc=False, **kw):
        fn = (lambda e: getattr(e, meth)(**{k: (v(e) if callable(v) else v) for k, v in kw.items()}))
        o = Op(eng, fn, dma or cc, cc)
        raw = set()
        war = set()
        for r in reads:
            w = self.res_w.get(r)
            if w is not None:
                raw.add(w)
        for wr in writes:
            w = self.res_w.get(wr)
            if w is not None:
                war.add(w)
            for rd in self.res_r.get(wr, ()):
                war.add(rd)
        raw.discard(o)
        war -= raw
        o.raw = list(raw)
        o.war = list(war)
        for r in reads:
            self.res_r.setdefault(r, []).append(o)
        for wr in writes:
            self.res_w[wr] = o
            self.res_r[wr] = []
        self.q[eng].append(o)
        return o

    def dma(self, eng, out, in_, reads=(), writes=(), **kw):
        return self.op(eng, "dma_start", reads, writes, dma=True, out=out, in_=in_, **kw)

    def end_phase(self):
        nc = self.nc
        for e in ENGS:
            for o in self.q[e]:
                for d in o.raw:
                    d.has_dep = True
                for d in o.war:
                    if d.dma or d.eng != o.eng or o.dma:
                        d.has_dep = True
            for o in reversed(self.q[e]):
                if not o.dma:
                    o.has_dep = True
                    break
        for e in ENGS:
            for o in self.q[e]:
                if o.cc:
                    self.cccnt += 1
                    o.dsem = self.ccsem
                    o.dval = self.cccnt
                elif o.dma:
                    s = self.dk[e] % NDMASEM
                    self.dk[e] += 1
                    self.dcnt[e][s] += 16
                    o.dsem = self.dsems[e][s]
                    o.dval = self.dcnt[e][s]
                elif o.has_dep:
                    self.cnt[e] += 1
                    o.sig = self.cnt[e]
        prev_barrier = self.barrier
        nb = [(self.sems[e], self.cnt[e]) for e in ENGS if self.cnt[e] > 0]
        for e in DMAQ:
            for s in range(NDMASEM):
                if self.dcnt[e][s] > 0:
                    nb.append((self.dsems[e][s], self.dcnt[e][s]))
        if self.cccnt > 0:
            nb.append((self.ccsem, self.cccnt))
        self.barrier = nb
        sems = self.sems

        def run_engine(e, eng):
            waited = self.waited[e]

            def wait(sem, val):
                if waited.get(sem, 0) >= val:
                    return
                eng.wait_ge(sem, val)
                waited[sem] = val

            if prev_barrier is not None:
                for s, v in prev_barrier:
                    wait(s, v)
            for o in self.q[e]:
                for d, is_raw in [(d, True) for d in o.raw] + [(d, False) for d in o.war]:
                    if d.dma:
                        wait(d.dsem, d.dval)
                    elif d.eng == e and not o.dma:
                        if is_raw:
                            wait(sems[e], d.sig)
                    else:
                        wait(sems[d.eng], d.sig)
                if o.cc:
                    if o.dval > 1:
                        wait(o.dsem, o.dval - 1)
                    o.fn(eng).then_inc(o.dsem)
                elif o.dma:
                    if o.dval > 16:
                        wait(o.dsem, o.dval - 16)
                    o.fn(eng).then_inc(o.dsem, 16)
                else:
                    ins = o.fn(eng)
                    if o.sig is not None:
                        ins.then_inc(sems[e], 1)
            if e == "sp":
                for s, v in nb:
                    wait(s, v)

        with nc.Block() as block:
            @block.tensor
            def _(eng):
                run_engine("pe", eng)

            @block.scalar
            def _(eng):
                run_engine("act", eng)

            @block.vector
            def _(eng):
                run_engine("dve", eng)

            @block.gpsimd
            def _(eng):
                run_engine("pool", eng)

            @block.sync
            def _(eng):
                run_engine("sp", eng)
        self.es.close()
        self._reset()
        self.nphase += 1

    def emit(self):
        self.end_phase()

    def close(self):
        self.es.close()
        self.gs.close()

D = 1024
EPS = 1e-6
SC_SWA = 64 ** -0.5
SC_RET = 64 ** -0.5
SC_DIFF = 32 ** -0.5


class Rot:
    def __init__(self, P, name, n, shape, dt=F32, psum=False):
        self.bufs = [(P.ps(shape, dt, f"{name}{i}") if psum else P.sb(shape, dt, f"{name}{i}")) for i in range(n)]
        self.name = name
        self.i = 0

    def next(self):
        k = self.i % len(self.bufs)
        self.i += 1
        return self.bufs[k], f"{self.name}{k}"


def _r(a, b):
    return list(range(a, b))


TOK_COLS = _r(0, 256) + _r(256, 384) + _r(1536, 1792) + _r(1792, 2048) + _r(512, 768) + _r(768, 1024) + _r(384, 512) + _r(1024, 1280) + _r(2048, 2304)
FEAT_COLS = _r(1280, 1536) + _r(2304, 2560) + _r(2560, 2816)


def rope_tables(n_lat):
    f32 = np.float32
    n = np.arange(n_lat)
    row = (n // 64).astype(f32)
    col = (n % 64).astype(f32)

    def axial(dim):
        nf = dim // 4
        inv = (f32(10000.0) ** (-(np.arange(nf, dtype=f32)) / f32(nf))).astype(f32)
        ang = np.concatenate([row[:, None] * inv, col[:, None] * inv], -1).astype(f32)
        return np.cos(ang).astype(f32), np.sin(ang).astype(f32)

    ca, sa = axial(64)
    cd, sd = axial(32)
    nf = 32
    inv = (f32(10000.0) ** (-(np.arange(nf, dtype=f32)) / f32(nf))).astype(f32)
    ang = (n.astype(f32)[:, None] * inv).astype(f32)
    cl, sl = np.cos(ang).astype(f32), np.sin(ang).astype(f32)
    return np.ascontiguousarray(np.concatenate([ca, sa, cd, sd, cl, sl], -1).astype(f32))


def mod_rows(P, cT, wada, bada, ncols, pmrot, name, extra=None):
    scT = P.sb([128, 8, 2], F32, name + "_scT")
    P.dma("sp", scT[:], cT, writes=[name + "_scT"])
    P.op("act", "activation", out=scT[:], in_=scT[:], func=AF.Silu, reads=[name + "_scT"], writes=[name + "_scT"])
    mrow = P.sb([2, ncols], F32, name + "_mrow")
    brot = Rot(P, name + "_brow", 2, [2, 512], F32)
    xrot_ = Rot(P, name + "_xrow", 2, [2, 512], F32)
    g2rot = Rot(P, name + "_g2row", 2, [2, 512], F32)
    wrot = Rot(P, name + "_wA", 1, [128, 8, 512], F32)
    wv = wada.rearrange("(k p) c -> p k c", p=128)
    ntot = ncols if extra is None else extra[0]
    for cb in range(ntot // 512):
        cs = slice(cb * 512, (cb + 1) * 512)
        wA, wk = wrot.next()
        P.dma("sp", wA[:], wv[:, :, cs], writes=[wk])
        brow, bk = brot.next()
        P.dma("sp", brow[0:1, :], bada[:, cs], writes=[bk])
        P.dma("sp", brow[1:2, :], bada[:, cs], writes=[bk])
        pm, pk = pmrot.next()
        for k in range(8):
            P.op("pe", "matmul", out=pm[0:2, :], lhsT=scT[:, k, :], rhs=wA[:, k, :], start=(k == 0), stop=(k == 7),
                 reads=[name + "_scT", wk], writes=[pk])
        if cb * 512 < ncols:
            P.op("dve", "tensor_tensor", out=mrow[:, cs], in0=pm[0:2, :], in1=brow[:, :], op=ALU.add, reads=[pk, bk], writes=[name + "_mrow"])
        else:
            xr, xk = xrot_.next()
            P.op("dve", "tensor_tensor", out=xr[:, :], in0=pm[0:2, :], in1=brow[:, :], op=ALU.add, reads=[pk, bk], writes=[xk])
            oc = cb * 512 - ncols
            if 2048 <= oc < 3072:
                g2, g2k = g2rot.next()
                P.dma("sp", g2[0:1, :], extra[1][:, oc - 2048:oc - 2048 + 512], writes=[g2k])
                P.dma("sp", g2[1:2, :], extra[1][:, oc - 2048:oc - 2048 + 512], writes=[g2k])
                P.op("dve", "scalar_tensor_tensor", out=xr[:, :], in0=xr[:, :], scalar=1.0, in1=g2[:, :], op0=ALU.add, op1=ALU.mult, reads=[xk, g2k], writes=[xk])
            P.dma("sp", extra[2][:, oc:oc + 512], xr[:, :], reads=[xk])
    return mrow, name + "_mrow"


def bcast_rows(P, row, rowkey, sel, v, pmrot, out, outkey):
    for hh in range(2):
        pm, pk = pmrot.next()
        P.op("pe", "matmul", out=pm[:, :], lhsT=sel[:, v, :], rhs=row[:, hh * 512:(hh + 1) * 512], start=True, stop=True,
             reads=[rowkey, "sel"], writes=[pk])
        P.op("act", "copy", out=out[:, hh * 512:(hh + 1) * 512], in_=pm[:, :], reads=[pk], writes=[outkey])


def rms_rstd(P, xt, xk, rots):
    sq, sqk = rots["sq"].next()
    st, stk = rots["st"].next()
    P.op("act", "activation", out=sq[:], in_=xt, func=AF.Square, accum_out=st[:, 0:1], reads=[xk], writes=[sqk, stk])
    P.op("dve", "tensor_scalar", out=st[:, 1:2], in0=st[:, 0:1], scalar1=1.0 / D, scalar2=EPS, op0=ALU.mult, op1=ALU.add, reads=[stk], writes=[stk])
    P.op("act", "activation", out=st[:, 2:3], in_=st[:, 1:2], func=AF.Sqrt, reads=[stk], writes=[stk])
    P.op("dve", "reciprocal", out=st[:, 3:4], in_=st[:, 2:3], reads=[stk], writes=[stk])
    return st, stk, sq, sqk


def norm_mod_transpose(P, xt, xk, Gb, Sb, gkeys, ident, hT, hTk, col0, rots):
    st, stk, sq, sqk = rms_rstd(P, xt, xk, rots)
    P.op("dve", "scalar_tensor_tensor", out=sq[:], in0=xt, scalar=st[:, 3:4], in1=Gb[:], op0=ALU.mult, op1=ALU.mult,
         reads=[xk, stk, gkeys[0]], writes=[sqk])
    hb, hbk = rots["hb"].next()
    P.op("pool", "tensor_tensor", out=hb[:], in0=sq[:], in1=Sb[:], op=ALU.add, reads=[sqk, gkeys[1]], writes=[hbk])
    ptr, ptk = rots["ptr"].next()
    for k in range(8):
        P.op("pe", "transpose", out=ptr[:, k * 128:(k + 1) * 128], in_=hb[:, k * 128:(k + 1) * 128], identity=ident[:],
             reads=[hbk, "ident"], writes=[ptk])
    P.op("act", "copy", out=hT[:, :, col0:col0 + 128], in_=ptr[:, :].rearrange("p (k t) -> p k t", k=8), reads=[ptk], writes=[hTk])


def head_cols(j):
    g = j // 2
    r = lambda a, n=64: list(range(a, a + n))
    tok = (r(1536 + j * 64) + r(0 + j * 64) + r(1792 + j * 64) + r(256 + g * 64) + r(512 + j * 64) + r(768 + j * 64)
           + r(384 + g * 64) + r(1024 + j * 64) + r(2048 + j * 64))
    feat = r(1280 + j * 64) + r(2304 + j * 64) + r(2560 + j * 64)
    return tok, feat


def emit_p(P, N, M, x_lat, x_ctx, cT, wada, bada, gn, gn2, wt_d, wf_d, tabs, ident_d, sel_d,
           s_qA, s_kB, s_rq, s_rk, s_tok, s_f32, s_modrows):
    nlt, nct = N // 128, M // 128
    ident = P.sb([128, 128], BF16, "ident")
    P.dma("sp", ident[:], ident_d, writes=["ident"])
    sel = P.sb([2, 2, 128], F32, "sel")
    P.dma("sp", sel[:], sel_d, writes=["sel"])
    Wt = P.sb([128, 8, 576], BF16, "Wt")
    Wf = P.sb([128, 8, 192], BF16, "Wf")
    P.dma("pool", Wt[:], wt_d.rearrange("(k p) c -> p k c", p=128), writes=["Wt"])
    P.dma("pool", Wf[:], wf_d.rearrange("(k p) c -> p k c", p=128), writes=["Wf"])
    pmrot = Rot(P, "pm", 3, [128, 512], F32, psum=True)
    mrow, mk = mod_rows(P, cT, wada, bada, 2048, pmrot, "mod", extra=(6144, gn2, s_modrows))
    grow = P.sb([2, D], F32, "grow")
    P.dma("sp", grow[0:1, :], gn, writes=["grow"])
    P.dma("sp", grow[1:2, :], gn, writes=["grow"])
    P.op("dve", "scalar_tensor_tensor", out=grow[:], in0=mrow[:, 1024:2048], scalar=1.0, in1=grow[:], op0=ALU.add, op1=ALU.mult,
         reads=[mk, "grow"], writes=["grow"])
    Gb = [P.sb([128, D], F32, f"Gb{v}") for v in range(2)]
    Sb = [P.sb([128, D], F32, f"Sb{v}") for v in range(2)]
    for v in range(2):
        bcast_rows(P, grow, "grow", sel, v, pmrot, Gb[v], f"Gb{v}")
        bcast_rows(P, mrow[:, 0:1024], mk, sel, v, pmrot, Sb[v], f"Sb{v}")
    rots = {
        "sq": Rot(P, "sq", 4, [128, D], F32),
        "st": Rot(P, "st", 8, [128, 4], F32),
        "hb": Rot(P, "hb", 4, [128, D], BF16),
        "ptr": Rot(P, "ptr", 2, [128, 1024], BF16, psum=True),
    }
    xrot = Rot(P, "xt", 5, [128, D], F32)
    hTrot = Rot(P, "hT", 3, [128, 8, 512], BF16)
    ptokrot = Rot(P, "ptok", 4, [128, 576], F32)
    tabrot = Rot(P, "tab", 4, [128, 160], F32)
    tmprot = Rot(P, "rtmp", 6, [128, 4, 128], F32)
    qkrot = Rot(P, "qk16", 4, [128, 384], BF16)
    vstrot = Rot(P, "vst", 4, [128, 256], BF16)
    ptArot = Rot(P, "ptA", 2, [128, 4, 128], BF16, psum=True)
    f16rot = Rot(P, "f16T", 2, [128, 4, 512], BF16)
    f32rot = Rot(P, "f32T", 2, [128, 2, 512], F32)
    pieces = [(0, 64, SC_DIFF), (64, 128, SC_SWA), (128, 320, 1.0), (320, 384, SC_RET), (384, 512, 1.0)]
    ei = 0
    groups = [(list(range(t, min(t + 4, nct))), 1) for t in range(0, nct, 4)] + [(list(range(t, min(t + 4, nlt))), 0) for t in range(0, nlt, 4)]
    def do_norm(gi, ti):
        tiles, v = groups[gi]
        src = x_ctx if v == 1 else x_lat
        tile = tiles[ti]
        xt, xk = xrot.next()
        P.dma("sp", xt[:], (src(tile) if callable(src) else src[tile * 128:(tile + 1) * 128, :]), reads=["g_x", "s_xc1"], writes=[xk])
        st, stk, sq, sqk = rms_rstd(P, xt[:], xk, rots)
        P.op("dve", "scalar_tensor_tensor", out=sq[:], in0=xt[:], scalar=st[:, 3:4], in1=Gb[v][:], op0=ALU.mult, op1=ALU.mult,
             reads=[xk, stk, f"Gb{v}"], writes=[sqk])
        hb, hbk = rots["hb"].next()
        P.op("pool", "tensor_tensor", out=hb[:], in0=sq[:], in1=Sb[v][:], op=ALU.add, reads=[sqk, f"Sb{v}"], writes=[hbk])
        pend[(gi, ti)] = (hb, hbk)

    pend = {}

    def do_norm_b(gi, ti):
        hb, hbk = pend.pop((gi, ti))
        hT, hTk = hts[gi]
        ptr, ptk = rots["ptr"].next()
        for k in range(8):
            P.op("pe", "transpose", out=ptr[:, k * 128:(k + 1) * 128], in_=hb[:, k * 128:(k + 1) * 128], identity=ident[:],
                 reads=[hbk, "ident"], writes=[ptk])
        P.op("act", "copy", out=hT[:, :, ti * 128:(ti + 1) * 128], in_=ptr[:, :].rearrange("p (k t) -> p k t", k=8), reads=[ptk], writes=[hTk])

    hts = {}
    st1 = {}

    def stage1(gi, ti):
        tiles, v = groups[gi]
        tile = tiles[ti]
        T0 = (tiles[0] * 128) if v == 1 else (M + tiles[0] * 128)
        hT, hTk = hts[gi]
        ptok, ptokk = ptokrot.next()
        pm, pk = pmrot.next()
        for k in range(8):
            P.op("pe", "matmul", out=pm[:, :], lhsT=hT[:, k, ti * 128:(ti + 1) * 128], rhs=Wt[:, k, 0:512], start=(k == 0), stop=(k == 7),
                 reads=[hTk, "Wt"], writes=[pk])
        for (c0, c1, sc) in pieces:
            P.op("act", "activation", out=ptok[:, c0:c1], in_=pm[:, c0:c1], func=AF.Copy, scale=float(sc), reads=[pk], writes=[ptokk])
        pm2, pk2 = pmrot.next()
        for k in range(8):
            P.op("pe", "matmul", out=pm2[:, 0:64], lhsT=hT[:, k, ti * 128:(ti + 1) * 128], rhs=Wt[:, k, 512:576], start=(k == 0), stop=(k == 7),
                 reads=[hTk, "Wt"], writes=[pk2])
        P.op("act", "copy", out=ptok[:, 512:576], in_=pm2[:, 0:64], reads=[pk2], writes=[ptokk])
        qk, qkk = qkrot.next()
        if v == 0:
            tab, tabk = tabrot.next()
            P.dma("sp", tab[:], tabs[tile * 128:(tile + 1) * 128, :], writes=[tabk])

            def views(t_, kind):
                if kind == "diff":
                    vv = t_[:, 0:256].rearrange("p (a c) -> p a c", a=2)[:, :, 0:64].rearrange("p a (h two d) -> p a h two d", h=2, two=2)
                    return vv[:, :, :, 0, :], vv[:, :, :, 1, :], [128, 2, 2, 16]
                if kind == "swa":
                    vv = t_[:, 0:256].rearrange("p (a c) -> p a c", a=2)[:, :, 64:128].rearrange("p a (two d) -> p a two d", two=2)
                    return vv[:, :, 0, :], vv[:, :, 1, :], [128, 2, 32]
                vv = t_[:, 256:384].rearrange("p (h two d) -> p h two d", h=2, two=2)
                return vv[:, :, 0, :], vv[:, :, 1, :], [128, 2, 32]
            for ki, (kind, hd, tc0) in enumerate((("diff", 16, 64), ("swa", 32, 0), ("ret", 32, 96))):
                x1, x2, shp = views(ptok, kind)
                o1, o2, _ = views(qk, kind)
                cosb = tab[:, tc0:tc0 + hd]
                sinb = tab[:, tc0 + hd:tc0 + 2 * hd]
                for _ in range(len(shp) - 2):
                    cosb = cosb.unsqueeze(1)
                    sinb = sinb.unsqueeze(1)
                cosb = cosb.to_broadcast(shp)
                sinb = sinb.to_broadcast(shp)
                tmp, tmpk = tmprot.next()
                nel = int(np.prod(shp[1:]))
                if len(shp) == 4:
                    T_ = [tmp[:, i, 0:nel].rearrange("p (a h d) -> p a h d", a=shp[1], h=shp[2]) for i in range(4)]
                else:
                    T_ = [tmp[:, i, 0:nel].rearrange("p (a d) -> p a d", a=shp[1]) for i in range(4)]
                e1 = "dve" if (ti + ki) % 2 == 0 else "pool"
                e2 = "pool" if (ti + ki) % 2 == 0 else "dve"
                P.op(e1, "tensor_tensor", out=T_[0], in0=x1, in1=cosb, op=ALU.mult, reads=[ptokk, tabk], writes=[tmpk + "a"])
                P.op(e1, "tensor_tensor", out=T_[1], in0=x2, in1=sinb, op=ALU.mult, reads=[ptokk, tabk], writes=[tmpk + "b"])
                P.op(e2, "tensor_tensor", out=T_[2], in0=x2, in1=cosb, op=ALU.mult, reads=[ptokk, tabk], writes=[tmpk + "c"])
                P.op(e2, "tensor_tensor", out=T_[3], in0=x1, in1=sinb, op=ALU.mult, reads=[ptokk, tabk], writes=[tmpk + "d"])
                P.op(e1, "tensor_tensor", out=o1, in0=T_[0], in1=T_[1], op=ALU.subtract, reads=[tmpk + "a", tmpk + "b"], writes=[qkk])
                P.op(e2, "tensor_tensor", out=o2, in0=T_[2], in1=T_[3], op=ALU.add, reads=[tmpk + "c", tmpk + "d"], writes=[qkk])
        else:
            P.op("dve", "tensor_copy", out=qk[:, :], in_=ptok[:, 0:384], reads=[ptokk], writes=[qkk])
        vst, vstk = vstrot.next()
        P.op("pool", "tensor_copy", out=vst[:, 64:256], in_=ptok[:, 384:576], reads=[ptokk], writes=[vstk])
        P.op("pool", "tensor_copy", out=vst[:, 0:64], in_=qk[:, 320:384], reads=[qkk], writes=[vstk])
        P.dma("sp", s_tok[T0 + ti * 128:T0 + (ti + 1) * 128, :], vst[:], reads=[vstk], writes=["s_tok"])
        st1[(gi, ti)] = (qk, qkk)

    def stage2(gi, ti, f16T, f16k):
        qk, qkk = st1.pop((gi, ti))
        ptA, ptAk = ptArot.next()
        P.op("pe", "transpose", out=ptA[:, 0, :], in_=qk[:, 0:128], identity=ident[:], reads=[qkk, "ident"], writes=[ptAk])
        P.op("pe", "transpose", out=ptA[:, 1, :], in_=qk[:, 128:256], identity=ident[:], reads=[qkk, "ident"], writes=[ptAk])
        P.op("pe", "transpose", out=ptA[0:64, 2, :], in_=qk[:, 256:320], identity=ident[:], reads=[qkk, "ident"], writes=[ptAk])
        P.op("pe", "transpose", out=ptA[0:64, 3, :], in_=qk[:, 320:384], identity=ident[:], reads=[qkk, "ident"], writes=[ptAk])
        P.op("dve", "tensor_copy", out=f16T[:, 0:2, ti * 128:(ti + 1) * 128], in_=ptA[:, 0:2, :], reads=[ptAk], writes=[f16k])
        P.op("dve", "tensor_copy", out=f16T[0:64, 2:4, ti * 128:(ti + 1) * 128], in_=ptA[0:64, 2:4, :], reads=[ptAk], writes=[f16k])

    hts[0] = hTrot.next()
    for ti in range(len(groups[0][0])):
        do_norm(0, ti)
    for ti in range(len(groups[0][0])):
        do_norm_b(0, ti)
    for gi, (tiles, v) in enumerate(groups):
        ntok = len(tiles) * 128
        T0 = (tiles[0] * 128) if v == 1 else (M + tiles[0] * 128)
        hT, hTk = hts[gi]
        nxt = gi + 1 if gi + 1 < len(groups) else None
        if nxt is not None:
            hts[nxt] = hTrot.next()
        f16T, f16k = f16rot.next()
        for ti in range(len(tiles)):
            stage1(gi, ti)
            if nxt is not None and ti < len(groups[nxt][0]):
                do_norm(nxt, ti)
            if ti > 0:
                stage2(gi, ti - 1, f16T, f16k)
        if nxt is not None:
            for ti in range(len(tiles), len(groups[nxt][0])):
                do_norm(nxt, ti)
        stage2(gi, len(tiles) - 1, f16T, f16k)
        if nxt is not None:
            for ti in range(len(groups[nxt][0])):
                do_norm_b(nxt, ti)
        P.dma("sp", s_qA[:, T0:T0 + ntok], f16T[:, 0, 0:ntok], reads=[f16k], writes=["s_qA"])
        P.dma("sp", s_kB[:, T0:T0 + ntok], f16T[:, 1, 0:ntok], reads=[f16k], writes=["s_kB"])
        P.dma("sp", s_rq[:, T0:T0 + ntok], f16T[0:64, 2, 0:ntok], reads=[f16k], writes=["s_rq"])
        P.dma("sp", s_rk[:, T0:T0 + ntok], f16T[0:64, 3, 0:ntok], reads=[f16k], writes=["s_rk"])
        f32T, f32k = f32rot.next()
        for fb, (c0, c1) in enumerate(((0, 128), (128, 192))):
            w = c1 - c0
            pm, pk = pmrot.next()
            for k in range(8):
                P.op("pe", "matmul", out=pm[0:w, 0:ntok], lhsT=Wf[:, k, c0:c1], rhs=hT[:, k, 0:ntok], start=(k == 0), stop=(k == 7), reads=[hTk, "Wf"], writes=[pk])
            P.op("act", "copy", out=f32T[0:w, fb, 0:ntok], in_=pm[0:w, 0:ntok], reads=[pk], writes=[f32k])
        P.dma("sp", s_f32[0:128, T0:T0 + ntok], f32T[:, 0, 0:ntok], reads=[f32k], writes=["s_f32"])
        P.dma("sp", s_f32[128:192, T0:T0 + ntok], f32T[0:64, 1, 0:ntok], reads=[f32k], writes=["s_f32"])
        del hts[gi]
    P.end_phase()


def lam_scalar_col(P, lamv_d, lam_init, pmrot):
    lv = P.sb([1, 128], F32, "lv")
    P.dma("sp", lv[:], lamv_d, writes=["lv"])
    pr = P.sb([1, 64], F32, "lvpr")
    lvv = lv[:, :].rearrange("o (a b d) -> o a b d", a=2, b=2)
    P.op("dve", "tensor_tensor", out=pr[:, :].rearrange("o (a d) -> o a d", a=2), in0=lvv[:, :, 0, :], in1=lvv[:, :, 1, :], op=ALU.mult, reads=["lv"], writes=["lvpr"])
    sm = P.sb([1, 4], F32, "lvsm")
    P.op("dve", "tensor_reduce", out=sm[:, 0:2], in_=pr[:, :].rearrange("o (a d) -> o a d", a=2), axis=AX.X, op=ALU.add, reads=["lvpr"], writes=["lvsm"])
    P.op("act", "activation", out=sm[:, 0:2], in_=sm[:, 0:2], func=AF.Exp, reads=["lvsm"], writes=["lvsm"])
    P.op("dve", "tensor_tensor", out=sm[:, 2:3], in0=sm[:, 0:1], in1=sm[:, 1:2], op=ALU.subtract, reads=["lvsm"], writes=["lvsm"])
    P.op("dve", "tensor_scalar", out=sm[:, 3:4], in0=sm[:, 2:3], scalar1=float(lam_init), scalar2=-1.0, op0=ALU.add, op1=ALU.mult, reads=["lvsm"], writes=["lvsm"])
    P.op("dve", "tensor_scalar", out=sm[:, 2:3], in0=sm[:, 2:3], scalar1=float(lam_init), scalar2=None, op0=ALU.add, reads=["lvsm"], writes=["lvsm"])
    ones1 = P.sb([1, 128], F32, "ones1")
    P.op("pool", "memset", ap=ones1[:], constant=1.0, writes=["ones1"])
    pm, pk = pmrot.next()
    P.op("pe", "matmul", out=pm[:, 0:2], lhsT=ones1[:, :], rhs=sm[:, 2:4], start=True, stop=True, reads=["ones1", "lvsm"], writes=[pk])
    lamc = P.sb([128, 2], F32, "lamc")
    P.op("dve", "tensor_copy", out=lamc[:], in_=pm[:, 0:2], reads=[pk], writes=["lamc"])
    return lamc


def headnorm_T(P, o32, ok, ncol, ones64, eprot, gcol, gkey, mul, out_ap, outk, tmprot, eng_a="dve", eng_b="pool"):
    sq, sqk = tmprot.next()
    P.op(eng_b, "tensor_tensor", out=sq[0:64, 0:ncol], in0=o32, in1=o32, op=ALU.mult, reads=[ok], writes=[sqk])
    pm, pk = eprot.next()
    P.op("pe", "matmul", out=pm[0:64, 0:ncol], lhsT=ones64[0:64, 0:64], rhs=sq[0:64, 0:ncol], start=True, stop=True, reads=[sqk, "ones64"], writes=[pk])
    P.op(eng_a, "tensor_scalar", out=sq[0:64, 0:ncol], in0=pm[0:64, 0:ncol], scalar1=1.0 / 64, scalar2=EPS, op0=ALU.mult, op1=ALU.add, reads=[pk], writes=[sqk])
    P.op("act", "activation", out=sq[0:64, 0:ncol], in_=sq[0:64, 0:ncol], func=AF.Sqrt, reads=[sqk], writes=[sqk])
    P.op(eng_a, "reciprocal", out=sq[0:64, 0:ncol], in_=sq[0:64, 0:ncol], reads=[sqk], writes=[sqk])
    P.op(eng_a, "tensor_tensor", out=sq[0:64, 0:ncol], in0=sq[0:64, 0:ncol], in1=o32, op=ALU.mult, reads=[sqk, ok], writes=[sqk])
    P.op(eng_a, "tensor_scalar", out=out_ap, in0=sq[0:64, 0:ncol], scalar1=gcol, scalar2=float(mul), op0=ALU.mult, op1=ALU.mult, reads=[sqk, gkey], writes=[outk])


def emit_diff(P, N, M, need_ctx, lam_init, qT_d, kT_d, v_d, lamv_d, gn_d, oT_d):
    T = N + M
    NKB = T // 128

    qT = P.sb([64, T], BF16, "qT")
    kT = P.sb([64, T], BF16, "kT")
    V1 = P.sb([128, NKB, 128], BF16, "V1")
    nchunk = 4
    cw = T // nchunk
    for i in range(nchunk):
        P.dma("sp", qT[:, i * cw:(i + 1) * cw], qT_d[:, i * cw:(i + 1) * cw], writes=["qT"])
        P.dma("sp", kT[:, i * cw:(i + 1) * cw], kT_d[:, i * cw:(i + 1) * cw], writes=["kT"])
    vv = v_d.rearrange("(kb p) d -> p kb d", p=128)
    step = max(1, NKB // 8)
    for k0 in range(0, NKB, step):
        k1 = min(NKB, k0 + step)
        P.dma("sp", V1[:, k0:k1, 0:64], vv[:, k0:k1, :], writes=["V1"])
    P.op("pool", "memset", ap=V1[:, :, 64:65], constant=1.0, writes=["V1"])
    P.op("pool", "memset", ap=V1[:, :, 65:128], constant=0.0, writes=["V1"])
    gcol = P.sb([64, 1], F32, "gcol")
    P.dma("sp", gcol[:], gn_d, writes=["gcol"])
    ones64 = P.sb([128, 64], F32, "ones64")
    P.op("pool", "memset", ap=ones64[:], constant=1.0, writes=["ones64"])
    srot = Rot(P, "S", 3, [128, 512], F32, psum=True)
    eprot = Rot(P, "ep", 1, [128, 512], F32, psum=True)
    accrot = [Rot(P, f"acc{m}_", 2, [128, 512], F32, psum=True) for m in range(2)]
    lamc = lam_scalar_col(P, lamv_d, lam_init, eprot)
    ptrot = Rot(P, "PT", 6, [128, 512], BF16)
    rlrot = Rot(P, "rl", 2, [128, 2, 512], F32)
    bcrot = Rot(P, "bc", 2, [64, 2, 512], F32)
    t32rot = Rot(P, "t32", 3, [64, 512], F32)
    orot = Rot(P, "ob", 2, [64, 512], BF16)

    def epilogue(acc, q0, nq):
        rl, rlk = rlrot.next()
        for m in range(2):
            P.op("dve", "reciprocal", out=rl[64:65, m, 0:nq], in_=acc[m][0][64:65, 0:nq], reads=[acc[m][1]], writes=[rlk])
        P.op("dve", "tensor_scalar", out=rl[64:65, 1, 0:nq], in0=rl[64:65, 1, 0:nq], scalar1=lamc[64:65, 1:2], scalar2=None, op0=ALU.mult, reads=[rlk, "lamc"], writes=[rlk])
        bc, bck = bcrot.next()
        for m in range(2):
            pm, pk = eprot.next()
            P.op("pe", "matmul", out=pm[0:64, 0:nq], lhsT=ones64[64:65, 0:64], rhs=rl[64:65, m, 0:nq], start=True, stop=True, reads=[rlk, "ones64"], writes=[pk])
            P.op("dve", "tensor_copy", out=bc[:, m, 0:nq], in_=pm[0:64, 0:nq], reads=[pk], writes=[bck])
        t0, t0k = t32rot.next()
        t1, t1k = t32rot.next()
        P.op("dve", "tensor_tensor", out=t0[:, 0:nq], in0=acc[0][0][0:64, 0:nq], in1=bc[:, 0, 0:nq], op=ALU.mult, reads=[acc[0][1], bck], writes=[t0k])
        P.op("dve", "tensor_tensor", out=t1[:, 0:nq], in0=acc[1][0][0:64, 0:nq], in1=bc[:, 1, 0:nq], op=ALU.mult, reads=[acc[1][1], bck], writes=[t1k])
        P.op("pool", "tensor_tensor", out=t0[:, 0:nq], in0=t0[:, 0:nq], in1=t1[:, 0:nq], op=ALU.add, reads=[t0k, t1k], writes=[t0k])
        ob, obk = orot.next()
        headnorm_T(P, t0[:, 0:nq], t0k, nq, ones64, eprot, gcol[:, 0:1], "gcol", 1.0 - lam_init, ob[:, 0:nq], obk, t32rot)
        P.dma("sp", oT_d[:, q0:q0 + nq], ob[:, 0:nq], reads=[obk])
    QG = 2
    sgroups = []
    if need_ctx:
        sgroups.append(([(0, M)], 0, M // 128))
    lat = [(g0, min(512, T - g0)) for g0 in range(M, T, 512)]
    for i in range(0, len(lat), QG):
        sgroups.append((lat[i:i + QG], 0, NKB))
    LOOK = 1
    for (qgs, kb0, kb1) in sgroups:
        ng = len(qgs)
        acc = [[accrot[m].next() for m in range(2)] for _ in range(ng)]
        steps = [(kb, m) for kb in range(kb0, kb1) for m in range(2)]
        pts = {}

        def issue_s(i):
            kb, m = steps[i]
            for gi, (q0, nq) in enumerate(qgs):
                S, Sk = srot.next()
                P.op("pe", "matmul", out=S[:, 0:nq], lhsT=kT[m * 32:(m + 1) * 32, kb * 128:(kb + 1) * 128], rhs=qT[m * 32:(m + 1) * 32, q0:q0 + nq],
                     start=True, stop=True, reads=["kT", "qT"], writes=[Sk])
                PT, PTk = ptrot.next()
                P.op("act", "activation", out=PT[:, 0:nq], in_=S[:, 0:nq], func=AF.Exp, reads=[Sk], writes=[PTk])
                pts[(i, gi)] = (PT, PTk)

        def issue_pv(i):
            kb, m = steps[i]
            for gi, (q0, nq) in enumerate(qgs):
                PT, PTk = pts.pop((i, gi))
                P.op("pe", "matmul", out=acc[gi][m][0][:, 0:nq], lhsT=V1[:, kb, :], rhs=PT[:, 0:nq], start=(kb == kb0), stop=(kb == kb1 - 1),
                     reads=["V1", PTk], writes=[acc[gi][m][1]])
        for i in range(len(steps) + LOOK):
            if i < len(steps):
                issue_s(i)
            if i - LOOK >= 0:
                issue_pv(i - LOOK)
        for gi, (q0, nq) in enumerate(qgs):
            epilogue(acc[gi], q0, nq)
    P.end_phase()


def swa_masks():
    b = np.arange(128)[:, None]
    a = np.arange(128)[None, :]
    m = np.concatenate([(b <= a), np.ones((128, 128), bool), (a <= b)], 1)
    return m.astype(np.float32).astype(ml_dtypes.bfloat16)


def emit_swa(P, N, M, need_ctx, qT_d, kT_d, v_d, sink_d, m3_d, oT_d):
    T = N + M
    NKB = T // 128
    MB = M // 128
    nb = N // 128

    qT = P.sb([64, T], BF16, "qT")
    kT = P.sb([64, T], BF16, "kT")
    V1 = P.sb([128, NKB, 65], BF16, "V1")
    nchunk = 4
    cw = T // nchunk
    for i in range(nchunk):
        P.dma("sp", qT[:, i * cw:(i + 1) * cw], qT_d[:, i * cw:(i + 1) * cw], writes=["qT"])
        P.dma("sp", kT[:, i * cw:(i + 1) * cw], kT_d[:, i * cw:(i + 1) * cw], writes=["kT"])
    vv = v_d.rearrange("(kb p) d -> p kb d", p=128)
    step = max(1, NKB // 8)
    for k0 in range(0, NKB, step):
        k1 = min(NKB, k0 + step)
        P.dma("sp", V1[:, k0:k1, 0:64], vv[:, k0:k1, :], writes=["V1"])
    P.op("pool", "memset", ap=V1[:, :, 64:65], constant=1.0, writes=["V1"])
    m3 = P.sb([128, 384], BF16, "m3")
    P.dma("sp", m3[:], m3_d, writes=["m3"])
    es = P.sb([128, 1], F32, "es")
    P.dma("sp", es[:], sink_d, writes=["es"])
    P.op("act", "activation", out=es[:], in_=es[:], func=AF.Exp, reads=["es"], writes=["es"])
    ones64 = P.sb([128, 64], F32, "ones64")
    P.op("pool", "memset", ap=ones64[:], constant=1.0, writes=["ones64"])
    accrot = Rot(P, "acc", 2, [128, 512], F32, psum=True)
    srot = Rot(P, "S", 3, [128, 512], F32, psum=True)
    eprot = Rot(P, "ep", 2, [128, 512], F32, psum=True)
    ptrot = Rot(P, "PT", 4, [128, 512], BF16)
    rlrot = Rot(P, "rl", 2, [128, 512], F32)
    bcrot = Rot(P, "bc", 2, [64, 512], F32)
    orot = Rot(P, "ob", 2, [64, 512], BF16)

    qgroups = []
    if need_ctx:
        qgroups.append((0, M, None))
    for s0 in range(0, nb, 4):
        qgroups.append((M + s0 * 128, min(4, nb - s0) * 128, s0))
    ei = 0
    for (q0, nq, s0) in qgroups:
        acc, acck = accrot.next()
        items = [(kb, 0, nq, None) for kb in range(MB)]
        if s0 is not None:
            ns = nq // 128
            for r in range(s0 - 1, s0 + ns + 1):
                if r < 0 or r >= nb:
                    continue
                sa = max(r - 1, s0)
                sb_ = min(r + 1, s0 + ns - 1)
                items.append((MB + r, (sa - s0) * 128, (sb_ - s0 + 1) * 128, (sa - (r - 1)) * 128))
        for ii, (kb, c0, c1, mc) in enumerate(items):
            w = c1 - c0
            S, Sk = srot.next()
            P.op("pe", "matmul", out=S[:, 0:w], lhsT=kT[:, kb * 128:(kb + 1) * 128], rhs=qT[:, q0 + c0:q0 + c1], start=True, stop=True,
                 reads=["kT", "qT"], writes=[Sk])
            PT, PTk = ptrot.next()
            P.op("act", "activation", out=PT[:, 0:w], in_=S[:, 0:w], func=AF.Exp, reads=[Sk], writes=[PTk])
            if mc is not None:
                eng = "dve" if ei % 2 == 0 else "pool"
                ei += 1
                P.op(eng, "tensor_tensor", out=PT[:, 0:w], in0=PT[:, 0:w], in1=m3[:, mc:mc + w], op=ALU.mult, reads=[PTk, "m3"], writes=[PTk])
            P.op("pe", "matmul", out=acc[0:65, c0:c1], lhsT=V1[:, kb, :], rhs=PT[:, 0:w], start=(ii == 0), stop=(ii == len(items) - 1),
                 reads=["V1", PTk], writes=[acck])
        rl, rlk = rlrot.next()
        P.op("dve", "tensor_scalar", out=rl[64:65, 0:nq], in0=acc[64:65, 0:nq], scalar1=es[64:65, 0:1], scalar2=None, op0=ALU.add, reads=[acck, "es"], writes=[rlk])
        P.op("dve", "reciprocal", out=rl[64:65, 0:nq], in_=rl[64:65, 0:nq], reads=[rlk], writes=[rlk])
        pm, pk = eprot.next()
        P.op("pe", "matmul", out=pm[0:64, 0:nq], lhsT=ones64[64:65, 0:64], rhs=rl[64:65, 0:nq], start=True, stop=True, reads=[rlk, "ones64"], writes=[pk])
        bc, bck = bcrot.next()
        P.op("act", "copy", out=bc[:, 0:nq], in_=pm[0:64, 0:nq], reads=[pk], writes=[bck])
        ob, obk = orot.next()
        P.op("dve", "tensor_tensor", out=ob[:, 0:nq], in0=acc[0:64, 0:nq], in1=bc[:, 0:nq], op=ALU.mult, reads=[acck, bck], writes=[obk])
        P.dma("sp", oT_d[:, q0:q0 + nq], ob[:, 0:nq], reads=[obk])
    P.end_phase()


def ret_consts():
    m = np.arange(128)[:, None].astype(np.float32)
    n = np.arange(128)[None, :].astype(np.float32)
    c = np.zeros((128, 6, 128), np.float32)
    c[:, 0] = np.maximum(n - m, 0)
    c[:, 1] = (n >= m)
    c[:, 2] = np.maximum(m - n, 0)
    c[:, 3] = (m >= n)
    c[:, 4] = n + 1
    c[:, 5] = 128 - n
    z = np.zeros((128, 2), np.float32)
    z[:, 0] = 127 - np.arange(128)
    z[:, 1] = np.arange(128)
    return c, z


def emit_ret(P, N, M, need_ctx, qT_d, kT_d, kt_d, v_d, gT_d, lg_d, gn_d, rc_d, rz_d, oT_d):
    T = N + M
    NC = T // 128
    MB = M // 128

    ktok = P.sb([128, NC, 64], BF16, "ktok")
    V = P.sb([128, NC, 64], BF16, "V")
    vv = v_d.rearrange("(kb p) d -> p kb d", p=128)
    kv = kt_d.rearrange("(kb p) d -> p kb d", p=128)
    step = max(1, NC // 8)
    for k0 in range(0, NC, step):
        k1 = min(NC, k0 + step)
        P.dma("sp", V[:, k0:k1, :], vv[:, k0:k1, :], writes=["V"])
        P.dma("sp", ktok[:, k0:k1, :], kv[:, k0:k1, :], writes=["ktok"])
    rc = P.sb([128, 6, 128], F32, "rc")
    P.dma("sp", rc[:], rc_d, writes=["rc"])
    rz = P.sb([128, 2], F32, "rz")
    P.dma("sp", rz[:], rz_d, writes=["rz"])
    gcol = P.sb([64, 1], F32, "gcol")
    P.dma("sp", gcol[:], gn_d, writes=["gcol"])
    lgc = P.sb([128, 2], F32, "lgc")
    P.dma("sp", lgc[:], lg_d, writes=["lgc"])
    P.op("act", "activation", out=lgc[:], in_=lgc[:], func=AF.Exp, scale=-1.0, reads=["lgc"], writes=["lgc"])
    P.op("dve", "tensor_scalar", out=lgc[:], in0=lgc[:], scalar1=1.0, scalar2=None, op0=ALU.add, reads=["lgc"], writes=["lgc"])
    P.op("act", "activation", out=lgc[:], in_=lgc[:], func=AF.Ln, reads=["lgc"], writes=["lgc"])
    P.op("dve", "tensor_scalar", out=lgc[:], in0=lgc[:], scalar1=-1.0, scalar2=None, op0=ALU.mult, reads=["lgc"], writes=["lgc"])
    DcT = P.sb([128, 128], F32, "DcT")
    E2 = P.sb([128, 128], F32, "E2")
    P.op("act", "activation", out=DcT[:], in_=rc[:, 0, :], func=AF.Exp, scale=lgc[:, 0:1], reads=["rc", "lgc"], writes=["DcT"])
    P.op("act", "activation", out=E2[:], in_=rc[:, 2, :], func=AF.Exp, scale=lgc[:, 1:2], reads=["rc", "lgc"], writes=["E2"])
    P.op("dve", "tensor_tensor", out=DcT[:], in0=DcT[:], in1=rc[:, 1, :], op=ALU.mult, reads=["DcT", "rc"], writes=["DcT"])
    P.op("dve", "tensor_tensor", out=E2[:], in0=E2[:], in1=rc[:, 3, :], op=ALU.mult, reads=["E2", "rc"], writes=["E2"])
    P.op("dve", "tensor_tensor", out=DcT[:], in0=DcT[:], in1=E2[:], op=ALU.add, reads=["DcT", "E2"], writes=["DcT"])
    xi = P.sb([64, 2, 128], F32, "xi")
    P.op("act", "activation", out=xi[:, 0, :], in_=rc[0:64, 4, :], func=AF.Exp, scale=lgc[0:64, 0:1], reads=["rc", "lgc"], writes=["xi"])
    P.op("act", "activation", out=xi[:, 1, :], in_=rc[0:64, 5, :], func=AF.Exp, scale=lgc[0:64, 1:2], reads=["rc", "lgc"], writes=["xi"])
    zc = P.sb([128, 2], F32, "zc")
    P.op("act", "activation", out=zc[:, 0:1], in_=rz[:, 0:1], func=AF.Exp, scale=lgc[:, 0:1], reads=["rz", "lgc"], writes=["zc"])
    P.op("act", "activation", out=zc[:, 1:2], in_=rz[:, 1:2], func=AF.Exp, scale=lgc[:, 1:2], reads=["rz", "lgc"], writes=["zc"])
    gc = P.sb([64, 2], F32, "gc")
    P.op("act", "activation", out=gc[:], in_=lgc[0:64, :], func=AF.Exp, scale=128.0, reads=["lgc"], writes=["gc"])
    ones64 = P.sb([128, 64], F32, "ones64")
    P.op("pool", "memset", ap=ones64[:], constant=1.0, writes=["ones64"])

    U = [P.sb([64, NC, 64], F32, f"U{d}") for d in range(2)]
    S16 = [P.sb([64, NC, 64], BF16, f"S16_{d}") for d in range(2)]
    urot = Rot(P, "ups", 2, [128, 512], F32, psum=True)
    kzrot = Rot(P, "kz", 4, [128, 64], BF16)
    for d in range(2):
        for c0 in range(0, NC, 8):
            c1 = min(NC, c0 + 8)
            pm, pk = urot.next()
            for c in range(c0, c1):
                kz, kzk = kzrot.next()
                P.op("dve" if d == 0 else "pool", "tensor_scalar", out=kz[:], in0=ktok[:, c, :], scalar1=zc[:, d:d + 1], scalar2=None, op0=ALU.mult,
                     reads=["ktok", "zc"], writes=[kzk])
                P.op("pe", "matmul", out=pm[0:64, (c - c0) * 64:(c - c0 + 1) * 64], lhsT=kz[:], rhs=V[:, c, :], start=True, stop=True,
                     reads=[kzk, "V"], writes=[pk])
            P.op("act", "copy", out=U[d][:, c0:c1, :], in_=pm[0:64, 0:(c1 - c0) * 64].rearrange("p (c e) -> p c e", e=64), reads=[pk], writes=[f"U{d}"])
    order = [list(range(NC)), list(range(MB - 1, -1, -1)) + list(range(NC - 1, MB - 1, -1))]
    prev = [{}, {}]
    for d in range(2):
        eng = "dve"
        od = order[d]
        for i in range(1, NC):
            prev[d][od[i]] = od[i - 1]
            P.op(eng, "scalar_tensor_tensor", out=U[d][:, od[i], :], in0=U[d][:, od[i - 1], :], scalar=gc[:, d:d + 1], in1=U[d][:, od[i], :],
                 op0=ALU.mult, op1=ALU.add, reads=[f"U{d}", "gc"], writes=[f"U{d}"])
        P.op(eng, "tensor_copy", out=S16[d][:], in_=U[d][:], reads=[f"U{d}"], writes=[f"S16_{d}"])
    accrot = Rot(P, "acc", 2, [128, 512], F32, psum=True)
    srot = Rot(P, "S", 2, [128, 512], F32, psum=True)
    eprot = Rot(P, "ep", 2, [128, 512], F32, psum=True)
    qrot = Rot(P, "qg", 2, [64, 512], BF16)
    krot = Rot(P, "kg", 2, [64, 512], BF16)
    grot = Rot(P, "gg", 2, [64, 512], F32)
    amrot = Rot(P, "am", 3, [128, 128], BF16)
    qxrot = Rot(P, "qx", 3, [64, 2, 128], BF16)
    t32rot = Rot(P, "t32", 3, [64, 512], F32)
    orot = Rot(P, "ob", 2, [64, 512], BF16)
    groups = []
    if need_ctx:
        groups.append(list(range(0, MB)))
    for c0 in range(MB, NC, 4):
        groups.append(list(range(c0, min(NC, c0 + 4))))
    for grp in groups:
        t0_ = grp[0] * 128
        nq = len(grp) * 128
        qg, qgk = qrot.next()
        kg, kgk = krot.next()
        gg, ggk = grot.next()
        P.dma("sp", qg[:, 0:nq], qT_d[:, t0_:t0_ + nq], writes=[qgk])
        P.dma("sp", kg[:, 0:nq], kT_d[:, t0_:t0_ + nq], writes=[kgk])
        P.dma("sp", gg[:, 0:nq], gT_d[:, t0_:t0_ + nq], writes=[ggk])
        P.op("act", "activation", out=gg[:, 0:nq], in_=gg[:, 0:nq], func=AF.Silu, reads=[ggk], writes=[ggk])
        acc, acck = accrot.next()
        for ci, c in enumerate(grp):
            cs = slice(ci * 128, (ci + 1) * 128)
            S, Sk = srot.next()
            P.op("pe", "matmul", out=S[:, 0:128], lhsT=kg[:, cs], rhs=qg[:, cs], start=True, stop=True, reads=[kgk, qgk], writes=[Sk])
            am, amk = amrot.next()
            P.op("dve", "tensor_tensor", out=am[:], in0=S[:, 0:128], in1=DcT[:], op=ALU.mult, reads=[Sk, "DcT"], writes=[amk])
            qx, qxk = qxrot.next()
            P.op("pool", "tensor_tensor", out=qx[:], in0=qg[:, cs].unsqueeze(1).to_broadcast([64, 2, 128]), in1=xi[:], op=ALU.mult, reads=[qgk, "xi"], writes=[qxk])
            mms = [(V[:, c, :], am[:], ["V", amk])]
            for d in range(2):
                if c in prev[d]:
                    mms.append((S16[d][:, prev[d][c], :], qx[:, d, :], [f"S16_{d}", qxk]))
            for mi, (l_, r_, rk_) in enumerate(mms):
                P.op("pe", "matmul", out=acc[0:64, cs], lhsT=l_, rhs=r_, start=(mi == 0), stop=(mi == len(mms) - 1), reads=rk_, writes=[acck])
        y, yk = t32rot.next()
        P.op("act", "copy", out=y[:, 0:nq], in_=acc[0:64, 0:nq], reads=[acck], writes=[yk])
        yn, ynk = t32rot.next()
        headnorm_T(P, y[:, 0:nq], yk, nq, ones64, eprot, gcol[:, 0:1], "gcol", 1.0, yn[:, 0:nq], ynk, t32rot)
        ob, obk = orot.next()
        P.op("pool", "tensor_tensor", out=ob[:, 0:nq], in0=yn[:, 0:nq], in1=gg[:, 0:nq], op=ALU.mult, reads=[ynk, ggk], writes=[obk])
        P.dma("sp", oT_d[:, t0_:t0_ + nq], ob[:, 0:nq], reads=[obk])
    P.end_phase()


def _bk(name, c0, c1):
    return [f"{name}{b}" for b in range(c0 // 512, (c1 - 1) // 512 + 1)]


def emit_lru(P, N, M, lx_d, ly_d, cw_d, cb_d, wa_d, wx_d, ba_d, bx_d, lam_d, i2_d, oT_d):
    T = N + M

    XX = P.sb([128, T], F32, "XX")
    UU = P.sb([128, T], F32, "UU")
    CH = 2048
    for c0 in range(0, T, CH):
        c1 = min(T, c0 + CH)
        P.dma("sp", XX[0:64, c0:c1], lx_d[:, c0:c1], writes=_bk("XX", c0, c1))
        P.dma("sp", XX[64:128, c0:c1], lx_d[:, c0:c1], writes=_bk("XX", c0, c1))
    small = {}
    for nm, d_, shp in (("cw", cw_d, [128, 4]), ("cb", cb_d, [128, 1]), ("wa", wa_d, [64, 128]), ("wx", wx_d, [64, 128]),
                        ("ba", ba_d, [128, 1]), ("bx", bx_d, [128, 1]), ("lam", lam_d, [128, 1]), ("i2", i2_d, [128, 64])):
        t_ = P.sb(shp, F32, nm)
        P.dma("sp", t_[:], d_, writes=[nm])
        small[nm] = t_
    cw, cb, wa, wx, ba, bx, lam, i2 = (small[k] for k in ("cw", "cb", "wa", "wx", "ba", "bx", "lam", "i2"))
    c8 = P.sb([128, 1], F32, "c8")
    P.op("act", "activation", out=c8[:], in_=lam[:], func=AF.Exp, scale=-1.0, reads=["lam"], writes=["c8"])
    P.op("dve", "tensor_scalar", out=c8[:], in0=c8[:], scalar1=1.0, scalar2=None, op0=ALU.add, reads=["c8"], writes=["c8"])
    P.op("act", "activation", out=c8[:], in_=c8[:], func=AF.Ln, reads=["c8"], writes=["c8"])
    P.op("dve", "tensor_scalar", out=c8[:], in0=c8[:], scalar1=-8.0, scalar2=None, op0=ALU.mult, reads=["c8"], writes=["c8"])
    for (s0, s1) in ((0, M), (M, T)):
        for c0 in range(s0, s1, CH):
            c1 = min(s1, c0 + CH)
            P.op("dve", "tensor_scalar", out=UU[:, c0:c1], in0=XX[:, c0:c1], scalar1=cw[:, 1:2], scalar2=cb[:, 0:1], op0=ALU.mult, op1=ALU.add,
                 reads=_bk("XX", c0, c1) + ["cw", "cb"], writes=_bk("UU", c0, c1))
            for (tap, sh) in ((0, -1), (2, 1), (3, 2)):
                o0, o1 = max(c0, s0 - sh), min(c1, s1 - sh)
                if o1 <= o0:
                    continue
                P.op("dve", "scalar_tensor_tensor", out=UU[:, o0:o1], in0=XX[:, o0 + sh:o1 + sh], scalar=cw[:, tap:tap + 1], in1=UU[:, o0:o1],
                     op0=ALU.mult, op1=ALU.add, reads=_bk("XX", o0 + sh, o1 + sh) + _bk("UU", o0, o1) + ["cw"], writes=_bk("UU", o0, o1))
    grot = Rot(P, "gps", 4, [128, 512], F32, psum=True)
    rsrot = Rot(P, "rs", 2, [128, 512], F32)
    isrot = Rot(P, "is", 2, [128, 512], F32)
    t1rot = Rot(P, "t1", 2, [128, 512], F32)
    for c0 in range(0, T, 512):
        c1 = min(T, c0 + 512)
        w = c1 - c0
        uk = _bk("UU", c0, c1)
        xk = _bk("XX", c0, c1)
        pr, prk = grot.next()
        pi, pik = grot.next()
        P.op("pe", "matmul", out=pr[:, 0:w], lhsT=wa[:, :], rhs=UU[0:64, c0:c1], start=True, stop=True, reads=uk + ["wa"], writes=[prk])
        P.op("pe", "matmul", out=pi[:, 0:w], lhsT=wx[:, :], rhs=UU[0:64, c0:c1], start=True, stop=True, reads=uk + ["wx"], writes=[pik])
        rs, rsk = rsrot.next()
        is_, isk = isrot.next()
        P.op("act", "activation", out=rs[:, 0:w], in_=pr[:, 0:w], func=AF.Sigmoid, bias=ba[:, 0:1], reads=[prk, "ba"], writes=[rsk])
        P.op("act", "activation", out=is_[:, 0:w], in_=pi[:, 0:w], func=AF.Sigmoid, bias=bx[:, 0:1], reads=[pik, "bx"], writes=[isk])
        P.op("act", "activation", out=XX[:, c0:c1], in_=rs[:, 0:w], func=AF.Exp, scale=c8[:, 0:1], reads=[rsk, "c8"], writes=xk)
        t1, t1k = t1rot.next()
        P.op("pool", "tensor_tensor", out=t1[:, 0:w], in0=XX[:, c0:c1], in1=XX[:, c0:c1], op=ALU.mult, reads=xk, writes=[t1k])
        P.op("pool", "tensor_scalar", out=t1[:, 0:w], in0=t1[:, 0:w], scalar1=-1.0, scalar2=1.0, op0=ALU.mult, op1=ALU.add, reads=[t1k], writes=[t1k])
        P.op("act", "activation", out=t1[:, 0:w], in_=t1[:, 0:w], func=AF.Sqrt, reads=[t1k], writes=[t1k])
        P.op("dve", "tensor_tensor", out=is_[:, 0:w], in0=is_[:, 0:w], in1=UU[:, c0:c1], op=ALU.mult, reads=[isk] + uk, writes=[isk])
        P.op("dve", "tensor_tensor", out=UU[:, c0:c1], in0=is_[:, 0:w], in1=t1[:, 0:w], op=ALU.mult, reads=[isk, t1k], writes=uk)
    prev = None
    for c0 in range(0, T, CH):
        c1 = min(T, c0 + CH)
        init = 0.0 if prev is None else UU[0:64, c0 - 1:c0]
        P.op("dve", "tensor_tensor_scan", out=UU[0:64, c0:c1], data0=XX[0:64, c0:c1], data1=UU[0:64, c0:c1], initial=init, op0=ALU.mult, op1=ALU.add,
             reads=_bk("XX", c0, c1) + _bk("UU", max(c0 - 1, 0), c1) + ["scanf"], writes=_bk("UU", c0, c1) + ["scanf"])
        prev = c0
    chunks = [(c0, min(M, c0 + CH)) for c0 in range(0, M, CH)][::-1] + [(c0, min(T, c0 + CH)) for c0 in range(M, T, CH)][::-1]
    prev_lo = None
    for (c0, c1) in chunks:
        init = 0.0 if prev_lo is None else UU[64:128, prev_lo:prev_lo + 1]
        rk = [] if prev_lo is None else _bk("UU", prev_lo, prev_lo + 1)
        P.op("dve", "tensor_tensor_scan", out=UU[64:128, c0:c1][:, ::-1], data0=XX[64:128, c0:c1][:, ::-1], data1=UU[64:128, c0:c1][:, ::-1], initial=init,
             op0=ALU.mult, op1=ALU.add, reads=_bk("XX", c0, c1) + _bk("UU", c0, c1) + rk + ["scanb"], writes=_bk("UU", c0, c1) + ["scanb"])
        prev_lo = c0
    hrot = Rot(P, "hps", 2, [128, 512], F32, psum=True)
    yrot = Rot(P, "yy", 2, [64, 512], F32)
    zrot = Rot(P, "zz", 2, [64, 512], F32)
    orot = Rot(P, "ob", 2, [64, 512], BF16)
    for c0 in range(0, T, 512):
        c1 = min(T, c0 + 512)
        w = c1 - c0
        hp, hpk = hrot.next()
        P.op("pe", "matmul", out=hp[0:64, 0:w], lhsT=i2[:, :], rhs=UU[:, c0:c1], start=True, stop=True, reads=_bk("UU", c0, c1) + ["i2"], writes=[hpk])
        yy, yk = yrot.next()
        zz, zk = zrot.next()
        P.dma("sp", yy[:, 0:w], ly_d[:, c0:c1], writes=[yk])
        P.op("pool", "tensor_tensor", out=zz[:, 0:w], in0=yy[:, 0:w], in1=yy[:, 0:w], op=ALU.mult, reads=[yk], writes=[zk])
        P.op("pool", "tensor_scalar", out=zz[:, 0:w], in0=zz[:, 0:w], scalar1=0.044715, scalar2=1.0, op0=ALU.mult, op1=ALU.add, reads=[zk], writes=[zk])
        P.op("pool", "tensor_tensor", out=zz[:, 0:w], in0=zz[:, 0:w], in1=yy[:, 0:w], op=ALU.mult, reads=[zk, yk], writes=[zk])
        P.op("act", "activation", out=zz[:, 0:w], in_=zz[:, 0:w], func=AF.Sigmoid, scale=2.0 * math.sqrt(2.0 / math.pi), reads=[zk], writes=[zk])
        P.op("pool", "tensor_tensor", out=zz[:, 0:w], in0=zz[:, 0:w], in1=yy[:, 0:w], op=ALU.mult, reads=[zk, yk], writes=[zk])
        ob, obk = orot.next()
        P.op("dve", "tensor_tensor", out=ob[:, 0:w], in0=hp[0:64, 0:w], in1=zz[:, 0:w], op=ALU.mult, reads=[hpk, zk], writes=[obk])
        P.dma("sp", oT_d[:, c0:c1], ob[:, 0:w], reads=[obk])
    P.end_phase()


def emit_f(P, tile_specs, nv, E, FF, moe, final, modrows_d, sel_d, wo_d, w1_d, w3_d, w2_d, ident_d, wr_d=None, id32_d=None, fn_d=None, GT=8):
    ntile = len(tile_specs)
    NFB = FF // 128
    nhalf = 2
    HB = NFB // nhalf
    ident = P.sb([128, 128], BF16, "ident")
    P.dma("sp", ident[:], ident_d, writes=["ident"])
    sel = P.sb([2, 2, 128], F32, "sel")
    P.dma("sp", sel[:], sel_d, writes=["sel"])
    mrrot = Rot(P, "mrowF", 2, [2, 1024], F32)
    pmrot = Rot(P, "pm", 3, [128, 512], F32, psum=True)
    mt = [[P.sb([128, D], F32, f"mt{v}_{i}") for i in range(4)] for v in range(nv)]
    for i in range(4):
        mrow, mrk = mrrot.next()
        P.dma("sp", mrow[:], modrows_d[:, i * 1024:(i + 1) * 1024], writes=[mrk])
        for v in range(nv):
            bcast_rows(P, mrow, mrk, sel, v, pmrot, mt[v][i], f"mt{v}_{i}")
    if final:
        fnb = P.sb([128, D], F32, "fnb")
        P.dma("sp", fnb[:], fn_d, writes=["fnb"])
    Wo = P.sb([128, 8, D], BF16, "Wo")
    wov = wo_d.rearrange("(k p) c -> p k c", p=128)
    for k in range(8):
        P.dma("pool", Wo[:, k, :], wov[:, k, :], writes=["Wo"])
    if moe:
        Wr = P.sb([128, 8, E], F32, "Wr")
        P.dma("sp", Wr[:], wr_d.rearrange("(k p) e -> p k e", p=128), writes=["Wr"])
        id32 = P.sb([128, 128], F32, "id32")
        P.dma("sp", id32[:], id32_d, writes=["id32"])
        gates = P.sb([128, GT, E], F32, "gates")
        h32T = P.sb([128, 8, 128], F32, "h32T")
        rsm = Rot(P, "rsm", 2, [128, 40], F32)
    purot = Rot(P, "pu", 4, [128, 512], F32, psum=True)
    rots = {
        "sq": Rot(P, "sq", 2, [128, D], F32),
        "st": Rot(P, "st", 4, [128, 4], F32),
        "hb": Rot(P, "hb", 2, [128, D], BF16),
        "ptr": Rot(P, "ptr", 1, [128, 1024], BF16, psum=True),
    }
    xrot = Rot(P, "xt", 2, [128, D], F32)
    otrot = Rot(P, "oTt", 2, [128, 8, 128], BF16)
    acc = P.sb([128, GT, D], F32, "acc")
    h2T = P.sb([128, 8, GT * 128], BF16, "h2T")
    gT = P.sb([128, HB, GT * 128], BF16, "gT")
    W2 = P.sb([128, HB, D], BF16, "W2")
    w1rot = Rot(P, "w1b", 2, [128, 8, 256], BF16)
    w3rot = Rot(P, "w3b", 2, [128, 8, 256], BF16)
    srot = Rot(P, "sil", 2, [128, 512], F32)
    tmprot = Rot(P, "ftmp", 2, [128, 512], F32)
    orot = Rot(P, "xout", 1, [128, D], F32) if final else None

    tiles_v = [ts_[0] for ts_ in tile_specs]
    for g0 in range(0, ntile, GT):
        tiles = list(range(g0, min(ntile, g0 + GT)))
        ng = len(tiles)
        ntok = ng * 128
        for ti, tile in enumerate(tiles):
            v = tiles_v[tile]
            acck = f"acc{ti}"
            xt, xk = xrot.next()
            P.dma("sp", xt[:], tile_specs[tile][1], writes=[xk])
            ot, otk = otrot.next()
            P.dma("sp", ot[:], tile_specs[tile][2], reads=["l_oT"], writes=[otk])
            for hh in range(2):
                pm, pk = pmrot.next()
                for k in range(8):
                    P.op("pe", "matmul", out=pm[:, :], lhsT=ot[:, k, :], rhs=Wo[:, k, hh * 512:(hh + 1) * 512], start=(k == 0), stop=(k == 7),
                         reads=[otk, "Wo"], writes=[pk])
                hs = slice(hh * 512, (hh + 1) * 512)
                P.op("dve", "tensor_tensor", out=acc[:, ti, hs], in0=pm[:, :], in1=mt[v][0][:, hs], op=ALU.mult, reads=[pk, f"mt{v}_0"], writes=[acck])
                P.op("pool", "tensor_tensor", out=acc[:, ti, hs], in0=acc[:, ti, hs], in1=xt[:, hs], op=ALU.add, reads=[acck, xk], writes=[acck])
            st, stk, sq, sqk = rms_rstd(P, acc[:, ti, :], acck, rots)
            P.op("dve", "scalar_tensor_tensor", out=sq[:], in0=acc[:, ti, :], scalar=st[:, 3:4], in1=mt[v][2][:], op0=ALU.mult, op1=ALU.mult,
                 reads=[acck, stk, f"mt{v}_2"], writes=[sqk])
            P.op("pool", "tensor_tensor", out=sq[:], in0=sq[:], in1=mt[v][1][:], op=ALU.add, reads=[sqk, f"mt{v}_1"], writes=[sqk])
            hb, hbk = rots["hb"].next()
            P.op("act", "copy", out=hb[:], in_=sq[:], reads=[sqk], writes=[hbk])
            ptr, ptk = rots["ptr"].next()
            for k in range(8):
                P.op("pe", "transpose", out=ptr[:, k * 128:(k + 1) * 128], in_=hb[:, k * 128:(k + 1) * 128], identity=ident[:], reads=[hbk, "ident"], writes=[ptk])
            P.op("act", "copy", out=h2T[:, :, ti * 128:(ti + 1) * 128], in_=ptr[:, :].rearrange("p (k t) -> p k t", k=8), reads=[ptk], writes=[f"h2T{ti}"])
            if moe:
                for hh in range(2):
                    pm, pk = pmrot.next()
                    for k4 in range(4):
                        k = hh * 4 + k4
                        P.op("pe", "transpose", out=pm[:, k4 * 128:(k4 + 1) * 128], in_=sq[:, k * 128:(k + 1) * 128], identity=id32[:], reads=[sqk, "id32"], writes=[pk])
                    P.op("act", "copy", out=h32T[:, hh * 4:(hh + 1) * 4, :], in_=pm[:, :].rearrange("p (k t) -> p k t", k=4), reads=[pk], writes=["h32T"])
                pm, pk = pmrot.next()
                for k in range(8):
                    P.op("pe", "matmul", out=pm[:, 0:E], lhsT=h32T[:, k, :], rhs=Wr[:, k, :], start=(k == 0), stop=(k == 7), reads=["h32T", "Wr"], writes=[pk])
                r, rk = rsm.next()
                lg, eq1, lg2, eq2 = r[:, 0:E], r[:, 8:8 + E], r[:, 16:16 + E], r[:, 24:24 + E]
                m1, m2, dd, ww1, ww2 = (r[:, 32 + i:33 + i] for i in range(5))
                P.op("act", "copy", out=lg, in_=pm[:, 0:E], reads=[pk], writes=[rk])
                P.op("dve", "tensor_reduce", out=m1, in_=lg, axis=AX.X, op=ALU.max, reads=[rk], writes=[rk])
                P.op("dve", "tensor_scalar", out=eq1, in0=lg, scalar1=m1, scalar2=None, op0=ALU.is_equal, reads=[rk], writes=[rk])
                P.op("dve", "scalar_tensor_tensor", out=lg2, in0=eq1, scalar=-1e30, in1=lg, op0=ALU.mult, op1=ALU.add, reads=[rk], writes=[rk])
                P.op("dve", "tensor_reduce", out=m2, in_=lg2, axis=AX.X, op=ALU.max, reads=[rk], writes=[rk])
                P.op("dve", "tensor_scalar", out=eq2, in0=lg2, scalar1=m2, scalar2=None, op0=ALU.is_equal, reads=[rk], writes=[rk])
                P.op("dve", "tensor_tensor", out=dd, in0=m2, in1=m1, op=ALU.subtract, reads=[rk], writes=[rk])
                P.op("act", "activation", out=dd, in_=dd, func=AF.Exp, reads=[rk], writes=[rk])
                P.op("dve", "tensor_scalar", out=ww1, in0=dd, scalar1=1.0, scalar2=None, op0=ALU.add, reads=[rk], writes=[rk])
                P.op("dve", "reciprocal", out=ww1, in_=ww1, reads=[rk], writes=[rk])
                P.op("dve", "tensor_tensor", out=ww2, in0=dd, in1=ww1, op=ALU.mult, reads=[rk], writes=[rk])
                P.op("dve", "tensor_scalar", out=gates[:, ti, :], in0=eq1, scalar1=ww1, scalar2=None, op0=ALU.mult, reads=[rk], writes=[f"gates{ti}"])
                P.op("dve", "scalar_tensor_tensor", out=gates[:, ti, :], in0=eq2, scalar=ww2, in1=gates[:, ti, :], op0=ALU.mult, op1=ALU.add, reads=[rk, f"gates{ti}"], writes=[f"gates{ti}"])
        h2keys = [f"h2T{ti}" for ti in range(ng)]
        chunks = [(c0, min(ntok, c0 + 512)) for c0 in range(0, ntok, 512)]
        for e in range(E):
            for half in range(nhalf):
                fb0 = half * HB
                w2v = w2_d[e, fb0 * 128:(fb0 + HB) * 128, :].rearrange("(f p) c -> p f c", p=128)
                for fp in range(0, HB, 2):
                    nb_ = min(2, HB - fp)
                    w1b, w1k = w1rot.next()
                    w3b, w3k = w3rot.next()
                    cs = slice((fb0 + fp) * 128, (fb0 + fp + nb_) * 128)
                    P.dma("pool", w1b[:, :, 0:nb_ * 128], w1_d[e, :, cs].rearrange("(k p) c -> p k c", p=128), writes=[w1k])
                    P.dma("pool", w3b[:, :, 0:nb_ * 128], w3_d[e, :, cs].rearrange("(k p) c -> p k c", p=128), writes=[w3k])
                    P.dma("pool", W2[:, fp:fp + nb_, :], w2v[:, fp:fp + nb_, :], writes=["W2"])
                    for bi in range(nb_):
                        fbi = fp + bi
                        for (c0, c1) in chunks:
                            w = c1 - c0
                            hk = h2keys[c0 // 128:(c1 - 1) // 128 + 1]
                            p1, p1k = purot.next()
                            p3, p3k = purot.next()
                            for k in range(8):
                                P.op("pe", "matmul", out=p1[:, 0:w], lhsT=w1b[:, k, bi * 128:(bi + 1) * 128], rhs=h2T[:, k, c0:c1], start=(k == 0), stop=(k == 7),
                                     reads=[w1k] + hk, writes=[p1k])
                            for k in range(8):
                                P.op("pe", "matmul", out=p3[:, 0:w], lhsT=w3b[:, k, bi * 128:(bi + 1) * 128], rhs=h2T[:, k, c0:c1], start=(k == 0), stop=(k == 7),
                                     reads=[w3k] + hk, writes=[p3k])
                            sl, slk = srot.next()
                            P.op("act", "activation", out=sl[:, 0:w], in_=p1[:, 0:w], func=AF.Silu, reads=[p1k], writes=[slk])
                            P.op("dve", "tensor_tensor", out=gT[:, fbi, c0:c1], in0=p3[:, 0:w], in1=sl[:, 0:w], op=ALU.mult, reads=[p3k, slk], writes=[f"gT{c0 // 512}"])
                for ti, tile in enumerate(tiles):
                    v = tiles_v[tile]
                    for hh in range(2):
                        hs = slice(hh * 512, (hh + 1) * 512)
                        pm, pk = pmrot.next()
                        for fbi in range(HB):
                            P.op("pe", "matmul", out=pm[:, :], lhsT=gT[:, fbi, ti * 128:(ti + 1) * 128], rhs=W2[:, fbi, hs], start=(fbi == 0), stop=(fbi == HB - 1),
                                 reads=[f"gT{ti // 4}", "W2"], writes=[pk])
                        tmp, tmpk = tmprot.next()
                        if moe:
                            P.op("dve", "scalar_tensor_tensor", out=tmp[:], in0=pm[:, :], scalar=gates[:, ti, e:e + 1], in1=mt[v][3][:, hs], op0=ALU.mult, op1=ALU.mult,
                                 reads=[pk, f"gates{ti}", f"mt{v}_3"], writes=[tmpk])
                        else:
                            P.op("dve", "tensor_tensor", out=tmp[:], in0=pm[:, :], in1=mt[v][3][:, hs], op=ALU.mult, reads=[pk, f"mt{v}_3"], writes=[tmpk])
                        P.op("pool", "tensor_tensor", out=acc[:, ti, hs], in0=acc[:, ti, hs], in1=tmp[:], op=ALU.add, reads=[f"acc{ti}", tmpk], writes=[f"acc{ti}"])
        for ti, tile in enumerate(tiles):
            if final:
                st, stk, sq, sqk = rms_rstd(P, acc[:, ti, :], f"acc{ti}", rots)
                xo_t, xok = orot.next()
                P.op("dve", "scalar_tensor_tensor", out=xo_t[:], in0=acc[:, ti, :], scalar=st[:, 3:4], in1=fnb[:], op0=ALU.mult, op1=ALU.mult,
                     reads=[f"acc{ti}", stk, "fnb"], writes=[xok])
                P.dma("sp", tile_specs[tile][3], xo_t[:], reads=[xok], writes=["xout_d"])
            else:
                P.dma("sp", tile_specs[tile][3], acc[:, ti, :], reads=[f"acc{ti}"], writes=["xout_d", "xsrc"])
    P.end_phase()


def build_fused(N, M, E, FFD, FFE):
    T = N + M
    tps = N // 4
    nc = bass.Bass("TRN2", target_bir_lowering=False)
    P = Prog(nc)
    di = lambda n, s, d=F32: nc.dram_tensor(n, s, d, kind="ExternalInput").ap()
    do = lambda n, s, d=F32: nc.dram_tensor(n, s, d, kind="ExternalOutput").ap()
    sc = lambda n, s, d=F32: nc.dram_tensor(n, s, d).ap()
    xsh = di("xsh", [tps, D])
    ctx = di("ctx", [M, D])
    cT = di("cT", [128, 8, 2])
    tabs = di("tabs", [N, 160])
    ident_d = di("ident", [128, 128], BF16)
    id32_d = di("id32", [128, 128])
    sel_d = di("sel", [2, 2, 128])
    m3_d = di("m3", [128, 384], BF16)
    rc_d = di("retc", [128, 6, 128])
    rz_d = di("retz", [128, 2])
    i2_d = di("i2", [128, 64])
    fn_d = di("fnb", [128, D])
    L = []
    for l in range(2):
        L.append(dict(
            wada=di(f"wada{l}", [D, 6144]), bada=di(f"bada{l}", [1, 6144]), gn1=di(f"gn1_{l}", [1, D]), gn2=di(f"gn2_{l}", [1, D]),
            wt=di(f"wt{l}", [D, 576]), wf=di(f"wf{l}", [D, 192]), wo=di(f"wo{l}", [D, D]),
            sink=di(f"sink{l}", [128, 1]), logit=di(f"logit{l}", [128, 2]), rgn=di(f"rgn{l}", [64, 1]),
            lamv=di(f"lamv{l}", [1, 128]), dgn=di(f"dgn{l}", [64, 1]),
            cw=di(f"cw{l}", [128, 4]), cb=di(f"cb{l}", [128, 1]), wa=di(f"wa{l}", [64, 128]), wx=di(f"wx{l}", [64, 128]),
            ba=di(f"ba{l}", [128, 1]), bx=di(f"bx{l}", [128, 1]), lam=di(f"lam{l}", [128, 1])))
    fw1, fw3, fw2 = di("fw1", [1, D, FFD]), di("fw3", [1, D, FFD]), di("fw2", [1, FFD, D])
    mw1, mw3, mw2 = di("mw1", [E, D, FFE]), di("mw3", [E, D, FFE]), di("mw2", [E, FFE, D])
    wr_d = di("wr", [D, E])
    xo = do("xo", [tps, D])
    s_qA, s_kB = sc("s_qA", [128, T], BF16), sc("s_kB", [128, T], BF16)
    s_rq, s_rk = sc("s_rq", [64, T], BF16), sc("s_rk", [64, T], BF16)
    s_tok = sc("s_tok", [T, 256], BF16)
    s_f32 = sc("s_f32", [192, T])
    s_mod = sc("s_mod", [2, 4096])
    s_oT = sc("s_oT", [256, T], BF16)
    CHT = tps // 2
    NCH = N // CHT
    st_o = sc("st_o", [NCH * 256, CHT], BF16)
    st_oc = sc("st_oc", [256, M], BF16)
    g_o = sc("g_o", [NCH * 1024, CHT], BF16)
    g_oc = sc("g_oc", [1024, M], BF16)
    l_oT = sc("l_oT", [1024, tps], BF16)
    s_x0 = sc("s_x0", [tps, D])
    s_x1 = sc("s_x1", [tps, D])
    XR = min(256, tps)
    NXC = tps // XR
    g_x = [sc(f"g_x{c}", [4 * XR, D]) for c in range(NXC)]
    s_xc1 = sc("s_xc1", [M, D])
    RG = [[0, 1, 2, 3], [4, 5, 6, 7]]
    l_oTv = l_oT.rearrange("(k p) t -> p k t", p=128)
    g_ocv = g_oc.rearrange("(k p) t -> p k t", p=128)

    def gather_x(src):
        for c in range(NXC):
            P.op("pool", "collective_compute", cc=True, kind="AllGather", op=ALU.bypass, replica_groups=RG,
                 ins=[src[c * XR:(c + 1) * XR, :]], outs=[g_x[c]], reads=["xsrc"], writes=["g_x"])

    def x_tile(ti):
        t0 = ti * 128
        r, rem = t0 // tps, t0 % tps
        c, i = rem // XR, rem % XR
        return g_x[c][r * XR + i:r * XR + i + 128, :]
    for r0 in range(0, tps, max(128, tps // 4)):
        r1 = min(tps, r0 + max(128, tps // 4))
        P.dma("sp", s_x0[r0:r1, :], xsh[r0:r1, :], writes=["xsrc"])
    gather_x(s_x0)
    for l in range(2):
        W = L[l]
        need_ctx = (l == 0)
        lam_init = 0.8 - 0.6 * math.exp(-0.3 * l)
        x_lat = x_tile
        x_ctx = ctx if l == 0 else s_xc1
        emit_p(P, N, M, x_lat, x_ctx, cT, W["wada"], W["bada"], W["gn1"], W["gn2"], W["wt"], W["wf"], tabs, ident_d, sel_d,
               s_qA, s_kB, s_rq, s_rk, s_tok, s_f32, s_mod)
        emit_swa(P, N, M, need_ctx, s_qA[64:128, :], s_kB[64:128, :], s_tok[:, 64:128], W["sink"], m3_d, s_oT[0:64, :])
        emit_ret(P, N, M, need_ctx, s_rq, s_rk, s_tok[:, 0:64], s_tok[:, 128:192], s_f32[0:64, :], W["logit"], W["rgn"], rc_d, rz_d, s_oT[64:128, :])
        emit_diff(P, N, M, need_ctx, lam_init, s_qA[0:64, :], s_kB[0:64, :], s_tok[:, 192:256], W["lamv"], W["dgn"], s_oT[128:192, :])
        emit_lru(P, N, M, s_f32[64:128, :], s_f32[128:192, :], W["cw"], W["cb"], W["wa"], W["wx"], W["ba"], W["bx"], W["lam"], i2_d, s_oT[192:256, :])
        for c in range(NCH):
            P.dma("sp", st_o[c * 256:(c + 1) * 256, :], s_oT[:, M + c * CHT:M + (c + 1) * CHT], writes=[f"st_o{c}"])
        if need_ctx:
            P.dma("sp", st_oc[:, :], s_oT[:, 0:M], writes=["st_oc"])
        for c in range(NCH):
            P.op("pool", "collective_compute", cc=True, kind="AllGather", op=ALU.bypass, replica_groups=RG,
                 ins=[st_o[c * 256:(c + 1) * 256, :]], outs=[g_o[c * 1024:(c + 1) * 1024, :]], reads=[f"st_o{c}"], writes=["g_o"])
        if need_ctx:
            P.op("pool", "collective_compute", cc=True, kind="AllGather", op=ALU.bypass, replica_groups=RG,
                 ins=[st_oc], outs=[g_oc], reads=["st_oc"], writes=["g_oc"])
        P.end_phase()
        pidc = {}

        def shard_rows(e, pidc=pidc):
            if id(e) not in pidc:
                pidc[id(e)] = e.snap((e.partition_id() % 4) * 2048)
            return pidc[id(e)]
        for h in range(2):
            P.dma("sp", l_oT[:, h * CHT:(h + 1) * CHT],
                  (lambda e, h=h, sr=shard_rows: g_o[bass.ds(sr(e) + h * 1024, 1024), :]), writes=["l_oT"])
        specs = []
        for i in range(tps // 128):
            if l == 0:
                xs = xsh[i * 128:(i + 1) * 128, :]
                od = s_x1[i * 128:(i + 1) * 128, :]
            else:
                xs = s_x1[i * 128:(i + 1) * 128, :]
                od = xo[i * 128:(i + 1) * 128, :]
            specs.append((0, xs, l_oTv[:, :, i * 128:(i + 1) * 128], od))
        if need_ctx:
            for i in range(M // 128):
                specs.append((1, ctx[i * 128:(i + 1) * 128, :], g_ocv[:, :, i * 128:(i + 1) * 128], s_xc1[i * 128:(i + 1) * 128, :]))
        if l == 0:
            emit_f(P, specs, 2, 1, FFD, False, False, s_mod, sel_d, W["wo"], fw1, fw3, fw2, ident_d)
            gather_x(s_x1)
            P.end_phase()
        else:
            emit_f(P, specs, 1, E, FFE, True, True, s_mod, sel_d, W["wo"], mw1, mw3, mw2, ident_d, wr_d=wr_d, id32_d=id32_d, fn_d=fn_d)
    P.close()
    return nc


NCORES = 8
_cache = {}


def _get(name, fn, *a):
    key = (name,) + a
    if key not in _cache:
        _cache[key] = fn(*a)
    return _cache[key]


def _wo_perm():
    idx = np.zeros(1024, np.int64)
    for j in range(4):
        for m in range(4):
            idx[j * 256 + m * 64:j * 256 + (m + 1) * 64] = np.arange(m * 256 + j * 64, m * 256 + (j + 1) * 64)
    return idx


def make_maps(inp):
    f32 = np.float32
    B, N, _ = inp["x"].shape
    M = inp["ctx"].shape[1]
    ident = np.eye(128, dtype=f32).astype(ml_dtypes.bfloat16)
    sel = np.zeros((2, 2, 128), f32)
    sel[0, 0, :] = 1.0
    sel[1, 1, :] = 1.0
    rc, rz = ret_consts()
    tabs = rope_tables(N)
    common = {"ident": ident, "id32": np.eye(128, dtype=f32), "sel": sel, "m3": swa_masks(), "retc": rc, "retz": rz,
              "i2": np.concatenate([np.eye(64, dtype=f32)] * 2, 0), "tabs": tabs,
              "fnb": np.ascontiguousarray(np.broadcast_to(inp["final_norm"][None, :], (128, D))).astype(f32),
              "fw1": inp["ffn_w1"], "fw3": inp["ffn_w3"], "fw2": inp["ffn_w2"],
              "mw1": inp["moe_w1"][0], "mw3": inp["moe_w3"][0], "mw2": inp["moe_w2"][0], "wr": inp["moe_router"][0]}
    woidx = _wo_perm()
    for l in range(2):
        common[f"wada{l}"] = inp["w_ada"][l]
        common[f"bada{l}"] = np.ascontiguousarray(inp["b_ada"][l][None, :])
        common[f"gn1_{l}"] = np.ascontiguousarray(inp["g_norm1"][l][None, :])
        common[f"gn2_{l}"] = np.ascontiguousarray(inp["g_norm2"][l][None, :])
        common[f"wo{l}"] = np.ascontiguousarray(inp["w_out"][l][woidx, :])
        common[f"lamv{l}"] = np.ascontiguousarray(inp["diff_lambda"][l].reshape(1, 128))
    maps = []
    dup = lambda a: np.ascontiguousarray(np.concatenate([a, a], 0)).astype(f32)
    for core in range(NCORES):
        b, j = core // 4, core % 4
        s = slice(j * 64, (j + 1) * 64)
        m = dict(common)
        m["xsh"] = np.ascontiguousarray(inp["x"][b, j * (N // 4):(j + 1) * (N // 4)])
        m["ctx"] = np.ascontiguousarray(inp["ctx"][b])
        cv = np.stack([inp["c"][b], inp["c_ctx"]], 0)
        m["cT"] = np.ascontiguousarray(cv.reshape(2, 8, 128).transpose(2, 1, 0)).astype(f32)
        tok, feat = head_cols(j)
        for l in range(2):
            two = lambda a: np.ascontiguousarray(np.concatenate([a[0][s], a[1][s]], 0)[:, None]).astype(f32)
            m[f"wt{l}"] = np.ascontiguousarray(inp["w_in"][l][:, tok])
            m[f"wf{l}"] = np.ascontiguousarray(inp["w_in"][l][:, feat])
            m[f"sink{l}"] = np.full((128, 1), inp["attn_sink"][l][j], f32)
            m[f"logit{l}"] = np.ascontiguousarray(np.broadcast_to(inp["ret_decay_logit"][l][:, j][None, :], (128, 2))).astype(f32)
            m[f"rgn{l}"] = np.ascontiguousarray(inp["ret_gn"][l][s, None])
            m[f"dgn{l}"] = np.ascontiguousarray(inp["diff_gn"][l][s, None])
            m[f"cw{l}"] = dup(inp["conv_w"][l][:, s].T)
            m[f"cb{l}"] = dup(inp["conv_b"][l][s, None])
            m[f"wa{l}"] = np.ascontiguousarray(np.concatenate([inp["lru_wa"][l][0, j], inp["lru_wa"][l][1, j]], 1))
            m[f"wx{l}"] = np.ascontiguousarray(np.concatenate([inp["lru_wx"][l][0, j], inp["lru_wx"][l][1, j]], 1))
            m[f"ba{l}"] = two(inp["lru_ba"][l])
            m[f"bx{l}"] = two(inp["lru_bx"][l])
            m[f"lam{l}"] = two(inp["lru_lambda"][l])
        maps.append(m)
    return maps


def kernel(**inputs):
    inp = {k: np.asarray(v) for k, v in inputs.items()}
    B, N, _ = inp["x"].shape
    M = inp["ctx"].shape[1]
    E = inp["moe_w1"].shape[1]
    nc = _get("fused", build_fused, N, M, E, inp["ffn_w1"].shape[2], inp["moe_w1"].shape[3])
    res = run_bass_kernel_spmd(nc, make_maps(inp), core_ids=list(range(NCORES))).results
    out = np.stack([np.concatenate([res[b * 4 + s]["xo"] for s in range(4)], 0) for b in range(B)], 0)
    return out.astype(np.float32)
```

```python
from contextlib import ExitStack
import math
import numpy as np
import ml_dtypes
import concourse.bass as bass
import concourse.mybir as mybir
from concourse.bass_utils import run_bass_kernel_spmd

F32 = mybir.dt.float32
BF16 = mybir.dt.bfloat16
I32 = mybir.dt.int32
AF = mybir.ActivationFunctionType
ALU = mybir.AluOpType
AX = mybir.AxisListType

ENGS = ("pe", "act", "dve", "pool", "sp")
DMAQ = ("sp", "act", "pool")
NDMASEM = 8
_ENGATTR = {"pe": "tensor", "act": "scalar", "dve": "vector", "pool": "gpsimd", "sp": "sync"}


class Op:
    __slots__ = ("eng", "fn", "dma", "cc", "raw", "war", "sig", "dsem", "dval", "has_dep")

    def __init__(self, eng, fn, dma, cc=False):
        self.eng = eng
        self.fn = fn
        self.dma = dma
        self.cc = cc
        self.raw = []
        self.war = []
        self.sig = None
        self.dsem = None
        self.dval = None
        self.has_dep = False


class Prog:
    def __init__(self, nc):
        self.nc = nc
        self.gs = ExitStack()
        self.sems = {e: self.gs.enter_context(nc.semaphore(f"s_{e}")) for e in ENGS}
        self.dsems = {e: [self.gs.enter_context(nc.semaphore(f"d_{e}{i}")) for i in range(NDMASEM)] for e in DMAQ}
        self.cnt = {e: 0 for e in ENGS}
        self.dcnt = {e: [0] * NDMASEM for e in DMAQ}
        self.dk = {e: 0 for e in DMAQ}
        self.ccsem = self.gs.enter_context(nc.semaphore("s_cc"))
        self.cccnt = 0
        self.waited = {e: {} for e in ENGS}
        self.barrier = None
        self.nsb = 0
        self.nphase = 0
        self._reset()

    def _reset(self):
        self.q = {e: [] for e in ENGS}
        self.res_w = {}
        self.res_r = {}
        self.es = ExitStack()

    def sb(self, shape, dt=F32, name=None, persist=False):
        self.nsb += 1
        st = self.gs if persist else self.es
        return st.enter_context(self.nc.sbuf_tensor(f"s{self.nsb}_" + (name or "t"), list(shape), dt))

    def ps(self, shape, dt=F32, name=None):
        self.nsb += 1
        return self.es.enter_context(self.nc.psum_tensor(f"p{self.nsb}_" + (name or "t"), list(shape), dt))

    def op(self, eng, meth, reads=(), writes=(), dma=False, cc=False, **kw):
        fn = (lambda e: getattr(e, meth)(**{k: (v(e) if callable(v) else v) for k, v in kw.items()}))
        o = Op(eng, fn, dma or cc, cc)
        raw = set()
        war = set()
        for r in reads:
            w = self.res_w.get(r)
            if w is not None:
                raw.add(w)
        for wr in writes:
            w = self.res_w.get(wr)
            if w is not None:
                war.add(w)
            for rd in self.res_r.get(wr, ()):
                war.add(rd)
        raw.discard(o)
        war -= raw
        o.raw = list(raw)
        o.war = list(war)
        for r in reads:
            self.res_r.setdefault(r, []).append(o)
        for wr in writes:
            self.res_w[wr] = o
            self.res_r[wr] = []
        self.q[eng].append(o)
        return o

    def dma(self, eng, out, in_, reads=(), writes=(), **kw):
        return self.op(eng, "dma_start", reads, writes, dma=True, out=out, in_=in_, **kw)

    def end_phase(self):
        nc = self.nc
        for e in ENGS:
            for o in self.q[e]:
                for d in o.raw:
                    d.has_dep = True
                for d in o.war:
                    if d.dma or d.eng != o.eng or o.dma:
                        d.has_dep = True
            for o in reversed(self.q[e]):
                if not o.dma:
                    o.has_dep = True
                    break
        for e in ENGS:
            for o in self.q[e]:
                if o.cc:
                    self.cccnt += 1
                    o.dsem = self.ccsem
                    o.dval = self.cccnt
                elif o.dma:
                    s = self.dk[e] % NDMASEM
                    self.dk[e] += 1
                    self.dcnt[e][s] += 16
                    o.dsem = self.dsems[e][s]
                    o.dval = self.dcnt[e][s]
                elif o.has_dep:
                    self.cnt[e] += 1
                    o.sig = self.cnt[e]
        prev_barrier = self.barrier
        nb = [(self.sems[e], self.cnt[e]) for e in ENGS if self.cnt[e] > 0]
        for e in DMAQ:
            for s in range(NDMASEM):
                if self.dcnt[e][s] > 0:
                    nb.append((self.dsems[e][s], self.dcnt[e][s]))
        if self.cccnt > 0:
            nb.append((self.ccsem, self.cccnt))
        self.barrier = nb
        sems = self.sems

        def run_engine(e, eng):
            waited = self.waited[e]

            def wait(sem, val):
                if waited.get(sem, 0) >= val:
                    return
                eng.wait_ge(sem, val)
                waited[sem] = val

            if prev_barrier is not None:
                for s, v in prev_barrier:
                    wait(s, v)
            for o in self.q[e]:
                for d, is_raw in [(d, True) for d in o.raw] + [(d, False) for d in o.war]:
                    if d.dma:
                        wait(d.dsem, d.dval)
                    elif d.eng == e and not o.dma:
                        if is_raw:
                            wait(sems[e], d.sig)
                    else:
                        wait(sems[d.eng], d.sig)
                if o.cc:
                    if o.dval > 1:
                        wait(o.dsem, o.dval - 1)
                    o.fn(eng).then_inc(o.dsem)
                elif o.dma:
                    if o.dval > 16:
                        wait(o.dsem, o.dval - 16)
                    o.fn(eng).then_inc(o.dsem, 16)
                else:
                    ins = o.fn(eng)
                    if o.sig is not None:
                        ins.then_inc(sems[e], 1)
            if e == "sp":
                for s, v in nb:
                    wait(s, v)

        with nc.Block() as block:
            @block.tensor
            def _(eng):
                run_engine("pe", eng)

            @block.scalar
            def _(eng):
                run_engine("act", eng)

            @block.vector
            def _(eng):
                run_engine("dve", eng)

            @block.gpsimd
            def _(eng):
                run_engine("pool", eng)

            @block.sync
            def _(eng):
                run_engine("sp", eng)
        self.es.close()
        self._reset()
        self.nphase += 1

    def emit(self):
        self.end_phase()

    def close(self):
        self.es.close()
        self.gs.close()

D = 1024
EPS = 1e-6
SC_SWA = 64 ** -0.5
SC_RET = 64 ** -0.5
SC_DIFF = 32 ** -0.5


class Rot:
    def __init__(self, P, name, n, shape, dt=F32, psum=False):
        self.bufs = [(P.ps(shape, dt, f"{name}{i}") if psum else P.sb(shape, dt, f"{name}{i}")) for i in range(n)]
        self.name = name
        self.i = 0

    def next(self):
        k = self.i % len(self.bufs)
        self.i += 1
        return self.bufs[k], f"{self.name}{k}"


def _r(a, b):
    return list(range(a, b))


TOK_COLS = _r(0, 256) + _r(256, 384) + _r(1536, 1792) + _r(1792, 2048) + _r(512, 768) + _r(768, 1024) + _r(384, 512) + _r(1024, 1280) + _r(2048, 2304)
FEAT_COLS = _r(1280, 1536) + _r(2304, 2560) + _r(2560, 2816)


def rope_tables(n_lat):
    f32 = np.float32
    n = np.arange(n_lat)
    row = (n // 64).astype(f32)
    col = (n % 64).astype(f32)

    def axial(dim):
        nf = dim // 4
        inv = (f32(10000.0) ** (-(np.arange(nf, dtype=f32)) / f32(nf))).astype(f32)
        ang = np.concatenate([row[:, None] * inv, col[:, None] * inv], -1).astype(f32)
        return np.cos(ang).astype(f32), np.sin(ang).astype(f32)

    ca, sa = axial(64)
    cd, sd = axial(32)
    nf = 32
    inv = (f32(10000.0) ** (-(np.arange(nf, dtype=f32)) / f32(nf))).astype(f32)
    ang = (n.astype(f32)[:, None] * inv).astype(f32)
    cl, sl = np.cos(ang).astype(f32), np.sin(ang).astype(f32)
    return np.ascontiguousarray(np.concatenate([ca, sa, cd, sd, cl, sl], -1).astype(f32))


def mod_rows(P, cT, wada, bada, ncols, pmrot, name, extra=None):
    scT = P.sb([128, 8, 2], F32, name + "_scT")
    P.dma("sp", scT[:], cT, writes=[name + "_scT"])
    P.op("act", "activation", out=scT[:], in_=scT[:], func=AF.Silu, reads=[name + "_scT"], writes=[name + "_scT"])
    mrow = P.sb([2, ncols], F32, name + "_mrow")
    brot = Rot(P, name + "_brow", 2, [2, 512], F32)
    xrot_ = Rot(P, name + "_xrow", 2, [2, 512], F32)
    g2rot = Rot(P, name + "_g2row", 2, [2, 512], F32)
    wrot = Rot(P, name + "_wA", 2, [128, 8, 512], F32)
    wv = wada.rearrange("(k p) c -> p k c", p=128)
    ntot = ncols if extra is None else extra[0]
    for cb in range(ntot // 512):
        cs = slice(cb * 512, (cb + 1) * 512)
        wA, wk = wrot.next()
        P.dma("sp", wA[:], wv[:, :, cs], writes=[wk])
        brow, bk = brot.next()
        P.dma("sp", brow[0:1, :], bada[:, cs], writes=[bk])
        P.dma("sp", brow[1:2, :], bada[:, cs], writes=[bk])
        pm, pk = pmrot.next()
        for k in range(8):
            P.op("pe", "matmul", out=pm[0:2, :], lhsT=scT[:, k, :], rhs=wA[:, k, :], start=(k == 0), stop=(k == 7),
                 reads=[name + "_scT", wk], writes=[pk])
        if cb * 512 < ncols:
            P.op("dve", "tensor_tensor", out=mrow[:, cs], in0=pm[0:2, :], in1=brow[:, :], op=ALU.add, reads=[pk, bk], writes=[name + "_mrow"])
        else:
            xr, xk = xrot_.next()
            P.op("dve", "tensor_tensor", out=xr[:, :], in0=pm[0:2, :], in1=brow[:, :], op=ALU.add, reads=[pk, bk], writes=[xk])
            oc = cb * 512 - ncols
            if 2048 <= oc < 3072:
                g2, g2k = g2rot.next()
                P.dma("sp", g2[0:1, :], extra[1][:, oc - 2048:oc - 2048 + 512], writes=[g2k])
                P.dma("sp", g2[1:2, :], extra[1][:, oc - 2048:oc - 2048 + 512], writes=[g2k])
                P.op("dve", "scalar_tensor_tensor", out=xr[:, :], in0=xr[:, :], scalar=1.0, in1=g2[:, :], op0=ALU.add, op1=ALU.mult, reads=[xk, g2k], writes=[xk])
            P.dma("sp", extra[2][:, oc:oc + 512], xr[:, :], reads=[xk])
    return mrow, name + "_mrow"


def bcast_rows(P, row, rowkey, sel, v, pmrot, out, outkey):
    for hh in range(2):
        pm, pk = pmrot.next()
        P.op("pe", "matmul", out=pm[:, :], lhsT=sel[:, v, :], rhs=row[:, hh * 512:(hh + 1) * 512], start=True, stop=True,
             reads=[rowkey, "sel"], writes=[pk])
        P.op("act", "copy", out=out[:, hh * 512:(hh + 1) * 512], in_=pm[:, :], reads=[pk], writes=[outkey])


def rms_rstd(P, xt, xk, rots):
    sq, sqk = rots["sq"].next()
    st, stk = rots["st"].next()
    P.op("act", "activation", out=sq[:], in_=xt, func=AF.Square, accum_out=st[:, 0:1], reads=[xk], writes=[sqk, stk])
    P.op("dve", "tensor_scalar", out=st[:, 1:2], in0=st[:, 0:1], scalar1=1.0 / D, scalar2=EPS, op0=ALU.mult, op1=ALU.add, reads=[stk], writes=[stk])
    P.op("act", "activation", out=st[:, 2:3], in_=st[:, 1:2], func=AF.Sqrt, reads=[stk], writes=[stk])
    P.op("dve", "reciprocal", out=st[:, 3:4], in_=st[:, 2:3], reads=[stk], writes=[stk])
    return st, stk, sq, sqk


def norm_mod_transpose(P, xt, xk, Gb, Sb, gkeys, ident, hT, hTk, col0, rots):
    st, stk, sq, sqk = rms_rstd(P, xt, xk, rots)
    P.op("dve", "scalar_tensor_tensor", out=sq[:], in0=xt, scalar=st[:, 3:4], in1=Gb[:], op0=ALU.mult, op1=ALU.mult,
         reads=[xk, stk, gkeys[0]], writes=[sqk])
    hb, hbk = rots["hb"].next()
    P.op("pool", "tensor_tensor", out=hb[:], in0=sq[:], in1=Sb[:], op=ALU.add, reads=[sqk, gkeys[1]], writes=[hbk])
    ptr, ptk = rots["ptr"].next()
    for k in range(8):
        P.op("pe", "transpose", out=ptr[:, k * 128:(k + 1) * 128], in_=hb[:, k * 128:(k + 1) * 128], identity=ident[:],
             reads=[hbk, "ident"], writes=[ptk])
    P.op("act", "copy", out=hT[:, :, col0:col0 + 128], in_=ptr[:, :].rearrange("p (k t) -> p k t", k=8), reads=[ptk], writes=[hTk])


def head_cols(j):
    g = j // 2
    r = lambda a, n=64: list(range(a, a + n))
    tok = (r(1536 + j * 64) + r(0 + j * 64) + r(1792 + j * 64) + r(256 + g * 64) + r(512 + j * 64) + r(768 + j * 64)
           + r(384 + g * 64) + r(1024 + j * 64) + r(2048 + j * 64))
    feat = r(1280 + j * 64) + r(2304 + j * 64) + r(2560 + j * 64)
    return tok, feat


def emit_p(P, N, M, x_lat, x_ctx, cT, wada, bada, gn, gn2, wt_d, wf_d, tabs, ident_d, sel_d,
           s_qA, s_kB, s_rq, s_rk, s_tok, s_f32, s_modrows):
    nlt, nct = N // 128, M // 128
    ident = P.sb([128, 128], BF16, "ident")
    P.dma("sp", ident[:], ident_d, writes=["ident"])
    sel = P.sb([2, 2, 128], F32, "sel")
    P.dma("sp", sel[:], sel_d, writes=["sel"])
    Wt = P.sb([128, 8, 576], BF16, "Wt")
    Wf = P.sb([128, 8, 192], BF16, "Wf")
    P.dma("pool", Wt[:], wt_d.rearrange("(k p) c -> p k c", p=128), writes=["Wt"])
    P.dma("pool", Wf[:], wf_d.rearrange("(k p) c -> p k c", p=128), writes=["Wf"])
    pmrot = Rot(P, "pm", 3, [128, 512], F32, psum=True)
    mrow, mk = mod_rows(P, cT, wada, bada, 2048, pmrot, "mod", extra=(6144, gn2, s_modrows))
    grow = P.sb([2, D], F32, "grow")
    P.dma("sp", grow[0:1, :], gn, writes=["grow"])
    P.dma("sp", grow[1:2, :], gn, writes=["grow"])
    P.op("dve", "scalar_tensor_tensor", out=grow[:], in0=mrow[:, 1024:2048], scalar=1.0, in1=grow[:], op0=ALU.add, op1=ALU.mult,
         reads=[mk, "grow"], writes=["grow"])
    Gb = [P.sb([128, D], F32, f"Gb{v}") for v in range(2)]
    Sb = [P.sb([128, D], F32, f"Sb{v}") for v in range(2)]
    for v in range(2):
        bcast_rows(P, grow, "grow", sel, v, pmrot, Gb[v], f"Gb{v}")
        bcast_rows(P, mrow[:, 0:1024], mk, sel, v, pmrot, Sb[v], f"Sb{v}")
    rots = {
        "sq": Rot(P, "sq", 4, [128, D], F32),
        "st": Rot(P, "st", 8, [128, 4], F32),
        "hb": Rot(P, "hb", 4, [128, D], BF16),
        "ptr": Rot(P, "ptr", 2, [128, 1024], BF16, psum=True),
    }
    xrot = Rot(P, "xt", 5, [128, D], F32)
    hTrot = Rot(P, "hT", 3, [128, 8, 512], BF16)
    ptokrot = Rot(P, "ptok", 4, [128, 576], F32)
    tabrot = Rot(P, "tab", 4, [128, 160], F32)
    tmprot = Rot(P, "rtmp", 6, [128, 4, 128], F32)
    qkrot = Rot(P, "qk16", 4, [128, 384], BF16)
    vstrot = Rot(P, "vst", 4, [128, 256], BF16)
    ptArot = Rot(P, "ptA", 2, [128, 4, 128], BF16, psum=True)
    f16rot = Rot(P, "f16T", 2, [128, 4, 512], BF16)
    f32rot = Rot(P, "f32T", 2, [128, 2, 512], F32)
    pieces = [(0, 64, SC_DIFF), (64, 128, SC_SWA), (128, 320, 1.0), (320, 384, SC_RET), (384, 512, 1.0)]
    ei = 0
    groups = [(list(range(t, min(t + 4, nct))), 1) for t in range(0, nct, 4)] + [(list(range(t, min(t + 4, nlt))), 0) for t in range(0, nlt, 4)]
    def do_norm(gi, ti):
        tiles, v = groups[gi]
        src = x_ctx if v == 1 else x_lat
        tile = tiles[ti]
        xt, xk = xrot.next()
        P.dma("sp", xt[:], (src(tile) if callable(src) else src[tile * 128:(tile + 1) * 128, :]), reads=["g_x", "s_xc1"], writes=[xk])
        st, stk, sq, sqk = rms_rstd(P, xt[:], xk, rots)
        P.op("dve", "scalar_tensor_tensor", out=sq[:], in0=xt[:], scalar=st[:, 3:4], in1=Gb[v][:], op0=ALU.mult, op1=ALU.mult,
             reads=[xk, stk, f"Gb{v}"], writes=[sqk])
        hb, hbk = rots["hb"].next()
        P.op("pool", "tensor_tensor", out=hb[:], in0=sq[:], in1=Sb[v][:], op=ALU.add, reads=[sqk, f"Sb{v}"], writes=[hbk])
        pend[(gi, ti)] = (hb, hbk)

    pend = {}

    def do_norm_b(gi, ti):
        hb, hbk = pend.pop((gi, ti))
        hT, hTk = hts[gi]
        ptr, ptk = rots["ptr"].next()
        for k in range(8):
            P.op("pe", "transpose", out=ptr[:, k * 128:(k + 1) * 128], in_=hb[:, k * 128:(k + 1) * 128], identity=ident[:],
                 reads=[hbk, "ident"], writes=[ptk])
        P.op("act", "copy", out=hT[:, :, ti * 128:(ti + 1) * 128], in_=ptr[:, :].rearrange("p (k t) -> p k t", k=8), reads=[ptk], writes=[hTk])

    hts = {}
    st1 = {}

    def stage1(gi, ti):
        tiles, v = groups[gi]
        tile = tiles[ti]
        T0 = (tiles[0] * 128) if v == 1 else (M + tiles[0] * 128)
        hT, hTk = hts[gi]
        ptok, ptokk = ptokrot.next()
        pm, pk = pmrot.next()
        for k in range(8):
            P.op("pe", "matmul", out=pm[:, :], lhsT=hT[:, k, ti * 128:(ti + 1) * 128], rhs=Wt[:, k, 0:512], start=(k == 0), stop=(k == 7),
                 reads=[hTk, "Wt"], writes=[pk])
        for (c0, c1, sc) in pieces:
            P.op("act", "activation", out=ptok[:, c0:c1], in_=pm[:, c0:c1], func=AF.Copy, scale=float(sc), reads=[pk], writes=[ptokk])
        pm2, pk2 = pmrot.next()
        for k in range(8):
            P.op("pe", "matmul", out=pm2[:, 0:64], lhsT=hT[:, k, ti * 128:(ti + 1) * 128], rhs=Wt[:, k, 512:576], start=(k == 0), stop=(k == 7),
                 reads=[hTk, "Wt"], writes=[pk2])
        P.op("act", "copy", out=ptok[:, 512:576], in_=pm2[:, 0:64], reads=[pk2], writes=[ptokk])
        qk, qkk = qkrot.next()
        if v == 0:
            tab, tabk = tabrot.next()
            P.dma("sp", tab[:], tabs[tile * 128:(tile + 1) * 128, :], writes=[tabk])

            def views(t_, kind):
                if kind == "diff":
                    vv = t_[:, 0:256].rearrange("p (a c) -> p a c", a=2)[:, :, 0:64].rearrange("p a (h two d) -> p a h two d", h=2, two=2)
                    return vv[:, :, :, 0, :], vv[:, :, :, 1, :], [128, 2, 2, 16]
                if kind == "swa":
                    vv = t_[:, 0:256].rearrange("p (a c) -> p a c", a=2)[:, :, 64:128].rearrange("p a (two d) -> p a two d", two=2)
                    return vv[:, :, 0, :], vv[:, :, 1, :], [128, 2, 32]
                vv = t_[:, 256:384].rearrange("p (h two d) -> p h two d", h=2, two=2)
                return vv[:, :, 0, :], vv[:, :, 1, :], [128, 2, 32]
            for ki, (kind, hd, tc0) in enumerate((("diff", 16, 64), ("swa", 32, 0), ("ret", 32, 96))):
                x1, x2, shp = views(ptok, kind)
                o1, o2, _ = views(qk, kind)
                cosb = tab[:, tc0:tc0 + hd]
                sinb = tab[:, tc0 + hd:tc0 + 2 * hd]
                for _ in range(len(shp) - 2):
                    cosb = cosb.unsqueeze(1)
                    sinb = sinb.unsqueeze(1)
                cosb = cosb.to_broadcast(shp)
                sinb = sinb.to_broadcast(shp)
                tmp, tmpk = tmprot.next()
                nel = int(np.prod(shp[1:]))
                if len(shp) == 4:
                    T_ = [tmp[:, i, 0:nel].rearrange("p (a h d) -> p a h d", a=shp[1], h=shp[2]) for i in range(4)]
                else:
                    T_ = [tmp[:, i, 0:nel].rearrange("p (a d) -> p a d", a=shp[1]) for i in range(4)]
                e1 = "dve" if (ti + ki) % 2 == 0 else "pool"
                e2 = "pool" if (ti + ki) % 2 == 0 else "dve"
                P.op(e1, "tensor_tensor", out=T_[0], in0=x1, in1=cosb, op=ALU.mult, reads=[ptokk, tabk], writes=[tmpk + "a"])
                P.op(e1, "tensor_tensor", out=T_[1], in0=x2, in1=sinb, op=ALU.mult, reads=[ptokk, tabk], writes=[tmpk + "b"])
                P.op(e2, "tensor_tensor", out=T_[2], in0=x2, in1=cosb, op=ALU.mult, reads=[ptokk, tabk], writes=[tmpk + "c"])
                P.op(e2, "tensor_tensor", out=T_[3], in0=x1, in1=sinb, op=ALU.mult, reads=[ptokk, tabk], writes=[tmpk + "d"])
                P.op(e1, "tensor_tensor", out=o1, in0=T_[0], in1=T_[1], op=ALU.subtract, reads=[tmpk + "a", tmpk + "b"], writes=[qkk])
                P.op(e2, "tensor_tensor", out=o2, in0=T_[2], in1=T_[3], op=ALU.add, reads=[tmpk + "c", tmpk + "d"], writes=[qkk])
        else:
            P.op("dve", "tensor_copy", out=qk[:, :], in_=ptok[:, 0:384], reads=[ptokk], writes=[qkk])
        vst, vstk = vstrot.next()
        P.op("pool", "tensor_copy", out=vst[:, 64:256], in_=ptok[:, 384:576], reads=[ptokk], writes=[vstk])
        P.op("pool", "tensor_copy", out=vst[:, 0:64], in_=qk[:, 320:384], reads=[qkk], writes=[vstk])
        P.dma("sp", s_tok[T0 + ti * 128:T0 + (ti + 1) * 128, :], vst[:], reads=[vstk], writes=["s_tok"])
        st1[(gi, ti)] = (qk, qkk)

    def stage2(gi, ti, f16T, f16k):
        qk, qkk = st1.pop((gi, ti))
        ptA, ptAk = ptArot.next()
        P.op("pe", "transpose", out=ptA[:, 0, :], in_=qk[:, 0:128], identity=ident[:], reads=[qkk, "ident"], writes=[ptAk])
        P.op("pe", "transpose", out=ptA[:, 1, :], in_=qk[:, 128:256], identity=ident[:], reads=[qkk, "ident"], writes=[ptAk])
        P.op("pe", "transpose", out=ptA[0:64, 2, :], in_=qk[:, 256:320], identity=ident[:], reads=[qkk, "ident"], writes=[ptAk])
        P.op("pe", "transpose", out=ptA[0:64, 3, :], in_=qk[:, 320:384], identity=ident[:], reads=[qkk, "ident"], writes=[ptAk])
        P.op("dve", "tensor_copy", out=f16T[:, 0:2, ti * 128:(ti + 1) * 128], in_=ptA[:, 0:2, :], reads=[ptAk], writes=[f16k])
        P.op("dve", "tensor_copy", out=f16T[0:64, 2:4, ti * 128:(ti + 1) * 128], in_=ptA[0:64, 2:4, :], reads=[ptAk], writes=[f16k])

    hts[0] = hTrot.next()
    for ti in range(len(groups[0][0])):
        do_norm(0, ti)
    for ti in range(len(groups[0][0])):
        do_norm_b(0, ti)
    for gi, (tiles, v) in enumerate(groups):
        ntok = len(tiles) * 128
        T0 = (tiles[0] * 128) if v == 1 else (M + tiles[0] * 128)
        hT, hTk = hts[gi]
        nxt = gi + 1 if gi + 1 < len(groups) else None
        if nxt is not None:
            hts[nxt] = hTrot.next()
        f16T, f16k = f16rot.next()
        for ti in range(len(tiles)):
            stage1(gi, ti)
            if nxt is not None and ti < len(groups[nxt][0]):
                do_norm(nxt, ti)
            if ti > 0:
                stage2(gi, ti - 1, f16T, f16k)
        if nxt is not None:
            for ti in range(len(tiles), len(groups[nxt][0])):
                do_norm(nxt, ti)
        stage2(gi, len(tiles) - 1, f16T, f16k)
        if nxt is not None:
            for ti in range(len(groups[nxt][0])):
                do_norm_b(nxt, ti)
        P.dma("sp", s_qA[:, T0:T0 + ntok], f16T[:, 0, 0:ntok], reads=[f16k], writes=["s_qA"])
        P.dma("sp", s_kB[:, T0:T0 + ntok], f16T[:, 1, 0:ntok], reads=[f16k], writes=["s_kB"])
        P.dma("sp", s_rq[:, T0:T0 + ntok], f16T[0:64, 2, 0:ntok], reads=[f16k], writes=["s_rq"])
        P.dma("sp", s_rk[:, T0:T0 + ntok], f16T[0:64, 3, 0:ntok], reads=[f16k], writes=["s_rk"])
        f32T, f32k = f32rot.next()
        for fb, (c0, c1) in enumerate(((0, 128), (128, 192))):
            w = c1 - c0
            pm, pk = pmrot.next()
            for k in range(8):
                P.op("pe", "matmul", out=pm[0:w, 0:ntok], lhsT=Wf[:, k, c0:c1], rhs=hT[:, k, 0:ntok], start=(k == 0), stop=(k == 7), reads=[hTk, "Wf"], writes=[pk])
            P.op("act", "copy", out=f32T[0:w, fb, 0:ntok], in_=pm[0:w, 0:ntok], reads=[pk], writes=[f32k])
        P.dma("sp", s_f32[0:128, T0:T0 + ntok], f32T[:, 0, 0:ntok], reads=[f32k], writes=["s_f32"])
        P.dma("sp", s_f32[128:192, T0:T0 + ntok], f32T[0:64, 1, 0:ntok], reads=[f32k], writes=["s_f32"])
        del hts[gi]
    P.end_phase()


def lam_scalar_col(P, lamv_d, lam_init, pmrot):
    lv = P.sb([1, 128], F32, "lv")
    P.dma("sp", lv[:], lamv_d, writes=["lv"])
    pr = P.sb([1, 64], F32, "lvpr")
    lvv = lv[:, :].rearrange("o (a b d) -> o a b d", a=2, b=2)
    P.op("dve", "tensor_tensor", out=pr[:, :].rearrange("o (a d) -> o a d", a=2), in0=lvv[:, :, 0, :], in1=lvv[:, :, 1, :], op=ALU.mult, reads=["lv"], writes=["lvpr"])
    sm = P.sb([1, 4], F32, "lvsm")
    P.op("dve", "tensor_reduce", out=sm[:, 0:2], in_=pr[:, :].rearrange("o (a d) -> o a d", a=2), axis=AX.X, op=ALU.add, reads=["lvpr"], writes=["lvsm"])
    P.op("act", "activation", out=sm[:, 0:2], in_=sm[:, 0:2], func=AF.Exp, reads=["lvsm"], writes=["lvsm"])
    P.op("dve", "tensor_tensor", out=sm[:, 2:3], in0=sm[:, 0:1], in1=sm[:, 1:2], op=ALU.subtract, reads=["lvsm"], writes=["lvsm"])
    P.op("dve", "tensor_scalar", out=sm[:, 3:4], in0=sm[:, 2:3], scalar1=float(lam_init), scalar2=-1.0, op0=ALU.add, op1=ALU.mult, reads=["lvsm"], writes=["lvsm"])
    P.op("dve", "tensor_scalar", out=sm[:, 2:3], in0=sm[:, 2:3], scalar1=float(lam_init), scalar2=None, op0=ALU.add, reads=["lvsm"], writes=["lvsm"])
    ones1 = P.sb([1, 128], F32, "ones1")
    P.op("pool", "memset", ap=ones1[:], constant=1.0, writes=["ones1"])
    pm, pk = pmrot.next()
    P.op("pe", "matmul", out=pm[:, 0:2], lhsT=ones1[:, :], rhs=sm[:, 2:4], start=True, stop=True, reads=["ones1", "lvsm"], writes=[pk])
    lamc = P.sb([128, 2], F32, "lamc")
    P.op("dve", "tensor_copy", out=lamc[:], in_=pm[:, 0:2], reads=[pk], writes=["lamc"])
    return lamc


def headnorm_T(P, o32, ok, ncol, ones64, eprot, gcol, gkey, mul, out_ap, outk, tmprot, eng_a="dve", eng_b="pool"):
    sq, sqk = tmprot.next()
    P.op(eng_b, "tensor_tensor", out=sq[0:64, 0:ncol], in0=o32, in1=o32, op=ALU.mult, reads=[ok], writes=[sqk])
    pm, pk = eprot.next()
    P.op("pe", "matmul", out=pm[0:64, 0:ncol], lhsT=ones64[0:64, 0:64], rhs=sq[0:64, 0:ncol], start=True, stop=True, reads=[sqk, "ones64"], writes=[pk])
    P.op(eng_a, "tensor_scalar", out=sq[0:64, 0:ncol], in0=pm[0:64, 0:ncol], scalar1=1.0 / 64, scalar2=EPS, op0=ALU.mult, op1=ALU.add, reads=[pk], writes=[sqk])
    P.op("act", "activation", out=sq[0:64, 0:ncol], in_=sq[0:64, 0:ncol], func=AF.Sqrt, reads=[sqk], writes=[sqk])
    P.op(eng_a, "reciprocal", out=sq[0:64, 0:ncol], in_=sq[0:64, 0:ncol], reads=[sqk], writes=[sqk])
    P.op(eng_a, "tensor_tensor", out=sq[0:64, 0:ncol], in0=sq[0:64, 0:ncol], in1=o32, op=ALU.mult, reads=[sqk, ok], writes=[sqk])
    P.op(eng_a, "tensor_scalar", out=out_ap, in0=sq[0:64, 0:ncol], scalar1=gcol, scalar2=float(mul), op0=ALU.mult, op1=ALU.mult, reads=[sqk, gkey], writes=[outk])


def emit_diff(P, N, M, need_ctx, lam_init, qT_d, kT_d, v_d, lamv_d, gn_d, oT_d):
    T = N + M
    NKB = T // 128

    qT = P.sb([64, T], BF16, "qT")
    kT = P.sb([64, T], BF16, "kT")
    V1 = P.sb([128, NKB, 128], BF16, "V1")
    nchunk = 4
    cw = T // nchunk
    for i in range(nchunk):
        P.dma("sp", qT[:, i * cw:(i + 1) * cw], qT_d[:, i * cw:(i + 1) * cw], writes=["qT"])
        P.dma("sp", kT[:, i * cw:(i + 1) * cw], kT_d[:, i * cw:(i + 1) * cw], writes=["kT"])
    vv = v_d.rearrange("(kb p) d -> p kb d", p=128)
    step = max(1, NKB // 8)
    for k0 in range(0, NKB, step):
        k1 = min(NKB, k0 + step)
        P.dma("sp", V1[:, k0:k1, 0:64], vv[:, k0:k1, :], writes=["V1"])
    P.op("pool", "memset", ap=V1[:, :, 64:65], constant=1.0, writes=["V1"])
    P.op("pool", "memset", ap=V1[:, :, 65:128], constant=0.0, writes=["V1"])
    gcol = P.sb([64, 1], F32, "gcol")
    P.dma("sp", gcol[:], gn_d, writes=["gcol"])
    ones64 = P.sb([128, 64], F32, "ones64")
    P.op("pool", "memset", ap=ones64[:], constant=1.0, writes=["ones64"])
    srot = Rot(P, "S", 3, [128, 512], F32, psum=True)
    eprot = Rot(P, "ep", 1, [128, 512], F32, psum=True)
    accrot = [Rot(P, f"acc{m}_", 2, [128, 512], F32, psum=True) for m in range(2)]
    lamc = lam_scalar_col(P, lamv_d, lam_init, eprot)
    ptrot = Rot(P, "PT", 6, [128, 512], BF16)
    rlrot = Rot(P, "rl", 2, [128, 2, 512], F32)
    bcrot = Rot(P, "bc", 2, [64, 2, 512], F32)
    t32rot = Rot(P, "t32", 3, [64, 512], F32)
    orot = Rot(P, "ob", 2, [64, 512], BF16)

    def epilogue(acc, q0, nq):
        rl, rlk = rlrot.next()
        for m in range(2):
            P.op("dve", "reciprocal", out=rl[64:65, m, 0:nq], in_=acc[m][0][64:65, 0:nq], reads=[acc[m][1]], writes=[rlk])
        P.op("dve", "tensor_scalar", out=rl[64:65, 1, 0:nq], in0=rl[64:65, 1, 0:nq], scalar1=lamc[64:65, 1:2], scalar2=None, op0=ALU.mult, reads=[rlk, "lamc"], writes=[rlk])
        bc, bck = bcrot.next()
        for m in range(2):
            pm, pk = eprot.next()
            P.op("pe", "matmul", out=pm[0:64, 0:nq], lhsT=ones64[64:65, 0:64], rhs=rl[64:65, m, 0:nq], start=True, stop=True, reads=[rlk, "ones64"], writes=[pk])
            P.op("dve", "tensor_copy", out=bc[:, m, 0:nq], in_=pm[0:64, 0:nq], reads=[pk], writes=[bck])
        t0, t0k = t32rot.next()
        t1, t1k = t32rot.next()
        P.op("dve", "tensor_tensor", out=t0[:, 0:nq], in0=acc[0][0][0:64, 0:nq], in1=bc[:, 0, 0:nq], op=ALU.mult, reads=[acc[0][1], bck], writes=[t0k])
        P.op("dve", "tensor_tensor", out=t1[:, 0:nq], in0=acc[1][0][0:64, 0:nq], in1=bc[:, 1, 0:nq], op=ALU.mult, reads=[acc[1][1], bck], writes=[t1k])
        P.op("pool", "tensor_tensor", out=t0[:, 0:nq], in0=t0[:, 0:nq], in1=t1[:, 0:nq], op=ALU.add, reads=[t0k, t1k], writes=[t0k])
        ob, obk = orot.next()
        headnorm_T(P, t0[:, 0:nq], t0k, nq, ones64, eprot, gcol[:, 0:1], "gcol", 1.0 - lam_init, ob[:, 0:nq], obk, t32rot)
        P.dma("sp", oT_d[:, q0:q0 + nq], ob[:, 0:nq], reads=[obk])
    QG = 2
    sgroups = []
    if need_ctx:
        sgroups.append(([(0, M)], 0, M // 128))
    lat = [(g0, min(512, T - g0)) for g0 in range(M, T, 512)]
    for i in range(0, len(lat), QG):
        sgroups.append((lat[i:i + QG], 0, NKB))
    LOOK = 1
    for (qgs, kb0, kb1) in sgroups:
        ng = len(qgs)
        acc = [[accrot[m].next() for m in range(2)] for _ in range(ng)]
        steps = [(kb, m) for kb in range(kb0, kb1) for m in range(2)]
        pts = {}

        def issue_s(i):
            kb, m = steps[i]
            for gi, (q0, nq) in enumerate(qgs):
                S, Sk = srot.next()
                P.op("pe", "matmul", out=S[:, 0:nq], lhsT=kT[m * 32:(m + 1) * 32, kb * 128:(kb + 1) * 128], rhs=qT[m * 32:(m + 1) * 32, q0:q0 + nq],
                     start=True, stop=True, reads=["kT", "qT"], writes=[Sk])
                PT, PTk = ptrot.next()
                P.op("act", "activation", out=PT[:, 0:nq], in_=S[:, 0:nq], func=AF.Exp, reads=[Sk], writes=[PTk])
                pts[(i, gi)] = (PT, PTk)

        def issue_pv(i):
            kb, m = steps[i]
            for gi, (q0, nq) in enumerate(qgs):
                PT, PTk = pts.pop((i, gi))
                P.op("pe", "matmul", out=acc[gi][m][0][:, 0:nq], lhsT=V1[:, kb, :], rhs=PT[:, 0:nq], start=(kb == kb0), stop=(kb == kb1 - 1),
                     reads=["V1", PTk], writes=[acc[gi][m][1]])
        for i in range(len(steps) + LOOK):
            if i < len(steps):
                issue_s(i)
            if i - LOOK >= 0:
                issue_pv(i - LOOK)
        for gi, (q0, nq) in enumerate(qgs):
            epilogue(acc[gi], q0, nq)
    P.end_phase()


def swa_masks():
    b = np.arange(128)[:, None]
    a = np.arange(128)[None, :]
    m = np.concatenate([(b <= a), np.ones((128, 128), bool), (a <= b)], 1)
    return m.astype(np.float32).astype(ml_dtypes.bfloat16)


def emit_swa(P, N, M, need_ctx, qT_d, kT_d, v_d, sink_d, m3_d, oT_d):
    T = N + M
    NKB = T // 128
    MB = M // 128
    nb = N // 128

    qT = P.sb([64, T], BF16, "qT")
    kT = P.sb([64, T], BF16, "kT")
    V1 = P.sb([128, NKB, 65], BF16, "V1")
    nchunk = 4
    cw = T // nchunk
    for i in range(nchunk):
        P.dma("sp", qT[:, i * cw:(i + 1) * cw], qT_d[:, i * cw:(i + 1) * cw], writes=["qT"])
        P.dma("sp", kT[:, i * cw:(i + 1) * cw], kT_d[:, i * cw:(i + 1) * cw], writes=["kT"])
    vv = v_d.rearrange("(kb p) d -> p kb d", p=128)
    step = max(1, NKB // 8)
    for k0 in range(0, NKB, step):
        k1 = min(NKB, k0 + step)
        P.dma("sp", V1[:, k0:k1, 0:64], vv[:, k0:k1, :], writes=["V1"])
    P.op("pool", "memset", ap=V1[:, :, 64:65], constant=1.0, writes=["V1"])
    m3 = P.sb([128, 384], BF16, "m3")
    P.dma("sp", m3[:], m3_d, writes=["m3"])
    es = P.sb([128, 1], F32, "es")
    P.dma("sp", es[:], sink_d, writes=["es"])
    P.op("act", "activation", out=es[:], in_=es[:], func=AF.Exp, reads=["es"], writes=["es"])
    ones64 = P.sb([128, 64], F32, "ones64")
    P.op("pool", "memset", ap=ones64[:], constant=1.0, writes=["ones64"])
    accrot = Rot(P, "acc", 2, [128, 512], F32, psum=True)
    srot = Rot(P, "S", 3, [128, 512], F32, psum=True)
    eprot = Rot(P, "ep", 2, [128, 512], F32, psum=True)
    ptrot = Rot(P, "PT", 4, [128, 512], BF16)
    rlrot = Rot(P, "rl", 2, [128, 512], F32)
    bcrot = Rot(P, "bc", 2, [64, 512], F32)
    orot = Rot(P, "ob", 2, [64, 512], BF16)

    qgroups = []
    if need_ctx:
        qgroups.append((0, M, None))
    for s0 in range(0, nb, 4):
        qgroups.append((M + s0 * 128, min(4, nb - s0) * 128, s0))
    ei = 0
    for (q0, nq, s0) in qgroups:
        acc, acck = accrot.next()
        items = [(kb, 0, nq, None) for kb in range(MB)]
        if s0 is not None:
            ns = nq // 128
            for r in range(s0 - 1, s0 + ns + 1):
                if r < 0 or r >= nb:
                    continue
                sa = max(r - 1, s0)
                sb_ = min(r + 1, s0 + ns - 1)
                items.append((MB + r, (sa - s0) * 128, (sb_ - s0 + 1) * 128, (sa - (r - 1)) * 128))
        for ii, (kb, c0, c1, mc) in enumerate(items):
            w = c1 - c0
            S, Sk = srot.next()
            P.op("pe", "matmul", out=S[:, 0:w], lhsT=kT[:, kb * 128:(kb + 1) * 128], rhs=qT[:, q0 + c0:q0 + c1], start=True, stop=True,
                 reads=["kT", "qT"], writes=[Sk])
            PT, PTk = ptrot.next()
            P.op("act", "activation", out=PT[:, 0:w], in_=S[:, 0:w], func=AF.Exp, reads=[Sk], writes=[PTk])
            if mc is not None:
                eng = "dve" if ei % 2 == 0 else "pool"
                ei += 1
                P.op(eng, "tensor_tensor", out=PT[:, 0:w], in0=PT[:, 0:w], in1=m3[:, mc:mc + w], op=ALU.mult, reads=[PTk, "m3"], writes=[PTk])
            P.op("pe", "matmul", out=acc[0:65, c0:c1], lhsT=V1[:, kb, :], rhs=PT[:, 0:w], start=(ii == 0), stop=(ii == len(items) - 1),
                 reads=["V1", PTk], writes=[acck])
        rl, rlk = rlrot.next()
        P.op("dve", "tensor_scalar", out=rl[64:65, 0:nq], in0=acc[64:65, 0:nq], scalar1=es[64:65, 0:1], scalar2=None, op0=ALU.add, reads=[acck, "es"], writes=[rlk])
        P.op("dve", "reciprocal", out=rl[64:65, 0:nq], in_=rl[64:65, 0:nq], reads=[rlk], writes=[rlk])
        pm, pk = eprot.next()
        P.op("pe", "matmul", out=pm[0:64, 0:nq], lhsT=ones64[64:65, 0:64], rhs=rl[64:65, 0:nq], start=True, stop=True, reads=[rlk, "ones64"], writes=[pk])
        bc, bck = bcrot.next()
        P.op("act", "copy", out=bc[:, 0:nq], in_=pm[0:64, 0:nq], reads=[pk], writes=[bck])
        ob, obk = orot.next()
        P.op("dve", "tensor_tensor", out=ob[:, 0:nq], in0=acc[0:64, 0:nq], in1=bc[:, 0:nq], op=ALU.mult, reads=[acck, bck], writes=[obk])
        P.dma("sp", oT_d[:, q0:q0 + nq], ob[:, 0:nq], reads=[obk])
    P.end_phase()


def ret_consts():
    m = np.arange(128)[:, None].astype(np.float32)
    n = np.arange(128)[None, :].astype(np.float32)
    c = np.zeros((128, 6, 128), np.float32)
    c[:, 0] = np.maximum(n - m, 0)
    c[:, 1] = (n >= m)
    c[:, 2] = np.maximum(m - n, 0)
    c[:, 3] = (m >= n)
    c[:, 4] = n + 1
    c[:, 5] = 128 - n
    z = np.zeros((128, 2), np.float32)
    z[:, 0] = 127 - np.arange(128)
    z[:, 1] = np.arange(128)
    return c, z


def emit_ret(P, N, M, need_ctx, qT_d, kT_d, kt_d, v_d, gT_d, lg_d, gn_d, rc_d, rz_d, oT_d):
    T = N + M
    NC = T // 128
    MB = M // 128

    ktok = P.sb([128, NC, 64], BF16, "ktok")
    V = P.sb([128, NC, 64], BF16, "V")
    vv = v_d.rearrange("(kb p) d -> p kb d", p=128)
    kv = kt_d.rearrange("(kb p) d -> p kb d", p=128)
    step = max(1, NC // 8)
    for k0 in range(0, NC, step):
        k1 = min(NC, k0 + step)
        P.dma("sp", V[:, k0:k1, :], vv[:, k0:k1, :], writes=["V"])
        P.dma("sp", ktok[:, k0:k1, :], kv[:, k0:k1, :], writes=["ktok"])
    rc = P.sb([128, 6, 128], F32, "rc")
    P.dma("sp", rc[:], rc_d, writes=["rc"])
    rz = P.sb([128, 2], F32, "rz")
    P.dma("sp", rz[:], rz_d, writes=["rz"])
    gcol = P.sb([64, 1], F32, "gcol")
    P.dma("sp", gcol[:], gn_d, writes=["gcol"])
    lgc = P.sb([128, 2], F32, "lgc")
    P.dma("sp", lgc[:], lg_d, writes=["lgc"])
    P.op("act", "activation", out=lgc[:], in_=lgc[:], func=AF.Exp, scale=-1.0, reads=["lgc"], writes=["lgc"])
    P.op("dve", "tensor_scalar", out=lgc[:], in0=lgc[:], scalar1=1.0, scalar2=None, op0=ALU.add, reads=["lgc"], writes=["lgc"])
    P.op("act", "activation", out=lgc[:], in_=lgc[:], func=AF.Ln, reads=["lgc"], writes=["lgc"])
    P.op("dve", "tensor_scalar", out=lgc[:], in0=lgc[:], scalar1=-1.0, scalar2=None, op0=ALU.mult, reads=["lgc"], writes=["lgc"])
    DcT = P.sb([128, 128], F32, "DcT")
    E2 = P.sb([128, 128], F32, "E2")
    P.op("act", "activation", out=DcT[:], in_=rc[:, 0, :], func=AF.Exp, scale=lgc[:, 0:1], reads=["rc", "lgc"], writes=["DcT"])
    P.op("act", "activation", out=E2[:], in_=rc[:, 2, :], func=AF.Exp, scale=lgc[:, 1:2], reads=["rc", "lgc"], writes=["E2"])
    P.op("dve", "tensor_tensor", out=DcT[:], in0=DcT[:], in1=rc[:, 1, :], op=ALU.mult, reads=["DcT", "rc"], writes=["DcT"])
    P.op("dve", "tensor_tensor", out=E2[:], in0=E2[:], in1=rc[:, 3, :], op=ALU.mult, reads=["E2", "rc"], writes=["E2"])
    P.op("dve", "tensor_tensor", out=DcT[:], in0=DcT[:], in1=E2[:], op=ALU.add, reads=["DcT", "E2"], writes=["DcT"])
    xi = P.sb([64, 2, 128], F32, "xi")
    P.op("act", "activation", out=xi[:, 0, :], in_=rc[0:64, 4, :], func=AF.Exp, scale=lgc[0:64, 0:1], reads=["rc", "lgc"], writes=["xi"])
    P.op("act", "activation", out=xi[:, 1, :], in_=rc[0:64, 5, :], func=AF.Exp, scale=lgc[0:64, 1:2], reads=["rc", "lgc"], writes=["xi"])
    zc = P.sb([128, 2], F32, "zc")
    P.op("act", "activation", out=zc[:, 0:1], in_=rz[:, 0:1], func=AF.Exp, scale=lgc[:, 0:1], reads=["rz", "lgc"], writes=["zc"])
    P.op("act", "activation", out=zc[:, 1:2], in_=rz[:, 1:2], func=AF.Exp, scale=lgc[:, 1:2], reads=["rz", "lgc"], writes=["zc"])
    gc = P.sb([64, 2], F32, "gc")
    P.op("act", "activation", out=gc[:], in_=lgc[0:64, :], func=AF.Exp, scale=128.0, reads=["lgc"], writes=["gc"])
    ones64 = P.sb([128, 64], F32, "ones64")
    P.op("pool", "memset", ap=ones64[:], constant=1.0, writes=["ones64"])

    U = [P.sb([64, NC, 64], F32, f"U{d}") for d in range(2)]
    S16 = [P.sb([64, NC, 64], BF16, f"S16_{d}") for d in range(2)]
    urot = Rot(P, "ups", 2, [128, 512], F32, psum=True)
    kzrot = Rot(P, "kz", 4, [128, 64], BF16)
    for d in range(2):
        for c0 in range(0, NC, 8):
            c1 = min(NC, c0 + 8)
            pm, pk = urot.next()
            for c in range(c0, c1):
                kz, kzk = kzrot.next()
                P.op("dve" if d == 0 else "pool", "tensor_scalar", out=kz[:], in0=ktok[:, c, :], scalar1=zc[:, d:d + 1], scalar2=None, op0=ALU.mult,
                     reads=["ktok", "zc"], writes=[kzk])
                P.op("pe", "matmul", out=pm[0:64, (c - c0) * 64:(c - c0 + 1) * 64], lhsT=kz[:], rhs=V[:, c, :], start=True, stop=True,
                     reads=[kzk, "V"], writes=[pk])
            P.op("act", "copy", out=U[d][:, c0:c1, :], in_=pm[0:64, 0:(c1 - c0) * 64].rearrange("p (c e) -> p c e", e=64), reads=[pk], writes=[f"U{d}"])
    order = [list(range(NC)), list(range(MB - 1, -1, -1)) + list(range(NC - 1, MB - 1, -1))]
    prev = [{}, {}]
    for d in range(2):
        eng = "dve"
        od = order[d]
        for i in range(1, NC):
            prev[d][od[i]] = od[i - 1]
            P.op(eng, "scalar_tensor_tensor", out=U[d][:, od[i], :], in0=U[d][:, od[i - 1], :], scalar=gc[:, d:d + 1], in1=U[d][:, od[i], :],
                 op0=ALU.mult, op1=ALU.add, reads=[f"U{d}", "gc"], writes=[f"U{d}"])
        P.op(eng, "tensor_copy", out=S16[d][:], in_=U[d][:], reads=[f"U{d}"], writes=[f"S16_{d}"])
    accrot = Rot(P, "acc", 2, [128, 512], F32, psum=True)
    srot = Rot(P, "S", 2, [128, 512], F32, psum=True)
    eprot = Rot(P, "ep", 2, [128, 512], F32, psum=True)
    qrot = Rot(P, "qg", 2, [64, 512], BF16)
    krot = Rot(P, "kg", 2, [64, 512], BF16)
    grot = Rot(P, "gg", 2, [64, 512], F32)
    amrot = Rot(P, "am", 3, [128, 128], BF16)
    qxrot = Rot(P, "qx", 3, [64, 2, 128], BF16)
    t32rot = Rot(P, "t32", 3, [64, 512], F32)
    orot = Rot(P, "ob", 2, [64, 512], BF16)
    groups = []
    if need_ctx:
        groups.append(list(range(0, MB)))
    for c0 in range(MB, NC, 4):
        groups.append(list(range(c0, min(NC, c0 + 4))))
    for grp in groups:
        t0_ = grp[0] * 128
        nq = len(grp) * 128
        qg, qgk = qrot.next()
        kg, kgk = krot.next()
        gg, ggk = grot.next()
        P.dma("sp", qg[:, 0:nq], qT_d[:, t0_:t0_ + nq], writes=[qgk])
        P.dma("sp", kg[:, 0:nq], kT_d[:, t0_:t0_ + nq], writes=[kgk])
        P.dma("sp", gg[:, 0:nq], gT_d[:, t0_:t0_ + nq], writes=[ggk])
        P.op("act", "activation", out=gg[:, 0:nq], in_=gg[:, 0:nq], func=AF.Silu, reads=[ggk], writes=[ggk])
        acc, acck = accrot.next()
        for ci, c in enumerate(grp):
            cs = slice(ci * 128, (ci + 1) * 128)
            S, Sk = srot.next()
            P.op("pe", "matmul", out=S[:, 0:128], lhsT=kg[:, cs], rhs=qg[:, cs], start=True, stop=True, reads=[kgk, qgk], writes=[Sk])
            am, amk = amrot.next()
            P.op("dve", "tensor_tensor", out=am[:], in0=S[:, 0:128], in1=DcT[:], op=ALU.mult, reads=[Sk, "DcT"], writes=[amk])
            qx, qxk = qxrot.next()
            P.op("pool", "tensor_tensor", out=qx[:], in0=qg[:, cs].unsqueeze(1).to_broadcast([64, 2, 128]), in1=xi[:], op=ALU.mult, reads=[qgk, "xi"], writes=[qxk])
            mms = [(V[:, c, :], am[:], ["V", amk])]
            for d in range(2):
                if c in prev[d]:
                    mms.append((S16[d][:, prev[d][c], :], qx[:, d, :], [f"S16_{d}", qxk]))
            for mi, (l_, r_, rk_) in enumerate(mms):
                P.op("pe", "matmul", out=acc[0:64, cs], lhsT=l_, rhs=r_, start=(mi == 0), stop=(mi == len(mms) - 1), reads=rk_, writes=[acck])
        y, yk = t32rot.next()
        P.op("act", "copy", out=y[:, 0:nq], in_=acc[0:64, 0:nq], reads=[acck], writes=[yk])
        yn, ynk = t32rot.next()
        headnorm_T(P, y[:, 0:nq], yk, nq, ones64, eprot, gcol[:, 0:1], "gcol", 1.0, yn[:, 0:nq], ynk, t32rot)
        ob, obk = orot.next()
        P.op("pool", "tensor_tensor", out=ob[:, 0:nq], in0=yn[:, 0:nq], in1=gg[:, 0:nq], op=ALU.mult, reads=[ynk, ggk], writes=[obk])
        P.dma("sp", oT_d[:, t0_:t0_ + nq], ob[:, 0:nq], reads=[obk])
    P.end_phase()


def _bk(name, c0, c1):
    return [f"{name}{b}" for b in range(c0 // 512, (c1 - 1) // 512 + 1)]


def emit_lru(P, N, M, lx_d, ly_d, cw_d, cb_d, wa_d, wx_d, ba_d, bx_d, lam_d, i2_d, oT_d):
    T = N + M

    XX = P.sb([128, T], F32, "XX")
    UU = P.sb([128, T], F32, "UU")
    CH = 2048
    for c0 in range(0, T, CH):
        c1 = min(T, c0 + CH)
        P.dma("sp", XX[0:64, c0:c1], lx_d[:, c0:c1], writes=_bk("XX", c0, c1))
        P.dma("sp", XX[64:128, c0:c1], lx_d[:, c0:c1], writes=_bk("XX", c0, c1))
    small = {}
    for nm, d_, shp in (("cw", cw_d, [128, 4]), ("cb", cb_d, [128, 1]), ("wa", wa_d, [64, 128]), ("wx", wx_d, [64, 128]),
                        ("ba", ba_d, [128, 1]), ("bx", bx_d, [128, 1]), ("lam", lam_d, [128, 1]), ("i2", i2_d, [128, 64])):
        t_ = P.sb(shp, F32, nm)
        P.dma("sp", t_[:], d_, writes=[nm])
        small[nm] = t_
    cw, cb, wa, wx, ba, bx, lam, i2 = (small[k] for k in ("cw", "cb", "wa", "wx", "ba", "bx", "lam", "i2"))
    c8 = P.sb([128, 1], F32, "c8")
    P.op("act", "activation", out=c8[:], in_=lam[:], func=AF.Exp, scale=-1.0, reads=["lam"], writes=["c8"])
    P.op("dve", "tensor_scalar", out=c8[:], in0=c8[:], scalar1=1.0, scalar2=None, op0=ALU.add, reads=["c8"], writes=["c8"])
    P.op("act", "activation", out=c8[:], in_=c8[:], func=AF.Ln, reads=["c8"], writes=["c8"])
    P.op("dve", "tensor_scalar", out=c8[:], in0=c8[:], scalar1=-8.0, scalar2=None, op0=ALU.mult, reads=["c8"], writes=["c8"])
    for (s0, s1) in ((0, M), (M, T)):
        for c0 in range(s0, s1, CH):
            c1 = min(s1, c0 + CH)
            P.op("dve", "tensor_scalar", out=UU[:, c0:c1], in0=XX[:, c0:c1], scalar1=cw[:, 1:2], scalar2=cb[:, 0:1], op0=ALU.mult, op1=ALU.add,
                 reads=_bk("XX", c0, c1) + ["cw", "cb"], writes=_bk("UU", c0, c1))
            for (tap, sh) in ((0, -1), (2, 1), (3, 2)):
                o0, o1 = max(c0, s0 - sh), min(c1, s1 - sh)
                if o1 <= o0:
                    continue
                P.op("dve", "scalar_tensor_tensor", out=UU[:, o0:o1], in0=XX[:, o0 + sh:o1 + sh], scalar=cw[:, tap:tap + 1], in1=UU[:, o0:o1],
                     op0=ALU.mult, op1=ALU.add, reads=_bk("XX", o0 + sh, o1 + sh) + _bk("UU", o0, o1) + ["cw"], writes=_bk("UU", o0, o1))
    grot = Rot(P, "gps", 4, [128, 512], F32, psum=True)
    rsrot = Rot(P, "rs", 2, [128, 512], F32)
    isrot = Rot(P, "is", 2, [128, 512], F32)
    t1rot = Rot(P, "t1", 2, [128, 512], F32)
    for c0 in range(0, T, 512):
        c1 = min(T, c0 + 512)
        w = c1 - c0
        uk = _bk("UU", c0, c1)
        xk = _bk("XX", c0, c1)
        pr, prk = grot.next()
        pi, pik = grot.next()
        P.op("pe", "matmul", out=pr[:, 0:w], lhsT=wa[:, :], rhs=UU[0:64, c0:c1], start=True, stop=True, reads=uk + ["wa"], writes=[prk])
        P.op("pe", "matmul", out=pi[:, 0:w], lhsT=wx[:, :], rhs=UU[0:64, c0:c1], start=True, stop=True, reads=uk + ["wx"], writes=[pik])
        rs, rsk = rsrot.next()
        is_, isk = isrot.next()
        P.op("act", "activation", out=rs[:, 0:w], in_=pr[:, 0:w], func=AF.Sigmoid, bias=ba[:, 0:1], reads=[prk, "ba"], writes=[rsk])
        P.op("act", "activation", out=is_[:, 0:w], in_=pi[:, 0:w], func=AF.Sigmoid, bias=bx[:, 0:1], reads=[pik, "bx"], writes=[isk])
        P.op("act", "activation", out=XX[:, c0:c1], in_=rs[:, 0:w], func=AF.Exp, scale=c8[:, 0:1], reads=[rsk, "c8"], writes=xk)
        t1, t1k = t1rot.next()
        P.op("pool", "tensor_tensor", out=t1[:, 0:w], in0=XX[:, c0:c1], in1=XX[:, c0:c1], op=ALU.mult, reads=xk, writes=[t1k])
        P.op("pool", "tensor_scalar", out=t1[:, 0:w], in0=t1[:, 0:w], scalar1=-1.0, scalar2=1.0, op0=ALU.mult, op1=ALU.add, reads=[t1k], writes=[t1k])
        P.op("act", "activation", out=t1[:, 0:w], in_=t1[:, 0:w], func=AF.Sqrt, reads=[t1k], writes=[t1k])
        P.op("dve", "tensor_tensor", out=is_[:, 0:w], in0=is_[:, 0:w], in1=UU[:, c0:c1], op=ALU.mult, reads=[isk] + uk, writes=[isk])
        P.op("dve", "tensor_tensor", out=UU[:, c0:c1], in0=is_[:, 0:w], in1=t1[:, 0:w], op=ALU.mult, reads=[isk, t1k], writes=uk)
    prev = None
    for c0 in range(0, T, CH):
        c1 = min(T, c0 + CH)
        init = 0.0 if prev is None else UU[0:64, c0 - 1:c0]
        P.op("dve", "tensor_tensor_scan", out=UU[0:64, c0:c1], data0=XX[0:64, c0:c1], data1=UU[0:64, c0:c1], initial=init, op0=ALU.mult, op1=ALU.add,
             reads=_bk("XX", c0, c1) + _bk("UU", max(c0 - 1, 0), c1) + ["scanf"], writes=_bk("UU", c0, c1) + ["scanf"])
        prev = c0
    chunks = [(c0, min(M, c0 + CH)) for c0 in range(0, M, CH)][::-1] + [(c0, min(T, c0 + CH)) for c0 in range(M, T, CH)][::-1]
    prev_lo = None
    for (c0, c1) in chunks:
        init = 0.0 if prev_lo is None else UU[64:128, prev_lo:prev_lo + 1]
        rk = [] if prev_lo is None else _bk("UU", prev_lo, prev_lo + 1)
        P.op("dve", "tensor_tensor_scan", out=UU[64:128, c0:c1][:, ::-1], data0=XX[64:128, c0:c1][:, ::-1], data1=UU[64:128, c0:c1][:, ::-1], initial=init,
             op0=ALU.mult, op1=ALU.add, reads=_bk("XX", c0, c1) + _bk("UU", c0, c1) + rk + ["scanb"], writes=_bk("UU", c0, c1) + ["scanb"])
        prev_lo = c0
    hrot = Rot(P, "hps", 2, [128, 512], F32, psum=True)
    yrot = Rot(P, "yy", 2, [64, 512], F32)
    zrot = Rot(P, "zz", 2, [64, 512], F32)
    orot = Rot(P, "ob", 2, [64, 512], BF16)
    for c0 in range(0, T, 512):
        c1 = min(T, c0 + 512)
        w = c1 - c0
        hp, hpk = hrot.next()
        P.op("pe", "matmul", out=hp[0:64, 0:w], lhsT=i2[:, :], rhs=UU[:, c0:c1], start=True, stop=True, reads=_bk("UU", c0, c1) + ["i2"], writes=[hpk])
        yy, yk = yrot.next()
        zz, zk = zrot.next()
        P.dma("sp", yy[:, 0:w], ly_d[:, c0:c1], writes=[yk])
        P.op("pool", "tensor_tensor", out=zz[:, 0:w], in0=yy[:, 0:w], in1=yy[:, 0:w], op=ALU.mult, reads=[yk], writes=[zk])
        P.op("pool", "tensor_scalar", out=zz[:, 0:w], in0=zz[:, 0:w], scalar1=0.044715, scalar2=1.0, op0=ALU.mult, op1=ALU.add, reads=[zk], writes=[zk])
        P.op("pool", "tensor_tensor", out=zz[:, 0:w], in0=zz[:, 0:w], in1=yy[:, 0:w], op=ALU.mult, reads=[zk, yk], writes=[zk])
        P.op("act", "activation", out=zz[:, 0:w], in_=zz[:, 0:w], func=AF.Sigmoid, scale=2.0 * math.sqrt(2.0 / math.pi), reads=[zk], writes=[zk])
        P.op("pool", "tensor_tensor", out=zz[:, 0:w], in0=zz[:, 0:w], in1=yy[:, 0:w], op=ALU.mult, reads=[zk, yk], writes=[zk])
        ob, obk = orot.next()
        P.op("dve", "tensor_tensor", out=ob[:, 0:w], in0=hp[0:64, 0:w], in1=zz[:, 0:w], op=ALU.mult, reads=[hpk, zk], writes=[obk])
        P.dma("sp", oT_d[:, c0:c1], ob[:, 0:w], reads=[obk])
    P.end_phase()


def emit_f(P, tile_specs, nv, E, FF, moe, final, modrows_d, sel_d, wo_d, w1_d, w3_d, w2_d, ident_d, wr_d=None, id32_d=None, fn_d=None, GT=8, after_tile=None):
    ntile = len(tile_specs)
    NFB = FF // 128
    nhalf = 2
    HB = NFB // nhalf
    ident = P.sb([128, 128], BF16, "ident")
    P.dma("sp", ident[:], ident_d, writes=["ident"])
    sel = P.sb([2, 2, 128], F32, "sel")
    P.dma("sp", sel[:], sel_d, writes=["sel"])
    mrrot = Rot(P, "mrowF", 2, [2, 1024], F32)
    pmrot = Rot(P, "pm", 3, [128, 512], F32, psum=True)
    mt = [[P.sb([128, D], F32, f"mt{v}_{i}") for i in range(4)] for v in range(nv)]
    for i in range(4):
        mrow, mrk = mrrot.next()
        P.dma("sp", mrow[:], modrows_d[:, i * 1024:(i + 1) * 1024], writes=[mrk])
        for v in range(nv):
            bcast_rows(P, mrow, mrk, sel, v, pmrot, mt[v][i], f"mt{v}_{i}")
    if final:
        fnb = P.sb([128, D], F32, "fnb")
        P.dma("sp", fnb[:], fn_d, writes=["fnb"])
    Wo = P.sb([128, 8, D], BF16, "Wo")
    wov = wo_d.rearrange("(k p) c -> p k c", p=128)
    for k in range(8):
        P.dma("pool", Wo[:, k, :], wov[:, k, :], writes=["Wo"])
    if moe:
        Wr = P.sb([128, 8, E], F32, "Wr")
        P.dma("sp", Wr[:], wr_d.rearrange("(k p) e -> p k e", p=128), writes=["Wr"])
        id32 = P.sb([128, 128], F32, "id32")
        P.dma("sp", id32[:], id32_d, writes=["id32"])
        gates = P.sb([128, GT, E], F32, "gates")
        h32T = P.sb([128, 8, 128], F32, "h32T")
        rsm = Rot(P, "rsm", 2, [128, 40], F32)
    purot = Rot(P, "pu", 4, [128, 512], F32, psum=True)
    rots = {
        "sq": Rot(P, "sq", 2, [128, D], F32),
        "st": Rot(P, "st", 4, [128, 4], F32),
        "hb": Rot(P, "hb", 2, [128, D], BF16),
        "ptr": Rot(P, "ptr", 1, [128, 1024], BF16, psum=True),
    }
    xrot = Rot(P, "xt", 2, [128, D], F32)
    otrot = Rot(P, "oTt", 2, [128, 8, 128], BF16)
    acc = P.sb([128, GT, D], F32, "acc")
    h2T = P.sb([128, 8, GT * 128], BF16, "h2T")
    gT = P.sb([128, HB, GT * 128], BF16, "gT")
    W2 = P.sb([128, HB, D], BF16, "W2")
    w1rot = Rot(P, "w1b", 2, [128, 8, 256], BF16)
    w3rot = Rot(P, "w3b", 2, [128, 8, 256], BF16)
    srot = Rot(P, "sil", 2, [128, 512], F32)
    tmprot = Rot(P, "ftmp", 2, [128, 512], F32)
    orot = Rot(P, "xout", 1, [128, D], F32) if final else None

    tiles_v = [ts_[0] for ts_ in tile_specs]
    for g0 in range(0, ntile, GT):
        tiles = list(range(g0, min(ntile, g0 + GT)))
        ng = len(tiles)
        ntok = ng * 128
        for ti, tile in enumerate(tiles):
            v = tiles_v[tile]
            acck = f"acc{ti}"
            xt, xk = xrot.next()
            P.dma("sp", xt[:], tile_specs[tile][1], writes=[xk])
            ot, otk = otrot.next()
            P.dma("sp", ot[:], tile_specs[tile][2], reads=["l_oT"], writes=[otk])
            for hh in range(2):
                pm, pk = pmrot.next()
                for k in range(8):
                    P.op("pe", "matmul", out=pm[:, :], lhsT=ot[:, k, :], rhs=Wo[:, k, hh * 512:(hh + 1) * 512], start=(k == 0), stop=(k == 7),
                         reads=[otk, "Wo"], writes=[pk])
                hs = slice(hh * 512, (hh + 1) * 512)
                P.op("dve", "tensor_tensor", out=acc[:, ti, hs], in0=pm[:, :], in1=mt[v][0][:, hs], op=ALU.mult, reads=[pk, f"mt{v}_0"], writes=[acck])
                P.op("pool", "tensor_tensor", out=acc[:, ti, hs], in0=acc[:, ti, hs], in1=xt[:, hs], op=ALU.add, reads=[acck, xk], writes=[acck])
            st, stk, sq, sqk = rms_rstd(P, acc[:, ti, :], acck, rots)
            P.op("dve", "scalar_tensor_tensor", out=sq[:], in0=acc[:, ti, :], scalar=st[:, 3:4], in1=mt[v][2][:], op0=ALU.mult, op1=ALU.mult,
                 reads=[acck, stk, f"mt{v}_2"], writes=[sqk])
            P.op("pool", "tensor_tensor", out=sq[:], in0=sq[:], in1=mt[v][1][:], op=ALU.add, reads=[sqk, f"mt{v}_1"], writes=[sqk])
            hb, hbk = rots["hb"].next()
            P.op("act", "copy", out=hb[:], in_=sq[:], reads=[sqk], writes=[hbk])
            ptr, ptk = rots["ptr"].next()
            for k in range(8):
                P.op("pe", "transpose", out=ptr[:, k * 128:(k + 1) * 128], in_=hb[:, k * 128:(k + 1) * 128], identity=ident[:], reads=[hbk, "ident"], writes=[ptk])
            P.op("act", "copy", out=h2T[:, :, ti * 128:(ti + 1) * 128], in_=ptr[:, :].rearrange("p (k t) -> p k t", k=8), reads=[ptk], writes=[f"h2T{ti}"])
            if moe:
                for hh in range(2):
                    pm, pk = pmrot.next()
                    for k4 in range(4):
                        k = hh * 4 + k4
                        P.op("pe", "transpose", out=pm[:, k4 * 128:(k4 + 1) * 128], in_=sq[:, k * 128:(k + 1) * 128], identity=id32[:], reads=[sqk, "id32"], writes=[pk])
                    P.op("act", "copy", out=h32T[:, hh * 4:(hh + 1) * 4, :], in_=pm[:, :].rearrange("p (k t) -> p k t", k=4), reads=[pk], writes=["h32T"])
                pm, pk = pmrot.next()
                for k in range(8):
                    P.op("pe", "matmul", out=pm[:, 0:E], lhsT=h32T[:, k, :], rhs=Wr[:, k, :], start=(k == 0), stop=(k == 7), reads=["h32T", "Wr"], writes=[pk])
                r, rk = rsm.next()
                lg, eq1, lg2, eq2 = r[:, 0:E], r[:, 8:8 + E], r[:, 16:16 + E], r[:, 24:24 + E]
                m1, m2, dd, ww1, ww2 = (r[:, 32 + i:33 + i] for i in range(5))
                P.op("act", "copy", out=lg, in_=pm[:, 0:E], reads=[pk], writes=[rk])
                P.op("dve", "tensor_reduce", out=m1, in_=lg, axis=AX.X, op=ALU.max, reads=[rk], writes=[rk])
                P.op("dve", "tensor_scalar", out=eq1, in0=lg, scalar1=m1, scalar2=None, op0=ALU.is_equal, reads=[rk], writes=[rk])
                P.op("dve", "scalar_tensor_tensor", out=lg2, in0=eq1, scalar=-1e30, in1=lg, op0=ALU.mult, op1=ALU.add, reads=[rk], writes=[rk])
                P.op("dve", "tensor_reduce", out=m2, in_=lg2, axis=AX.X, op=ALU.max, reads=[rk], writes=[rk])
                P.op("dve", "tensor_scalar", out=eq2, in0=lg2, scalar1=m2, scalar2=None, op0=ALU.is_equal, reads=[rk], writes=[rk])
                P.op("dve", "tensor_tensor", out=dd, in0=m2, in1=m1, op=ALU.subtract, reads=[rk], writes=[rk])
                P.op("act", "activation", out=dd, in_=dd, func=AF.Exp, reads=[rk], writes=[rk])
                P.op("dve", "tensor_scalar", out=ww1, in0=dd, scalar1=1.0, scalar2=None, op0=ALU.add, reads=[rk], writes=[rk])
                P.op("dve", "reciprocal", out=ww1, in_=ww1, reads=[rk], writes=[rk])
                P.op("dve", "tensor_tensor", out=ww2, in0=dd, in1=ww1, op=ALU.mult, reads=[rk], writes=[rk])
                P.op("dve", "tensor_scalar", out=gates[:, ti, :], in0=eq1, scalar1=ww1, scalar2=None, op0=ALU.mult, reads=[rk], writes=[f"gates{ti}"])
                P.op("dve", "scalar_tensor_tensor", out=gates[:, ti, :], in0=eq2, scalar=ww2, in1=gates[:, ti, :], op0=ALU.mult, op1=ALU.add, reads=[rk, f"gates{ti}"], writes=[f"gates{ti}"])
        h2keys = [f"h2T{ti}" for ti in range(ng)]
        chunks = [(c0, min(ntok, c0 + 512)) for c0 in range(0, ntok, 512)]
        for e in range(E):
            for half in range(nhalf):
                fb0 = half * HB
                w2v = w2_d[e, fb0 * 128:(fb0 + HB) * 128, :].rearrange("(f p) c -> p f c", p=128)
                for fp in range(0, HB, 2):
                    nb_ = min(2, HB - fp)
                    w1b, w1k = w1rot.next()
                    w3b, w3k = w3rot.next()
                    cs = slice((fb0 + fp) * 128, (fb0 + fp + nb_) * 128)
                    P.dma("pool", w1b[:, :, 0:nb_ * 128], w1_d[e, :, cs].rearrange("(k p) c -> p k c", p=128), writes=[w1k])
                    P.dma("pool", w3b[:, :, 0:nb_ * 128], w3_d[e, :, cs].rearrange("(k p) c -> p k c", p=128), writes=[w3k])
                    P.dma("pool", W2[:, fp:fp + nb_, :], w2v[:, fp:fp + nb_, :], writes=["W2"])
                    for bi in range(nb_):
                        fbi = fp + bi
                        for (c0, c1) in chunks:
                            w = c1 - c0
                            hk = h2keys[c0 // 128:(c1 - 1) // 128 + 1]
                            p1, p1k = purot.next()
                            p3, p3k = purot.next()
                            for k in range(8):
                                P.op("pe", "matmul", out=p1[:, 0:w], lhsT=w1b[:, k, bi * 128:(bi + 1) * 128], rhs=h2T[:, k, c0:c1], start=(k == 0), stop=(k == 7),
                                     reads=[w1k] + hk, writes=[p1k])
                            for k in range(8):
                                P.op("pe", "matmul", out=p3[:, 0:w], lhsT=w3b[:, k, bi * 128:(bi + 1) * 128], rhs=h2T[:, k, c0:c1], start=(k == 0), stop=(k == 7),
                                     reads=[w3k] + hk, writes=[p3k])
                            sl, slk = srot.next()
                            P.op("act", "activation", out=sl[:, 0:w], in_=p1[:, 0:w], func=AF.Silu, reads=[p1k], writes=[slk])
                            P.op("dve", "tensor_tensor", out=gT[:, fbi, c0:c1], in0=p3[:, 0:w], in1=sl[:, 0:w], op=ALU.mult, reads=[p3k, slk], writes=[f"gT{c0 // 512}"])
                for ti, tile in enumerate(tiles):
                    v = tiles_v[tile]
                    for hh in range(2):
                        hs = slice(hh * 512, (hh + 1) * 512)
                        pm, pk = pmrot.next()
                        for fbi in range(HB):
                            P.op("pe", "matmul", out=pm[:, :], lhsT=gT[:, fbi, ti * 128:(ti + 1) * 128], rhs=W2[:, fbi, hs], start=(fbi == 0), stop=(fbi == HB - 1),
                                 reads=[f"gT{ti // 4}", "W2"], writes=[pk])
                        tmp, tmpk = tmprot.next()
                        if moe:
                            P.op("dve", "scalar_tensor_tensor", out=tmp[:], in0=pm[:, :], scalar=gates[:, ti, e:e + 1], in1=mt[v][3][:, hs], op0=ALU.mult, op1=ALU.mult,
                                 reads=[pk, f"gates{ti}", f"mt{v}_3"], writes=[tmpk])
                        else:
                            P.op("dve", "tensor_tensor", out=tmp[:], in0=pm[:, :], in1=mt[v][3][:, hs], op=ALU.mult, reads=[pk, f"mt{v}_3"], writes=[tmpk])
                        P.op("pool", "tensor_tensor", out=acc[:, ti, hs], in0=acc[:, ti, hs], in1=tmp[:], op=ALU.add, reads=[f"acc{ti}", tmpk], writes=[f"acc{ti}"])
        for ti, tile in enumerate(tiles):
            if final:
                st, stk, sq, sqk = rms_rstd(P, acc[:, ti, :], f"acc{ti}", rots)
                xo_t, xok = orot.next()
                P.op("dve", "scalar_tensor_tensor", out=xo_t[:], in0=acc[:, ti, :], scalar=st[:, 3:4], in1=fnb[:], op0=ALU.mult, op1=ALU.mult,
                     reads=[f"acc{ti}", stk, "fnb"], writes=[xok])
                P.dma("sp", tile_specs[tile][3], xo_t[:], reads=[xok], writes=["xout_d"])
            else:
                P.dma("sp", tile_specs[tile][3], acc[:, ti, :], reads=[f"acc{ti}"], writes=["xout_d", "xsrc", f"xo{tile}"])
                if after_tile is not None:
                    after_tile(tile)
    P.end_phase()


def build_fused(N, M, E, FFD, FFE):
    T = N + M
    tps = N // 4
    nc = bass.Bass("TRN2", target_bir_lowering=False)
    P = Prog(nc)
    di = lambda n, s, d=F32: nc.dram_tensor(n, s, d, kind="ExternalInput").ap()
    do = lambda n, s, d=F32: nc.dram_tensor(n, s, d, kind="ExternalOutput").ap()
    sc = lambda n, s, d=F32: nc.dram_tensor(n, s, d).ap()
    xsh = di("xsh", [tps, D])
    ctx = di("ctx", [M, D])
    cT = di("cT", [128, 8, 2])
    tabs = di("tabs", [N, 160])
    ident_d = di("ident", [128, 128], BF16)
    id32_d = di("id32", [128, 128])
    sel_d = di("sel", [2, 2, 128])
    m3_d = di("m3", [128, 384], BF16)
    rc_d = di("retc", [128, 6, 128])
    rz_d = di("retz", [128, 2])
    i2_d = di("i2", [128, 64])
    fn_d = di("fnb", [128, D])
    L = []
    for l in range(2):
        L.append(dict(
            wada=di(f"wada{l}", [D, 6144]), bada=di(f"bada{l}", [1, 6144]), gn1=di(f"gn1_{l}", [1, D]), gn2=di(f"gn2_{l}", [1, D]),
            wt=di(f"wt{l}", [D, 576]), wf=di(f"wf{l}", [D, 192]), wo=di(f"wo{l}", [D, D]),
            sink=di(f"sink{l}", [128, 1]), logit=di(f"logit{l}", [128, 2]), rgn=di(f"rgn{l}", [64, 1]),
            lamv=di(f"lamv{l}", [1, 128]), dgn=di(f"dgn{l}", [64, 1]),
            cw=di(f"cw{l}", [128, 4]), cb=di(f"cb{l}", [128, 1]), wa=di(f"wa{l}", [64, 128]), wx=di(f"wx{l}", [64, 128]),
            ba=di(f"ba{l}", [128, 1]), bx=di(f"bx{l}", [128, 1]), lam=di(f"lam{l}", [128, 1])))
    fw1, fw3, fw2 = di("fw1", [1, D, FFD]), di("fw3", [1, D, FFD]), di("fw2", [1, FFD, D])
    mw1, mw3, mw2 = di("mw1", [E, D, FFE]), di("mw3", [E, D, FFE]), di("mw2", [E, FFE, D])
    wr_d = di("wr", [D, E])
    xo = do("xo", [tps, D])
    s_qA, s_kB = sc("s_qA", [128, T], BF16), sc("s_kB", [128, T], BF16)
    s_rq, s_rk = sc("s_rq", [64, T], BF16), sc("s_rk", [64, T], BF16)
    s_tok = sc("s_tok", [T, 256], BF16)
    s_f32 = sc("s_f32", [192, T])
    s_mod = sc("s_mod", [2, 4096])
    s_oT = sc("s_oT", [256, T], BF16)
    CHT = tps // 2
    NCH = N // CHT
    st_o = sc("st_o", [NCH * 256, CHT], BF16)
    st_oc = sc("st_oc", [256, M], BF16)
    g_o = sc("g_o", [NCH * 1024, CHT], BF16)
    g_oc = sc("g_oc", [1024, M], BF16)
    l_oT = sc("l_oT", [1024, tps], BF16)
    s_x0 = sc("s_x0", [tps, D])
    s_x1 = sc("s_x1", [tps, D])
    XR = min(256, tps)
    NXC = tps // XR
    g_x = [sc(f"g_x{c}", [4 * XR, D]) for c in range(NXC)]
    s_xc1 = sc("s_xc1", [M, D])
    RG = [[0, 1, 2, 3], [4, 5, 6, 7]]
    l_oTv = l_oT.rearrange("(k p) t -> p k t", p=128)
    g_ocv = g_oc.rearrange("(k p) t -> p k t", p=128)

    def gather_x(src):
        for c in range(NXC):
            P.op("pool", "collective_compute", cc=True, kind="AllGather", op=ALU.bypass, replica_groups=RG,
                 ins=[src[c * XR:(c + 1) * XR, :]], outs=[g_x[c]], reads=["xsrc"], writes=["g_x"])

    def x_tile(ti):
        t0 = ti * 128
        r, rem = t0 // tps, t0 % tps
        c, i = rem // XR, rem % XR
        return g_x[c][r * XR + i:r * XR + i + 128, :]
    for r0 in range(0, tps, max(128, tps // 4)):
        r1 = min(tps, r0 + max(128, tps // 4))
        P.dma("sp", s_x0[r0:r1, :], xsh[r0:r1, :], writes=["xsrc"])
    gather_x(s_x0)
    for l in range(2):
        W = L[l]
        need_ctx = (l == 0)
        lam_init = 0.8 - 0.6 * math.exp(-0.3 * l)
        x_lat = x_tile
        x_ctx = ctx if l == 0 else s_xc1
        emit_p(P, N, M, x_lat, x_ctx, cT, W["wada"], W["bada"], W["gn1"], W["gn2"], W["wt"], W["wf"], tabs, ident_d, sel_d,
               s_qA, s_kB, s_rq, s_rk, s_tok, s_f32, s_mod)
        emit_swa(P, N, M, need_ctx, s_qA[64:128, :], s_kB[64:128, :], s_tok[:, 64:128], W["sink"], m3_d, s_oT[0:64, :])
        emit_ret(P, N, M, need_ctx, s_rq, s_rk, s_tok[:, 0:64], s_tok[:, 128:192], s_f32[0:64, :], W["logit"], W["rgn"], rc_d, rz_d, s_oT[64:128, :])
        emit_diff(P, N, M, need_ctx, lam_init, s_qA[0:64, :], s_kB[0:64, :], s_tok[:, 192:256], W["lamv"], W["dgn"], s_oT[128:192, :])
        emit_lru(P, N, M, s_f32[64:128, :], s_f32[128:192, :], W["cw"], W["cb"], W["wa"], W["wx"], W["ba"], W["bx"], W["lam"], i2_d, s_oT[192:256, :])
        for c in range(NCH):
            P.dma("sp", st_o[c * 256:(c + 1) * 256, :], s_oT[:, M + c * CHT:M + (c + 1) * CHT], writes=[f"st_o{c}"])
        if need_ctx:
            P.dma("sp", st_oc[:, :], s_oT[:, 0:M], writes=["st_oc"])
        for c in range(NCH):
            P.op("pool", "collective_compute", cc=True, kind="AllGather", op=ALU.bypass, replica_groups=RG,
                 ins=[st_o[c * 256:(c + 1) * 256, :]], outs=[g_o[c * 1024:(c + 1) * 1024, :]], reads=[f"st_o{c}"], writes=["g_o"])
        if need_ctx:
            P.op("pool", "collective_compute", cc=True, kind="AllGather", op=ALU.bypass, replica_groups=RG,
                 ins=[st_oc], outs=[g_oc], reads=["st_oc"], writes=["g_oc"])
        P.end_phase()
        pidc = {}

        def shard_rows(e, pidc=pidc):
            if id(e) not in pidc:
                pidc[id(e)] = e.snap((e.partition_id() % 4) * 2048)
            return pidc[id(e)]
        for h in range(2):
            P.dma("sp", l_oT[:, h * CHT:(h + 1) * CHT],
                  (lambda e, h=h, sr=shard_rows: g_o[bass.ds(sr(e) + h * 1024, 1024), :]), writes=["l_oT"])
        specs = []
        for i in range(tps // 128):
            if l == 0:
                xs = xsh[i * 128:(i + 1) * 128, :]
                od = s_x1[i * 128:(i + 1) * 128, :]
            else:
                xs = s_x1[i * 128:(i + 1) * 128, :]
                od = xo[i * 128:(i + 1) * 128, :]
            specs.append((0, xs, l_oTv[:, :, i * 128:(i + 1) * 128], od))
        if need_ctx:
            for i in range(M // 128):
                specs.append((1, ctx[i * 128:(i + 1) * 128, :], g_ocv[:, :, i * 128:(i + 1) * 128], s_xc1[i * 128:(i + 1) * 128, :]))
        if l == 0:
            def after_tile(tile):
                per = XR // 128
                if tile < tps // 128 and tile % per == per - 1:
                    c = tile // per
                    P.op("pool", "collective_compute", cc=True, kind="AllGather", op=ALU.bypass, replica_groups=RG,
                         ins=[s_x1[c * XR:(c + 1) * XR, :]], outs=[g_x[c]], reads=[f"xo{t_}" for t_ in range(c * per, (c + 1) * per)], writes=["g_x"])
            emit_f(P, specs, 2, 1, FFD, False, False, s_mod, sel_d, W["wo"], fw1, fw3, fw2, ident_d, after_tile=after_tile)
        else:
            emit_f(P, specs, 1, E, FFE, True, True, s_mod, sel_d, W["wo"], mw1, mw3, mw2, ident_d, wr_d=wr_d, id32_d=id32_d, fn_d=fn_d)
    P.close()
    return nc


NCORES = 8
_cache = {}


def _get(name, fn, *a):
    key = (name,) + a
    if key not in _cache:
        _cache[key] = fn(*a)
    return _cache[key]


def _wo_perm():
    idx = np.zeros(1024, np.int64)
    for j in range(4):
        for m in range(4):
            idx[j * 256 + m * 64:j * 256 + (m + 1) * 64] = np.arange(m * 256 + j * 64, m * 256 + (j + 1) * 64)
    return idx


def make_maps(inp):
    f32 = np.float32
    B, N, _ = inp["x"].shape
    M = inp["ctx"].shape[1]
    ident = np.eye(128, dtype=f32).astype(ml_dtypes.bfloat16)
    sel = np.zeros((2, 2, 128), f32)
    sel[0, 0, :] = 1.0
    sel[1, 1, :] = 1.0
    rc, rz = ret_consts()
    tabs = rope_tables(N)
    common = {"ident": ident, "id32": np.eye(128, dtype=f32), "sel": sel, "m3": swa_masks(), "retc": rc, "retz": rz,
              "i2": np.concatenate([np.eye(64, dtype=f32)] * 2, 0), "tabs": tabs,
              "fnb": np.ascontiguousarray(np.broadcast_to(inp["final_norm"][None, :], (128, D))).astype(f32),
              "fw1": inp["ffn_w1"], "fw3": inp["ffn_w3"], "fw2": inp["ffn_w2"],
              "mw1": inp["moe_w1"][0], "mw3": inp["moe_w3"][0], "mw2": inp["moe_w2"][0], "wr": inp["moe_router"][0]}
    woidx = _wo_perm()
    for l in range(2):
        common[f"wada{l}"] = inp["w_ada"][l]
        common[f"bada{l}"] = np.ascontiguousarray(inp["b_ada"][l][None, :])
        common[f"gn1_{l}"] = np.ascontiguousarray(inp["g_norm1"][l][None, :])
        common[f"gn2_{l}"] = np.ascontiguousarray(inp["g_norm2"][l][None, :])
        common[f"wo{l}"] = np.ascontiguousarray(inp["w_out"][l][woidx, :])
        common[f"lamv{l}"] = np.ascontiguousarray(inp["diff_lambda"][l].reshape(1, 128))
    maps = []
    dup = lambda a: np.ascontiguousarray(np.concatenate([a, a], 0)).astype(f32)
    for core in range(NCORES):
        b, j = core // 4, core % 4
        s = slice(j * 64, (j + 1) * 64)
        m = dict(common)
        m["xsh"] = np.ascontiguousarray(inp["x"][b, j * (N // 4):(j + 1) * (N // 4)])
        m["ctx"] = np.ascontiguousarray(inp["ctx"][b])
        cv = np.stack([inp["c"][b], inp["c_ctx"]], 0)
        m["cT"] = np.ascontiguousarray(cv.reshape(2, 8, 128).transpose(2, 1, 0)).astype(f32)
        tok, feat = head_cols(j)
        for l in range(2):
            two = lambda a: np.ascontiguousarray(np.concatenate([a[0][s], a[1][s]], 0)[:, None]).astype(f32)
            m[f"wt{l}"] = np.ascontiguousarray(inp["w_in"][l][:, tok])
            m[f"wf{l}"] = np.ascontiguousarray(inp["w_in"][l][:, feat])
            m[f"sink{l}"] = np.full((128, 1), inp["attn_sink"][l][j], f32)
            m[f"logit{l}"] = np.ascontiguousarray(np.broadcast_to(inp["ret_decay_logit"][l][:, j][None, :], (128, 2))).astype(f32)
            m[f"rgn{l}"] = np.ascontiguousarray(inp["ret_gn"][l][s, None])
            m[f"dgn{l}"] = np.ascontiguousarray(inp["diff_gn"][l][s, None])
            m[f"cw{l}"] = dup(inp["conv_w"][l][:, s].T)
            m[f"cb{l}"] = dup(inp["conv_b"][l][s, None])
            m[f"wa{l}"] = np.ascontiguousarray(np.concatenate([inp["lru_wa"][l][0, j], inp["lru_wa"][l][1, j]], 1))
            m[f"wx{l}"] = np.ascontiguousarray(np.concatenate([inp["lru_wx"][l][0, j], inp["lru_wx"][l][1, j]], 1))
            m[f"ba{l}"] = two(inp["lru_ba"][l])
            m[f"bx{l}"] = two(inp["lru_bx"][l])
            m[f"lam{l}"] = two(inp["lru_lambda"][l])
        maps.append(m)
    return maps


def kernel(**inputs):
    inp = {k: np.asarray(v) for k, v in inputs.items()}
    B, N, _ = inp["x"].shape
    M = inp["ctx"].shape[1]
    E = inp["moe_w1"].shape[1]
    nc = _get("fused", build_fused, N, M, E, inp["ffn_w1"].shape[2], inp["moe_w1"].shape[3])
    res = run_bass_kernel_spmd(nc, make_maps(inp), core_ids=list(range(NCORES))).results
    out = np.stack([np.concatenate([res[b * 4 + s]["xo"] for s in range(4)], 0) for b in range(B)], 0)
    return out.astype(np.float32)
```

```python
from contextlib import ExitStack
import math
import numpy as np
import ml_dtypes
import concourse.bass as bass
import concourse.mybir as mybir
from concourse.bass_utils import run_bass_kernel_spmd

F32 = mybir.dt.float32
BF16 = mybir.dt.bfloat16
I32 = mybir.dt.int32
AF = mybir.ActivationFunctionType
ALU = mybir.AluOpType
AX = mybir.AxisListType

ENGS = ("pe", "act", "dve", "pool", "sp")
DMAQ = ("sp", "act", "pool")
NDMASEM = 8
_ENGATTR = {"pe": "tensor", "act": "scalar", "dve": "vector", "pool": "gpsimd", "sp": "sync"}


class Op:
    __slots__ = ("eng", "fn", "dma", "cc", "raw", "war", "sig", "dsem", "dval", "has_dep")

    def __init__(self, eng, fn, dma, cc=False):
        self.eng = eng
        self.fn = fn
        self.dma = dma
        self.cc = cc
        self.raw = []
        self.war = []
        self.sig = None
        self.dsem = None
        self.dval = None
        self.has_dep = False


class Prog:
    def __init__(self, nc):
        self.nc = nc
        self.gs = ExitStack()
        self.sems = {e: self.gs.enter_context(nc.semaphore(f"s_{e}")) for e in ENGS}
        self.dsems = {e: [self.gs.enter_context(nc.semaphore(f"d_{e}{i}")) for i in range(NDMASEM)] for e in DMAQ}
        self.cnt = {e: 0 for e in ENGS}
        self.dcnt = {e: [0] * NDMASEM for e in DMAQ}
        self.dk = {e: 0 for e in DMAQ}
        self.ccsem = self.gs.enter_context(nc.semaphore("s_cc"))
        self.cccnt = 0
        self.waited = {e: {} for e in ENGS}
        self.barrier = None
        self.nsb = 0
        self.nphase = 0
        self._reset()

    def _reset(self):
        self.q = {e: [] for e in ENGS}
        self.res_w = {}
        self.res_r = {}
        self.es = ExitStack()

    def sb(self, shape, dt=F32, name=None, persist=False):
        self.nsb += 1
        st = self.gs if persist else self.es
        return st.enter_context(self.nc.sbuf_tensor(f"s{self.nsb}_" + (name or "t"), list(shape), dt))

    def ps(self, shape, dt=F32, name=None):
        self.nsb += 1
        return self.es.enter_context(self.nc.psum_tensor(f"p{self.nsb}_" + (name or "t"), list(shape), dt))

    def op(self, eng, meth, reads=(), writes=(), dma=False, cc=False, **kw):
        fn = (lambda e: getattr(e, meth)(**{k: (v(e) if callable(v) else v) for k, v in kw.items()}))
        o = Op(eng, fn, dma or cc, cc)
        raw = set()
        war = set()
        for r in reads:
            w = self.res_w.get(r)
            if w is not None:
                raw.add(w)
        for wr in writes:
            w = self.res_w.get(wr)
            if w is not None:
                war.add(w)
            for rd in self.res_r.get(wr, ()):
                war.add(rd)
        raw.discard(o)
        war -= raw
        o.raw = list(raw)
        o.war = list(war)
        for r in reads:
            self.res_r.setdefault(r, []).append(o)
        for wr in writes:
            self.res_w[wr] = o
            self.res_r[wr] = []
        self.q[eng].append(o)
        return o

    def dma(self, eng, out, in_, reads=(), writes=(), **kw):
        return self.op(eng, "dma_start", reads, writes, dma=True, out=out, in_=in_, **kw)

    def end_phase(self):
        nc = self.nc
        for e in ENGS:
            for o in self.q[e]:
                for d in o.raw:
                    d.has_dep = True
                for d in o.war:
                    if d.dma or d.eng != o.eng or o.dma:
                        d.has_dep = True
            for o in reversed(self.q[e]):
                if not o.dma:
                    o.has_dep = True
                    break
        for e in ENGS:
            for o in self.q[e]:
                if o.cc:
                    self.cccnt += 1
                    o.dsem = self.ccsem
                    o.dval = self.cccnt
                elif o.dma:
                    s = self.dk[e] % NDMASEM
                    self.dk[e] += 1
                    self.dcnt[e][s] += 16
                    o.dsem = self.dsems[e][s]
                    o.dval = self.dcnt[e][s]
                elif o.has_dep:
                    self.cnt[e] += 1
                    o.sig = self.cnt[e]
        prev_barrier = self.barrier
        nb = [(self.sems[e], self.cnt[e]) for e in ENGS if self.cnt[e] > 0]
        for e in DMAQ:
            for s in range(NDMASEM):
                if self.dcnt[e][s] > 0:
                    nb.append((self.dsems[e][s], self.dcnt[e][s]))
        if self.cccnt > 0:
            nb.append((self.ccsem, self.cccnt))
        self.barrier = nb
        sems = self.sems

        def run_engine(e, eng):
            waited = self.waited[e]

            def wait(sem, val):
                if waited.get(sem, 0) >= val:
                    return
                eng.wait_ge(sem, val)
                waited[sem] = val

            if prev_barrier is not None:
                for s, v in prev_barrier:
                    wait(s, v)
            for o in self.q[e]:
                for d, is_raw in [(d, True) for d in o.raw] + [(d, False) for d in o.war]:
                    if d.dma:
                        wait(d.dsem, d.dval)
                    elif d.eng == e and not o.dma:
                        if is_raw:
                            wait(sems[e], d.sig)
                    else:
                        wait(sems[d.eng], d.sig)
                if o.cc:
                    if o.dval > 1:
                        wait(o.dsem, o.dval - 1)
                    o.fn(eng).then_inc(o.dsem)
                elif o.dma:
                    if o.dval > 16:
                        wait(o.dsem, o.dval - 16)
                    o.fn(eng).then_inc(o.dsem, 16)
                else:
                    ins = o.fn(eng)
                    if o.sig is not None:
                        ins.then_inc(sems[e], 1)
            if e == "sp":
                for s, v in nb:
                    wait(s, v)

        with nc.Block() as block:
            @block.tensor
            def _(eng):
                run_engine("pe", eng)

            @block.scalar
            def _(eng):
                run_engine("act", eng)

            @block.vector
            def _(eng):
                run_engine("dve", eng)

            @block.gpsimd
            def _(eng):
                run_engine("pool", eng)

            @block.sync
            def _(eng):
                run_engine("sp", eng)
        self.es.close()
        self._reset()
        self.nphase += 1

    def emit(self):
        self.end_phase()

    def close(self):
        self.es.close()
        self.gs.close()

D = 1024
EPS = 1e-6
SC_SWA = 64 ** -0.5
SC_RET = 64 ** -0.5
SC_DIFF = 32 ** -0.5


class Rot:
    def __init__(self, P, name, n, shape, dt=F32, psum=False):
        self.bufs = [(P.ps(shape, dt, f"{name}{i}") if psum else P.sb(shape, dt, f"{name}{i}")) for i in range(n)]
        self.name = name
        self.i = 0

    def next(self):
        k = self.i % len(self.bufs)
        self.i += 1
        return self.bufs[k], f"{self.name}{k}"


def _r(a, b):
    return list(range(a, b))


TOK_COLS = _r(0, 256) + _r(256, 384) + _r(1536, 1792) + _r(1792, 2048) + _r(512, 768) + _r(768, 1024) + _r(384, 512) + _r(1024, 1280) + _r(2048, 2304)
FEAT_COLS = _r(1280, 1536) + _r(2304, 2560) + _r(2560, 2816)


def rope_tables(n_lat):
    f32 = np.float32
    n = np.arange(n_lat)
    row = (n // 64).astype(f32)
    col = (n % 64).astype(f32)

    def axial(dim):
        nf = dim // 4
        inv = (f32(10000.0) ** (-(np.arange(nf, dtype=f32)) / f32(nf))).astype(f32)
        ang = np.concatenate([row[:, None] * inv, col[:, None] * inv], -1).astype(f32)
        return np.cos(ang).astype(f32), np.sin(ang).astype(f32)

    ca, sa = axial(64)
    cd, sd = axial(32)
    nf = 32
    inv = (f32(10000.0) ** (-(np.arange(nf, dtype=f32)) / f32(nf))).astype(f32)
    ang = (n.astype(f32)[:, None] * inv).astype(f32)
    cl, sl = np.cos(ang).astype(f32), np.sin(ang).astype(f32)
    return np.ascontiguousarray(np.concatenate([ca, sa, cd, sd, cl, sl], -1).astype(f32))


def mod_rows(P, cT, wada, bada, ncols, pmrot, name, extra=None):
    scT = P.sb([128, 8, 2], F32, name + "_scT")
    P.dma("sp", scT[:], cT, writes=[name + "_scT"])
    P.op("act", "activation", out=scT[:], in_=scT[:], func=AF.Silu, reads=[name + "_scT"], writes=[name + "_scT"])
    mrow = P.sb([2, ncols], F32, name + "_mrow")
    brot = Rot(P, name + "_brow", 2, [2, 512], F32)
    xrot_ = Rot(P, name + "_xrow", 2, [2, 512], F32)
    g2rot = Rot(P, name + "_g2row", 2, [2, 512], F32)
    wrot = Rot(P, name + "_wA", 2, [128, 8, 512], F32)
    wv = wada.rearrange("(k p) c -> p k c", p=128)
    ntot = ncols if extra is None else extra[0]
    for cb in range(ntot // 512):
        cs = slice(cb * 512, (cb + 1) * 512)
        wA, wk = wrot.next()
        P.dma("sp", wA[:], wv[:, :, cs], writes=[wk])
        brow, bk = brot.next()
        P.dma("sp", brow[0:1, :], bada[:, cs], writes=[bk])
        P.dma("sp", brow[1:2, :], bada[:, cs], writes=[bk])
        pm, pk = pmrot.next()
        for k in range(8):
            P.op("pe", "matmul", out=pm[0:2, :], lhsT=scT[:, k, :], rhs=wA[:, k, :], start=(k == 0), stop=(k == 7),
                 reads=[name + "_scT", wk], writes=[pk])
        if cb * 512 < ncols:
            P.op("dve", "tensor_tensor", out=mrow[:, cs], in0=pm[0:2, :], in1=brow[:, :], op=ALU.add, reads=[pk, bk], writes=[name + "_mrow"])
        else:
            xr, xk = xrot_.next()
            P.op("dve", "tensor_tensor", out=xr[:, :], in0=pm[0:2, :], in1=brow[:, :], op=ALU.add, reads=[pk, bk], writes=[xk])
            oc = cb * 512 - ncols
            if 2048 <= oc < 3072:
                g2, g2k = g2rot.next()
                P.dma("sp", g2[0:1, :], extra[1][:, oc - 2048:oc - 2048 + 512], writes=[g2k])
                P.dma("sp", g2[1:2, :], extra[1][:, oc - 2048:oc - 2048 + 512], writes=[g2k])
                P.op("dve", "scalar_tensor_tensor", out=xr[:, :], in0=xr[:, :], scalar=1.0, in1=g2[:, :], op0=ALU.add, op1=ALU.mult, reads=[xk, g2k], writes=[xk])
            P.dma("sp", extra[2][:, oc:oc + 512], xr[:, :], reads=[xk])
    return mrow, name + "_mrow"


def bcast_rows(P, row, rowkey, sel, v, pmrot, out, outkey):
    for hh in range(2):
        pm, pk = pmrot.next()
        P.op("pe", "matmul", out=pm[:, :], lhsT=sel[:, v, :], rhs=row[:, hh * 512:(hh + 1) * 512], start=True, stop=True,
             reads=[rowkey, "sel"], writes=[pk])
        P.op("act", "copy", out=out[:, hh * 512:(hh + 1) * 512], in_=pm[:, :], reads=[pk], writes=[outkey])


def rms_rstd(P, xt, xk, rots):
    sq, sqk = rots["sq"].next()
    st, stk = rots["st"].next()
    P.op("act", "activation", out=sq[:], in_=xt, func=AF.Square, accum_out=st[:, 0:1], reads=[xk], writes=[sqk, stk])
    P.op("dve", "tensor_scalar", out=st[:, 1:2], in0=st[:, 0:1], scalar1=1.0 / D, scalar2=EPS, op0=ALU.mult, op1=ALU.add, reads=[stk], writes=[stk])
    P.op("act", "activation", out=st[:, 2:3], in_=st[:, 1:2], func=AF.Sqrt, reads=[stk], writes=[stk])
    P.op("dve", "reciprocal", out=st[:, 3:4], in_=st[:, 2:3], reads=[stk], writes=[stk])
    return st, stk, sq, sqk


def norm_mod_transpose(P, xt, xk, Gb, Sb, gkeys, ident, hT, hTk, col0, rots):
    st, stk, sq, sqk = rms_rstd(P, xt, xk, rots)
    P.op("dve", "scalar_tensor_tensor", out=sq[:], in0=xt, scalar=st[:, 3:4], in1=Gb[:], op0=ALU.mult, op1=ALU.mult,
         reads=[xk, stk, gkeys[0]], writes=[sqk])
    hb, hbk = rots["hb"].next()
    P.op("pool", "tensor_tensor", out=hb[:], in0=sq[:], in1=Sb[:], op=ALU.add, reads=[sqk, gkeys[1]], writes=[hbk])
    ptr, ptk = rots["ptr"].next()
    for k in range(8):
        P.op("pe", "transpose", out=ptr[:, k * 128:(k + 1) * 128], in_=hb[:, k * 128:(k + 1) * 128], identity=ident[:],
             reads=[hbk, "ident"], writes=[ptk])
    P.op("act", "copy", out=hT[:, :, col0:col0 + 128], in_=ptr[:, :].rearrange("p (k t) -> p k t", k=8), reads=[ptk], writes=[hTk])


def head_cols(j):
    g = j // 2
    r = lambda a, n=64: list(range(a, a + n))
    tok = (r(1536 + j * 64) + r(0 + j * 64) + r(1792 + j * 64) + r(256 + g * 64) + r(512 + j * 64) + r(768 + j * 64)
           + r(384 + g * 64) + r(1024 + j * 64) + r(2048 + j * 64))
    feat = r(1280 + j * 64) + r(2304 + j * 64) + r(2560 + j * 64)
    return tok, feat


def emit_p(P, N, M, x_lat, x_ctx, cT, wada, bada, gn, gn2, wt_d, wf_d, tabs, ident_d, sel_d,
           s_qA, s_kB, s_rq, s_rk, s_tok, s_f32, s_modrows):
    nlt, nct = N // 128, M // 128
    ident = P.sb([128, 128], BF16, "ident")
    P.dma("sp", ident[:], ident_d, writes=["ident"])
    sel = P.sb([2, 2, 128], F32, "sel")
    P.dma("sp", sel[:], sel_d, writes=["sel"])
    Wt = P.sb([128, 8, 576], BF16, "Wt")
    Wf = P.sb([128, 8, 192], BF16, "Wf")
    P.dma("pool", Wt[:], wt_d.rearrange("(k p) c -> p k c", p=128), writes=["Wt"])
    P.dma("pool", Wf[:], wf_d.rearrange("(k p) c -> p k c", p=128), writes=["Wf"])
    pmrot = Rot(P, "pm", 3, [128, 512], F32, psum=True)
    mrow, mk = mod_rows(P, cT, wada, bada, 2048, pmrot, "mod", extra=(6144, gn2, s_modrows))
    grow = P.sb([2, D], F32, "grow")
    P.dma("sp", grow[0:1, :], gn, writes=["grow"])
    P.dma("sp", grow[1:2, :], gn, writes=["grow"])
    P.op("dve", "scalar_tensor_tensor", out=grow[:], in0=mrow[:, 1024:2048], scalar=1.0, in1=grow[:], op0=ALU.add, op1=ALU.mult,
         reads=[mk, "grow"], writes=["grow"])
    Gb = [P.sb([128, D], F32, f"Gb{v}") for v in range(2)]
    Sb = [P.sb([128, D], F32, f"Sb{v}") for v in range(2)]
    for v in range(2):
        bcast_rows(P, grow, "grow", sel, v, pmrot, Gb[v], f"Gb{v}")
        bcast_rows(P, mrow[:, 0:1024], mk, sel, v, pmrot, Sb[v], f"Sb{v}")
    rots = {
        "sq": Rot(P, "sq", 4, [128, D], F32),
        "st": Rot(P, "st", 8, [128, 4], F32),
        "hb": Rot(P, "hb", 4, [128, D], BF16),
        "ptr": Rot(P, "ptr", 2, [128, 1024], BF16, psum=True),
    }
    xrot = Rot(P, "xt", 5, [128, D], F32)
    hTrot = Rot(P, "hT", 3, [128, 8, 512], BF16)
    ptokrot = Rot(P, "ptok", 4, [128, 576], F32)
    tabrot = Rot(P, "tab", 4, [128, 160], F32)
    tmprot = Rot(P, "rtmp", 6, [128, 4, 128], F32)
    qkrot = Rot(P, "qk16", 4, [128, 384], BF16)
    vstrot = Rot(P, "vst", 4, [128, 256], BF16)
    ptArot = Rot(P, "ptA", 2, [128, 4, 128], BF16, psum=True)
    f16rot = Rot(P, "f16T", 2, [128, 4, 512], BF16)
    f32rot = Rot(P, "f32T", 2, [128, 2, 512], F32)
    pieces = [(0, 64, SC_DIFF), (64, 128, SC_SWA), (128, 320, 1.0), (320, 384, SC_RET), (384, 512, 1.0)]
    ei = 0
    groups = [(list(range(t, min(t + 4, nct))), 1) for t in range(0, nct, 4)] + [(list(range(t, min(t + 4, nlt))), 0) for t in range(0, nlt, 4)]
    def do_norm(gi, ti):
        tiles, v = groups[gi]
        src = x_ctx if v == 1 else x_lat
        tile = tiles[ti]
        xt, xk = xrot.next()
        P.dma("sp", xt[:], (src(tile) if callable(src) else src[tile * 128:(tile + 1) * 128, :]), reads=["g_x", "s_xc1"], writes=[xk])
        st, stk, sq, sqk = rms_rstd(P, xt[:], xk, rots)
        P.op("dve", "scalar_tensor_tensor", out=sq[:], in0=xt[:], scalar=st[:, 3:4], in1=Gb[v][:], op0=ALU.mult, op1=ALU.mult,
             reads=[xk, stk, f"Gb{v}"], writes=[sqk])
        hb, hbk = rots["hb"].next()
        P.op("pool", "tensor_tensor", out=hb[:], in0=sq[:], in1=Sb[v][:], op=ALU.add, reads=[sqk, f"Sb{v}"], writes=[hbk])
        pend[(gi, ti)] = (hb, hbk)

    pend = {}

    def do_norm_b(gi, ti):
        hb, hbk = pend.pop((gi, ti))
        hT, hTk = hts[gi]
        ptr, ptk = rots["ptr"].next()
        for k in range(8):
            P.op("pe", "transpose", out=ptr[:, k * 128:(k + 1) * 128], in_=hb[:, k * 128:(k + 1) * 128], identity=ident[:],
                 reads=[hbk, "ident"], writes=[ptk])
        P.op("act", "copy", out=hT[:, :, ti * 128:(ti + 1) * 128], in_=ptr[:, :].rearrange("p (k t) -> p k t", k=8), reads=[ptk], writes=[hTk])

    hts = {}
    st1 = {}

    def stage1(gi, ti):
        tiles, v = groups[gi]
        tile = tiles[ti]
        T0 = (tiles[0] * 128) if v == 1 else (M + tiles[0] * 128)
        hT, hTk = hts[gi]
        ptok, ptokk = ptokrot.next()
        pm, pk = pmrot.next()
        for k in range(8):
            P.op("pe", "matmul", out=pm[:, :], lhsT=hT[:, k, ti * 128:(ti + 1) * 128], rhs=Wt[:, k, 0:512], start=(k == 0), stop=(k == 7),
                 reads=[hTk, "Wt"], writes=[pk])
        for (c0, c1, sc) in pieces:
            P.op("act", "activation", out=ptok[:, c0:c1], in_=pm[:, c0:c1], func=AF.Copy, scale=float(sc), reads=[pk], writes=[ptokk])
        pm2, pk2 = pmrot.next()
        for k in range(8):
            P.op("pe", "matmul", out=pm2[:, 0:64], lhsT=hT[:, k, ti * 128:(ti + 1) * 128], rhs=Wt[:, k, 512:576], start=(k == 0), stop=(k == 7),
                 reads=[hTk, "Wt"], writes=[pk2])
        P.op("act", "copy", out=ptok[:, 512:576], in_=pm2[:, 0:64], reads=[pk2], writes=[ptokk])
        qk, qkk = qkrot.next()
        if v == 0:
            tab, tabk = tabrot.next()
            P.dma("sp", tab[:], tabs[tile * 128:(tile + 1) * 128, :], writes=[tabk])

            def views(t_, kind):
                if kind == "diff":
                    vv = t_[:, 0:256].rearrange("p (a c) -> p a c", a=2)[:, :, 0:64].rearrange("p a (h two d) -> p a h two d", h=2, two=2)
                    return vv[:, :, :, 0, :], vv[:, :, :, 1, :], [128, 2, 2, 16]
                if kind == "swa":
                    vv = t_[:, 0:256].rearrange("p (a c) -> p a c", a=2)[:, :, 64:128].rearrange("p a (two d) -> p a two d", two=2)
                    return vv[:, :, 0, :], vv[:, :, 1, :], [128, 2, 32]
                vv = t_[:, 256:384].rearrange("p (h two d) -> p h two d", h=2, two=2)
                return vv[:, :, 0, :], vv[:, :, 1, :], [128, 2, 32]
            for ki, (kind, hd, tc0) in enumerate((("diff", 16, 64), ("swa", 32, 0), ("ret", 32, 96))):
                x1, x2, shp = views(ptok, kind)
                o1, o2, _ = views(qk, kind)
                cosb = tab[:, tc0:tc0 + hd]
                sinb = tab[:, tc0 + hd:tc0 + 2 * hd]
                for _ in range(len(shp) - 2):
                    cosb = cosb.unsqueeze(1)
                    sinb = sinb.unsqueeze(1)
                cosb = cosb.to_broadcast(shp)
                sinb = sinb.to_broadcast(shp)
                tmp, tmpk = tmprot.next()
                nel = int(np.prod(shp[1:]))
                if len(shp) == 4:
                    T_ = [tmp[:, i, 0:nel].rearrange("p (a h d) -> p a h d", a=shp[1], h=shp[2]) for i in range(4)]
                else:
                    T_ = [tmp[:, i, 0:nel].rearrange("p (a d) -> p a d", a=shp[1]) for i in range(4)]
                e1 = "dve" if (ti + ki) % 2 == 0 else "pool"
                e2 = "pool" if (ti + ki) % 2 == 0 else "dve"
                P.op(e1, "tensor_tensor", out=T_[0], in0=x1, in1=cosb, op=ALU.mult, reads=[ptokk, tabk], writes=[tmpk + "a"])
                P.op(e1, "tensor_tensor", out=T_[1], in0=x2, in1=sinb, op=ALU.mult, reads=[ptokk, tabk], writes=[tmpk + "b"])
                P.op(e2, "tensor_tensor", out=T_[2], in0=x2, in1=cosb, op=ALU.mult, reads=[ptokk, tabk], writes=[tmpk + "c"])
                P.op(e2, "tensor_tensor", out=T_[3], in0=x1, in1=sinb, op=ALU.mult, reads=[ptokk, tabk], writes=[tmpk + "d"])
                P.op(e1, "tensor_tensor", out=o1, in0=T_[0], in1=T_[1], op=ALU.subtract, reads=[tmpk + "a", tmpk + "b"], writes=[qkk])
                P.op(e2, "tensor_tensor", out=o2, in0=T_[2], in1=T_[3], op=ALU.add, reads=[tmpk + "c", tmpk + "d"], writes=[qkk])
        else:
            P.op("dve", "tensor_copy", out=qk[:, :], in_=ptok[:, 0:384], reads=[ptokk], writes=[qkk])
        vst, vstk = vstrot.next()
        P.op("pool", "tensor_copy", out=vst[:, 64:256], in_=ptok[:, 384:576], reads=[ptokk], writes=[vstk])
        P.op("pool", "tensor_copy", out=vst[:, 0:64], in_=qk[:, 320:384], reads=[qkk], writes=[vstk])
        P.dma("sp", s_tok[T0 + ti * 128:T0 + (ti + 1) * 128, :], vst[:], reads=[vstk], writes=["s_tok"])
        st1[(gi, ti)] = (qk, qkk)

    def stage2(gi, ti, f16T, f16k):
        qk, qkk = st1.pop((gi, ti))
        ptA, ptAk = ptArot.next()
        P.op("pe", "transpose", out=ptA[:, 0, :], in_=qk[:, 0:128], identity=ident[:], reads=[qkk, "ident"], writes=[ptAk])
        P.op("pe", "transpose", out=ptA[:, 1, :], in_=qk[:, 128:256], identity=ident[:], reads=[qkk, "ident"], writes=[ptAk])
        P.op("pe", "transpose", out=ptA[0:64, 2, :], in_=qk[:, 256:320], identity=ident[:], reads=[qkk, "ident"], writes=[ptAk])
        P.op("pe", "transpose", out=ptA[0:64, 3, :], in_=qk[:, 320:384], identity=ident[:], reads=[qkk, "ident"], writes=[ptAk])
        P.op("dve", "tensor_copy", out=f16T[:, 0:2, ti * 128:(ti + 1) * 128], in_=ptA[:, 0:2, :], reads=[ptAk], writes=[f16k])
        P.op("dve", "tensor_copy", out=f16T[0:64, 2:4, ti * 128:(ti + 1) * 128], in_=ptA[0:64, 2:4, :], reads=[ptAk], writes=[f16k])

    hts[0] = hTrot.next()
    for ti in range(len(groups[0][0])):
        do_norm(0, ti)
    for ti in range(len(groups[0][0])):
        do_norm_b(0, ti)
    for gi, (tiles, v) in enumerate(groups):
        ntok = len(tiles) * 128
        T0 = (tiles[0] * 128) if v == 1 else (M + tiles[0] * 128)
        hT, hTk = hts[gi]
        nxt = gi + 1 if gi + 1 < len(groups) else None
        if nxt is not None:
            hts[nxt] = hTrot.next()
        f16T, f16k = f16rot.next()
        for ti in range(len(tiles)):
            stage1(gi, ti)
            if nxt is not None and ti < len(groups[nxt][0]):
                do_norm(nxt, ti)
            if ti > 0:
                stage2(gi, ti - 1, f16T, f16k)
        if nxt is not None:
            for ti in range(len(tiles), len(groups[nxt][0])):
                do_norm(nxt, ti)
        stage2(gi, len(tiles) - 1, f16T, f16k)
        if nxt is not None:
            for ti in range(len(groups[nxt][0])):
                do_norm_b(nxt, ti)
        P.dma("sp", s_qA[:, T0:T0 + ntok], f16T[:, 0, 0:ntok], reads=[f16k], writes=["s_qA"])
        P.dma("sp", s_kB[:, T0:T0 + ntok], f16T[:, 1, 0:ntok], reads=[f16k], writes=["s_kB"])
        P.dma("sp", s_rq[:, T0:T0 + ntok], f16T[0:64, 2, 0:ntok], reads=[f16k], writes=["s_rq"])
        P.dma("sp", s_rk[:, T0:T0 + ntok], f16T[0:64, 3, 0:ntok], reads=[f16k], writes=["s_rk"])
        f32T, f32k = f32rot.next()
        for fb, (c0, c1) in enumerate(((0, 128), (128, 192))):
            w = c1 - c0
            pm, pk = pmrot.next()
            for k in range(8):
                P.op("pe", "matmul", out=pm[0:w, 0:ntok], lhsT=Wf[:, k, c0:c1], rhs=hT[:, k, 0:ntok], start=(k == 0), stop=(k == 7), reads=[hTk, "Wf"], writes=[pk])
            P.op("act", "copy", out=f32T[0:w, fb, 0:ntok], in_=pm[0:w, 0:ntok], reads=[pk], writes=[f32k])
        P.dma("sp", s_f32[0:128, T0:T0 + ntok], f32T[:, 0, 0:ntok], reads=[f32k], writes=["s_f32"])
        P.dma("sp", s_f32[128:192, T0:T0 + ntok], f32T[0:64, 1, 0:ntok], reads=[f32k], writes=["s_f32"])
        del hts[gi]
    P.end_phase()


def lam_scalar_col(P, lamv_d, lam_init, pmrot):
    lv = P.sb([1, 128], F32, "lv")
    P.dma("sp", lv[:], lamv_d, writes=["lv"])
    pr = P.sb([1, 64], F32, "lvpr")
    lvv = lv[:, :].rearrange("o (a b d) -> o a b d", a=2, b=2)
    P.op("dve", "tensor_tensor", out=pr[:, :].rearrange("o (a d) -> o a d", a=2), in0=lvv[:, :, 0, :], in1=lvv[:, :, 1, :], op=ALU.mult, reads=["lv"], writes=["lvpr"])
    sm = P.sb([1, 4], F32, "lvsm")
    P.op("dve", "tensor_reduce", out=sm[:, 0:2], in_=pr[:, :].rearrange("o (a d) -> o a d", a=2), axis=AX.X, op=ALU.add, reads=["lvpr"], writes=["lvsm"])
    P.op("act", "activation", out=sm[:, 0:2], in_=sm[:, 0:2], func=AF.Exp, reads=["lvsm"], writes=["lvsm"])
    P.op("dve", "tensor_tensor", out=sm[:, 2:3], in0=sm[:, 0:1], in1=sm[:, 1:2], op=ALU.subtract, reads=["lvsm"], writes=["lvsm"])
    P.op("dve", "tensor_scalar", out=sm[:, 3:4], in0=sm[:, 2:3], scalar1=float(lam_init), scalar2=-1.0, op0=ALU.add, op1=ALU.mult, reads=["lvsm"], writes=["lvsm"])
    P.op("dve", "tensor_scalar", out=sm[:, 2:3], in0=sm[:, 2:3], scalar1=float(lam_init), scalar2=None, op0=ALU.add, reads=["lvsm"], writes=["lvsm"])
    ones1 = P.sb([1, 128], F32, "ones1")
    P.op("pool", "memset", ap=ones1[:], constant=1.0, writes=["ones1"])
    pm, pk = pmrot.next()
    P.op("pe", "matmul", out=pm[:, 0:2], lhsT=ones1[:, :], rhs=sm[:, 2:4], start=True, stop=True, reads=["ones1", "lvsm"], writes=[pk])
    lamc = P.sb([128, 2], F32, "lamc")
    P.op("dve", "tensor_copy", out=lamc[:], in_=pm[:, 0:2], reads=[pk], writes=["lamc"])
    return lamc


def headnorm_T(P, o32, ok, ncol, ones64, eprot, gcol, gkey, mul, out_ap, outk, tmprot, eng_a="dve", eng_b="pool"):
    sq, sqk = tmprot.next()
    P.op(eng_b, "tensor_tensor", out=sq[0:64, 0:ncol], in0=o32, in1=o32, op=ALU.mult, reads=[ok], writes=[sqk])
    pm, pk = eprot.next()
    P.op("pe", "matmul", out=pm[0:64, 0:ncol], lhsT=ones64[0:64, 0:64], rhs=sq[0:64, 0:ncol], start=True, stop=True, reads=[sqk, "ones64"], writes=[pk])
    P.op(eng_a, "tensor_scalar", out=sq[0:64, 0:ncol], in0=pm[0:64, 0:ncol], scalar1=1.0 / 64, scalar2=EPS, op0=ALU.mult, op1=ALU.add, reads=[pk], writes=[sqk])
    P.op("act", "activation", out=sq[0:64, 0:ncol], in_=sq[0:64, 0:ncol], func=AF.Sqrt, reads=[sqk], writes=[sqk])
    P.op(eng_a, "reciprocal", out=sq[0:64, 0:ncol], in_=sq[0:64, 0:ncol], reads=[sqk], writes=[sqk])
    P.op(eng_a, "tensor_tensor", out=sq[0:64, 0:ncol], in0=sq[0:64, 0:ncol], in1=o32, op=ALU.mult, reads=[sqk, ok], writes=[sqk])
    P.op(eng_a, "tensor_scalar", out=out_ap, in0=sq[0:64, 0:ncol], scalar1=gcol, scalar2=float(mul), op0=ALU.mult, op1=ALU.mult, reads=[sqk, gkey], writes=[outk])


def emit_diff(P, N, M, need_ctx, lam_init, qT_d, kT_d, v_d, lamv_d, gn_d, oT_d):
    T = N + M
    NKB = T // 128

    qT = P.sb([64, T], BF16, "qT")
    kT = P.sb([64, T], BF16, "kT")
    V1 = P.sb([128, NKB, 128], BF16, "V1")
    nchunk = 4
    cw = T // nchunk
    for i in range(nchunk):
        P.dma("sp", qT[:, i * cw:(i + 1) * cw], qT_d[:, i * cw:(i + 1) * cw], writes=["qT"])
        P.dma("sp", kT[:, i * cw:(i + 1) * cw], kT_d[:, i * cw:(i + 1) * cw], writes=["kT"])
    vv = v_d.rearrange("(kb p) d -> p kb d", p=128)
    step = max(1, NKB // 8)
    for k0 in range(0, NKB, step):
        k1 = min(NKB, k0 + step)
        P.dma("sp", V1[:, k0:k1, 0:64], vv[:, k0:k1, :], writes=["V1"])
    P.op("pool", "memset", ap=V1[:, :, 64:65], constant=1.0, writes=["V1"])
    P.op("pool", "memset", ap=V1[:, :, 65:128], constant=0.0, writes=["V1"])
    gcol = P.sb([64, 1], F32, "gcol")
    P.dma("sp", gcol[:], gn_d, writes=["gcol"])
    ones64 = P.sb([128, 64], F32, "ones64")
    P.op("pool", "memset", ap=ones64[:], constant=1.0, writes=["ones64"])
    srot = Rot(P, "S", 3, [128, 512], F32, psum=True)
    eprot = Rot(P, "ep", 1, [128, 512], F32, psum=True)
    accrot = [Rot(P, f"acc{m}_", 2, [128, 512], F32, psum=True) for m in range(2)]
    lamc = lam_scalar_col(P, lamv_d, lam_init, eprot)
    ptrot = Rot(P, "PT", 6, [128, 512], BF16)
    rlrot = Rot(P, "rl", 2, [128, 2, 512], F32)
    bcrot = Rot(P, "bc", 2, [64, 2, 512], F32)
    t32rot = Rot(P, "t32", 3, [64, 512], F32)
    orot = Rot(P, "ob", 2, [64, 512], BF16)

    def epilogue(acc, q0, nq):
        rl, rlk = rlrot.next()
        for m in range(2):
            P.op("dve", "reciprocal", out=rl[64:65, m, 0:nq], in_=acc[m][0][64:65, 0:nq], reads=[acc[m][1]], writes=[rlk])
        P.op("dve", "tensor_scalar", out=rl[64:65, 1, 0:nq], in0=rl[64:65, 1, 0:nq], scalar1=lamc[64:65, 1:2], scalar2=None, op0=ALU.mult, reads=[rlk, "lamc"], writes=[rlk])
        bc, bck = bcrot.next()
        for m in range(2):
            pm, pk = eprot.next()
            P.op("pe", "matmul", out=pm[0:64, 0:nq], lhsT=ones64[64:65, 0:64], rhs=rl[64:65, m, 0:nq], start=True, stop=True, reads=[rlk, "ones64"], writes=[pk])
            P.op("dve", "tensor_copy", out=bc[:, m, 0:nq], in_=pm[0:64, 0:nq], reads=[pk], writes=[bck])
        t0, t0k = t32rot.next()
        t1, t1k = t32rot.next()
        P.op("dve", "tensor_tensor", out=t0[:, 0:nq], in0=acc[0][0][0:64, 0:nq], in1=bc[:, 0, 0:nq], op=ALU.mult, reads=[acc[0][1], bck], writes=[t0k])
        P.op("dve", "tensor_tensor", out=t1[:, 0:nq], in0=acc[1][0][0:64, 0:nq], in1=bc[:, 1, 0:nq], op=ALU.mult, reads=[acc[1][1], bck], writes=[t1k])
        P.op("pool", "tensor_tensor", out=t0[:, 0:nq], in0=t0[:, 0:nq], in1=t1[:, 0:nq], op=ALU.add, reads=[t0k, t1k], writes=[t0k])
        ob, obk = orot.next()
        headnorm_T(P, t0[:, 0:nq], t0k, nq, ones64, eprot, gcol[:, 0:1], "gcol", 1.0 - lam_init, ob[:, 0:nq], obk, t32rot)
        P.dma("sp", oT_d[:, q0:q0 + nq], ob[:, 0:nq], reads=[obk])
    QG = 2
    sgroups = []
    if need_ctx:
        sgroups.append(([(0, M)], 0, M // 128))
    lat = [(g0, min(512, T - g0)) for g0 in range(M, T, 512)]
    for i in range(0, len(lat), QG):
        sgroups.append((lat[i:i + QG], 0, NKB))
    LOOK = 1
    for (qgs, kb0, kb1) in sgroups:
        ng = len(qgs)
        acc = [[accrot[m].next() for m in range(2)] for _ in range(ng)]
        steps = [(kb, m) for kb in range(kb0, kb1) for m in range(2)]
        pts = {}

        def issue_s(i):
            kb, m = steps[i]
            for gi, (q0, nq) in enumerate(qgs):
                S, Sk = srot.next()
                P.op("pe", "matmul", out=S[:, 0:nq], lhsT=kT[m * 32:(m + 1) * 32, kb * 128:(kb + 1) * 128], rhs=qT[m * 32:(m + 1) * 32, q0:q0 + nq],
                     start=True, stop=True, reads=["kT", "qT"], writes=[Sk])
                PT, PTk = ptrot.next()
                P.op("act", "activation", out=PT[:, 0:nq], in_=S[:, 0:nq], func=AF.Exp, reads=[Sk], writes=[PTk])
                pts[(i, gi)] = (PT, PTk)

        def issue_pv(i):
            kb, m = steps[i]
            for gi, (q0, nq) in enumerate(qgs):
                PT, PTk = pts.pop((i, gi))
                P.op("pe", "matmul", out=acc[gi][m][0][:, 0:nq], lhsT=V1[:, kb, :], rhs=PT[:, 0:nq], start=(kb == kb0), stop=(kb == kb1 - 1),
                     reads=["V1", PTk], writes=[acc[gi][m][1]])
        for i in range(len(steps) + LOOK):
            if i < len(steps):
                issue_s(i)
            if i - LOOK >= 0:
                issue_pv(i - LOOK)
        for gi, (q0, nq) in enumerate(qgs):
            epilogue(acc[gi], q0, nq)
    P.end_phase()


def swa_masks():
    b = np.arange(128)[:, None]
    a = np.arange(128)[None, :]
    m = np.concatenate([(b <= a), np.ones((128, 128), bool), (a <= b)], 1)
    return m.astype(np.float32).astype(ml_dtypes.bfloat16)


def emit_swa(P, N, M, need_ctx, qT_d, kT_d, v_d, sink_d, m3_d, oT_d):
    T = N + M
    NKB = T // 128
    MB = M // 128
    nb = N // 128

    qT = P.sb([64, T], BF16, "qT")
    kT = P.sb([64, T], BF16, "kT")
    V1 = P.sb([128, NKB, 65], BF16, "V1")
    nchunk = 4
    cw = T // nchunk
    for i in range(nchunk):
        P.dma("sp", qT[:, i * cw:(i + 1) * cw], qT_d[:, i * cw:(i + 1) * cw], writes=["qT"])
        P.dma("sp", kT[:, i * cw:(i + 1) * cw], kT_d[:, i * cw:(i + 1) * cw], writes=["kT"])
    vv = v_d.rearrange("(kb p) d -> p kb d", p=128)
    step = max(1, NKB // 8)
    for k0 in range(0, NKB, step):
        k1 = min(NKB, k0 + step)
        P.dma("sp", V1[:, k0:k1, 0:64], vv[:, k0:k1, :], writes=["V1"])
    P.op("pool", "memset", ap=V1[:, :, 64:65], constant=1.0, writes=["V1"])
    m3 = P.sb([128, 384], BF16, "m3")
    P.dma("sp", m3[:], m3_d, writes=["m3"])
    es = P.sb([128, 1], F32, "es")
    P.dma("sp", es[:], sink_d, writes=["es"])
    P.op("act", "activation", out=es[:], in_=es[:], func=AF.Exp, reads=["es"], writes=["es"])
    ones64 = P.sb([128, 64], F32, "ones64")
    P.op("pool", "memset", ap=ones64[:], constant=1.0, writes=["ones64"])
    accrot = Rot(P, "acc", 2, [128, 512], F32, psum=True)
    srot = Rot(P, "S", 3, [128, 512], F32, psum=True)
    eprot = Rot(P, "ep", 2, [128, 512], F32, psum=True)
    ptrot = Rot(P, "PT", 4, [128, 512], BF16)
    rlrot = Rot(P, "rl", 2, [128, 512], F32)
    bcrot = Rot(P, "bc", 2, [64, 512], F32)
    orot = Rot(P, "ob", 2, [64, 512], BF16)

    qgroups = []
    if need_ctx:
        qgroups.append((0, M, None))
    for s0 in range(0, nb, 4):
        qgroups.append((M + s0 * 128, min(4, nb - s0) * 128, s0))
    ei = 0
    for (q0, nq, s0) in qgroups:
        acc, acck = accrot.next()
        items = [(kb, 0, nq, None) for kb in range(MB)]
        if s0 is not None:
            ns = nq // 128
            for r in range(s0 - 1, s0 + ns + 1):
                if r < 0 or r >= nb:
                    continue
                sa = max(r - 1, s0)
                sb_ = min(r + 1, s0 + ns - 1)
                items.append((MB + r, (sa - s0) * 128, (sb_ - s0 + 1) * 128, (sa - (r - 1)) * 128))
        pend = {}
        eic = [ei]

        def issue_s(ii):
            kb, c0, c1, mc = items[ii]
            w = c1 - c0
            S, Sk = srot.next()
            P.op("pe", "matmul", out=S[:, 0:w], lhsT=kT[:, kb * 128:(kb + 1) * 128], rhs=qT[:, q0 + c0:q0 + c1], start=True, stop=True,
                 reads=["kT", "qT"], writes=[Sk])
            PT, PTk = ptrot.next()
            P.op("act", "activation", out=PT[:, 0:w], in_=S[:, 0:w], func=AF.Exp, reads=[Sk], writes=[PTk])
            if mc is not None:
                eng = "dve" if eic[0] % 2 == 0 else "pool"
                eic[0] += 1
                P.op(eng, "tensor_tensor", out=PT[:, 0:w], in0=PT[:, 0:w], in1=m3[:, mc:mc + w], op=ALU.mult, reads=[PTk, "m3"], writes=[PTk])
            pend[ii] = (PT, PTk)

        def issue_pv(ii):
            kb, c0, c1, mc = items[ii]
            w = c1 - c0
            PT, PTk = pend.pop(ii)
            P.op("pe", "matmul", out=acc[0:65, c0:c1], lhsT=V1[:, kb, :], rhs=PT[:, 0:w], start=(ii == 0), stop=(ii == len(items) - 1),
                 reads=["V1", PTk], writes=[acck])
        LOOK = 1
        for i in range(len(items) + LOOK):
            if i < len(items):
                issue_s(i)
            if i - LOOK >= 0:
                issue_pv(i - LOOK)
        ei = eic[0]
        rl, rlk = rlrot.next()
        P.op("dve", "tensor_scalar", out=rl[64:65, 0:nq], in0=acc[64:65, 0:nq], scalar1=es[64:65, 0:1], scalar2=None, op0=ALU.add, reads=[acck, "es"], writes=[rlk])
        P.op("dve", "reciprocal", out=rl[64:65, 0:nq], in_=rl[64:65, 0:nq], reads=[rlk], writes=[rlk])
        pm, pk = eprot.next()
        P.op("pe", "matmul", out=pm[0:64, 0:nq], lhsT=ones64[64:65, 0:64], rhs=rl[64:65, 0:nq], start=True, stop=True, reads=[rlk, "ones64"], writes=[pk])
        bc, bck = bcrot.next()
        P.op("act", "copy", out=bc[:, 0:nq], in_=pm[0:64, 0:nq], reads=[pk], writes=[bck])
        ob, obk = orot.next()
        P.op("dve", "tensor_tensor", out=ob[:, 0:nq], in0=acc[0:64, 0:nq], in1=bc[:, 0:nq], op=ALU.mult, reads=[acck, bck], writes=[obk])
        P.dma("sp", oT_d[:, q0:q0 + nq], ob[:, 0:nq], reads=[obk])
    P.end_phase()


def ret_consts():
    m = np.arange(128)[:, None].astype(np.float32)
    n = np.arange(128)[None, :].astype(np.float32)
    c = np.zeros((128, 6, 128), np.float32)
    c[:, 0] = np.maximum(n - m, 0)
    c[:, 1] = (n >= m)
    c[:, 2] = np.maximum(m - n, 0)
    c[:, 3] = (m >= n)
    c[:, 4] = n + 1
    c[:, 5] = 128 - n
    z = np.zeros((128, 2), np.float32)
    z[:, 0] = 127 - np.arange(128)
    z[:, 1] = np.arange(128)
    return c, z


def emit_ret(P, N, M, need_ctx, qT_d, kT_d, kt_d, v_d, gT_d, lg_d, gn_d, rc_d, rz_d, oT_d):
    T = N + M
    NC = T // 128
    MB = M // 128

    ktok = P.sb([128, NC, 64], BF16, "ktok")
    V = P.sb([128, NC, 64], BF16, "V")
    vv = v_d.rearrange("(kb p) d -> p kb d", p=128)
    kv = kt_d.rearrange("(kb p) d -> p kb d", p=128)
    step = max(1, NC // 8)
    for k0 in range(0, NC, step):
        k1 = min(NC, k0 + step)
        P.dma("sp", V[:, k0:k1, :], vv[:, k0:k1, :], writes=["V"])
        P.dma("sp", ktok[:, k0:k1, :], kv[:, k0:k1, :], writes=["ktok"])
    rc = P.sb([128, 6, 128], F32, "rc")
    P.dma("sp", rc[:], rc_d, writes=["rc"])
    rz = P.sb([128, 2], F32, "rz")
    P.dma("sp", rz[:], rz_d, writes=["rz"])
    gcol = P.sb([64, 1], F32, "gcol")
    P.dma("sp", gcol[:], gn_d, writes=["gcol"])
    lgc = P.sb([128, 2], F32, "lgc")
    P.dma("sp", lgc[:], lg_d, writes=["lgc"])
    P.op("act", "activation", out=lgc[:], in_=lgc[:], func=AF.Exp, scale=-1.0, reads=["lgc"], writes=["lgc"])
    P.op("dve", "tensor_scalar", out=lgc[:], in0=lgc[:], scalar1=1.0, scalar2=None, op0=ALU.add, reads=["lgc"], writes=["lgc"])
    P.op("act", "activation", out=lgc[:], in_=lgc[:], func=AF.Ln, reads=["lgc"], writes=["lgc"])
    P.op("dve", "tensor_scalar", out=lgc[:], in0=lgc[:], scalar1=-1.0, scalar2=None, op0=ALU.mult, reads=["lgc"], writes=["lgc"])
    DcT = P.sb([128, 128], F32, "DcT")
    E2 = P.sb([128, 128], F32, "E2")
    P.op("act", "activation", out=DcT[:], in_=rc[:, 0, :], func=AF.Exp, scale=lgc[:, 0:1], reads=["rc", "lgc"], writes=["DcT"])
    P.op("act", "activation", out=E2[:], in_=rc[:, 2, :], func=AF.Exp, scale=lgc[:, 1:2], reads=["rc", "lgc"], writes=["E2"])
    P.op("dve", "tensor_tensor", out=DcT[:], in0=DcT[:], in1=rc[:, 1, :], op=ALU.mult, reads=["DcT", "rc"], writes=["DcT"])
    P.op("dve", "tensor_tensor", out=E2[:], in0=E2[:], in1=rc[:, 3, :], op=ALU.mult, reads=["E2", "rc"], writes=["E2"])
    P.op("dve", "tensor_tensor", out=DcT[:], in0=DcT[:], in1=E2[:], op=ALU.add, reads=["DcT", "E2"], writes=["DcT"])
    xi = P.sb([64, 2, 128], F32, "xi")
    P.op("act", "activation", out=xi[:, 0, :], in_=rc[0:64, 4, :], func=AF.Exp, scale=lgc[0:64, 0:1], reads=["rc", "lgc"], writes=["xi"])
    P.op("act", "activation", out=xi[:, 1, :], in_=rc[0:64, 5, :], func=AF.Exp, scale=lgc[0:64, 1:2], reads=["rc", "lgc"], writes=["xi"])
    zc = P.sb([128, 2], F32, "zc")
    P.op("act", "activation", out=zc[:, 0:1], in_=rz[:, 0:1], func=AF.Exp, scale=lgc[:, 0:1], reads=["rz", "lgc"], writes=["zc"])
    P.op("act", "activation", out=zc[:, 1:2], in_=rz[:, 1:2], func=AF.Exp, scale=lgc[:, 1:2], reads=["rz", "lgc"], writes=["zc"])
    gc = P.sb([64, 2], F32, "gc")
    P.op("act", "activation", out=gc[:], in_=lgc[0:64, :], func=AF.Exp, scale=128.0, reads=["lgc"], writes=["gc"])
    ones64 = P.sb([128, 64], F32, "ones64")
    P.op("pool", "memset", ap=ones64[:], constant=1.0, writes=["ones64"])

    U = [P.sb([64, NC, 64], F32, f"U{d}") for d in range(2)]
    S16 = [P.sb([64, NC, 64], BF16, f"S16_{d}") for d in range(2)]
    urot = Rot(P, "ups", 2, [128, 512], F32, psum=True)
    kzrot = Rot(P, "kz", 4, [128, 64], BF16)
    for d in range(2):
        for c0 in range(0, NC, 8):
            c1 = min(NC, c0 + 8)
            pm, pk = urot.next()
            for c in range(c0, c1):
                kz, kzk = kzrot.next()
                P.op("dve" if d == 0 else "pool", "tensor_scalar", out=kz[:], in0=ktok[:, c, :], scalar1=zc[:, d:d + 1], scalar2=None, op0=ALU.mult,
                     reads=["ktok", "zc"], writes=[kzk])
                P.op("pe", "matmul", out=pm[0:64, (c - c0) * 64:(c - c0 + 1) * 64], lhsT=kz[:], rhs=V[:, c, :], start=True, stop=True,
                     reads=[kzk, "V"], writes=[pk])
            P.op("act", "copy", out=U[d][:, c0:c1, :], in_=pm[0:64, 0:(c1 - c0) * 64].rearrange("p (c e) -> p c e", e=64), reads=[pk], writes=[f"U{d}"])
    order = [list(range(NC)), list(range(MB - 1, -1, -1)) + list(range(NC - 1, MB - 1, -1))]
    prev = [{}, {}]
    for d in range(2):
        eng = "dve"
        od = order[d]
        for i in range(1, NC):
            prev[d][od[i]] = od[i - 1]
            P.op(eng, "scalar_tensor_tensor", out=U[d][:, od[i], :], in0=U[d][:, od[i - 1], :], scalar=gc[:, d:d + 1], in1=U[d][:, od[i], :],
                 op0=ALU.mult, op1=ALU.add, reads=[f"U{d}", "gc"], writes=[f"U{d}"])
        P.op(eng, "tensor_copy", out=S16[d][:], in_=U[d][:], reads=[f"U{d}"], writes=[f"S16_{d}"])
    accrot = Rot(P, "acc", 2, [128, 512], F32, psum=True)
    srot = Rot(P, "S", 2, [128, 512], F32, psum=True)
    eprot = Rot(P, "ep", 2, [128, 512], F32, psum=True)
    qrot = Rot(P, "qg", 2, [64, 512], BF16)
    krot = Rot(P, "kg", 2, [64, 512], BF16)
    grot = Rot(P, "gg", 2, [64, 512], F32)
    amrot = Rot(P, "am", 3, [128, 128], BF16)
    qxrot = Rot(P, "qx", 3, [64, 2, 128], BF16)
    t32rot = Rot(P, "t32", 3, [64, 512], F32)
    orot = Rot(P, "ob", 2, [64, 512], BF16)
    groups = []
    if need_ctx:
        groups.append(list(range(0, MB)))
    for c0 in range(MB, NC, 4):
        groups.append(list(range(c0, min(NC, c0 + 4))))
    for grp in groups:
        t0_ = grp[0] * 128
        nq = len(grp) * 128
        qg, qgk = qrot.next()
        kg, kgk = krot.next()
        gg, ggk = grot.next()
        P.dma("sp", qg[:, 0:nq], qT_d[:, t0_:t0_ + nq], writes=[qgk])
        P.dma("sp", kg[:, 0:nq], kT_d[:, t0_:t0_ + nq], writes=[kgk])
        P.dma("sp", gg[:, 0:nq], gT_d[:, t0_:t0_ + nq], writes=[ggk])
        P.op("act", "activation", out=gg[:, 0:nq], in_=gg[:, 0:nq], func=AF.Silu, reads=[ggk], writes=[ggk])
        acc, acck = accrot.next()
        for ci, c in enumerate(grp):
            cs = slice(ci * 128, (ci + 1) * 128)
            S, Sk = srot.next()
            P.op("pe", "matmul", out=S[:, 0:128], lhsT=kg[:, cs], rhs=qg[:, cs], start=True, stop=True, reads=[kgk, qgk], writes=[Sk])
            am, amk = amrot.next()
            P.op("dve", "tensor_tensor", out=am[:], in0=S[:, 0:128], in1=DcT[:], op=ALU.mult, reads=[Sk, "DcT"], writes=[amk])
            qx, qxk = qxrot.next()
            P.op("pool", "tensor_tensor", out=qx[:], in0=qg[:, cs].unsqueeze(1).to_broadcast([64, 2, 128]), in1=xi[:], op=ALU.mult, reads=[qgk, "xi"], writes=[qxk])
            mms = [(V[:, c, :], am[:], ["V", amk])]
            for d in range(2):
                if c in prev[d]:
                    mms.append((S16[d][:, prev[d][c], :], qx[:, d, :], [f"S16_{d}", qxk]))
            for mi, (l_, r_, rk_) in enumerate(mms):
                P.op("pe", "matmul", out=acc[0:64, cs], lhsT=l_, rhs=r_, start=(mi == 0), stop=(mi == len(mms) - 1), reads=rk_, writes=[acck])
        y, yk = t32rot.next()
        P.op("act", "copy", out=y[:, 0:nq], in_=acc[0:64, 0:nq], reads=[acck], writes=[yk])
        yn, ynk = t32rot.next()
        headnorm_T(P, y[:, 0:nq], yk, nq, ones64, eprot, gcol[:, 0:1], "gcol", 1.0, yn[:, 0:nq], ynk, t32rot)
        ob, obk = orot.next()
        P.op("pool", "tensor_tensor", out=ob[:, 0:nq], in0=yn[:, 0:nq], in1=gg[:, 0:nq], op=ALU.mult, reads=[ynk, ggk], writes=[obk])
        P.dma("sp", oT_d[:, t0_:t0_ + nq], ob[:, 0:nq], reads=[obk])
    P.end_phase()


def _bk(name, c0, c1):
    return [f"{name}{b}" for b in range(c0 // 512, (c1 - 1) // 512 + 1)]


def emit_lru(P, N, M, lx_d, ly_d, cw_d, cb_d, wa_d, wx_d, ba_d, bx_d, lam_d, i2_d, oT_d):
    T = N + M

    XX = P.sb([128, T], F32, "XX")
    UU = P.sb([128, T], F32, "UU")
    CH = 2048
    for c0 in range(0, T, CH):
        c1 = min(T, c0 + CH)
        P.dma("sp", XX[0:64, c0:c1], lx_d[:, c0:c1], writes=_bk("XX", c0, c1))
        P.dma("sp", XX[64:128, c0:c1], lx_d[:, c0:c1], writes=_bk("XX", c0, c1))
    small = {}
    for nm, d_, shp in (("cw", cw_d, [128, 4]), ("cb", cb_d, [128, 1]), ("wa", wa_d, [64, 128]), ("wx", wx_d, [64, 128]),
                        ("ba", ba_d, [128, 1]), ("bx", bx_d, [128, 1]), ("lam", lam_d, [128, 1]), ("i2", i2_d, [128, 64])):
        t_ = P.sb(shp, F32, nm)
        P.dma("sp", t_[:], d_, writes=[nm])
        small[nm] = t_
    cw, cb, wa, wx, ba, bx, lam, i2 = (small[k] for k in ("cw", "cb", "wa", "wx", "ba", "bx", "lam", "i2"))
    c8 = P.sb([128, 1], F32, "c8")
    P.op("act", "activation", out=c8[:], in_=lam[:], func=AF.Exp, scale=-1.0, reads=["lam"], writes=["c8"])
    P.op("dve", "tensor_scalar", out=c8[:], in0=c8[:], scalar1=1.0, scalar2=None, op0=ALU.add, reads=["c8"], writes=["c8"])
    P.op("act", "activation", out=c8[:], in_=c8[:], func=AF.Ln, reads=["c8"], writes=["c8"])
    P.op("dve", "tensor_scalar", out=c8[:], in0=c8[:], scalar1=-8.0, scalar2=None, op0=ALU.mult, reads=["c8"], writes=["c8"])
    for (s0, s1) in ((0, M), (M, T)):
        for c0 in range(s0, s1, CH):
            c1 = min(s1, c0 + CH)
            P.op("dve", "tensor_scalar", out=UU[:, c0:c1], in0=XX[:, c0:c1], scalar1=cw[:, 1:2], scalar2=cb[:, 0:1], op0=ALU.mult, op1=ALU.add,
                 reads=_bk("XX", c0, c1) + ["cw", "cb"], writes=_bk("UU", c0, c1))
            for (tap, sh) in ((0, -1), (2, 1), (3, 2)):
                o0, o1 = max(c0, s0 - sh), min(c1, s1 - sh)
                if o1 <= o0:
                    continue
                P.op("dve", "scalar_tensor_tensor", out=UU[:, o0:o1], in0=XX[:, o0 + sh:o1 + sh], scalar=cw[:, tap:tap + 1], in1=UU[:, o0:o1],
                     op0=ALU.mult, op1=ALU.add, reads=_bk("XX", o0 + sh, o1 + sh) + _bk("UU", o0, o1) + ["cw"], writes=_bk("UU", o0, o1))
    grot = Rot(P, "gps", 4, [128, 512], F32, psum=True)
    rsrot = Rot(P, "rs", 2, [128, 512], F32)
    isrot = Rot(P, "is", 2, [128, 512], F32)
    t1rot = Rot(P, "t1", 2, [128, 512], F32)
    for c0 in range(0, T, 512):
        c1 = min(T, c0 + 512)
        w = c1 - c0
        uk = _bk("UU", c0, c1)
        xk = _bk("XX", c0, c1)
        pr, prk = grot.next()
        pi, pik = grot.next()
        P.op("pe", "matmul", out=pr[:, 0:w], lhsT=wa[:, :], rhs=UU[0:64, c0:c1], start=True, stop=True, reads=uk + ["wa"], writes=[prk])
        P.op("pe", "matmul", out=pi[:, 0:w], lhsT=wx[:, :], rhs=UU[0:64, c0:c1], start=True, stop=True, reads=uk + ["wx"], writes=[pik])
        rs, rsk = rsrot.next()
        is_, isk = isrot.next()
        P.op("act", "activation", out=rs[:, 0:w], in_=pr[:, 0:w], func=AF.Sigmoid, bias=ba[:, 0:1], reads=[prk, "ba"], writes=[rsk])
        P.op("act", "activation", out=is_[:, 0:w], in_=pi[:, 0:w], func=AF.Sigmoid, bias=bx[:, 0:1], reads=[pik, "bx"], writes=[isk])
        P.op("act", "activation", out=XX[:, c0:c1], in_=rs[:, 0:w], func=AF.Exp, scale=c8[:, 0:1], reads=[rsk, "c8"], writes=xk)
        t1, t1k = t1rot.next()
        P.op("pool", "tensor_tensor", out=t1[:, 0:w], in0=XX[:, c0:c1], in1=XX[:, c0:c1], op=ALU.mult, reads=xk, writes=[t1k])
        P.op("pool", "tensor_scalar", out=t1[:, 0:w], in0=t1[:, 0:w], scalar1=-1.0, scalar2=1.0, op0=ALU.mult, op1=ALU.add, reads=[t1k], writes=[t1k])
        P.op("act", "activation", out=t1[:, 0:w], in_=t1[:, 0:w], func=AF.Sqrt, reads=[t1k], writes=[t1k])
        P.op("dve", "tensor_tensor", out=is_[:, 0:w], in0=is_[:, 0:w], in1=UU[:, c0:c1], op=ALU.mult, reads=[isk] + uk, writes=[isk])
        P.op("dve", "tensor_tensor", out=UU[:, c0:c1], in0=is_[:, 0:w], in1=t1[:, 0:w], op=ALU.mult, reads=[isk, t1k], writes=uk)
    prev = None
    for c0 in range(0, T, CH):
        c1 = min(T, c0 + CH)
        init = 0.0 if prev is None else UU[0:64, c0 - 1:c0]
        P.op("dve", "tensor_tensor_scan", out=UU[0:64, c0:c1], data0=XX[0:64, c0:c1], data1=UU[0:64, c0:c1], initial=init, op0=ALU.mult, op1=ALU.add,
             reads=_bk("XX", c0, c1) + _bk("UU", max(c0 - 1, 0), c1) + ["scanf"], writes=_bk("UU", c0, c1) + ["scanf"])
        prev = c0
    chunks = [(c0, min(M, c0 + CH)) for c0 in range(0, M, CH)][::-1] + [(c0, min(T, c0 + CH)) for c0 in range(M, T, CH)][::-1]
    prev_lo = None
    for (c0, c1) in chunks:
        init = 0.0 if prev_lo is None else UU[64:128, prev_lo:prev_lo + 1]
        rk = [] if prev_lo is None else _bk("UU", prev_lo, prev_lo + 1)
        P.op("dve", "tensor_tensor_scan", out=UU[64:128, c0:c1][:, ::-1], data0=XX[64:128, c0:c1][:, ::-1], data1=UU[64:128, c0:c1][:, ::-1], initial=init,
             op0=ALU.mult, op1=ALU.add, reads=_bk("XX", c0, c1) + _bk("UU", c0, c1) + rk + ["scanb"], writes=_bk("UU", c0, c1) + ["scanb"])
        prev_lo = c0
    hrot = Rot(P, "hps", 2, [128, 512], F32, psum=True)
    yrot = Rot(P, "yy", 2, [64, 512], F32)
    zrot = Rot(P, "zz", 2, [64, 512], F32)
    orot = Rot(P, "ob", 2, [64, 512], BF16)
    for c0 in range(0, T, 512):
        c1 = min(T, c0 + 512)
        w = c1 - c0
        hp, hpk = hrot.next()
        P.op("pe", "matmul", out=hp[0:64, 0:w], lhsT=i2[:, :], rhs=UU[:, c0:c1], start=True, stop=True, reads=_bk("UU", c0, c1) + ["i2"], writes=[hpk])
        yy, yk = yrot.next()
        zz, zk = zrot.next()
        P.dma("sp", yy[:, 0:w], ly_d[:, c0:c1], writes=[yk])
        P.op("pool", "tensor_tensor", out=zz[:, 0:w], in0=yy[:, 0:w], in1=yy[:, 0:w], op=ALU.mult, reads=[yk], writes=[zk])
        P.op("pool", "tensor_scalar", out=zz[:, 0:w], in0=zz[:, 0:w], scalar1=0.044715, scalar2=1.0, op0=ALU.mult, op1=ALU.add, reads=[zk], writes=[zk])
        P.op("pool", "tensor_tensor", out=zz[:, 0:w], in0=zz[:, 0:w], in1=yy[:, 0:w], op=ALU.mult, reads=[zk, yk], writes=[zk])
        P.op("act", "activation", out=zz[:, 0:w], in_=zz[:, 0:w], func=AF.Sigmoid, scale=2.0 * math.sqrt(2.0 / math.pi), reads=[zk], writes=[zk])
        P.op("pool", "tensor_tensor", out=zz[:, 0:w], in0=zz[:, 0:w], in1=yy[:, 0:w], op=ALU.mult, reads=[zk, yk], writes=[zk])
        ob, obk = orot.next()
        P.op("dve", "tensor_tensor", out=ob[:, 0:w], in0=hp[0:64, 0:w], in1=zz[:, 0:w], op=ALU.mult, reads=[hpk, zk], writes=[obk])
        P.dma("sp", oT_d[:, c0:c1], ob[:, 0:w], reads=[obk])
    P.end_phase()


def emit_f(P, tile_specs, nv, E, FF, moe, final, modrows_d, sel_d, wo_d, w1_d, w3_d, w2_d, ident_d, wr_d=None, id32_d=None, fn_d=None, GT=8, after_tile=None):
    ntile = len(tile_specs)
    NFB = FF // 128
    nhalf = 2
    HB = NFB // nhalf
    ident = P.sb([128, 128], BF16, "ident")
    P.dma("sp", ident[:], ident_d, writes=["ident"])
    sel = P.sb([2, 2, 128], F32, "sel")
    P.dma("sp", sel[:], sel_d, writes=["sel"])
    mrrot = Rot(P, "mrowF", 2, [2, 1024], F32)
    pmrot = Rot(P, "pm", 3, [128, 512], F32, psum=True)
    mt = [[P.sb([128, D], F32, f"mt{v}_{i}") for i in range(4)] for v in range(nv)]
    for i in range(4):
        mrow, mrk = mrrot.next()
        P.dma("sp", mrow[:], modrows_d[:, i * 1024:(i + 1) * 1024], writes=[mrk])
        for v in range(nv):
            bcast_rows(P, mrow, mrk, sel, v, pmrot, mt[v][i], f"mt{v}_{i}")
    if final:
        fnb = P.sb([128, D], F32, "fnb")
        P.dma("sp", fnb[:], fn_d, writes=["fnb"])
    Wo = P.sb([128, 8, D], BF16, "Wo")
    wov = wo_d.rearrange("(k p) c -> p k c", p=128)
    for k in range(8):
        P.dma("pool", Wo[:, k, :], wov[:, k, :], writes=["Wo"])
    if moe:
        Wr = P.sb([128, 8, E], F32, "Wr")
        P.dma("sp", Wr[:], wr_d.rearrange("(k p) e -> p k e", p=128), writes=["Wr"])
        id32 = P.sb([128, 128], F32, "id32")
        P.dma("sp", id32[:], id32_d, writes=["id32"])
        gates = P.sb([128, GT, E], F32, "gates")
        h32T = P.sb([128, 8, 128], F32, "h32T")
        rsm = Rot(P, "rsm", 2, [128, 40], F32)
    purot = Rot(P, "pu", 4, [128, 512], F32, psum=True)
    rots = {
        "sq": Rot(P, "sq", 2, [128, D], F32),
        "st": Rot(P, "st", 4, [128, 4], F32),
        "hb": Rot(P, "hb", 2, [128, D], BF16),
        "ptr": Rot(P, "ptr", 1, [128, 1024], BF16, psum=True),
    }
    xrot = Rot(P, "xt", 2, [128, D], F32)
    otrot = Rot(P, "oTt", 2, [128, 8, 128], BF16)
    acc = P.sb([128, GT, D], F32, "acc")
    h2T = P.sb([128, 8, GT * 128], BF16, "h2T")
    gT = P.sb([128, HB, GT * 128], BF16, "gT")
    W2 = P.sb([128, HB, D], BF16, "W2")
    w1rot = Rot(P, "w1b", 2, [128, 8, 256], BF16)
    w3rot = Rot(P, "w3b", 2, [128, 8, 256], BF16)
    srot = Rot(P, "sil", 2, [128, 512], F32)
    tmprot = Rot(P, "ftmp", 2, [128, 512], F32)
    orot = Rot(P, "xout", 1, [128, D], F32) if final else None

    tiles_v = [ts_[0] for ts_ in tile_specs]
    for g0 in range(0, ntile, GT):
        tiles = list(range(g0, min(ntile, g0 + GT)))
        ng = len(tiles)
        ntok = ng * 128
        for ti, tile in enumerate(tiles):
            v = tiles_v[tile]
            acck = f"acc{ti}"
            xt, xk = xrot.next()
            P.dma("sp", xt[:], tile_specs[tile][1], writes=[xk])
            ot, otk = otrot.next()
            P.dma("sp", ot[:], tile_specs[tile][2], reads=["l_oT"], writes=[otk])
            for hh in range(2):
                pm, pk = pmrot.next()
                for k in range(8):
                    P.op("pe", "matmul", out=pm[:, :], lhsT=ot[:, k, :], rhs=Wo[:, k, hh * 512:(hh + 1) * 512], start=(k == 0), stop=(k == 7),
                         reads=[otk, "Wo"], writes=[pk])
                hs = slice(hh * 512, (hh + 1) * 512)
                P.op("dve", "tensor_tensor", out=acc[:, ti, hs], in0=pm[:, :], in1=mt[v][0][:, hs], op=ALU.mult, reads=[pk, f"mt{v}_0"], writes=[acck])
                P.op("pool", "tensor_tensor", out=acc[:, ti, hs], in0=acc[:, ti, hs], in1=xt[:, hs], op=ALU.add, reads=[acck, xk], writes=[acck])
            st, stk, sq, sqk = rms_rstd(P, acc[:, ti, :], acck, rots)
            P.op("dve", "scalar_tensor_tensor", out=sq[:], in0=acc[:, ti, :], scalar=st[:, 3:4], in1=mt[v][2][:], op0=ALU.mult, op1=ALU.mult,
                 reads=[acck, stk, f"mt{v}_2"], writes=[sqk])
            P.op("pool", "tensor_tensor", out=sq[:], in0=sq[:], in1=mt[v][1][:], op=ALU.add, reads=[sqk, f"mt{v}_1"], writes=[sqk])
            hb, hbk = rots["hb"].next()
            P.op("act", "copy", out=hb[:], in_=sq[:], reads=[sqk], writes=[hbk])
            ptr, ptk = rots["ptr"].next()
            for k in range(8):
                P.op("pe", "transpose", out=ptr[:, k * 128:(k + 1) * 128], in_=hb[:, k * 128:(k + 1) * 128], identity=ident[:], reads=[hbk, "ident"], writes=[ptk])
            P.op("act", "copy", out=h2T[:, :, ti * 128:(ti + 1) * 128], in_=ptr[:, :].rearrange("p (k t) -> p k t", k=8), reads=[ptk], writes=[f"h2T{ti}"])
            if moe:
                for hh in range(2):
                    pm, pk = pmrot.next()
                    for k4 in range(4):
                        k = hh * 4 + k4
                        P.op("pe", "transpose", out=pm[:, k4 * 128:(k4 + 1) * 128], in_=sq[:, k * 128:(k + 1) * 128], identity=id32[:], reads=[sqk, "id32"], writes=[pk])
                    P.op("act", "copy", out=h32T[:, hh * 4:(hh + 1) * 4, :], in_=pm[:, :].rearrange("p (k t) -> p k t", k=4), reads=[pk], writes=["h32T"])
                pm, pk = pmrot.next()
                for k in range(8):
                    P.op("pe", "matmul", out=pm[:, 0:E], lhsT=h32T[:, k, :], rhs=Wr[:, k, :], start=(k == 0), stop=(k == 7), reads=["h32T", "Wr"], writes=[pk])
                r, rk = rsm.next()
                lg, eq1, lg2, eq2 = r[:, 0:E], r[:, 8:8 + E], r[:, 16:16 + E], r[:, 24:24 + E]
                m1, m2, dd, ww1, ww2 = (r[:, 32 + i:33 + i] for i in range(5))
                P.op("act", "copy", out=lg, in_=pm[:, 0:E], reads=[pk], writes=[rk])
                P.op("dve", "tensor_reduce", out=m1, in_=lg, axis=AX.X, op=ALU.max, reads=[rk], writes=[rk])
                P.op("dve", "tensor_scalar", out=eq1, in0=lg, scalar1=m1, scalar2=None, op0=ALU.is_equal, reads=[rk], writes=[rk])
                P.op("dve", "scalar_tensor_tensor", out=lg2, in0=eq1, scalar=-1e30, in1=lg, op0=ALU.mult, op1=ALU.add, reads=[rk], writes=[rk])
                P.op("dve", "tensor_reduce", out=m2, in_=lg2, axis=AX.X, op=ALU.max, reads=[rk], writes=[rk])
                P.op("dve", "tensor_scalar", out=eq2, in0=lg2, scalar1=m2, scalar2=None, op0=ALU.is_equal, reads=[rk], writes=[rk])
                P.op("dve", "tensor_tensor", out=dd, in0=m2, in1=m1, op=ALU.subtract, reads=[rk], writes=[rk])
                P.op("act", "activation", out=dd, in_=dd, func=AF.Exp, reads=[rk], writes=[rk])
                P.op("dve", "tensor_scalar", out=ww1, in0=dd, scalar1=1.0, scalar2=None, op0=ALU.add, reads=[rk], writes=[rk])
                P.op("dve", "reciprocal", out=ww1, in_=ww1, reads=[rk], writes=[rk])
                P.op("dve", "tensor_tensor", out=ww2, in0=dd, in1=ww1, op=ALU.mult, reads=[rk], writes=[rk])
                P.op("dve", "tensor_scalar", out=gates[:, ti, :], in0=eq1, scalar1=ww1, scalar2=None, op0=ALU.mult, reads=[rk], writes=[f"gates{ti}"])
                P.op("dve", "scalar_tensor_tensor", out=gates[:, ti, :], in0=eq2, scalar=ww2, in1=gates[:, ti, :], op0=ALU.mult, op1=ALU.add, reads=[rk, f"gates{ti}"], writes=[f"gates{ti}"])
        h2keys = [f"h2T{ti}" for ti in range(ng)]
        chunks = [(c0, min(ntok, c0 + 512)) for c0 in range(0, ntok, 512)]
        for e in range(E):
            for half in range(nhalf):
                fb0 = half * HB
                w2v = w2_d[e, fb0 * 128:(fb0 + HB) * 128, :].rearrange("(f p) c -> p f c", p=128)
                for fp in range(0, HB, 2):
                    nb_ = min(2, HB - fp)
                    w1b, w1k = w1rot.next()
                    w3b, w3k = w3rot.next()
                    cs = slice((fb0 + fp) * 128, (fb0 + fp + nb_) * 128)
                    P.dma("pool", w1b[:, :, 0:nb_ * 128], w1_d[e, :, cs].rearrange("(k p) c -> p k c", p=128), writes=[w1k])
                    P.dma("pool", w3b[:, :, 0:nb_ * 128], w3_d[e, :, cs].rearrange("(k p) c -> p k c", p=128), writes=[w3k])
                    P.dma("pool", W2[:, fp:fp + nb_, :], w2v[:, fp:fp + nb_, :], writes=["W2"])
                    for bi in range(nb_):
                        fbi = fp + bi
                        for (c0, c1) in chunks:
                            w = c1 - c0
                            hk = h2keys[c0 // 128:(c1 - 1) // 128 + 1]
                            p1, p1k = purot.next()
                            p3, p3k = purot.next()
                            for k in range(8):
                                P.op("pe", "matmul", out=p1[:, 0:w], lhsT=w1b[:, k, bi * 128:(bi + 1) * 128], rhs=h2T[:, k, c0:c1], start=(k == 0), stop=(k == 7),
                                     reads=[w1k] + hk, writes=[p1k])
                            for k in range(8):
                                P.op("pe", "matmul", out=p3[:, 0:w], lhsT=w3b[:, k, bi * 128:(bi + 1) * 128], rhs=h2T[:, k, c0:c1], start=(k == 0), stop=(k == 7),
                                     reads=[w3k] + hk, writes=[p3k])
                            sl, slk = srot.next()
                            P.op("act", "activation", out=sl[:, 0:w], in_=p1[:, 0:w], func=AF.Silu, reads=[p1k], writes=[slk])
                            P.op("dve", "tensor_tensor", out=gT[:, fbi, c0:c1], in0=p3[:, 0:w], in1=sl[:, 0:w], op=ALU.mult, reads=[p3k, slk], writes=[f"gT{c0 // 512}"])
                for ti, tile in enumerate(tiles):
                    v = tiles_v[tile]
                    for hh in range(2):
                        hs = slice(hh * 512, (hh + 1) * 512)
                        pm, pk = pmrot.next()
                        for fbi in range(HB):
                            P.op("pe", "matmul", out=pm[:, :], lhsT=gT[:, fbi, ti * 128:(ti + 1) * 128], rhs=W2[:, fbi, hs], start=(fbi == 0), stop=(fbi == HB - 1),
                                 reads=[f"gT{ti // 4}", "W2"], writes=[pk])
                        tmp, tmpk = tmprot.next()
                        if moe:
                            P.op("dve", "scalar_tensor_tensor", out=tmp[:], in0=pm[:, :], scalar=gates[:, ti, e:e + 1], in1=mt[v][3][:, hs], op0=ALU.mult, op1=ALU.mult,
                                 reads=[pk, f"gates{ti}", f"mt{v}_3"], writes=[tmpk])
                        else:
                            P.op("dve", "tensor_tensor", out=tmp[:], in0=pm[:, :], in1=mt[v][3][:, hs], op=ALU.mult, reads=[pk, f"mt{v}_3"], writes=[tmpk])
                        P.op("pool", "tensor_tensor", out=acc[:, ti, hs], in0=acc[:, ti, hs], in1=tmp[:], op=ALU.add, reads=[f"acc{ti}", tmpk], writes=[f"acc{ti}"])
        for ti, tile in enumerate(tiles):
            if final:
                st, stk, sq, sqk = rms_rstd(P, acc[:, ti, :], f"acc{ti}", rots)
                xo_t, xok = orot.next()
                P.op("dve", "scalar_tensor_tensor", out=xo_t[:], in0=acc[:, ti, :], scalar=st[:, 3:4], in1=fnb[:], op0=ALU.mult, op1=ALU.mult,
                     reads=[f"acc{ti}", stk, "fnb"], writes=[xok])
                P.dma("sp", tile_specs[tile][3], xo_t[:], reads=[xok], writes=["xout_d"])
            else:
                P.dma("sp", tile_specs[tile][3], acc[:, ti, :], reads=[f"acc{ti}"], writes=["xout_d", "xsrc", f"xo{tile}"])
                if after_tile is not None:
                    after_tile(tile)
    P.end_phase()


def build_fused(N, M, E, FFD, FFE):
    T = N + M
    tps = N // 4
    nc = bass.Bass("TRN2", target_bir_lowering=False)
    P = Prog(nc)
    di = lambda n, s, d=F32: nc.dram_tensor(n, s, d, kind="ExternalInput").ap()
    do = lambda n, s, d=F32: nc.dram_tensor(n, s, d, kind="ExternalOutput").ap()
    sc = lambda n, s, d=F32: nc.dram_tensor(n, s, d).ap()
    xsh = di("xsh", [tps, D])
    ctx = di("ctx", [M, D])
    cT = di("cT", [128, 8, 2])
    tabs = di("tabs", [N, 160])
    ident_d = di("ident", [128, 128], BF16)
    id32_d = di("id32", [128, 128])
    sel_d = di("sel", [2, 2, 128])
    m3_d = di("m3", [128, 384], BF16)
    rc_d = di("retc", [128, 6, 128])
    rz_d = di("retz", [128, 2])
    i2_d = di("i2", [128, 64])
    fn_d = di("fnb", [128, D])
    L = []
    for l in range(2):
        L.append(dict(
            wada=di(f"wada{l}", [D, 6144]), bada=di(f"bada{l}", [1, 6144]), gn1=di(f"gn1_{l}", [1, D]), gn2=di(f"gn2_{l}", [1, D]),
            wt=di(f"wt{l}", [D, 576]), wf=di(f"wf{l}", [D, 192]), wo=di(f"wo{l}", [D, D]),
            sink=di(f"sink{l}", [128, 1]), logit=di(f"logit{l}", [128, 2]), rgn=di(f"rgn{l}", [64, 1]),
            lamv=di(f"lamv{l}", [1, 128]), dgn=di(f"dgn{l}", [64, 1]),
            cw=di(f"cw{l}", [128, 4]), cb=di(f"cb{l}", [128, 1]), wa=di(f"wa{l}", [64, 128]), wx=di(f"wx{l}", [64, 128]),
            ba=di(f"ba{l}", [128, 1]), bx=di(f"bx{l}", [128, 1]), lam=di(f"lam{l}", [128, 1])))
    fw1, fw3, fw2 = di("fw1", [1, D, FFD]), di("fw3", [1, D, FFD]), di("fw2", [1, FFD, D])
    mw1, mw3, mw2 = di("mw1", [E, D, FFE]), di("mw3", [E, D, FFE]), di("mw2", [E, FFE, D])
    wr_d = di("wr", [D, E])
    xo = do("xo", [tps, D])
    s_qA, s_kB = sc("s_qA", [128, T], BF16), sc("s_kB", [128, T], BF16)
    s_rq, s_rk = sc("s_rq", [64, T], BF16), sc("s_rk", [64, T], BF16)
    s_tok = sc("s_tok", [T, 256], BF16)
    s_f32 = sc("s_f32", [192, T])
    s_mod = sc("s_mod", [2, 4096])
    s_oT = sc("s_oT", [256, T], BF16)
    CHT = tps // 2
    NCH = N // CHT
    st_o = sc("st_o", [NCH * 256, CHT], BF16)
    st_oc = sc("st_oc", [256, M], BF16)
    g_o = sc("g_o", [NCH * 1024, CHT], BF16)
    g_oc = sc("g_oc", [1024, M], BF16)
    l_oT = sc("l_oT", [1024, tps], BF16)
    s_x0 = sc("s_x0", [tps, D])
    s_x1 = sc("s_x1", [tps, D])
    XR = min(256, tps)
    NXC = tps // XR
    g_x = [sc(f"g_x{c}", [4 * XR, D]) for c in range(NXC)]
    s_xc1 = sc("s_xc1", [M, D])
    RG = [[0, 1, 2, 3], [4, 5, 6, 7]]
    l_oTv = l_oT.rearrange("(k p) t -> p k t", p=128)
    g_ocv = g_oc.rearrange("(k p) t -> p k t", p=128)

    def gather_x(src):
        for c in range(NXC):
            P.op("pool", "collective_compute", cc=True, kind="AllGather", op=ALU.bypass, replica_groups=RG,
                 ins=[src[c * XR:(c + 1) * XR, :]], outs=[g_x[c]], reads=["xsrc"], writes=["g_x"])

    def x_tile(ti):
        t0 = ti * 128
        r, rem = t0 // tps, t0 % tps
        c, i = rem // XR, rem % XR
        return g_x[c][r * XR + i:r * XR + i + 128, :]
    for r0 in range(0, tps, max(128, tps // 4)):
        r1 = min(tps, r0 + max(128, tps // 4))
        P.dma("sp", s_x0[r0:r1, :], xsh[r0:r1, :], writes=["xsrc"])
    gather_x(s_x0)
    for l in range(2):
        W = L[l]
        need_ctx = (l == 0)
        lam_init = 0.8 - 0.6 * math.exp(-0.3 * l)
        x_lat = x_tile
        x_ctx = ctx if l == 0 else s_xc1
        emit_p(P, N, M, x_lat, x_ctx, cT, W["wada"], W["bada"], W["gn1"], W["gn2"], W["wt"], W["wf"], tabs, ident_d, sel_d,
               s_qA, s_kB, s_rq, s_rk, s_tok, s_f32, s_mod)
        emit_swa(P, N, M, need_ctx, s_qA[64:128, :], s_kB[64:128, :], s_tok[:, 64:128], W["sink"], m3_d, s_oT[0:64, :])
        emit_ret(P, N, M, need_ctx, s_rq, s_rk, s_tok[:, 0:64], s_tok[:, 128:192], s_f32[0:64, :], W["logit"], W["rgn"], rc_d, rz_d, s_oT[64:128, :])
        emit_diff(P, N, M, need_ctx, lam_init, s_qA[0:64, :], s_kB[0:64, :], s_tok[:, 192:256], W["lamv"], W["dgn"], s_oT[128:192, :])
        emit_lru(P, N, M, s_f32[64:128, :], s_f32[128:192, :], W["cw"], W["cb"], W["wa"], W["wx"], W["ba"], W["bx"], W["lam"], i2_d, s_oT[192:256, :])
        for c in range(NCH):
            P.dma("sp", st_o[c * 256:(c + 1) * 256, :], s_oT[:, M + c * CHT:M + (c + 1) * CHT], writes=[f"st_o{c}"])
        if need_ctx:
            P.dma("sp", st_oc[:, :], s_oT[:, 0:M], writes=["st_oc"])
        for c in range(NCH):
            P.op("pool", "collective_compute", cc=True, kind="AllGather", op=ALU.bypass, replica_groups=RG,
                 ins=[st_o[c * 256:(c + 1) * 256, :]], outs=[g_o[c * 1024:(c + 1) * 1024, :]], reads=[f"st_o{c}"], writes=["g_o"])
        if need_ctx:
            P.op("pool", "collective_compute", cc=True, kind="AllGather", op=ALU.bypass, replica_groups=RG,
                 ins=[st_oc], outs=[g_oc], reads=["st_oc"], writes=["g_oc"])
        P.end_phase()
        pidc = {}

        def shard_rows(e, pidc=pidc):
            if id(e) not in pidc:
                pidc[id(e)] = e.snap((e.partition_id() % 4) * 2048)
            return pidc[id(e)]
        for h in range(2):
            P.dma("sp", l_oT[:, h * CHT:(h + 1) * CHT],
                  (lambda e, h=h, sr=shard_rows: g_o[bass.ds(sr(e) + h * 1024, 1024), :]), writes=["l_oT"])
        specs = []
        for i in range(tps // 128):
            if l == 0:
                xs = xsh[i * 128:(i + 1) * 128, :]
                od = s_x1[i * 128:(i + 1) * 128, :]
            else:
                xs = s_x1[i * 128:(i + 1) * 128, :]
                od = xo[i * 128:(i + 1) * 128, :]
            specs.append((0, xs, l_oTv[:, :, i * 128:(i + 1) * 128], od))
        if need_ctx:
            for i in range(M // 128):
                specs.append((1, ctx[i * 128:(i + 1) * 128, :], g_ocv[:, :, i * 128:(i + 1) * 128], s_xc1[i * 128:(i + 1) * 128, :]))
        if l == 0:
            def after_tile(tile):
                per = XR // 128
                if tile < tps // 128 and tile % per == per - 1:
                    c = tile // per
                    P.op("pool", "collective_compute", cc=True, kind="AllGather", op=ALU.bypass, replica_groups=RG,
                         ins=[s_x1[c * XR:(c + 1) * XR, :]], outs=[g_x[c]], reads=[f"xo{t_}" for t_ in range(c * per, (c + 1) * per)], writes=["g_x"])
            emit_f(P, specs, 2, 1, FFD, False, False, s_mod, sel_d, W["wo"], fw1, fw3, fw2, ident_d, after_tile=after_tile)
        else:
            emit_f(P, specs, 1, E, FFE, True, True, s_mod, sel_d, W["wo"], mw1, mw3, mw2, ident_d, wr_d=wr_d, id32_d=id32_d, fn_d=fn_d)
    P.close()
    return nc


NCORES = 8
_cache = {}


def _get(name, fn, *a):
    key = (name,) + a
    if key not in _cache:
        _cache[key] = fn(*a)
    return _cache[key]


def _wo_perm():
    idx = np.zeros(1024, np.int64)
    for j in range(4):
        for m in range(4):
            idx[j * 256 + m * 64:j * 256 + (m + 1) * 64] = np.arange(m * 256 + j * 64, m * 256 + (j + 1) * 64)
    return idx


def make_maps(inp):
    f32 = np.float32
    B, N, _ = inp["x"].shape
    M = inp["ctx"].shape[1]
    ident = np.eye(128, dtype=f32).astype(ml_dtypes.bfloat16)
    sel = np.zeros((2, 2, 128), f32)
    sel[0, 0, :] = 1.0
    sel[1, 1, :] = 1.0
    rc, rz = ret_consts()
    tabs = rope_tables(N)
    common = {"ident": ident, "id32": np.eye(128, dtype=f32), "sel": sel, "m3": swa_masks(), "retc": rc, "retz": rz,
              "i2": np.concatenate([np.eye(64, dtype=f32)] * 2, 0), "tabs": tabs,
              "fnb": np.ascontiguousarray(np.broadcast_to(inp["final_norm"][None, :], (128, D))).astype(f32),
              "fw1": inp["ffn_w1"], "fw3": inp["ffn_w3"], "fw2": inp["ffn_w2"],
              "mw1": inp["moe_w1"][0], "mw3": inp["moe_w3"][0], "mw2": inp["moe_w2"][0], "wr": inp["moe_router"][0]}
    woidx = _wo_perm()
    for l in range(2):
        common[f"wada{l}"] = inp["w_ada"][l]
        common[f"bada{l}"] = np.ascontiguousarray(inp["b_ada"][l][None, :])
        common[f"gn1_{l}"] = np.ascontiguousarray(inp["g_norm1"][l][None, :])
        common[f"gn2_{l}"] = np.ascontiguousarray(inp["g_norm2"][l][None, :])
        common[f"wo{l}"] = np.ascontiguousarray(inp["w_out"][l][woidx, :])
        common[f"lamv{l}"] = np.ascontiguousarray(inp["diff_lambda"][l].reshape(1, 128))
    maps = []
    dup = lambda a: np.ascontiguousarray(np.concatenate([a, a], 0)).astype(f32)
    for core in range(NCORES):
        b, j = core // 4, core % 4
        s = slice(j * 64, (j + 1) * 64)
        m = dict(common)
        m["xsh"] = np.ascontiguousarray(inp["x"][b, j * (N // 4):(j + 1) * (N // 4)])
        m["ctx"] = np.ascontiguousarray(inp["ctx"][b])
        cv = np.stack([inp["c"][b], inp["c_ctx"]], 0)
        m["cT"] = np.ascontiguousarray(cv.reshape(2, 8, 128).transpose(2, 1, 0)).astype(f32)
        tok, feat = head_cols(j)
        for l in range(2):
            two = lambda a: np.ascontiguousarray(np.concatenate([a[0][s], a[1][s]], 0)[:, None]).astype(f32)
            m[f"wt{l}"] = np.ascontiguousarray(inp["w_in"][l][:, tok])
            m[f"wf{l}"] = np.ascontiguousarray(inp["w_in"][l][:, feat])
            m[f"sink{l}"] = np.full((128, 1), inp["attn_sink"][l][j], f32)
            m[f"logit{l}"] = np.ascontiguousarray(np.broadcast_to(inp["ret_decay_logit"][l][:, j][None, :], (128, 2))).astype(f32)
            m[f"rgn{l}"] = np.ascontiguousarray(inp["ret_gn"][l][s, None])
            m[f"dgn{l}"] = np.ascontiguousarray(inp["diff_gn"][l][s, None])
            m[f"cw{l}"] = dup(inp["conv_w"][l][:, s].T)
            m[f"cb{l}"] = dup(inp["conv_b"][l][s, None])
            m[f"wa{l}"] = np.ascontiguousarray(np.concatenate([inp["lru_wa"][l][0, j], inp["lru_wa"][l][1, j]], 1))
            m[f"wx{l}"] = np.ascontiguousarray(np.concatenate([inp["lru_wx"][l][0, j], inp["lru_wx"][l][1, j]], 1))
            m[f"ba{l}"] = two(inp["lru_ba"][l])
            m[f"bx{l}"] = two(inp["lru_bx"][l])
            m[f"lam{l}"] = two(inp["lru_lambda"][l])
        maps.append(m)
    return maps


def kernel(**inputs):
    inp = {k: np.asarray(v) for k, v in inputs.items()}
    B, N, _ = inp["x"].shape
    M = inp["ctx"].shape[1]
    E = inp["moe_w1"].shape[1]
    nc = _get("fused", build_fused, N, M, E, inp["ffn_w1"].shape[2], inp["moe_w1"].shape[3])
    res = run_bass_kernel_spmd(nc, make_maps(inp), core_ids=list(range(NCORES))).results
    out = np.stack([np.concatenate([res[b * 4 + s]["xo"] for s in range(4)], 0) for b in range(B)], 0)
    return out.astype(np.float32)
```
